# Optimizing a Trainium2 kernel written in Bass

```python
import jax
import jax.numpy as jnp
from jax import lax
import numpy as np

D_MODEL = 1024
BATCH = 16
SEQ = 4096
DEPTH = 2

GRID_W = 64
CTX_LEN = 256
HEAD_DIM = 64
ROPE_THETA = 10000.0
A_HEADS = 8
A_KV_HEADS = 2
Q_BLOCK = 128
NA_HEADS = 8
NA_WIN_R = 8
NA_WIN_C = 16
ML_HEADS = 4
ML_HEAD_DIM = 128
ML_CHUNK = 64
N_BRANCH = 3
BRANCH_W = 512
N_GROUPS = 4
EXPERTS_PER_GROUP = 8
N_EXPERTS = N_GROUPS * EXPERTS_PER_GROUP
TOP_K = 2
D_EXPERT = 256
EPS = 1e-6
NEG_INF = -1e30
IN_SPLITS = (
    A_HEADS * HEAD_DIM, A_KV_HEADS * HEAD_DIM, A_KV_HEADS * HEAD_DIM,
    NA_HEADS * HEAD_DIM, NA_HEADS * HEAD_DIM, NA_HEADS * HEAD_DIM,
    ML_HEADS * ML_HEAD_DIM, ML_HEADS * ML_HEAD_DIM, ML_HEADS * ML_HEAD_DIM, ML_HEADS * ML_HEAD_DIM,
    2 * 2 * ML_HEADS,
    N_BRANCH * D_MODEL,
)
IN_WIDTH = sum(IN_SPLITS)

kernel_name = 'hybrid_gqa_natten_mlstm_hmoe_dit'


def rms_norm(x, gain):
    x32 = x.astype(jnp.float32)
    y = x32 * lax.rsqrt(jnp.mean(x32 * x32, axis=-1, keepdims=True) + EPS)
    return (y * gain.astype(jnp.float32)).astype(x.dtype)


def modulate(h, shift, scale):
    return h * (1 + scale) + shift


def split_cols(p):
    out, start = [], 0
    for w in IN_SPLITS:
        out.append(p[..., start:start + w])
        start += w
    return out


def to_heads(a, n_heads, head_dim):
    return a.reshape(a.shape[:2] + (n_heads, head_dim))


def axial_rope(n_tok, dtype):
    t = jnp.arange(n_tok)
    row = (t // GRID_W).astype(jnp.float32)
    col = (t % GRID_W).astype(jnp.float32)
    n_freq = HEAD_DIM // 4
    inv = ROPE_THETA ** (-jnp.arange(n_freq, dtype=jnp.float32) / n_freq)
    ang = jnp.concatenate([row[:, None] * inv, col[:, None] * inv], axis=-1)
    return jnp.cos(ang).astype(dtype), jnp.sin(ang).astype(dtype)


def apply_rope(x, cos, sin):
    half = x.shape[-1] // 2
    x1, x2 = x[..., :half], x[..., half:]
    cs, sn = cos[None, :, None, :], sin[None, :, None, :]
    return jnp.concatenate([x1 * cs - x2 * sn, x2 * cs + x1 * sn], axis=-1)


def gqa_attend(q, k, v):
    s = jnp.einsum('bqkgd,bskd->bkgqs', q, k).astype(jnp.float32) * HEAD_DIM ** -0.5
    p = jax.nn.softmax(s, axis=-1).astype(v.dtype)
    return jnp.einsum('bkgqs,bskd->bqkgd', p, v)


def gqa_branch(q, k, v, q_c, k_c, v_c, need_ctx):
    bsz, n_tok = q.shape[:2]
    grp = A_HEADS // A_KV_HEADS
    kk = jnp.concatenate([k, k_c], axis=1)
    vv = jnp.concatenate([v, v_c], axis=1)
    nb = n_tok // Q_BLOCK
    qb = q.reshape(bsz, nb, Q_BLOCK, A_KV_HEADS, grp, HEAD_DIM).swapaxes(0, 1)
    o = lax.map(lambda qq: gqa_attend(qq, kk, vv), qb)
    y = o.swapaxes(0, 1).reshape(bsz, n_tok, A_HEADS * HEAD_DIM)
    yc = None
    if need_ctx:
        n_ctx = q_c.shape[1]
        yc = gqa_attend(q_c.reshape(bsz, n_ctx, A_KV_HEADS, grp, HEAD_DIM), k_c, v_c)
        yc = yc.reshape(bsz, n_ctx, A_HEADS * HEAD_DIM)
    return y, yc


def na_branch(q, k, v, q_c, k_c, v_c, rpb, rows, need_ctx):
    bsz = q.shape[0]
    win_r = min(NA_WIN_R, rows)
    qg = q.reshape(bsz, rows, GRID_W, NA_HEADS, HEAD_DIM).swapaxes(0, 1)
    kg = k.reshape(bsz, rows, GRID_W, NA_HEADS, HEAD_DIM)
    vg = v.reshape(bsz, rows, GRID_W, NA_HEADS, HEAD_DIM)
    col = jnp.arange(GRID_W)
    col_start = jnp.clip(col - NA_WIN_C // 2, 0, GRID_W - NA_WIN_C)
    col_mask = (col[None, :] >= col_start[:, None]) & (col[None, :] < col_start[:, None] + NA_WIN_C)
    dc_idx = jnp.clip(col[None, :] - col[:, None] + NA_WIN_C - 1, 0, 2 * NA_WIN_C - 2)
    scale = HEAD_DIM ** -0.5

    def row_block(args):
        r, qb = args
        rs = jnp.clip(r - win_r // 2, 0, rows - win_r)
        kb = lax.dynamic_slice_in_dim(kg, rs, win_r, axis=1)
        vb = lax.dynamic_slice_in_dim(vg, rs, win_r, axis=1)
        dr_idx = rs + jnp.arange(win_r) - r + NA_WIN_R - 1
        bias = rpb[:, dr_idx[None, :, None], dc_idx[:, None, :]].astype(jnp.float32)
        s_loc = jnp.einsum('bqhd,bjkhd->bhqjk', qb, kb).astype(jnp.float32) * scale + bias
        s_loc = jnp.where(col_mask[:, None, :], s_loc, NEG_INF)
        s_loc = s_loc.reshape(bsz, NA_HEADS, GRID_W, win_r * GRID_W)
        s_ctx = jnp.einsum('bqhd,bshd->bhqs', qb, k_c).astype(jnp.float32) * scale
        p = jax.nn.softmax(jnp.concatenate([s_loc, s_ctx], axis=-1), axis=-1).astype(v.dtype)
        p_loc = p[..., :win_r * GRID_W].reshape(bsz, NA_HEADS, GRID_W, win_r, GRID_W)
        p_ctx = p[..., win_r * GRID_W:]
        return (jnp.einsum('bhqjk,bjkhd->bqhd', p_loc, vb)
                + jnp.einsum('bhqs,bshd->bqhd', p_ctx, v_c))

    o = lax.map(row_block, (jnp.arange(rows), qg))
    y = o.swapaxes(0, 1).reshape(bsz, rows * GRID_W, NA_HEADS * HEAD_DIM)
    yc = None
    if need_ctx:
        n_ctx = q_c.shape[1]
        yc = gqa_attend(q_c[:, :, :, None, :], k_c, v_c).reshape(bsz, n_ctx, NA_HEADS * HEAD_DIM)
    return y, yc


def mlstm_chunk_step(carry, xs):
    c_mat, n_vec, m = carry
    q, k, v, ig, lf = xs
    lc = q.shape[2]
    lower = jnp.tril(jnp.ones((lc, lc), dtype=bool))
    b = jnp.cumsum(lf, axis=-1)
    a = b + m[..., None]
    dmat = jnp.where(lower, b[..., :, None] - b[..., None, :] + ig[..., None, :], NEG_INF)
    m_t = jnp.maximum(a, dmat.max(axis=-1))
    w_inter = jnp.exp(a - m_t)
    s = jnp.einsum('bhtd,bhsd->bhts', q, k) * jnp.exp(dmat - m_t[..., None])
    num = w_inter[..., None] * jnp.einsum('bhtd,bhde->bhte', q, c_mat) + jnp.einsum('bhts,bhse->bhte', s, v)
    den = w_inter * jnp.einsum('bhtd,bhd->bht', q, n_vec) + s.sum(axis=-1)
    h = num / jnp.maximum(jnp.abs(den), jnp.exp(-m_t))[..., None]
    b_end = b[..., -1]
    log_w = b_end[..., None] - b + ig
    m_new = jnp.maximum(b_end + m, log_w.max(axis=-1))
    g_inter = jnp.exp(b_end + m - m_new)
    g_s = jnp.exp(log_w - m_new[..., None])
    kg = k * g_s[..., None]
    c_new = g_inter[..., None, None] * c_mat + jnp.einsum('bhsd,bhse->bhde', kg, v)
    n_new = g_inter[..., None] * n_vec + kg.sum(axis=2)
    return (c_new, n_new, m_new), h


def mlstm_scan(q, k, v, ig, lf, state):
    bsz, n_tok, nh, hd = q.shape
    nc = n_tok // ML_CHUNK
    chunks = lambda a: a.reshape(bsz, nc, ML_CHUNK, nh, hd).transpose(1, 0, 3, 2, 4)
    gchunks = lambda a: a.reshape(bsz, nc, ML_CHUNK, nh).transpose(1, 0, 3, 2)
    state, h = lax.scan(mlstm_chunk_step, state, (chunks(q), chunks(k), chunks(v), gchunks(ig), gchunks(lf)))
    return h.transpose(1, 0, 3, 2, 4).reshape(bsz, n_tok, nh, hd), state


def maybe_flip(a, direction):
    return jnp.flip(a, axis=1) if direction == 1 else a


def mlstm_branch(q, k, v, o, pre, q_c, k_c, v_c, o_c, pre_c, g_ml, need_ctx):
    dt = q.dtype
    bsz = q.shape[0]
    f32 = jnp.float32
    qs, vs, ks = q.astype(f32), v.astype(f32), k.astype(f32) * ML_HEAD_DIM ** -0.5
    qcs, vcs, kcs = q_c.astype(f32), v_c.astype(f32), k_c.astype(f32) * ML_HEAD_DIM ** -0.5
    ig, lf = pre[:, :, :, 0, :], jax.nn.log_sigmoid(pre[:, :, :, 1, :])
    ig_c, lf_c = pre_c[:, :, :, 0, :], jax.nn.log_sigmoid(pre_c[:, :, :, 1, :])
    state0 = (jnp.zeros((bsz, ML_HEADS, ML_HEAD_DIM, ML_HEAD_DIM), f32),
              jnp.zeros((bsz, ML_HEADS, ML_HEAD_DIM), f32),
              jnp.zeros((bsz, ML_HEADS), f32))
    h_lat = jnp.zeros(qs.shape, f32)
    h_ctx = jnp.zeros(qcs.shape, f32)
    for d in range(2):
        hc_d, st = mlstm_scan(maybe_flip(qcs, d), maybe_flip(kcs, d), maybe_flip(vcs, d),
                              maybe_flip(ig_c[:, :, d], d), maybe_flip(lf_c[:, :, d], d), state0)
        hl_d, _ = mlstm_scan(maybe_flip(qs, d), maybe_flip(ks, d), maybe_flip(vs, d),
                             maybe_flip(ig[:, :, d], d), maybe_flip(lf[:, :, d], d), st)
        h_lat = h_lat + maybe_flip(hl_d, d)
        h_ctx = h_ctx + maybe_flip(hc_d, d)
    gain = g_ml.reshape(ML_HEADS, ML_HEAD_DIM)

    def finish(hsum, og):
        hn = rms_norm(hsum, gain).astype(dt)
        out = hn * jax.nn.sigmoid(og).reshape(hn.shape)
        return out.reshape(hn.shape[0], hn.shape[1], ML_HEADS * ML_HEAD_DIM)

    y = finish(h_lat, o)
    yc = finish(h_ctx, o_c) if need_ctx else None
    return y, yc


def token_mixers(h, hc, w_in, b_merge, g_qk, rpb, b_mlstm, g_ml, w_branch, w_out, rows, cos, sin, need_ctx):
    bsz, n_tok, _ = h.shape
    n_ctx = hc.shape[1]
    pl = split_cols(h @ w_in)
    pc = split_cols(hc @ w_in)
    qa = apply_rope(rms_norm(to_heads(pl[0], A_HEADS, HEAD_DIM), g_qk[0]), cos, sin)
    ka = apply_rope(rms_norm(to_heads(pl[1], A_KV_HEADS, HEAD_DIM), g_qk[1]), cos, sin)
    va = to_heads(pl[2], A_KV_HEADS, HEAD_DIM)
    qa_c = rms_norm(to_heads(pc[0], A_HEADS, HEAD_DIM), g_qk[0])
    ka_c = rms_norm(to_heads(pc[1], A_KV_HEADS, HEAD_DIM), g_qk[1])
    va_c = to_heads(pc[2], A_KV_HEADS, HEAD_DIM)
    ya, ya_c = gqa_branch(qa, ka, va, qa_c, ka_c, va_c, need_ctx)
    qb = rms_norm(to_heads(pl[3], NA_HEADS, HEAD_DIM), g_qk[2])
    kb = rms_norm(to_heads(pl[4], NA_HEADS, HEAD_DIM), g_qk[3])
    vb = to_heads(pl[5], NA_HEADS, HEAD_DIM)
    qb_c = rms_norm(to_heads(pc[3], NA_HEADS, HEAD_DIM), g_qk[2])
    kb_c = rms_norm(to_heads(pc[4], NA_HEADS, HEAD_DIM), g_qk[3])
    vb_c = to_heads(pc[5], NA_HEADS, HEAD_DIM)
    yb, yb_c = na_branch(qb, kb, vb, qb_c, kb_c, vb_c, rpb, rows, need_ctx)
    pre = (pl[10].reshape(bsz, n_tok, 2, 2, ML_HEADS) + b_mlstm).astype(jnp.float32)
    pre_c = (pc[10].reshape(bsz, n_ctx, 2, 2, ML_HEADS) + b_mlstm).astype(jnp.float32)
    ym, ym_c = mlstm_branch(to_heads(pl[6], ML_HEADS, ML_HEAD_DIM), to_heads(pl[7], ML_HEADS, ML_HEAD_DIM),
                            to_heads(pl[8], ML_HEADS, ML_HEAD_DIM), pl[9], pre,
                            to_heads(pc[6], ML_HEADS, ML_HEAD_DIM), to_heads(pc[7], ML_HEADS, ML_HEAD_DIM),
                            to_heads(pc[8], ML_HEADS, ML_HEAD_DIM), pc[9], pre_c, g_ml, need_ctx)

    def merge(outs, gate_cols, n):
        gates = jax.nn.sigmoid(gate_cols.reshape(bsz, n, N_BRANCH, D_MODEL) + b_merge)
        merged = gates[:, :, 0] * (outs[0] @ w_branch[0])
        for br in range(1, N_BRANCH):
            merged = merged + gates[:, :, br] * (outs[br] @ w_branch[br])
        return merged @ w_out

    y = merge((ya, yb, ym), pl[11], n_tok)
    yc = merge((ya_c, yb_c, ym_c), pc[11], n_ctx) if need_ctx else None
    return y, yc


def hier_moe(h, w_group, b_group, w_router, b_router, w_gate_up, w_down):
    shp = h.shape
    t = h.reshape(-1, D_MODEL)
    lg = (t @ w_group + b_group).astype(jnp.float32)
    pg = jax.nn.softmax(lg, axis=-1)
    gsel = jnp.argmax(lg, axis=-1)
    le = (t @ w_router + b_router).astype(jnp.float32).reshape(-1, N_GROUPS, EXPERTS_PER_GROUP)
    le_sel = jnp.take_along_axis(le, gsel[:, None, None], axis=1)[:, 0]
    pe = jax.nn.softmax(le_sel, axis=-1)
    top_p, top_i = lax.top_k(pe, TOP_K)
    pg_sel = jnp.take_along_axis(pg, gsel[:, None], axis=1)
    wts = pg_sel * top_p / jnp.sum(top_p, axis=-1, keepdims=True)
    eid = gsel[:, None] * EXPERTS_PER_GROUP + top_i
    dense_w = jnp.sum(jax.nn.one_hot(eid, N_EXPERTS, dtype=jnp.float32) * wts[..., None], axis=1).astype(t.dtype)
    out = jnp.zeros_like(t)
    for e in range(N_EXPERTS):
        gu = t @ w_gate_up[e]
        y = (jax.nn.silu(gu[:, :D_EXPERT]) * gu[:, D_EXPERT:]) @ w_down[e]
        out = out + dense_w[:, e:e + 1] * y
    return out.reshape(shp)


def setup_inputs(seed: int = 0) -> dict:
    key = jax.random.key(seed)
    ks = jax.random.split(key, 24)
    f32 = jnp.float32
    nrm = lambda k, shape, s: jax.random.normal(k, shape, f32) * s
    b_ml_i = nrm(ks[11], (DEPTH, 2, ML_HEADS), 0.1)
    b_ml_f = 3.0 + 3.0 * jax.random.uniform(ks[12], (DEPTH, 2, ML_HEADS), f32)
    return {
        'x': nrm(ks[0], (BATCH, SEQ, D_MODEL), 1.0),
        'c': nrm(ks[1], (BATCH, D_MODEL), 1.0),
        'ctx': nrm(ks[2], (BATCH, CTX_LEN, D_MODEL), 1.0),
        'c_ctx': nrm(ks[3], (D_MODEL,), 1.0),
        'w_mod': nrm(ks[4], (DEPTH, D_MODEL, 6 * D_MODEL), 0.5 * D_MODEL ** -0.5),
        'b_mod': nrm(ks[5], (DEPTH, 6 * D_MODEL), 0.02),
        'g_norm': 1.0 + nrm(ks[6], (DEPTH, 2, D_MODEL), 0.02),
        'w_in': nrm(ks[7], (DEPTH, D_MODEL, IN_WIDTH), D_MODEL ** -0.5),
        'b_merge': nrm(ks[8], (DEPTH, N_BRANCH, D_MODEL), 0.02),
        'g_qk': 1.0 + nrm(ks[9], (DEPTH, 4, HEAD_DIM), 0.02),
        'rpb': nrm(ks[10], (DEPTH, NA_HEADS, 2 * NA_WIN_R - 1, 2 * NA_WIN_C - 1), 0.1),
        'b_mlstm': jnp.stack([b_ml_i, b_ml_f], axis=2),
        'g_ml': 1.0 + nrm(ks[13], (DEPTH, ML_HEADS * ML_HEAD_DIM), 0.02),
        'w_branch': nrm(ks[14], (DEPTH, N_BRANCH, BRANCH_W, D_MODEL), BRANCH_W ** -0.5),
        'w_out': nrm(ks[15], (DEPTH, D_MODEL, D_MODEL), D_MODEL ** -0.5),
        'w_group': nrm(ks[16], (DEPTH, D_MODEL, N_GROUPS), D_MODEL ** -0.5),
        'b_group': nrm(ks[17], (DEPTH, N_GROUPS), 0.01),
        'w_router': nrm(ks[18], (DEPTH, D_MODEL, N_EXPERTS), D_MODEL ** -0.5),
        'b_router': nrm(ks[19], (DEPTH, N_EXPERTS), 0.01),
        'w_gate_up': nrm(ks[20], (DEPTH, N_EXPERTS, D_MODEL, 2 * D_EXPERT), D_MODEL ** -0.5),
        'w_down': nrm(ks[21], (DEPTH, N_EXPERTS, D_EXPERT, D_MODEL), D_EXPERT ** -0.5),
    }


def reference(x, c, ctx, c_ctx, w_mod, b_mod, g_norm, w_in, b_merge, g_qk, rpb, b_mlstm, g_ml,
              w_branch, w_out, w_group, b_group, w_router, b_router, w_gate_up, w_down):
    n_tok = x.shape[1]
    rows = n_tok // GRID_W
    cos, sin = axial_rope(n_tok, x.dtype)
    xc = ctx
    for l in range(DEPTH):
        need_ctx = l < DEPTH - 1
        mod = jax.nn.silu(c) @ w_mod[l] + b_mod[l]
        mod_c = jax.nn.silu(c_ctx) @ w_mod[l] + b_mod[l]
        sh1, sc1, gt1, sh2, sc2, gt2 = jnp.split(mod[:, None, :], 6, axis=-1)
        sh1c, sc1c, gt1c, sh2c, sc2c, gt2c = jnp.split(mod_c, 6, axis=-1)
        h = modulate(rms_norm(x, g_norm[l, 0]), sh1, sc1)
        hc = modulate(rms_norm(xc, g_norm[l, 0]), sh1c, sc1c)
        y, yc = token_mixers(h, hc, w_in[l], b_merge[l], g_qk[l], rpb[l], b_mlstm[l], g_ml[l],
                             w_branch[l], w_out[l], rows, cos, sin, need_ctx)
        x = x + gt1 * y
        h = modulate(rms_norm(x, g_norm[l, 1]), sh2, sc2)
        x = x + gt2 * hier_moe(h, w_group[l], b_group[l], w_router[l], b_router[l], w_gate_up[l], w_down[l])
        if need_ctx:
            xc = xc + gt1c * yc
            hc = modulate(rms_norm(xc, g_norm[l, 1]), sh2c, sc2c)
            xc = xc + gt2c * hier_moe(hc, w_group[l], b_group[l], w_router[l], b_router[l], w_gate_up[l], w_down[l])
    return x
```

```python
import numpy as np
import concourse.bass as bass
import concourse.mybir as mybir
from concourse.bass_utils import run_bass_kernel_spmd
from contextlib import ExitStack
import os

F32 = mybir.dt.float32
BF16 = mybir.dt.bfloat16
AF = mybir.ActivationFunctionType
ALU = mybir.AluOpType
AX = mybir.AxisListType

T = 4352
NT = 34
D = 1024
NEG = -1e30
SKIP_SAME = int(os.environ.get("SKIP_SAME", "0"))


class Buf:
    __slots__ = ("name", "w", "r", "dsem", "t", "grp")

    def __init__(self, name, t=None, grp=None):
        self.name = name
        self.grp = grp
        self.w = None
        self.r = {}
        self.dsem = None
        self.t = t

    @property
    def a(self):
        return self.t.ap() if hasattr(self.t, "ap") else self.t[:]


class Ctx:
    def __init__(self, nc, es):
        self.nc = nc
        self.es = es
        self.E = {"pe": nc.tensor, "act": nc.scalar, "dve": nc.vector, "pool": nc.gpsimd, "sp": nc.sync}
        self.sem = {}
        self.ecnt = {}
        for e in ("pe", "act", "dve", "pool"):
            self.sem[e] = es.enter_context(nc.semaphore("c_" + e))
            self.ecnt[e] = 0
        self.seen = {e: {} for e in self.E}
        self.ninst = 0
        self.uid = 0
        self.trace = {e: [] for e in self.E}
        self.shared = set()
        self.stack = [es]
        self.dtot = {}

    def sb(self, name, shape, dt, grp=None):
        self.uid += 1
        return Buf(name, self.stack[-1].enter_context(self.nc.sbuf_tensor("%s_%d" % (name, self.uid), list(shape), dt)), grp)

    def ps(self, name, shape, dt):
        self.uid += 1
        return Buf(name, self.stack[-1].enter_context(self.nc.psum_tensor("%s_%d" % (name, self.uid), list(shape), dt)))

    def dram(self, name, shape, dt, kind="Internal"):
        return Buf(name, self.nc.dram_tensor(name, list(shape), dt, kind=kind))

    def push(self):
        st = ExitStack()
        st.__enter__()
        self.stack.append(st)

    def pop(self):
        self.barrier()
        self.stack.pop().__exit__(None, None, None)

    def barrier(self):
        evs = [(e, self.ecnt[e]) for e in self.ecnt] + list(self.dtot.items())
        for e in self.E:
            self._wait(e, evs)

    def _wait(self, eng, deps):
        need = {}
        for k, v in deps:
            if eng == "pe" and k == "pe":
                continue
            if SKIP_SAME and k == eng and v <= self.ecnt[eng] - SKIP_SAME:
                continue
            if v > need.get(k, 0):
                need[k] = v
        seen = self.seen[eng]
        for k, v in need.items():
            if k in self.shared:
                v = self.dtot[k]
            if seen.get(k, 0) >= v:
                continue
            self.E[eng].wait_ge(self.sem[k], v)
            self.trace[eng].append(("w", k, v))
            self.ninst += 1
            seen[k] = v

    def _deps(self, reads, writes):
        deps = []
        for b in reads:
            if b.w is not None:
                deps.append(b.w)
        for b in writes:
            if b.w is not None:
                deps.append(b.w)
            deps.extend(b.r.items())
        return deps

    def _commit(self, ev, reads, writes):
        k, v = ev
        for b in reads:
            if b.r.get(k, 0) < v:
                b.r[k] = v
        for b in writes:
            b.w = ev
            b.r = {}

    def op(self, eng, f, reads=(), writes=()):
        self._wait(eng, self._deps(reads, writes))
        ins = f(self.E[eng])
        self.ecnt[eng] += 1
        ins.then_inc(self.sem[eng], 1)
        self.trace[eng].append(("i", eng, 1))
        self.ninst += 1
        self._commit((eng, self.ecnt[eng]), reads, writes)
        return ins

    def V(self, f, r=(), w=()):
        return self.op("dve", f, r, w)

    def A(self, f, r=(), w=()):
        return self.op("act", f, r, w)

    def G(self, f, r=(), w=()):
        return self.op("pool", f, r, w)

    def mm(self, f, reads=(), writes=(), last=True):
        self._wait("pe", self._deps(reads, writes))
        ins = f(self.E["pe"])
        self.ninst += 1
        if last:
            self.ecnt["pe"] += 1
            ins.then_inc(self.sem["pe"], 1)
            self.trace["pe"].append(("i", "pe", 1))
            self._commit(("pe", self.ecnt["pe"]), reads, writes)
        else:
            self._commit(("pe", self.ecnt["pe"] + 1), reads, writes)
        return ins

    def dma(self, q, out, in_, dst, src, **kw):
        self._wait(q, self._deps((src,), (dst,)))
        if dst.dsem is None:
            dst.dsem = "d_" + (dst.grp or dst.name)
            if dst.grp:
                self.shared.add(dst.dsem)
            if dst.dsem not in self.sem:
                self.sem[dst.dsem] = self.es.enter_context(self.nc.semaphore(dst.dsem))
                self.dtot[dst.dsem] = 0
        ins = self.E[q].dma_start(out=out, in_=in_, **kw)
        self.dtot[dst.dsem] += 16
        ins.then_inc(self.sem[dst.dsem], 16)
        self.trace[q].append(("i", dst.dsem, 16))
        self.ninst += 1
        self._commit((dst.dsem, self.dtot[dst.dsem]), (src,), (dst,))
        return ins


def pipeline(n, stage1, stage2, depth, s1_first=False):
    for si in range(min(depth, n)):
        stage1(si)
    for si in range(n):
        if s1_first and si + depth < n:
            stage1(si + depth)
        stage2(si)
        if not s1_first and si + depth < n:
            stage1(si + depth)


def na_plan(j):
    plan = []
    for kt in range(32):
        blocks = {}
        anyv = False
        for a in range(2):
            for b in range(2):
                qr = 2 * j + b
                kr = 2 * kt + a
                rs = min(max(qr - 4, 0), 56)
                ok = rs <= kr < rs + 8
                blocks[(a, b)] = (kr - qr + 7) if ok else None
                anyv = anyv or ok
        if anyv:
            plan.append((kt, blocks))
    return plan


def build(nb=2, nl=2, dbg=(), stop_after=None):
    nc = bass.Bass("TRN2", target_bir_lowering=False)
    es = ExitStack()
    with es:
        c = Ctx(nc, es)

        def inp(name, shape):
            return Buf(name, nc.dram_tensor(name, list(shape), F32, kind="ExternalInput"))

        x_in = inp("x", [nb, 4096, D]); ctx_in = inp("ctx", [nb, 256, D]); c_in = inp("c", [nb, D]); cctx_in = inp("c_ctx", [D])
        w_mod = inp("w_mod", [2, D, 6144]); b_mod = inp("b_mod", [2, 6144]); g_norm = inp("g_norm", [2, 2, D])
        w_in = inp("w_in", [2, D, 7440]); b_merge = inp("b_merge", [2, 3, D]); g_qk = inp("g_qk", [2, 4, 64])
        rpb = inp("rpb", [2, 8, 15, 31]); b_mlstm = inp("b_mlstm", [2, 16]); g_ml = inp("g_ml", [2, 512])
        w_branch = inp("w_branch", [2, 3, 512, D]); w_out = inp("w_out", [2, D, D])
        w_group = inp("w_group", [2, D, 4]); b_group = inp("b_group", [2, 4]); w_router = inp("w_router", [2, D, 32]); b_router = inp("b_router", [2, 32])
        w_gate_up = inp("w_gate_up", [2, 32, D, 512]); w_down = inp("w_down", [2, 32, 256, D])
        k_cos = inp("k_cos", [T, 32]); k_sin = inp("k_sin", [T, 32]); k_u = inp("k_u", [2, 128, 128]); k_id = inp("k_id", [128, 128])
        k_colmask = inp("k_colmask", [128, 64]); k_jd = inp("k_jd", [64, 128])
        y_out = c.dram("y", [nb, 4096, D], F32, kind="ExternalOutput")

        def scr(name, shape, dt):
            return c.dram(name, shape, dt, kind=("ExternalOutput" if name in dbg else "Internal"))

        XS = scr("XS", [nb, T, D], F32)
        QAT = scr("QAT", [128, 4, T], BF16); KAT = scr("KAT", [128, T], BF16); VA = scr("VA", [T, 2, 65], BF16)
        QBT = scr("QBT", [128, 4, T], BF16); KBT = scr("KBT", [128, 4, T], BF16); VB = scr("VB", [T, 8, 65], BF16)
        MQT = scr("MQT", [128, 4, T], BF16); MKT = scr("MKT", [128, 4, T], BF16); MK = scr("MK", [T, 512], BF16)
        MV = scr("MV", [T, 4, 129], BF16); MO = scr("MO", [T, 512], BF16); MG = scr("MG", [T, 16], F32)
        GATES = scr("GATES", [T, 3072], BF16)
        YAT = scr("YAT", [4, 128, T], BF16); YBT = scr("YBT", [4, 128, T], BF16); YCT = scr("YCT", [4, 128, T], BF16)
        HS = scr("HS", [T, 512], F32)
        H2T = scr("H2T", [128, 8, T], BF16); DW = scr("DW", [T, 32], F32)
        WGU = scr("WGU", [32, 128, 8, 512], BF16); WDN = scr("WDN", [32, 128, 2, D], BF16)
        MODD = scr("MODD", [2, 3, 6144], F32)
        RPBP = scr("RPBP", [7568], F32); TPD = scr("TPD", [128, 8, 15, 64], F32)

        ident_f = c.sb("ident_f", [128, 128], F32, grp="setup"); ident = c.sb("ident", [128, 128], BF16)
        U = c.sb("U", [128, 2, 128], F32, grp="setup")
        c.dma("sp", ident_f.a, k_id.a, ident_f, k_id)
        c.dma("sp", U.a, k_u.a.rearrange("d s t -> s d t"), U, k_u)
        c.V(lambda e: e.tensor_copy(out=ident.a, in_=ident_f.a), [ident_f], [ident])
        ones_col = c.sb("ones_col", [128, 8], BF16)
        c.V(lambda e: e.memset(ones_col.a, 1.0), [], [ones_col])

        def tile_src(l, b, i):
            if l == 0:
                if i < 2:
                    return ctx_in, ctx_in.a[b, i * 128:(i + 1) * 128, :]
                return x_in, x_in.a[b, (i - 2) * 128:(i - 1) * 128, :]
            return XS, XS.a[b, i * 128:(i + 1) * 128, :]

        for l in range(nl):
            last_layer = (l == nl - 1)
            first_tile = 2 if last_layer else 0
            c.push()
            c.push()
            cT = c.sb("cT", [128, 8, 3], F32, grp="setup"); cTb = c.sb("cTb", [128, 8, 3], BF16)
            c.V(lambda e: e.memset(cT.a, 0.0), [], [cT])
            for b in range(nb):
                c.dma("sp", cT.a[:, :, b], c_in.a[b].rearrange("(k p) -> p k", p=128), cT, c_in, allow_slow_non_contiguous=True)
            c.dma("sp", cT.a[:, :, 2], cctx_in.a.rearrange("(k p) -> p k", p=128), cT, cctx_in, allow_slow_non_contiguous=True)
            c.A(lambda e: e.activation(out=cTb.a, in_=cT.a, func=AF.Silu), [cT], [cTb])
            modrow = c.sb("modrow", [3, 6144], F32)
            bmrow = c.sb("bmrow", [3, 6144], F32, grp="setup")
            c.dma("sp", bmrow.a, b_mod.a[l].partition_broadcast(3), bmrow, b_mod)
            wm = [c.sb("wm%d" % i, [128, 8, 512], BF16) for i in range(2)]
            pmod = [c.ps("pmod%d" % i, [128, 512], F32) for i in range(2)]
            for n in range(12):
                w_ = wm[n % 2]; p_ = pmod[n % 2]
                c.dma("pool", w_.a, w_mod.a[l, :, n * 512:(n + 1) * 512].rearrange("(k p) n -> p k n", p=128), w_, w_mod)
                for k in range(8):
                    c.mm(lambda e: e.matmul(p_.a[0:3, :], lhsT=cTb.a[:, k, :], rhs=w_.a[:, k, :], start=(k == 0), stop=(k == 7)), [cTb, w_], [p_], last=(k == 7))
                c.V(lambda e: e.tensor_tensor(out=modrow.a[:, n * 512:(n + 1) * 512], in0=p_.a[0:3, :], in1=bmrow.a[:, n * 512:(n + 1) * 512], op=ALU.add), [p_, bmrow], [modrow])
            c.dma("sp", MODD.a[l], modrow.a, MODD, modrow)
            c.pop()
            modT = c.sb("modT", [128, 6, 8, 3], F32, grp="setup")
            for s in range(6):
                for j in range(3):
                    c.dma("sp", modT.a[:, s, :, j], MODD.a[l, j, s * 1024:(s + 1) * 1024].rearrange("(k p) -> p k", p=128), modT, MODD, allow_slow_non_contiguous=True)
            gn = c.sb("gn", [128, 2, 8], F32, grp="setup")
            c.dma("sp", gn.a, g_norm.a[l].rearrange("t (k p) -> p t k", p=128), gn, g_norm, allow_slow_non_contiguous=True)
            G1 = c.sb("G1", [128, 8, 3], F32); G2 = c.sb("G2", [128, 8, 3], F32)
            for (Gx, seg, t_) in ((G1, 1, 0), (G2, 4, 1)):
                c.V(lambda e: e.tensor_scalar(out=Gx.a, in0=modT.a[:, seg], scalar1=1.0, scalar2=None, op0=ALU.add), [modT], [Gx])
                c.V(lambda e: e.tensor_tensor(out=Gx.a, in0=Gx.a, in1=gn.a[:, t_, :].unsqueeze(2).to_broadcast([128, 8, 3]), op=ALU.mult), [Gx, gn], [Gx])
            GT = c.sb("GT", [128, 2, 3, D], F32, grp="setup")
            for gi, seg in ((0, 2), (1, 5)):
                for j in range(3):
                    c.dma("sp", GT.a[:, gi, j, :], MODD.a[l, j, seg * 1024:(seg + 1) * 1024].partition_broadcast(128), GT, MODD)
            gqk = c.sb("gqk", [128, 4, 64], F32, grp="setup")
            c.dma("sp", gqk.a, g_qk.a[l].partition_broadcast(128), gqk, g_qk)
            bml = c.sb("bml", [128, 16], F32, grp="setup")
            c.dma("sp", bml.a, b_mlstm.a[l].partition_broadcast(128), bml, b_mlstm)
            gml = c.sb("gml", [128, 512], F32, grp="setup")
            c.dma("sp", gml.a, g_ml.a[l].partition_broadcast(128), gml, g_ml)
            brt = c.sb("brt", [128, 36], F32, grp="setup")
            c.dma("sp", brt.a[:, 0:4], b_group.a[l].partition_broadcast(128), brt, b_group)
            c.dma("sp", brt.a[:, 4:36], b_router.a[l].partition_broadcast(128), brt, b_router)
            wrt = c.sb("wrt", [128, 8, 36], F32, grp="setup")
            c.dma("sp", wrt.a[:, :, 0:4], w_group.a[l].rearrange("(k p) n -> p k n", p=128), wrt, w_group, allow_slow_non_contiguous=True)
            c.dma("sp", wrt.a[:, :, 4:36], w_router.a[l].rearrange("(k p) n -> p k n", p=128), wrt, w_router, allow_slow_non_contiguous=True)
            c.push()
            zt = c.sb("zt", [1, 8192], F32, grp="setup")
            c.V(lambda e: e.memset(zt.a, 0.0), [], [zt])
            c.dma("sp", RPBP.a.rearrange("(o n) -> o n", o=1), zt.a[:, 0:7568], RPBP, zt)
            c.dma("sp", RPBP.a[64:64 + 3720].rearrange("(r j) -> r j", j=31), bass.AP(rpb.t, l * 3720 + 30, [[31, 120], [-1, 31]]), RPBP, rpb, allow_slow_non_contiguous=True)
            TPb = c.sb("TPb", [128, 8, 15, 64], F32, grp="setup")
            cm = c.sb("cm", [128, 64], F32, grp="setup")
            c.dma("sp", cm.a, k_colmask.a, cm, k_colmask)
            TPx = c.sb("TPx", [64, 8, 15, 64], F32, grp="setup")
            for h in range(8):
                src = bass.AP(RPBP.t, 64 - 48 + h * 15 * 31, [[1, 64], [31, 15], [1, 64]])
                c.dma("sp", TPx.a[:, h], src, TPx, RPBP)
            jd = c.sb("jd", [64, 128], F32, grp="setup")
            c.dma("sp", jd.a, k_jd.a, jd, k_jd)
            pJ = [c.ps("pJ%d" % i, [128, 512], F32) for i in range(2)]
            TPx2 = TPx.a.rearrange("p h r q -> p (h r q)")
            TPb2 = TPb.a.rearrange("p h r q -> p (h r) q")
            for n in range(15):
                p_ = pJ[n % 2]
                c.mm(lambda e: e.matmul(p_.a, lhsT=jd.a, rhs=TPx2[:, n * 512:(n + 1) * 512], start=True, stop=True), [jd, TPx], [p_])
                c.V(lambda e: e.tensor_tensor(out=TPb2[:, n * 8:(n + 1) * 8, :], in0=p_.a.rearrange("p (r q) -> p r q", q=64), in1=cm.a.unsqueeze(1).to_broadcast([128, 8, 64]), op=ALU.add), [p_, cm], [TPb])
            c.dma("sp", TPD.a, TPb.a, TPD, TPb)
            c.pop()
            c.push()
            cv = [c.sb("cv%d" % i, [128, 8, 512], BF16, grp="cv%d" % i) for i in range(2)]
            cd = [c.sb("cd%d" % i, [128, 2, D], BF16, grp="cv%d" % i) for i in range(2)]
            for e_ in range(32 if not os.environ.get("SKIP_CONV") else 0):
                a_ = cv[e_ % 2]; d_ = cd[e_ % 2]
                c.dma("pool", a_.a, w_gate_up.a[l, e_].rearrange("(k p) n -> p k n", p=128), a_, w_gate_up)
                c.dma("sp", WGU.a[e_], a_.a, WGU, a_)
                c.dma("pool", d_.a, w_down.a[l, e_].rearrange("(k p) n -> p k n", p=128), d_, w_down)
                c.dma("sp", WDN.a[e_], d_.a, WDN, d_)
            c.pop()
            if stop_after == "mod":
                break

            for b in range(nb):
                c.push()
                hT = c.sb("hT", [128, 8, T], BF16)
                xt = [c.sb("xt%d" % i, [128, D], F32) for i in range(2)]
                junk_L = [c.sb("junk%d" % i_, [128, D], F32) for i_ in range(2)]; junk = junk_L[0]
                ss_L = [c.sb("ss%d" % i_, [128, 1], F32) for i_ in range(2)]; ss = ss_L[0]; rstd_L = [c.sb("rstd%d" % i_, [128, 1], F32) for i_ in range(2)]; rstd = rstd_L[0]
                xn = [c.sb("xn%d" % i, [128, D], BF16) for i in range(2)]
                pT = [c.ps("pT%d" % i, [128, 8, 128], BF16) for i in range(2)]
                for i in range(NT):
                    x_ = xt[i % 2]; n_ = xn[i % 2]; p_ = pT[i % 2]
                    junk = junk_L[i % 2]; ss = ss_L[i % 2]; rstd = rstd_L[i % 2]
                    src, ap = tile_src(l, b, i)
                    j3 = 2 if i < 2 else b
                    c.dma("sp", x_.a, ap, x_, src)
                    c.V(lambda e: e.memset(ss.a, 0.0), [], [ss])
                    c.A(lambda e: e.activation(out=junk.a, in_=x_.a, func=AF.Square, accum_out=ss.a), [x_], [junk, ss])
                    c.V(lambda e: e.tensor_scalar(out=rstd.a, in0=ss.a, scalar1=1.0 / D, scalar2=1e-6, op0=ALU.mult, op1=ALU.add), [ss], [rstd])
                    c.A(lambda e: e.activation(out=rstd.a, in_=rstd.a, func=AF.Sqrt), [rstd], [rstd])
                    c.V(lambda e: e.reciprocal(out=rstd.a, in_=rstd.a), [rstd], [rstd])
                    c.V(lambda e: e.tensor_scalar(out=n_.a, in0=x_.a, scalar1=rstd.a[:, 0:1], scalar2=None, op0=ALU.mult), [x_, rstd], [n_])
                    for k in range(8):
                        c.mm(lambda e: e.transpose(out=p_.a[:, k, :], in_=n_.a[:, k * 128:(k + 1) * 128], identity=ident.a), [n_, ident], [p_], last=(k == 7))
                    for k in range(8):
                        eng = c.A if k % 2 == 0 else None
                        if k % 2 == 0:
                            c.A(lambda e: e.activation(out=hT.a[:, k, i * 128:(i + 1) * 128], in_=p_.a[:, k, :], func=AF.Identity, scale=G1.a[:, k, j3:j3 + 1], bias=modT.a[:, 0, k, j3:j3 + 1]), [p_, G1, modT], [hT])
                        else:
                            c.V(lambda e: e.tensor_scalar(out=hT.a[:, k, i * 128:(i + 1) * 128], in0=p_.a[:, k, :], scalar1=G1.a[:, k, j3:j3 + 1], scalar2=modT.a[:, 0, k, j3:j3 + 1], op0=ALU.mult, op1=ALU.add), [p_, G1, modT], [hT])
                if "HTD" in dbg:
                    HTD = scr("HTD", [128, 8, T], BF16)
                    c.dma("sp", HTD.a, hT.a, HTD, hT)
                cosb = c.sb("cosb", [128, NT, 32], F32, grp="setup"); sinb = c.sb("sinb", [128, NT, 32], F32, grp="setup")
                c.dma("sp", cosb.a, k_cos.a.rearrange("(i p) f -> p i f", p=128), cosb, k_cos)
                c.dma("sp", sinb.a, k_sin.a.rearrange("(i p) f -> p i f", p=128), sinb, k_sin)
                bmg = c.sb("bmg", [128, 3072], F32, grp="setup")
                c.dma("sp", bmg.a, b_merge.a[l].rearrange("t d -> (t d)").partition_broadcast(128), bmg, b_merge)
                wc = [c.sb("wc%d" % i, [128, 8, 512], BF16) for i in range(2)]
                pp = [c.ps("pp%d" % i, [128, 512], F32) for i in range(2)]
                ptr = [c.ps("ptr%d" % i, [128, 4, 128], BF16) for i in range(2)]
                sq_L = [c.sb("sq%d" % i_, [128, 512], F32) for i_ in range(2)]; sq = sq_L[0]; ssq_L = [c.sb("ssq%d" % i_, [128, 8], F32) for i_ in range(2)]; ssq = ssq_L[0]; rq_L = [c.sb("rq%d" % i_, [128, 8], F32) for i_ in range(2)]; rq = rq_L[0]
                qn_L = [c.sb("qn%d" % i_, [128, 512], F32) for i_ in range(2)]; qn = qn_L[0]; t1_L = [c.sb("t1%d" % i_, [128, 256], F32) for i_ in range(2)]; t1 = t1_L[0]; t2_L = [c.sb("t2%d" % i_, [128, 256], F32) for i_ in range(2)]; t2 = t2_L[0]
                qr_ = [c.sb("qr%d" % i, [128, 512], BF16) for i in range(2)]
                trs = [c.sb("trs%d" % i, [128, 4, 128], BF16) for i in range(2)]
                vst = [c.sb("vst%d" % i, [128, 8, 65], BF16) for i in range(2)]
                mvst = [c.sb("mvst%d" % i, [128, 4, 129], BF16) for i in range(2)]
                gst = [c.sb("gst%d" % i, [128, 512], BF16) for i in range(2)]
                mg1_L = [c.sb("mg1%d" % i_, [128, 16], F32) for i_ in range(2)]; mg1 = mg1_L[0]; mg2_L = [c.sb("mg2%d" % i_, [128, 8], F32) for i_ in range(2)]; mg2 = mg2_L[0]
                gpre_L = [c.sb("gpre%d" % i_, [128, 512], F32) for i_ in range(2)]; gpre = gpre_L[0]
                for st_ in vst:
                    c.V(lambda e: e.memset(st_.a, 1.0), [], [st_])
                for st_ in mvst:
                    c.V(lambda e: e.memset(st_.a, 1.0), [], [st_])
                chunks = [(0, 512, "Aq"), (512, 256, "Akv"), (768, 512, "Bq"), (1280, 512, "Bk"), (1792, 512, "Bv"),
                          (2304, 512, "Cq"), (2816, 512, "Ck"), (3328, 512, "Cv"), (3840, 512, "Co"), (4352, 16, "Cg")]
                chunks += [(4368 + 512 * m, 512, "Mg%d" % m) for m in range(6)]
                it = 0

                def qknorm(p_, ncol, gidx, dst):
                    nh = ncol // 64
                    c.A(lambda e: e.activation(out=sq.a[:, :ncol], in_=p_.a[:, :ncol], func=AF.Square), [p_], [sq])
                    c.V(lambda e: e.tensor_reduce(out=ssq.a[:, :nh], in_=sq.a[:, :ncol].rearrange("p (h d) -> p h d", d=64), axis=AX.X, op=ALU.add), [sq], [ssq])
                    c.V(lambda e: e.tensor_scalar(out=rq.a[:, :nh], in0=ssq.a[:, :nh], scalar1=1.0 / 64, scalar2=1e-6, op0=ALU.mult, op1=ALU.add), [ssq], [rq])
                    c.A(lambda e: e.activation(out=rq.a[:, :nh], in_=rq.a[:, :nh], func=AF.Sqrt), [rq], [rq])
                    c.V(lambda e: e.reciprocal(out=rq.a[:, :nh], in_=rq.a[:, :nh]), [rq], [rq])
                    c.V(lambda e: e.tensor_tensor(out=dst.rearrange("p (h d) -> p h d", d=64), in0=p_.a[:, :ncol].rearrange("p (h d) -> p h d", d=64),
                                                  in1=rq.a[:, :nh].unsqueeze(2).to_broadcast([128, nh, 64]), op=ALU.mult), [p_, rq], [qn])
                    c.V(lambda e: e.tensor_tensor(out=dst.rearrange("p (h d) -> p h d", d=64), in0=dst.rearrange("p (h d) -> p h d", d=64),
                                                  in1=gqk.a[:, gidx:gidx + 1, :].to_broadcast([128, nh, 64]), op=ALU.mult), [qn, gqk], [qn])

                def rope(src, nh, i, dst4):
                    s3 = src.rearrange("p (h t f) -> p h t f", t=2, f=32)
                    x1 = s3[:, :, 0, :]; x2 = s3[:, :, 1, :]
                    cb = cosb.a[:, i:i + 1, :].to_broadcast([128, nh, 32]); sb_ = sinb.a[:, i:i + 1, :].to_broadcast([128, nh, 32])
                    a1 = t1.a[:, :nh * 32].rearrange("p (h f) -> p h f", f=32); a2 = t2.a[:, :nh * 32].rearrange("p (h f) -> p h f", f=32)
                    c.V(lambda e: e.tensor_tensor(out=a1, in0=x1, in1=cb, op=ALU.mult), [qn, cosb], [t1])
                    c.G(lambda e: e.tensor_tensor(out=a2, in0=x2, in1=sb_, op=ALU.mult), [qn, sinb], [t2])
                    c.V(lambda e: e.tensor_tensor(out=dst4[:, :, 0, :], in0=a1, in1=a2, op=ALU.subtract), [t1, t2], [dstbuf[0]])
                    c.V(lambda e: e.tensor_tensor(out=a1, in0=x2, in1=cb, op=ALU.mult), [qn, cosb], [t1])
                    c.G(lambda e: e.tensor_tensor(out=a2, in0=x1, in1=sb_, op=ALU.mult), [qn, sinb], [t2])
                    c.V(lambda e: e.tensor_tensor(out=dst4[:, :, 1, :], in0=a1, in1=a2, op=ALU.add), [t1, t2], [dstbuf[0]])

                dstbuf = [None]

                def transposes(srcb, nblk, dstD, i, scale_src=None):
                    nonlocal it
                    pt = ptr[it % 2]; ts_ = trs[it % 2]
                    for m in range(nblk):
                        c.mm(lambda e: e.transpose(out=pt.a[:, m, :], in_=srcb.a[:, m * 128:(m + 1) * 128], identity=ident.a), [srcb, ident], [pt], last=(m == nblk - 1))
                    c.A(lambda e: e.copy(out=ts_.a[:, :nblk, :], in_=pt.a[:, :nblk, :]), [pt], [ts_])
                    if nblk == 1:
                        c.dma("sp", dstD.a[:, i * 128:(i + 1) * 128], ts_.a[:, 0, :], dstD, ts_)
                    else:
                        c.dma("sp", dstD.a[:, :, i * 128:(i + 1) * 128], ts_.a[:, :nblk, :], dstD, ts_)

                for ci, (c0, cw, kind) in enumerate(chunks):
                    w_ = wc[ci % 2]
                    c.dma("pool", w_.a[:, :, :cw], w_in.a[l, :, c0:c0 + cw].rearrange("(k p) n -> p k n", p=128), w_, w_in)
                    for i in range(NT):
                        it += 1
                        p_ = pp[it % 2]
                        sq = sq_L[it % 2]; ssq = ssq_L[it % 2]; rq = rq_L[it % 2]; qn = qn_L[it % 2]; t1 = t1_L[it % 2]; t2 = t2_L[it % 2]; mg1 = mg1_L[it % 2]; mg2 = mg2_L[it % 2]; gpre = gpre_L[it % 2]
                        for k in range(8):
                            c.mm(lambda e: e.matmul(p_.a[:, :cw], lhsT=hT.a[:, k, i * 128:(i + 1) * 128], rhs=w_.a[:, k, :cw], start=(k == 0), stop=(k == 7)), [hT, w_], [p_], last=(k == 7))
                        tok = slice(i * 128, (i + 1) * 128)
                        if kind == "Aq":
                            qknorm(p_, 512, 0, qn.a[:, :512])
                            q_ = qr_[it % 2]; dstbuf[0] = q_
                            d5 = q_.a.rearrange("p (m g t f) -> p g m t f", g=2, t=2, f=32)
                            for g in range(2):
                                rope(qn.a[:, g * 256:(g + 1) * 256], 4, i, d5[:, g])
                            transposes(q_, 4, QAT, i)
                        elif kind == "Akv":
                            qknorm(p_, 128, 1, qn.a[:, :128])
                            q_ = qr_[it % 2]; dstbuf[0] = q_
                            rope(qn.a[:, :128], 2, i, q_.a[:, :128].rearrange("p (h t f) -> p h t f", t=2, f=32))
                            transposes(q_, 1, KAT, i)
                            v_ = vst[it % 2]
                            c.A(lambda e: e.copy(out=v_.a[:, 0:2, 0:64], in_=p_.a[:, 128:256].rearrange("p (h d) -> p h d", d=64)), [p_], [v_])
                            c.dma("sp", VA.a[tok], v_.a[:, 0:2, :], VA, v_)
                        elif kind in ("Bq", "Bk"):
                            qknorm(p_, 512, 2 if kind == "Bq" else 3, qn.a[:, :512])
                            q_ = qr_[it % 2]
                            c.A(lambda e: e.copy(out=q_.a, in_=qn.a[:, :512]), [qn], [q_])
                            transposes(q_, 4, QBT if kind == "Bq" else KBT, i)
                        elif kind == "Bv":
                            v_ = vst[it % 2]
                            c.A(lambda e: e.copy(out=v_.a[:, :, 0:64], in_=p_.a.rearrange("p (h d) -> p h d", d=64)), [p_], [v_])
                            c.dma("sp", VB.a[tok], v_.a, VB, v_)
                        elif kind in ("Cq", "Ck"):
                            q_ = qr_[it % 2]
                            c.A(lambda e: e.activation(out=q_.a, in_=p_.a, func=AF.Identity, scale=(1.0 if kind == "Cq" else 128 ** -0.5)), [p_], [q_])
                            if kind == "Ck":
                                c.dma("sp", MK.a[tok], q_.a, MK, q_)
                            transposes(q_, 4, MQT if kind == "Cq" else MKT, i)
                        elif kind == "Cv":
                            v_ = mvst[it % 2]
                            c.A(lambda e: e.copy(out=v_.a[:, :, 0:128], in_=p_.a.rearrange("p (h d) -> p h d", d=128)), [p_], [v_])
                            c.dma("sp", MV.a[tok], v_.a, MV, v_)
                        elif kind == "Co":
                            g_ = gst[it % 2]
                            c.A(lambda e: e.activation(out=g_.a, in_=p_.a, func=AF.Sigmoid), [p_], [g_])
                            c.dma("sp", MO.a[tok], g_.a, MO, g_)
                        elif kind == "Cg":
                            c.V(lambda e: e.tensor_tensor(out=mg1.a, in0=p_.a[:, :16], in1=bml.a, op=ALU.add), [p_, bml], [mg1])
                            fv = mg1.a.rearrange("p (d t h) -> p d t h", d=2, t=2)[:, :, 1, :]
                            m2 = mg2.a.rearrange("p (d h) -> p d h", d=2)
                            c.A(lambda e: e.activation(out=m2, in_=fv, func=AF.Exp, scale=-1.0), [mg1], [mg2])
                            c.A(lambda e: e.activation(out=m2, in_=m2, func=AF.Ln, bias=1.0), [mg2], [mg2])
                            c.V(lambda e: e.tensor_scalar(out=fv, in0=m2, scalar1=-1.0, scalar2=None, op0=ALU.mult), [mg2], [mg1])
                            c.dma("sp", MG.a[tok], mg1.a, MG, mg1)
                        else:
                            m = int(kind[2:])
                            g_ = gst[it % 2]
                            c.V(lambda e: e.tensor_tensor(out=gpre.a, in0=p_.a, in1=bmg.a[:, m * 512:(m + 1) * 512], op=ALU.add), [p_, bmg], [gpre])
                            c.A(lambda e: e.activation(out=g_.a, in_=gpre.a, func=AF.Sigmoid), [gpre], [g_])
                            c.dma("sp", GATES.a[tok, m * 512:(m + 1) * 512], g_.a, GATES, g_)
                c.pop()
                if stop_after == "p1":
                    break

                c.push()
                kat = c.sb("kat", [128, T], BF16, grp="ka"); va = c.sb("va", [128, NT, 2, 65], BF16, grp="ka")
                c.dma("sp", kat.a, KAT.a, kat, KAT)
                c.dma("sp", va.a, VA.a.rearrange("(i p) g d -> p i g d", p=128), va, VA)
                qa = [[c.sb("qa%d_%d" % (i, g), [128, 4, 128], BF16, grp="qa%d" % i) for g in range(2)] for i in range(2)]
                for i in range(2):
                    for g in range(2):
                        c.V(lambda e: e.memset(qa[i][g].a, 0.0), [], [qa[i][g]])
                ND = 2
                pS = [c.ps("pS%d" % i, [128, 1024], F32) for i in range(ND)]
                pe_ = [c.sb("pe%d" % i, [128, 1024], BF16) for i in range(3)]
                pO = [c.ps("pO%d" % i, [128, 512], F32) for i in range(2)]
                pbc = c.ps("pbc", [128, 512], F32)
                ones_f = c.sb("ones_f", [128, 64], F32)
                c.V(lambda e: e.memset(ones_f.a, 1.0), [], [ones_f])
                dn = [c.sb("dn%d" % i, [128, 512], F32) for i in range(2)]
                bcs = [c.sb("bcs%d" % i, [64, 512], F32) for i in range(2)]
                yTa = [c.sb("yTa%d" % i, [64, 512], BF16) for i in range(2)]
                steps = []
                for i in range(first_tile, NT):
                    kts = list(range(0, 2)) if i < 2 else list(range(0, NT))
                    prs = [kts[j:j + 2] for j in range(0, len(kts), 2)]
                    for g in range(2):
                        for pi, pr in enumerate(prs):
                            steps.append((i, g, pr, pi == 0, pi == len(prs) - 1))

                def a_s1(si):
                    i, g, pr, first, last = steps[si]
                    q_ = qa[i % 2][g]
                    if g == 0 and first:
                        for g2 in range(2):
                            c.dma("sp", qa[i % 2][g2].a[g2 * 64:(g2 + 1) * 64], QAT.a[g2 * 64:(g2 + 1) * 64, :, i * 128:(i + 1) * 128], qa[i % 2][g2], QAT)
                    ps_ = pS[si % ND]; e_ = pe_[si % 3]
                    for j, kt in enumerate(pr):
                        c.mm(lambda e: e.matmul(ps_.a[:, j * 512:(j + 1) * 512], lhsT=kat.a[:, kt * 128:(kt + 1) * 128], rhs=q_.a.rearrange("p m t -> p (m t)"), start=True, stop=True), [kat, q_], [ps_], last=(j == len(pr) - 1))
                    c.A(lambda e: e.activation(out=e_.a, in_=ps_.a, func=AF.Exp, scale=0.125), [ps_], [e_])

                def a_s2(si):
                    i, g, pr, first, last = steps[si]
                    e_ = pe_[si % 3]
                    po = pO[g]
                    for j, kt in enumerate(pr):
                        c.mm(lambda e: e.matmul(po.a[0:65, :], lhsT=va.a[:, kt, g, :], rhs=e_.a[:, j * 512:(j + 1) * 512], start=(first and j == 0), stop=(last and j == len(pr) - 1)), [e_, va], [po], last=(j == len(pr) - 1))
                    if not last:
                        return
                    d_ = dn[g]; b_ = bcs[g]; y_ = yTa[g]
                    c.A(lambda e: e.copy(out=d_.a[64:65, :], in_=po.a[64:65, :]), [po], [d_])
                    c.V(lambda e: e.reciprocal(out=d_.a[64:65, :], in_=d_.a[64:65, :]), [d_], [d_])
                    c.mm(lambda e: e.matmul(pbc.a[0:64, :], lhsT=ones_f.a[64:65, :], rhs=d_.a[64:65, :], start=True, stop=True), [ones_f, d_], [pbc])
                    c.A(lambda e: e.copy(out=b_.a, in_=pbc.a[0:64, :]), [pbc], [b_])
                    c.V(lambda e: e.tensor_tensor(out=y_.a, in0=po.a[0:64, :], in1=b_.a, op=ALU.mult), [po, b_], [y_])
                    for m in range(4):
                        c.dma("sp", YAT.a[g * 2 + m // 2, (m % 2) * 64:(m % 2) * 64 + 64, i * 128:(i + 1) * 128], y_.a[:, m * 128:(m + 1) * 128], YAT, y_)

                pipeline(len(steps), a_s1, a_s2, ND, s1_first=True)
                c.pop()
                if stop_after == "p2a":
                    break

                c.push()
                kbt = c.sb("kbt", [128, 4, T], BF16, grp="kb"); vb = c.sb("vb", [128, NT, 8, 65], BF16, grp="kb")
                c.dma("sp", kbt.a, KBT.a, kbt, KBT)
                c.dma("sp", vb.a, VB.a.rearrange("(i p) h d -> p i h d", p=128), vb, VB)
                TP = c.sb("TP", [128, 8, 15, 64], F32, grp="kb")
                c.dma("sp", TP.a, TPD.a, TP, TPD)
                tabI = c.sb("tabI", [128, 5, 8, 128], F32); tabE = c.sb("tabE", [128, 5, 8, 128], F32)

                def build_tab(tab, plan):
                    for di, (kt, blocks) in enumerate(plan):
                        for (a, b2), dr in blocks.items():
                            o = tab.a[a * 64:(a + 1) * 64, di, :, b2 * 64:(b2 + 1) * 64]
                            if dr is None:
                                c.G(lambda e: e.memset(o, NEG), [], [tab])
                            else:
                                c.V(lambda e: e.tensor_copy(out=o, in_=TP.a[a * 64:(a + 1) * 64, :, dr, :]), [TP], [tab])

                build_tab(tabI, na_plan(5))
                qz = [[c.sb("qz%d_%d" % (i, p), [128, 4, 128], BF16, grp="qz%d" % i) for p in range(2)] for i in range(2)]
                for i in range(2):
                    for p in range(2):
                        c.V(lambda e: e.memset(qz[i][p].a, 0.0), [], [qz[i][p]])
                pS = [c.ps("pSb%d" % i, [128, 8, 128], F32) for i in range(2)]
                sS = c.sb("sS", [128, 8, 128], F32)
                pe_ = [c.sb("peb%d" % i, [128, 8, 128], BF16) for i in range(2)]
                pOb = c.ps("pOb", [128, 2, 512], F32)
                pO3 = [pOb.a[:, hh, 0:260].rearrange("p (m d) -> p m d", d=65) for hh in range(2)]
                rd = c.sb("rdb", [128, 4], F32)
                ya = [c.sb("yb%d" % i, [128, 512], BF16) for i in range(2)]
                pY = c.ps("pYb", [128, 4, 128], BF16); yT = c.sb("yTb", [128, 4, 128], BF16)
                steps = []
                for i in range(first_tile, NT):
                    keys = [(0, None, None), (1, None, None)]
                    plan = None
                    if i >= 2:
                        j = i - 2
                        plan = na_plan(j)
                        tab = tabI if 2 <= j <= 29 else tabE
                        keys = [(kt + 2, tab, di) for di, (kt, _) in enumerate(plan)] + keys
                    for ki, (kt, tab, di) in enumerate(keys):
                        steps.append((i, ki, kt, tab, di, len(keys), plan))

                def b_s1(si):
                    i, ki, kt, tab, di, nk, plan = steps[si]
                    qz_ = qz[i % 2]
                    if ki == 0:
                        for p in range(2):
                            c.dma("sp", qz_[p].a[p * 64:(p + 1) * 64], QBT.a[p * 64:(p + 1) * 64, :, i * 128:(i + 1) * 128], qz_[p], QBT)
                        if tab is tabE:
                            build_tab(tabE, plan)
                    ps_ = pS[si % 2]; e_ = pe_[si % 2]
                    for h in range(8):
                        par = h % 2; pr = h // 2
                        c.mm(lambda e: e.matmul(ps_.a[:, h, :], lhsT=kbt.a[:, pr, kt * 128:(kt + 1) * 128], rhs=qz_[par].a[:, pr, :], start=True, stop=True), [kbt, qz_[par]], [ps_], last=(h == 7))
                    if tab is not None:
                        c.V(lambda e: e.scalar_tensor_tensor(out=sS.a, in0=ps_.a, scalar=0.125, in1=tab.a[:, di], op0=ALU.mult, op1=ALU.add), [ps_, tab], [sS])
                        c.A(lambda e: e.activation(out=e_.a, in_=sS.a, func=AF.Exp), [sS], [e_])
                    else:
                        c.A(lambda e: e.activation(out=e_.a, in_=ps_.a, func=AF.Exp, scale=0.125), [ps_], [e_])

                def b_s2(si):
                    i, ki, kt, tab, di, nk, plan = steps[si]
                    e_ = pe_[si % 2]; y_ = ya[i % 2]
                    for h in range(8):
                        c.mm(lambda e: e.matmul(pO3[h // 4][:, h % 4, :], lhsT=e_.a[:, h, :], rhs=vb.a[:, kt, h, :], start=(ki == 0 and h % 4 == 0), stop=(ki == nk - 1)), [e_, vb], [pOb], last=(h == 7))
                    if ki != nk - 1:
                        return
                    for hh in range(2):
                        po3 = pO3[hh]
                        c.V(lambda e: e.reciprocal(out=rd.a, in_=po3[:, :, 64]), [pOb], [rd])
                        c.V(lambda e: e.tensor_tensor(out=y_.a[:, hh * 256:(hh + 1) * 256].rearrange("p (m d) -> p m d", d=64), in0=po3[:, :, 0:64], in1=rd.a.unsqueeze(2).to_broadcast([128, 4, 64]), op=ALU.mult), [pOb, rd], [y_])
                    for m in range(4):
                        c.mm(lambda e: e.transpose(out=pY.a[:, m, :], in_=y_.a[:, m * 128:(m + 1) * 128], identity=ident.a), [y_, ident], [pY], last=(m == 3))
                    c.A(lambda e: e.copy(out=yT.a, in_=pY.a), [pY], [yT])
                    c.dma("sp", YBT.a[:, :, i * 128:(i + 1) * 128].rearrange("m p t -> p m t"), yT.a, YBT, yT)

                pipeline(len(steps), b_s1, b_s2, 2)
                c.pop()
                if stop_after == "p2b":
                    break

                c.push()
                Cst = c.sb("Cst", [128, 4, 129], F32)
                Cb = [c.sb("Cb%d" % i, [128, 4, 129], BF16) for i in range(2)]
                mq = [c.sb("mq%d" % i, [128, 4, 128], BF16, grp="m%d" % i) for i in range(2)]
                mk = [c.sb("mk%d" % i, [128, 4, 128], BF16, grp="m%d" % i) for i in range(2)]
                mkt = [c.sb("mkt%d" % i, [128, 512], BF16, grp="m%d" % i) for i in range(2)]
                mv = [c.sb("mv%d" % i, [128, 4, 129], BF16, grp="m%d" % i) for i in range(2)]
                mg = [c.sb("mg%d" % i, [128, 16], F32, grp="m%d" % i) for i in range(2)]
                mo = [c.sb("mo%d" % i, [128, 512], BF16, grp="m%d" % i) for i in range(2)]
                hs = [c.sb("hs%d" % i, [128, 512], F32, grp="m%d" % i) for i in range(2)]
                pb = c.ps("pb", [128, 4], F32); pbB = c.ps("pbB", [128, 4, 128], F32); pST = c.ps("pST", [128, 4, 128], F32)
                pH = c.ps("pH", [128, 2, 512], F32); pC = c.ps("pC", [128, 2, 512], F32)
                pY = c.ps("pYc", [128, 4, 128], BF16)
                biasc = [c.sb("biasc%d" % i, [128, 4], F32) for i in range(2)]
                gcol = [c.sb("gcol%d" % i, [128, 4], F32) for i in range(2)]
                ebend = [c.sb("ebend%d" % i, [128, 4], F32) for i in range(2)]
                EB_L = [c.sb("EB%d" % i_, [128, 4, 128], F32) for i_ in range(2)]; EB = EB_L[0]
                qs = [c.sb("qs%d" % i, [128, 4, 128], BF16) for i in range(2)]
                DT_L = [c.sb("DT%d" % i_, [128, 4, 128], F32) for i_ in range(2)]; DT = DT_L[0]; DTm_L = [c.sb("DTm%d" % i_, [128, 4, 128], F32) for i_ in range(2)]; DTm = DTm_L[0]
                SD = [c.sb("SD%d" % i, [128, 4, 128], BF16) for i in range(2)]
                kg_L = [c.sb("kg%d" % i_, [128, 4, 128], BF16) for i_ in range(2)]; kg = kg_L[0]
                pCs = [c.sb("pCs%d" % i, [128, 2, 258], F32) for i in range(2)]
                den_L = [c.sb("den%d" % i_, [128, 4], F32) for i_ in range(2)]; den = den_L[0]; hout = [c.sb("hout%d" % i, [128, 512], F32) for i in range(2)]
                hsq_L = [c.sb("hsq%d" % i_, [128, 512], F32) for i_ in range(2)]; hsq = hsq_L[0]; hss_L = [c.sb("hss%d" % i_, [128, 4], F32) for i_ in range(2)]; hss = hss_L[0]; hn_L = [c.sb("hn%d" % i_, [128, 512], F32) for i_ in range(2)]; hn = hn_L[0]
                yc_L = [c.sb("yc%d" % i_, [128, 512], BF16) for i_ in range(2)]; yc = yc_L[0]; yT_L = [c.sb("yTc%d" % i_, [128, 4, 128], BF16) for i_ in range(2)]; yT = yT_L[0]

                def hreg(pt, h):
                    return pt.a[:, h // 2, (h % 2) * 129:(h % 2) * 129 + 129]

                def sreg(pt, h):
                    return pt.a[:, h // 2, (h % 2) * 129:(h % 2) * 129 + 129]

                for d in range(2):
                    order = list(range(NT)) if d == 0 else [1, 0] + list(range(NT - 1, 1, -1))
                    tend = 127 if d == 0 else 0
                    Ud = U.a[:, d, :]
                    c.V(lambda e: e.memset(Cst.a, 0.0), [], [Cst])
                    c.V(lambda e: e.memset(Cb[1].a, 0.0), [], [Cb[1]])

                    def l_s1(n):
                        kg_c = kg_L[n % 2]; EB_c = EB_L[n % 2]; DT_c = DT_L[n % 2]; DTm_c = DTm_L[n % 2]
                        i = order[n]
                        q_ = mq[n % 2]; k_ = mk[n % 2]; kt_ = mkt[n % 2]; v_ = mv[n % 2]; g_ = mg[n % 2]
                        bc_ = biasc[n % 2]; gc_ = gcol[n % 2]; eb_ = ebend[n % 2]; qs_ = qs[n % 2]; SD_ = SD[n % 2]; pcs = pCs[n % 2]
                        tok = slice(i * 128, (i + 1) * 128)
                        need_out = i >= first_tile
                        c.dma("sp", q_.a, MQT.a[:, :, tok], q_, MQT)
                        c.dma("sp", k_.a, MKT.a[:, :, tok], k_, MKT)
                        c.dma("sp", kt_.a, MK.a[tok], kt_, MK)
                        c.dma("sp", v_.a, MV.a[tok], v_, MV)
                        c.dma("sp", g_.a, MG.a[tok], g_, MG)
                        if need_out and d == 1:
                            c.dma("sp", hs[n % 2].a, HS.a[tok], hs[n % 2], HS)
                            c.dma("sp", mo[n % 2].a, MO.a[tok], mo[n % 2], MO)
                        gv = g_.a.rearrange("p (d t h) -> p d t h", d=2, t=2)
                        ig = gv[:, d, 0, :]; lf = gv[:, d, 1, :]
                        c.mm(lambda e: e.matmul(pb.a, lhsT=Ud, rhs=lf, start=True, stop=True), [U, g_], [pb])
                        for h in range(4):
                            c.mm(lambda e: e.matmul(pbB.a[:, h, :], lhsT=gv[:, d, 1, h:h + 1].to_broadcast([128, 128]), rhs=Ud, start=True, stop=True), [g_, U], [pbB], last=(h == 3))
                        if need_out:
                            for h in range(4):
                                c.mm(lambda e: e.matmul(pST.a[:, h, :], lhsT=k_.a[:, h, :], rhs=q_.a[:, h, :], start=True, stop=True), [k_, q_], [pST], last=(h == 3))
                        c.V(lambda e: e.tensor_tensor(out=bc_.a, in0=ig, in1=pb.a, op=ALU.subtract), [g_, pb], [bc_])
                        c.V(lambda e: e.tensor_tensor(out=gc_.a, in0=pbB.a[:, :, tend], in1=bc_.a, op=ALU.add), [pbB, bc_], [gc_])
                        c.A(lambda e: e.activation(out=gc_.a, in_=gc_.a, func=AF.Exp), [gc_], [gc_])
                        c.A(lambda e: e.activation(out=eb_.a, in_=pbB.a[:, :, tend], func=AF.Exp), [pbB], [eb_])
                        c.V(lambda e: e.tensor_tensor(out=kg_c.a, in0=kt_.a.rearrange("p (h d) -> p h d", d=128), in1=gc_.a.unsqueeze(2).to_broadcast([128, 4, 128]), op=ALU.mult), [kt_, gc_], [kg_c])
                        for h in range(4):
                            c.mm(lambda e: e.matmul(hreg(pC, h), lhsT=kg_c.a[:, h, :], rhs=v_.a[:, h, :], start=True, stop=True), [kg_c, v_], [pC], last=(h == 3))
                        for hh in range(2):
                            c.A(lambda e: e.copy(out=pcs.a[:, hh, :], in_=pC.a[:, hh, 0:258]), [pC], [pcs])
                        if need_out:
                            c.A(lambda e: e.activation(out=EB_c.a, in_=pbB.a, func=AF.Exp), [pbB], [EB_c])
                            c.V(lambda e: e.tensor_tensor(out=qs_.a, in0=q_.a, in1=EB_c.a, op=ALU.mult), [q_, EB_c], [qs_])
                            for h in range(4):
                                c.A(lambda e: e.activation(out=DT_c.a[:, h, :], in_=pbB.a[:, h, :], func=AF.Exp, bias=bc_.a[:, h:h + 1]), [pbB, bc_], [DT_c])
                            c.G(lambda e: e.tensor_tensor(out=DTm_c.a, in0=DT_c.a, in1=U.a[:, d:d + 1, :].to_broadcast([128, 4, 128]), op=ALU.mult), [DT_c, U], [DTm_c])
                            c.V(lambda e: e.tensor_tensor(out=SD_.a, in0=DTm_c.a, in1=pST.a, op=ALU.mult), [DTm_c, pST], [SD_])

                    def l_s2(n):
                        den_c = den_L[n % 2]; hsq_c = hsq_L[n % 2]; hss_c = hss_L[n % 2]; hn_c = hn_L[n % 2]; yc_c = yc_L[n % 2]; yT_c = yT_L[n % 2]
                        i = order[n]
                        v_ = mv[n % 2]; eb_ = ebend[n % 2]; qs_ = qs[n % 2]; SD_ = SD[n % 2]; pcs = pCs[n % 2]
                        cb_old = Cb[(n + 1) % 2]; cb_new = Cb[n % 2]
                        ho = hout[n % 2]
                        tok = slice(i * 128, (i + 1) * 128)
                        need_out = i >= first_tile
                        if need_out:
                            for h in range(4):
                                c.mm(lambda e: e.matmul(hreg(pH, h), lhsT=qs_.a[:, h, :], rhs=cb_old.a[:, h, :], start=True, stop=False), [qs_, cb_old], [pH], last=False)
                                c.mm(lambda e: e.matmul(hreg(pH, h), lhsT=SD_.a[:, h, :], rhs=v_.a[:, h, :], start=False, stop=True), [SD_, v_], [pH], last=(h == 3))
                        for h in range(4):
                            c.V(lambda e: e.scalar_tensor_tensor(out=Cst.a[:, h, :], in0=Cst.a[:, h, :], scalar=eb_.a[:, h:h + 1], in1=sreg(pcs, h), op0=ALU.mult, op1=ALU.add), [Cst, eb_, pcs], [Cst])
                        c.A(lambda e: e.copy(out=cb_new.a, in_=Cst.a), [Cst], [cb_new])
                        if not need_out:
                            return
                        for h in range(4):
                            c.A(lambda e: e.activation(out=den_c.a[:, h:h + 1], in_=hreg(pH, h)[:, 128:129], func=AF.Abs), [pH], [den_c])
                        c.V(lambda e: e.tensor_scalar_max(out=den_c.a, in0=den_c.a, scalar1=1.0), [den_c], [den_c])
                        c.V(lambda e: e.reciprocal(out=den_c.a, in_=den_c.a), [den_c], [den_c])
                        for h in range(4):
                            c.V(lambda e: e.tensor_scalar(out=ho.a[:, h * 128:(h + 1) * 128], in0=hreg(pH, h)[:, 0:128], scalar1=den_c.a[:, h:h + 1], scalar2=None, op0=ALU.mult), [pH, den_c], [ho])
                        if d == 0:
                            c.dma("sp", HS.a[tok], ho.a, HS, ho)
                            return
                        h_ = hs[n % 2]; o_ = mo[n % 2]
                        c.G(lambda e: e.tensor_tensor(out=ho.a, in0=ho.a, in1=h_.a, op=ALU.add), [ho, h_], [ho])
                        c.A(lambda e: e.activation(out=hsq_c.a, in_=ho.a, func=AF.Square), [ho], [hsq_c])
                        c.V(lambda e: e.tensor_reduce(out=hss_c.a, in_=hsq_c.a.rearrange("p (h d) -> p h d", d=128), axis=AX.X, op=ALU.add), [hsq_c], [hss_c])
                        c.V(lambda e: e.tensor_scalar(out=hss_c.a, in0=hss_c.a, scalar1=1.0 / 128, scalar2=1e-6, op0=ALU.mult, op1=ALU.add), [hss_c], [hss_c])
                        c.A(lambda e: e.activation(out=hss_c.a, in_=hss_c.a, func=AF.Sqrt), [hss_c], [hss_c])
                        c.V(lambda e: e.reciprocal(out=hss_c.a, in_=hss_c.a), [hss_c], [hss_c])
                        c.V(lambda e: e.tensor_tensor(out=hn_c.a.rearrange("p (h d) -> p h d", d=128), in0=ho.a.rearrange("p (h d) -> p h d", d=128), in1=hss_c.a.unsqueeze(2).to_broadcast([128, 4, 128]), op=ALU.mult), [ho, hss_c], [hn_c])
                        c.G(lambda e: e.tensor_tensor(out=hn_c.a, in0=hn_c.a, in1=gml.a, op=ALU.mult), [hn_c, gml], [hn_c])
                        c.V(lambda e: e.tensor_tensor(out=yc_c.a, in0=hn_c.a, in1=o_.a, op=ALU.mult), [hn_c, o_], [yc_c])
                        for m in range(4):
                            c.mm(lambda e: e.transpose(out=pY.a[:, m, :], in_=yc_c.a[:, m * 128:(m + 1) * 128], identity=ident.a), [yc_c, ident], [pY], last=(m == 3))
                        c.A(lambda e: e.copy(out=yT_c.a, in_=pY.a), [pY], [yT_c])
                        c.dma("sp", YCT.a[:, :, tok].rearrange("m p t -> p m t"), yT_c.a, YCT, yT_c)

                    pipeline(NT, l_s1, l_s2, 2)
                c.pop()
                if stop_after == "p2c":
                    break

                c.push()
                wbr = c.sb("wbr", [128, 3, 4, D], BF16, grp="w3"); wo = c.sb("wo", [128, 8, D], BF16, grp="w3")
                for br in range(3):
                    c.dma("pool", wbr.a[:, br], w_branch.a[l, br].rearrange("(k p) n -> p k n", p=128), wbr, w_branch)
                c.dma("pool", wo.a, w_out.a[l].rearrange("(k p) n -> p k n", p=128), wo, w_out)
                ybr = [c.sb("ybr%d" % i, [128, 3, 4, 128], BF16, grp="l3%d" % i) for i in range(2)]
                gt_ = [c.sb("gt%d" % i, [128, 3072], BF16, grp="l3%d" % i) for i in range(2)]
                xt = [c.sb("x3%d" % i, [128, D], F32, grp="l3%d" % i) for i in range(2)]
                pB = [c.ps("pB%d" % i, [128, 512], F32) for i in range(2)]
                mrg_L = [c.sb("mrg%d" % i_, [128, D], F32) for i_ in range(2)]; mrg = mrg_L[0]; mtmp_L = [c.sb("mtmp%d" % i_, [128, 512], F32) for i_ in range(2)]; mtmp = mtmp_L[0]; mrb_L = [c.sb("mrb%d" % i_, [128, D], BF16) for i_ in range(2)]; mrb = mrb_L[0]
                pT = c.ps("pT3", [128, 8, 128], BF16); mT_L = [c.sb("mT%d" % i_, [128, 8, 128], BF16) for i_ in range(2)]; mT = mT_L[0]
                pYo = c.ps("pYo", [128, D], F32)
                x1 = [c.sb("x1%d" % i, [128, D], F32) for i in range(2)]
                junk_L = [c.sb("junk3%d" % i_, [128, D], F32) for i_ in range(2)]; junk = junk_L[0]; ss_L = [c.sb("ss3%d" % i_, [128, 1], F32) for i_ in range(2)]; ss = ss_L[0]; rstd_L = [c.sb("rstd3%d" % i_, [128, 1], F32) for i_ in range(2)]; rstd = rstd_L[0]
                xnf_L = [c.sb("xnf%d" % i_, [128, D], F32) for i_ in range(2)]; xnf = xnf_L[0]
                pTf = c.ps("pTf", [128, 8, 128], F32)
                h2f_L = [c.sb("h2f%d" % i_, [128, 8, 128], F32) for i_ in range(2)]; h2f = h2f_L[0]; h2b = [c.sb("h2b%d" % i, [128, 8, 128], BF16) for i in range(2)]
                pL = c.ps("pL", [128, 36], F32)
                lg_L = [c.sb("lg%d" % i_, [128, 36], F32) for i_ in range(2)]; lg = lg_L[0]; gmax_L = [c.sb("gmax%d" % i_, [128, 1], F32) for i_ in range(2)]; gmax = gmax_L[0]; ngmax_L = [c.sb("ngmax%d" % i_, [128, 1], F32) for i_ in range(2)]; ngmax = ngmax_L[0]
                eg_L = [c.sb("eg%d" % i_, [128, 4], F32) for i_ in range(2)]; eg = eg_L[0]; sg_L = [c.sb("sg%d" % i_, [128, 1], F32) for i_ in range(2)]; sg = sg_L[0]; ohg_L = [c.sb("ohg%d" % i_, [128, 4], F32) for i_ in range(2)]; ohg = ohg_L[0]
                lem_L = [c.sb("lem%d" % i_, [128, 4, 8], F32) for i_ in range(2)]; lem = lem_L[0]; les_L = [c.sb("les%d" % i_, [128, 8], F32) for i_ in range(2)]; les = les_L[0]; m8_L = [c.sb("m8%d" % i_, [128, 8], F32) for i_ in range(2)]; m8 = m8_L[0]
                nv0_L = [c.sb("nv0%d" % i_, [128, 1], F32) for i_ in range(2)]; nv0 = nv0_L[0]; e8_L = [c.sb("e8%d" % i_, [128, 8], F32) for i_ in range(2)]; e8 = e8_L[0]; mk2_L = [c.sb("mk2%d" % i_, [128, 8], F32) for i_ in range(2)]; mk2 = mk2_L[0]
                w8_L = [c.sb("w8%d" % i_, [128, 8], F32) for i_ in range(2)]; w8 = w8_L[0]; sden_L = [c.sb("sden%d" % i_, [128, 1], F32) for i_ in range(2)]; sden = sden_L[0]
                dw = [c.sb("dw%d" % i, [128, 4, 8], F32) for i in range(2)]
                for i in range(first_tile, NT):
                    tok = slice(i * 128, (i + 1) * 128)
                    j3 = 2 if i < 2 else b
                    mrg = mrg_L[i % 2]; mtmp = mtmp_L[i % 2]; mrb = mrb_L[i % 2]; mT = mT_L[i % 2]; junk = junk_L[i % 2]; ss = ss_L[i % 2]; rstd = rstd_L[i % 2]; xnf = xnf_L[i % 2]; h2f = h2f_L[i % 2]; lg = lg_L[i % 2]; gmax = gmax_L[i % 2]; ngmax = ngmax_L[i % 2]; eg = eg_L[i % 2]; sg = sg_L[i % 2]; ohg = ohg_L[i % 2]; lem = lem_L[i % 2]; les = les_L[i % 2]; m8 = m8_L[i % 2]; nv0 = nv0_L[i % 2]; e8 = e8_L[i % 2]; mk2 = mk2_L[i % 2]; w8 = w8_L[i % 2]; sden = sden_L[i % 2]
                    y_ = ybr[i % 2]; g_ = gt_[i % 2]; x_ = xt[i % 2]; xo = x1[i % 2]; hb = h2b[i % 2]; dw_ = dw[i % 2]
                    for br, YT_ in enumerate((YAT, YBT, YCT)):
                        c.dma("sp", y_.a[:, br], YT_.a[:, :, tok].rearrange("m p t -> p m t"), y_, YT_)
                    c.dma("sp", g_.a, GATES.a[tok], g_, GATES)
                    src, ap = tile_src(l, b, i)
                    c.dma("sp", x_.a, ap, x_, src)
                    for half in range(2):
                        cs = slice(half * 512, (half + 1) * 512)
                        for br in range(3):
                            p_ = pB[(half * 3 + br) % 2]
                            for k in range(4):
                                c.mm(lambda e: e.matmul(p_.a, lhsT=y_.a[:, br, k, :], rhs=wbr.a[:, br, k, cs], start=(k == 0), stop=(k == 3)), [y_, wbr], [p_], last=(k == 3))
                            gsl = g_.a[:, br * 1024 + half * 512: br * 1024 + (half + 1) * 512]
                            if br == 0:
                                c.V(lambda e: e.tensor_tensor(out=mrg.a[:, cs], in0=p_.a, in1=gsl, op=ALU.mult), [p_, g_], [mrg])
                            else:
                                c.V(lambda e: e.tensor_tensor(out=mtmp.a, in0=p_.a, in1=gsl, op=ALU.mult), [p_, g_], [mtmp])
                                c.G(lambda e: e.tensor_tensor(out=mrg.a[:, cs], in0=mrg.a[:, cs], in1=mtmp.a, op=ALU.add), [mrg, mtmp], [mrg])
                    c.A(lambda e: e.copy(out=mrb.a, in_=mrg.a), [mrg], [mrb])
                    for k in range(8):
                        c.mm(lambda e: e.transpose(out=pT.a[:, k, :], in_=mrb.a[:, k * 128:(k + 1) * 128], identity=ident.a), [mrb, ident], [pT], last=(k == 7))
                    c.A(lambda e: e.copy(out=mT.a, in_=pT.a), [pT], [mT])
                    for half in range(2):
                        cs = slice(half * 512, (half + 1) * 512)
                        for k in range(8):
                            c.mm(lambda e: e.matmul(pYo.a[:, cs], lhsT=mT.a[:, k, :], rhs=wo.a[:, k, cs], start=(k == 0), stop=(k == 7)), [mT, wo], [pYo], last=(k == 7 and half == 1))
                    c.V(lambda e: e.tensor_tensor(out=xo.a, in0=pYo.a, in1=GT.a[:, 0, j3, :], op=ALU.mult), [pYo, GT], [xo])
                    c.G(lambda e: e.tensor_tensor(out=xo.a, in0=xo.a, in1=x_.a, op=ALU.add), [xo, x_], [xo])
                    c.dma("sp", XS.a[b, tok, :], xo.a, XS, xo)
                    c.V(lambda e: e.memset(ss.a, 0.0), [], [ss])
                    c.A(lambda e: e.activation(out=junk.a, in_=xo.a, func=AF.Square, accum_out=ss.a), [xo], [junk, ss])
                    c.V(lambda e: e.tensor_scalar(out=rstd.a, in0=ss.a, scalar1=1.0 / D, scalar2=1e-6, op0=ALU.mult, op1=ALU.add), [ss], [rstd])
                    c.A(lambda e: e.activation(out=rstd.a, in_=rstd.a, func=AF.Sqrt), [rstd], [rstd])
                    c.V(lambda e: e.reciprocal(out=rstd.a, in_=rstd.a), [rstd], [rstd])
                    c.V(lambda e: e.tensor_scalar(out=xnf.a, in0=xo.a, scalar1=rstd.a[:, 0:1], scalar2=None, op0=ALU.mult), [xo, rstd], [xnf])
                    for k in range(8):
                        c.mm(lambda e: e.transpose(out=pTf.a[:, k, :], in_=xnf.a[:, k * 128:(k + 1) * 128], identity=ident_f.a), [xnf, ident_f], [pTf], last=(k == 7))
                    for k in range(8):
                        c.V(lambda e: e.tensor_scalar(out=h2f.a[:, k, :], in0=pTf.a[:, k, :], scalar1=G2.a[:, k, j3:j3 + 1], scalar2=modT.a[:, 3, k, j3:j3 + 1], op0=ALU.mult, op1=ALU.add), [pTf, G2, modT], [h2f])
                    c.A(lambda e: e.copy(out=hb.a, in_=h2f.a), [h2f], [hb])
                    c.dma("sp", H2T.a[:, :, tok], hb.a, H2T, hb)
                    for k in range(8):
                        c.mm(lambda e: e.matmul(pL.a, lhsT=h2f.a[:, k, :], rhs=wrt.a[:, k, :], start=(k == 0), stop=(k == 7)), [h2f, wrt], [pL], last=(k == 7))
                    c.V(lambda e: e.tensor_tensor(out=lg.a, in0=pL.a, in1=brt.a, op=ALU.add), [pL, brt], [lg])
                    c.V(lambda e: e.tensor_reduce(out=gmax.a, in_=lg.a[:, 0:4], axis=AX.X, op=ALU.max), [lg], [gmax])
                    c.V(lambda e: e.tensor_scalar(out=ngmax.a, in0=gmax.a, scalar1=-1.0, scalar2=None, op0=ALU.mult), [gmax], [ngmax])
                    c.V(lambda e: e.memset(sg.a, 0.0), [], [sg])
                    c.A(lambda e: e.activation(out=eg.a, in_=lg.a[:, 0:4], func=AF.Exp, bias=ngmax.a[:, 0:1], accum_out=sg.a), [lg, ngmax], [eg, sg])
                    c.V(lambda e: e.tensor_scalar(out=ohg.a, in0=lg.a[:, 0:4], scalar1=gmax.a[:, 0:1], scalar2=None, op0=ALU.is_ge), [lg, gmax], [ohg])
                    c.V(lambda e: e.tensor_tensor(out=lem.a, in0=lg.a[:, 4:36].rearrange("p (g e) -> p g e", e=8), in1=ohg.a.unsqueeze(2).to_broadcast([128, 4, 8]), op=ALU.mult), [lg, ohg], [lem])
                    c.V(lambda e: e.tensor_reduce(out=les.a, in_=lem.a.rearrange("p g e -> p e g"), axis=AX.X, op=ALU.add), [lem], [les])
                    c.V(lambda e: e.max(out=m8.a, in_=les.a), [les], [m8])
                    c.V(lambda e: e.tensor_scalar(out=nv0.a, in0=m8.a[:, 0:1], scalar1=-1.0, scalar2=None, op0=ALU.mult), [m8], [nv0])
                    c.A(lambda e: e.activation(out=e8.a, in_=les.a, func=AF.Exp, bias=nv0.a[:, 0:1]), [les, nv0], [e8])
                    c.V(lambda e: e.tensor_scalar(out=mk2.a, in0=les.a, scalar1=m8.a[:, 1:2], scalar2=None, op0=ALU.is_ge), [les, m8], [mk2])
                    c.V(lambda e: e.tensor_tensor(out=w8.a, in0=e8.a, in1=mk2.a, op=ALU.mult), [e8, mk2], [w8])
                    c.V(lambda e: e.tensor_reduce(out=sden.a, in_=w8.a, axis=AX.X, op=ALU.add), [w8], [sden])
                    c.V(lambda e: e.tensor_tensor(out=sden.a, in0=sden.a, in1=sg.a, op=ALU.mult), [sden, sg], [sden])
                    c.V(lambda e: e.reciprocal(out=sden.a, in_=sden.a), [sden], [sden])
                    c.V(lambda e: e.tensor_scalar(out=w8.a, in0=w8.a, scalar1=sden.a[:, 0:1], scalar2=None, op0=ALU.mult), [w8, sden], [w8])
                    c.V(lambda e: e.tensor_tensor(out=dw_.a, in0=ohg.a.unsqueeze(2).to_broadcast([128, 4, 8]), in1=w8.a.unsqueeze(1).to_broadcast([128, 4, 8]), op=ALU.mult), [ohg, w8], [dw_])
                    c.dma("sp", DW.a[tok].rearrange("p (g e) -> p g e", e=8), dw_.a, DW, dw_)
                c.pop()
                if stop_after == "p3a":
                    break

                tiles = list(range(first_tile, NT))
                ng = 2
                per = (len(tiles) + ng - 1) // ng
                for gi in range(ng):
                    grp = tiles[gi * per:(gi + 1) * per]
                    t0 = grp[0]; G_ = len(grp)
                    c.push()
                    h2 = c.sb("h2", [128, 8, G_ * 128], BF16, grp="g4")
                    dwg = c.sb("dwg", [128, G_, 32], F32, grp="g4")
                    acc = c.sb("acc", [128, G_, D], F32)
                    c.dma("sp", h2.a, H2T.a[:, :, t0 * 128:(t0 + G_) * 128], h2, H2T)
                    c.dma("sp", dwg.a, DW.a[t0 * 128:(t0 + G_) * 128].rearrange("(i p) e -> p i e", p=128), dwg, DW)
                    wg = [c.sb("wg%d" % i, [128, 8, 512], BF16, grp="we%d" % i) for i in range(2)]
                    wd = [c.sb("wd%d" % i, [128, 2, D], BF16, grp="we%d" % i) for i in range(2)]
                    pGU = [c.ps("pGU%d" % i, [128, 2, 512], F32) for i in range(2)]
                    sl = [c.sb("sl%d" % i, [128, 512], F32) for i in range(2)]
                    aT = [c.sb("aT%d" % i, [128, 2, 512], BF16) for i in range(2)]
                    pD = [c.ps("pD%d" % i, [128, D], F32) for i in range(2)]
                    xt = [c.sb("x4%d" % i, [128, D], F32) for i in range(2)]
                    quads = [(tq, min(4, G_ - tq)) for tq in range(0, G_, 4)]
                    steps = [(e_, qi) for e_ in range(32) for qi in range(len(quads))]

                    def m_s1(si):
                        e_, qi = steps[si]
                        tq, nt = quads[qi]; N = nt * 128
                        g_ = wg[e_ % 2]; d_ = wd[e_ % 2]; at_ = aT[si % 2]
                        if qi == 0:
                            c.dma("sp", g_.a, WGU.a[e_], g_, WGU)
                            c.dma("sp", d_.a, WDN.a[e_], d_, WDN)
                        for cch in range(2):
                            pg = pGU[cch]; s_ = sl[cch]
                            for which in range(2):
                                col0 = which * 256 + cch * 128
                                for k in range(8):
                                    c.mm(lambda e: e.matmul(pg.a[:, which, :N], lhsT=g_.a[:, k, col0:col0 + 128], rhs=h2.a[:, k, tq * 128:tq * 128 + N], start=(k == 0), stop=(k == 7)), [g_, h2], [pg], last=(k == 7 and which == 1))
                            c.A(lambda e: e.activation(out=s_.a[:, :N], in_=pg.a[:, 0, :N], func=AF.Silu), [pg], [s_])
                            c.V(lambda e: e.tensor_tensor(out=at_.a[:, cch, :N], in0=s_.a[:, :N], in1=pg.a[:, 1, :N], op=ALU.mult), [s_, pg], [at_])

                    def m_s2(si):
                        e_, qi = steps[si]
                        tq, nt = quads[qi]
                        d_ = wd[e_ % 2]; at_ = aT[si % 2]
                        for tj in range(nt):
                            ti = tq + tj
                            pd = pD[tj % 2]
                            for half in range(2):
                                cs = slice(half * 512, (half + 1) * 512)
                                for k in range(2):
                                    c.mm(lambda e: e.matmul(pd.a[:, cs], lhsT=at_.a[:, k, tj * 128:(tj + 1) * 128], rhs=d_.a[:, k, cs], start=(k == 0), stop=(k == 1)), [at_, d_], [pd], last=(k == 1 and half == 1))
                            if e_ == 0:
                                c.V(lambda e: e.tensor_scalar(out=acc.a[:, ti, :], in0=pd.a, scalar1=dwg.a[:, ti, e_:e_ + 1], scalar2=None, op0=ALU.mult), [pd, dwg], [acc])
                            else:
                                c.V(lambda e: e.scalar_tensor_tensor(out=acc.a[:, ti, :], in0=pd.a, scalar=dwg.a[:, ti, e_:e_ + 1], in1=acc.a[:, ti, :], op0=ALU.mult, op1=ALU.add), [pd, dwg, acc], [acc])

                    pipeline(len(steps), m_s1, m_s2, 2)
                    for ti in range(G_):
                        i = t0 + ti
                        j3 = 2 if i < 2 else b
                        x_ = xt[ti % 2]
                        c.dma("sp", x_.a, XS.a[b, i * 128:(i + 1) * 128, :], x_, XS)
                        c.G(lambda e: e.tensor_tensor(out=acc.a[:, ti, :], in0=acc.a[:, ti, :], in1=GT.a[:, 1, j3, :], op=ALU.mult), [acc, GT], [acc])
                        c.V(lambda e: e.tensor_tensor(out=x_.a, in0=x_.a, in1=acc.a[:, ti, :], op=ALU.add), [x_, acc], [x_])
                        if last_layer:
                            c.dma("sp", y_out.a[b, (i - 2) * 128:(i - 1) * 128, :], x_.a, y_out, x_)
                        else:
                            c.dma("sp", XS.a[b, i * 128:(i + 1) * 128, :], x_.a, XS, x_)
                    c.pop()
            if stop_after is not None:
                break
            c.pop()
        c.barrier()
        while len(c.stack) > 1:
            c.stack.pop().__exit__(None, None, None)
        print("instructions:", c.ninst, "sems:", len(c.sem))
    nc._trace = c.trace
    return nc


def host_consts():
    t = np.arange(4096)
    row = (t // 64).astype(np.float32); col = (t % 64).astype(np.float32)
    inv = (10000.0 ** (-np.arange(16, dtype=np.float32) / 16)).astype(np.float32)
    ang = np.concatenate([row[:, None] * inv, col[:, None] * inv], axis=-1).astype(np.float32)
    cos = np.ones((T, 32), np.float32); sin = np.zeros((T, 32), np.float32)
    cos[256:] = np.cos(ang); sin[256:] = np.sin(ang)
    s = np.arange(128)
    u = np.stack([(s[:, None] <= s[None, :]), (s[:, None] >= s[None, :])]).astype(np.float32)
    kc = np.arange(64)[:, None]; qc = np.arange(64)[None, :]
    cs = np.clip(qc - 8, 0, 48)
    valid = (kc >= cs) & (kc < cs + 16)
    cm = np.where(valid, 0.0, NEG).astype(np.float32)
    jd = np.zeros((64, 128), np.float32)
    for kc in range(64):
        jd[63 - kc, kc] = 1.0; jd[63 - kc, 64 + kc] = 1.0
    return {"k_jd": jd, "k_cos": cos, "k_sin": sin, "k_u": u, "k_id": np.eye(128, dtype=np.float32), "k_colmask": np.concatenate([cm, cm], 0)}


def make_in_maps(inputs, nb, cores):
    consts = host_consts()
    maps = []
    for ci in cores:
        m = dict(consts)
        for k, v in inputs.items():
            v = np.ascontiguousarray(v, dtype=np.float32)
            if k in ("x", "c", "ctx"):
                m[k] = np.ascontiguousarray(v[ci * nb:(ci + 1) * nb])
            elif k == "b_mlstm":
                m[k] = v.reshape(2, 16)
            else:
                m[k] = v
        maps.append(m)
    return maps


def kernel(**inputs):
    nb = 2
    nc = build(nb=nb, nl=2)
    in_maps = make_in_maps(inputs, nb, list(range(8)))
    res = run_bass_kernel_spmd(nc, in_maps, core_ids=list(range(8)))
    return np.concatenate([r["y"] for r in res.results], axis=0).astype(np.float32)
```

```python
import numpy as np
import concourse.bass as bass
import concourse.mybir as mybir
from concourse.bass_utils import run_bass_kernel_spmd
from contextlib import ExitStack
import os

F32 = mybir.dt.float32
BF16 = mybir.dt.bfloat16
AF = mybir.ActivationFunctionType
ALU = mybir.AluOpType
AX = mybir.AxisListType

T = 4352
NT = 34
D = 1024
NEG = -1e30
SKIP_SAME = int(os.environ.get("SKIP_SAME", "0"))


class Buf:
    __slots__ = ("name", "w", "r", "dsem", "t", "grp")

    def __init__(self, name, t=None, grp=None):
        self.name = name
        self.grp = grp
        self.w = None
        self.r = {}
        self.dsem = None
        self.t = t

    @property
    def a(self):
        return self.t.ap() if hasattr(self.t, "ap") else self.t[:]


class Ctx:
    def __init__(self, nc, es):
        self.nc = nc
        self.es = es
        self.E = {"pe": nc.tensor, "act": nc.scalar, "dve": nc.vector, "pool": nc.gpsimd, "sp": nc.sync}
        self.sem = {}
        self.ecnt = {}
        for e in ("pe", "act", "dve", "pool"):
            self.sem[e] = es.enter_context(nc.semaphore("c_" + e))
            self.ecnt[e] = 0
        self.seen = {e: {} for e in self.E}
        self.ninst = 0
        self.uid = 0
        self.trace = {e: [] for e in self.E}
        self.shared = set()
        self.stack = [es]
        self.dtot = {}

    def sb(self, name, shape, dt, grp=None):
        self.uid += 1
        return Buf(name, self.stack[-1].enter_context(self.nc.sbuf_tensor("%s_%d" % (name, self.uid), list(shape), dt)), grp)

    def ps(self, name, shape, dt):
        self.uid += 1
        return Buf(name, self.stack[-1].enter_context(self.nc.psum_tensor("%s_%d" % (name, self.uid), list(shape), dt)))

    def dram(self, name, shape, dt, kind="Internal"):
        return Buf(name, self.nc.dram_tensor(name, list(shape), dt, kind=kind))

    def push(self):
        st = ExitStack()
        st.__enter__()
        self.stack.append(st)

    def pop(self):
        self.barrier()
        self.stack.pop().__exit__(None, None, None)

    def barrier(self):
        evs = [(e, self.ecnt[e]) for e in self.ecnt] + list(self.dtot.items())
        for e in self.E:
            self._wait(e, evs)

    def _wait(self, eng, deps):
        need = {}
        for k, v in deps:
            if eng == "pe" and k == "pe":
                continue
            if SKIP_SAME and k == eng and v <= self.ecnt[eng] - SKIP_SAME:
                continue
            if v > need.get(k, 0):
                need[k] = v
        seen = self.seen[eng]
        for k, v in need.items():
            if k in self.shared:
                v = self.dtot[k]
            if seen.get(k, 0) >= v:
                continue
            self.E[eng].wait_ge(self.sem[k], v)
            self.trace[eng].append(("w", k, v))
            self.ninst += 1
            seen[k] = v

    def _deps(self, reads, writes):
        deps = []
        for b in reads:
            if b.w is not None:
                deps.append(b.w)
        for b in writes:
            if b.w is not None:
                deps.append(b.w)
            deps.extend(b.r.items())
        return deps

    def _commit(self, ev, reads, writes):
        k, v = ev
        for b in reads:
            if b.r.get(k, 0) < v:
                b.r[k] = v
        for b in writes:
            b.w = ev
            b.r = {}

    def op(self, eng, f, reads=(), writes=()):
        self._wait(eng, self._deps(reads, writes))
        ins = f(self.E[eng])
        self.ecnt[eng] += 1
        ins.then_inc(self.sem[eng], 1)
        self.trace[eng].append(("i", eng, 1))
        self.ninst += 1
        self._commit((eng, self.ecnt[eng]), reads, writes)
        return ins

    def V(self, f, r=(), w=()):
        return self.op("dve", f, r, w)

    def A(self, f, r=(), w=()):
        return self.op("act", f, r, w)

    def G(self, f, r=(), w=()):
        return self.op("pool", f, r, w)

    def mm(self, f, reads=(), writes=(), last=True):
        self._wait("pe", self._deps(reads, writes))
        ins = f(self.E["pe"])
        self.ninst += 1
        if last:
            self.ecnt["pe"] += 1
            ins.then_inc(self.sem["pe"], 1)
            self.trace["pe"].append(("i", "pe", 1))
            self._commit(("pe", self.ecnt["pe"]), reads, writes)
        else:
            self._commit(("pe", self.ecnt["pe"] + 1), reads, writes)
        return ins

    def dma(self, q, out, in_, dst, src, **kw):
        self._wait(q, self._deps((src,), (dst,)))
        if dst.dsem is None:
            dst.dsem = "d_" + (dst.grp or dst.name)
            if dst.grp:
                self.shared.add(dst.dsem)
            if dst.dsem not in self.sem:
                self.sem[dst.dsem] = self.es.enter_context(self.nc.semaphore(dst.dsem))
                self.dtot[dst.dsem] = 0
        ins = self.E[q].dma_start(out=out, in_=in_, **kw)
        self.dtot[dst.dsem] += 16
        ins.then_inc(self.sem[dst.dsem], 16)
        self.trace[q].append(("i", dst.dsem, 16))
        self.ninst += 1
        self._commit((dst.dsem, self.dtot[dst.dsem]), (src,), (dst,))
        return ins


def pipeline(n, stage1, stage2, depth, s1_first=False):
    for si in range(min(depth, n)):
        stage1(si)
    for si in range(n):
        if s1_first and si + depth < n:
            stage1(si + depth)
        stage2(si)
        if not s1_first and si + depth < n:
            stage1(si + depth)


def rr(*gens):
    gens = [g for g in gens if g is not None]
    while gens:
        for g in list(gens):
            try:
                next(g)
            except StopIteration:
                gens.remove(g)


def na_plan(j):
    plan = []
    for kt in range(32):
        blocks = {}
        anyv = False
        for a in range(2):
            for b in range(2):
                qr = 2 * j + b
                kr = 2 * kt + a
                rs = min(max(qr - 4, 0), 56)
                ok = rs <= kr < rs + 8
                blocks[(a, b)] = (kr - qr + 7) if ok else None
                anyv = anyv or ok
        if anyv:
            plan.append((kt, blocks))
    return plan


def build(nb=2, nl=2, dbg=(), stop_after=None):
    nc = bass.Bass("TRN2", target_bir_lowering=False)
    es = ExitStack()
    with es:
        c = Ctx(nc, es)

        def inp(name, shape):
            return Buf(name, nc.dram_tensor(name, list(shape), F32, kind="ExternalInput"))

        x_in = inp("x", [nb, 4096, D]); ctx_in = inp("ctx", [nb, 256, D]); c_in = inp("c", [nb, D]); cctx_in = inp("c_ctx", [D])
        w_mod = inp("w_mod", [2, D, 6144]); b_mod = inp("b_mod", [2, 6144]); g_norm = inp("g_norm", [2, 2, D])
        w_in = inp("w_in", [2, D, 7440]); b_merge = inp("b_merge", [2, 3, D]); g_qk = inp("g_qk", [2, 4, 64])
        rpb = inp("rpb", [2, 8, 15, 31]); b_mlstm = inp("b_mlstm", [2, 16]); g_ml = inp("g_ml", [2, 512])
        w_branch = inp("w_branch", [2, 3, 512, D]); w_out = inp("w_out", [2, D, D])
        w_group = inp("w_group", [2, D, 4]); b_group = inp("b_group", [2, 4]); w_router = inp("w_router", [2, D, 32]); b_router = inp("b_router", [2, 32])
        w_gate_up = inp("w_gate_up", [2, 32, D, 512]); w_down = inp("w_down", [2, 32, 256, D])
        k_cos = inp("k_cos", [T, 32]); k_sin = inp("k_sin", [T, 32]); k_u = inp("k_u", [2, 128, 128]); k_id = inp("k_id", [128, 128])
        k_colmask = inp("k_colmask", [128, 64]); k_jd = inp("k_jd", [64, 128])
        y_out = c.dram("y", [nb, 4096, D], F32, kind="ExternalOutput")

        def scr(name, shape, dt):
            return c.dram(name, shape, dt, kind=("ExternalOutput" if name in dbg else "Internal"))

        XS = scr("XS", [nb, T, D], F32)
        QAT = scr("QAT", [128, 4, T], BF16); KAT = scr("KAT", [128, T], BF16); VA = scr("VA", [T, 2, 65], BF16)
        QBT = scr("QBT", [128, 4, T], BF16); KBT = scr("KBT", [128, 4, T], BF16); VB = scr("VB", [T, 8, 65], BF16)
        MQT = scr("MQT", [128, 4, T], BF16); MKT = scr("MKT", [128, 4, T], BF16); MK = scr("MK", [T, 512], BF16)
        MV = scr("MV", [T, 4, 129], BF16); MO = scr("MO", [T, 512], BF16); MG = scr("MG", [T, 16], F32)
        GATES = scr("GATES", [T, 3072], BF16)
        YAT = scr("YAT", [4, 128, T], BF16); YBT = scr("YBT", [4, 128, T], BF16); YCT = scr("YCT", [4, 128, T], BF16)
        HS = scr("HS", [T, 512], F32)
        H2T = scr("H2T", [128, 8, T], BF16); DW = scr("DW", [T, 32], F32)
        WGU = scr("WGU", [32, 128, 8, 512], BF16); WDN = scr("WDN", [32, 128, 2, D], BF16)
        MODD = scr("MODD", [2, 3, 6144], F32)
        RPBP = scr("RPBP", [7568], F32); TPD = scr("TPD", [128, 8, 15, 64], F32)

        ident_f = c.sb("ident_f", [128, 128], F32, grp="setup"); ident = c.sb("ident", [128, 128], BF16)
        U = c.sb("U", [128, 2, 128], F32, grp="setup")
        c.dma("sp", ident_f.a, k_id.a, ident_f, k_id)
        c.dma("sp", U.a, k_u.a.rearrange("d s t -> s d t"), U, k_u)
        c.V(lambda e: e.tensor_copy(out=ident.a, in_=ident_f.a), [ident_f], [ident])
        ones_col = c.sb("ones_col", [128, 8], BF16)
        c.V(lambda e: e.memset(ones_col.a, 1.0), [], [ones_col])

        def tile_src(l, b, i):
            if l == 0:
                if i < 2:
                    return ctx_in, ctx_in.a[b, i * 128:(i + 1) * 128, :]
                return x_in, x_in.a[b, (i - 2) * 128:(i - 1) * 128, :]
            return XS, XS.a[b, i * 128:(i + 1) * 128, :]

        for l in range(nl):
            last_layer = (l == nl - 1)
            first_tile = 2 if last_layer else 0
            c.push()
            c.push()
            cT = c.sb("cT", [128, 8, 3], F32, grp="setup"); cTb = c.sb("cTb", [128, 8, 3], BF16)
            c.V(lambda e: e.memset(cT.a, 0.0), [], [cT])
            for b in range(nb):
                c.dma("sp", cT.a[:, :, b], c_in.a[b].rearrange("(k p) -> p k", p=128), cT, c_in, allow_slow_non_contiguous=True)
            c.dma("sp", cT.a[:, :, 2], cctx_in.a.rearrange("(k p) -> p k", p=128), cT, cctx_in, allow_slow_non_contiguous=True)
            c.A(lambda e: e.activation(out=cTb.a, in_=cT.a, func=AF.Silu), [cT], [cTb])
            modrow = c.sb("modrow", [3, 6144], F32)
            bmrow = c.sb("bmrow", [3, 6144], F32, grp="setup")
            c.dma("sp", bmrow.a, b_mod.a[l].partition_broadcast(3), bmrow, b_mod)
            wm = [c.sb("wm%d" % i, [128, 8, 512], BF16) for i in range(2)]
            pmod = [c.ps("pmod%d" % i, [128, 512], F32) for i in range(2)]
            for n in range(12):
                w_ = wm[n % 2]; p_ = pmod[n % 2]
                c.dma("pool", w_.a, w_mod.a[l, :, n * 512:(n + 1) * 512].rearrange("(k p) n -> p k n", p=128), w_, w_mod)
                for k in range(8):
                    c.mm(lambda e: e.matmul(p_.a[0:3, :], lhsT=cTb.a[:, k, :], rhs=w_.a[:, k, :], start=(k == 0), stop=(k == 7)), [cTb, w_], [p_], last=(k == 7))
                c.V(lambda e: e.tensor_tensor(out=modrow.a[:, n * 512:(n + 1) * 512], in0=p_.a[0:3, :], in1=bmrow.a[:, n * 512:(n + 1) * 512], op=ALU.add), [p_, bmrow], [modrow])
            c.dma("sp", MODD.a[l], modrow.a, MODD, modrow)
            c.pop()
            modT = c.sb("modT", [128, 6, 8, 3], F32, grp="setup")
            for s in range(6):
                for j in range(3):
                    c.dma("sp", modT.a[:, s, :, j], MODD.a[l, j, s * 1024:(s + 1) * 1024].rearrange("(k p) -> p k", p=128), modT, MODD, allow_slow_non_contiguous=True)
            gn = c.sb("gn", [128, 2, 8], F32, grp="setup")
            c.dma("sp", gn.a, g_norm.a[l].rearrange("t (k p) -> p t k", p=128), gn, g_norm, allow_slow_non_contiguous=True)
            G1 = c.sb("G1", [128, 8, 3], F32); G2 = c.sb("G2", [128, 8, 3], F32)
            for (Gx, seg, t_) in ((G1, 1, 0), (G2, 4, 1)):
                c.V(lambda e: e.tensor_scalar(out=Gx.a, in0=modT.a[:, seg], scalar1=1.0, scalar2=None, op0=ALU.add), [modT], [Gx])
                c.V(lambda e: e.tensor_tensor(out=Gx.a, in0=Gx.a, in1=gn.a[:, t_, :].unsqueeze(2).to_broadcast([128, 8, 3]), op=ALU.mult), [Gx, gn], [Gx])
            GT = c.sb("GT", [128, 2, 3, D], F32, grp="setup")
            for gi, seg in ((0, 2), (1, 5)):
                for j in range(3):
                    c.dma("sp", GT.a[:, gi, j, :], MODD.a[l, j, seg * 1024:(seg + 1) * 1024].partition_broadcast(128), GT, MODD)
            gqk = c.sb("gqk", [128, 4, 64], F32, grp="setup")
            c.dma("sp", gqk.a, g_qk.a[l].partition_broadcast(128), gqk, g_qk)
            bml = c.sb("bml", [128, 16], F32, grp="setup")
            c.dma("sp", bml.a, b_mlstm.a[l].partition_broadcast(128), bml, b_mlstm)
            gml = c.sb("gml", [128, 512], F32, grp="setup")
            c.dma("sp", gml.a, g_ml.a[l].partition_broadcast(128), gml, g_ml)
            brt = c.sb("brt", [128, 36], F32, grp="setup")
            c.dma("sp", brt.a[:, 0:4], b_group.a[l].partition_broadcast(128), brt, b_group)
            c.dma("sp", brt.a[:, 4:36], b_router.a[l].partition_broadcast(128), brt, b_router)
            wrt = c.sb("wrt", [128, 8, 36], F32, grp="setup")
            c.dma("sp", wrt.a[:, :, 0:4], w_group.a[l].rearrange("(k p) n -> p k n", p=128), wrt, w_group, allow_slow_non_contiguous=True)
            c.dma("sp", wrt.a[:, :, 4:36], w_router.a[l].rearrange("(k p) n -> p k n", p=128), wrt, w_router, allow_slow_non_contiguous=True)
            c.push()
            zt = c.sb("zt", [1, 8192], F32, grp="setup")
            c.V(lambda e: e.memset(zt.a, 0.0), [], [zt])
            c.dma("sp", RPBP.a.rearrange("(o n) -> o n", o=1), zt.a[:, 0:7568], RPBP, zt)
            c.dma("sp", RPBP.a[64:64 + 3720].rearrange("(r j) -> r j", j=31), bass.AP(rpb.t, l * 3720 + 30, [[31, 120], [-1, 31]]), RPBP, rpb, allow_slow_non_contiguous=True)
            TPb = c.sb("TPb", [128, 8, 15, 64], F32, grp="setup")
            cm = c.sb("cm", [128, 64], F32, grp="setup")
            c.dma("sp", cm.a, k_colmask.a, cm, k_colmask)
            TPx = c.sb("TPx", [64, 8, 15, 64], F32, grp="setup")
            for h in range(8):
                src = bass.AP(RPBP.t, 64 - 48 + h * 15 * 31, [[1, 64], [31, 15], [1, 64]])
                c.dma("sp", TPx.a[:, h], src, TPx, RPBP)
            jd = c.sb("jd", [64, 128], F32, grp="setup")
            c.dma("sp", jd.a, k_jd.a, jd, k_jd)
            pJ = [c.ps("pJ%d" % i, [128, 512], F32) for i in range(2)]
            TPx2 = TPx.a.rearrange("p h r q -> p (h r q)")
            TPb2 = TPb.a.rearrange("p h r q -> p (h r) q")
            for n in range(15):
                p_ = pJ[n % 2]
                c.mm(lambda e: e.matmul(p_.a, lhsT=jd.a, rhs=TPx2[:, n * 512:(n + 1) * 512], start=True, stop=True), [jd, TPx], [p_])
                c.V(lambda e: e.tensor_tensor(out=TPb2[:, n * 8:(n + 1) * 8, :], in0=p_.a.rearrange("p (r q) -> p r q", q=64), in1=cm.a.unsqueeze(1).to_broadcast([128, 8, 64]), op=ALU.add), [p_, cm], [TPb])
            c.dma("sp", TPD.a, TPb.a, TPD, TPb)
            c.pop()
            c.push()
            cv = [c.sb("cv%d" % i, [128, 8, 512], BF16, grp="cv%d" % i) for i in range(2)]
            cd = [c.sb("cd%d" % i, [128, 2, D], BF16, grp="cv%d" % i) for i in range(2)]
            for e_ in range(32 if not os.environ.get("SKIP_CONV") else 0):
                a_ = cv[e_ % 2]; d_ = cd[e_ % 2]
                c.dma("pool", a_.a, w_gate_up.a[l, e_].rearrange("(k p) n -> p k n", p=128), a_, w_gate_up)
                c.dma("sp", WGU.a[e_], a_.a, WGU, a_)
                c.dma("pool", d_.a, w_down.a[l, e_].rearrange("(k p) n -> p k n", p=128), d_, w_down)
                c.dma("sp", WDN.a[e_], d_.a, WDN, d_)
            c.pop()
            if stop_after == "mod":
                break

            for b in range(nb):
                c.push()
                hT = c.sb("hT", [128, 8, T], BF16)
                xt = [c.sb("xt%d" % i, [128, D], F32) for i in range(2)]
                junk_L = [c.sb("junk%d" % i_, [128, D], F32) for i_ in range(2)]; junk = junk_L[0]
                ss_L = [c.sb("ss%d" % i_, [128, 1], F32) for i_ in range(2)]; ss = ss_L[0]; rstd_L = [c.sb("rstd%d" % i_, [128, 1], F32) for i_ in range(2)]; rstd = rstd_L[0]
                xn = [c.sb("xn%d" % i, [128, D], BF16) for i in range(2)]
                pT = [c.ps("pT%d" % i, [128, 8, 128], BF16) for i in range(2)]
                for i in range(NT):
                    x_ = xt[i % 2]; n_ = xn[i % 2]; p_ = pT[i % 2]
                    junk = junk_L[i % 2]; ss = ss_L[i % 2]; rstd = rstd_L[i % 2]
                    src, ap = tile_src(l, b, i)
                    j3 = 2 if i < 2 else b
                    c.dma("sp", x_.a, ap, x_, src)
                    c.V(lambda e: e.memset(ss.a, 0.0), [], [ss])
                    c.A(lambda e: e.activation(out=junk.a, in_=x_.a, func=AF.Square, accum_out=ss.a), [x_], [junk, ss])
                    c.V(lambda e: e.tensor_scalar(out=rstd.a, in0=ss.a, scalar1=1.0 / D, scalar2=1e-6, op0=ALU.mult, op1=ALU.add), [ss], [rstd])
                    c.A(lambda e: e.activation(out=rstd.a, in_=rstd.a, func=AF.Sqrt), [rstd], [rstd])
                    c.V(lambda e: e.reciprocal(out=rstd.a, in_=rstd.a), [rstd], [rstd])
                    c.V(lambda e: e.tensor_scalar(out=n_.a, in0=x_.a, scalar1=rstd.a[:, 0:1], scalar2=None, op0=ALU.mult), [x_, rstd], [n_])
                    for k in range(8):
                        c.mm(lambda e: e.transpose(out=p_.a[:, k, :], in_=n_.a[:, k * 128:(k + 1) * 128], identity=ident.a), [n_, ident], [p_], last=(k == 7))
                    for k in range(8):
                        eng = c.A if k % 2 == 0 else None
                        if k % 2 == 0:
                            c.A(lambda e: e.activation(out=hT.a[:, k, i * 128:(i + 1) * 128], in_=p_.a[:, k, :], func=AF.Identity, scale=G1.a[:, k, j3:j3 + 1], bias=modT.a[:, 0, k, j3:j3 + 1]), [p_, G1, modT], [hT])
                        else:
                            c.V(lambda e: e.tensor_scalar(out=hT.a[:, k, i * 128:(i + 1) * 128], in0=p_.a[:, k, :], scalar1=G1.a[:, k, j3:j3 + 1], scalar2=modT.a[:, 0, k, j3:j3 + 1], op0=ALU.mult, op1=ALU.add), [p_, G1, modT], [hT])
                if "HTD" in dbg:
                    HTD = scr("HTD", [128, 8, T], BF16)
                    c.dma("sp", HTD.a, hT.a, HTD, hT)
                cosb = c.sb("cosb", [128, NT, 32], F32, grp="setup"); sinb = c.sb("sinb", [128, NT, 32], F32, grp="setup")
                c.dma("sp", cosb.a, k_cos.a.rearrange("(i p) f -> p i f", p=128), cosb, k_cos)
                c.dma("sp", sinb.a, k_sin.a.rearrange("(i p) f -> p i f", p=128), sinb, k_sin)
                bmg = c.sb("bmg", [128, 3072], F32, grp="setup")
                c.dma("sp", bmg.a, b_merge.a[l].rearrange("t d -> (t d)").partition_broadcast(128), bmg, b_merge)
                wc = [c.sb("wc%d" % i, [128, 8, 512], BF16) for i in range(2)]
                pp = [c.ps("pp%d" % i, [128, 512], F32) for i in range(2)]
                ptr = [c.ps("ptr%d" % i, [128, 4, 128], BF16) for i in range(2)]
                sq_L = [c.sb("sq%d" % i_, [128, 512], F32) for i_ in range(2)]; sq = sq_L[0]; ssq_L = [c.sb("ssq%d" % i_, [128, 8], F32) for i_ in range(2)]; ssq = ssq_L[0]; rq_L = [c.sb("rq%d" % i_, [128, 8], F32) for i_ in range(2)]; rq = rq_L[0]
                qn_L = [c.sb("qn%d" % i_, [128, 512], F32) for i_ in range(2)]; qn = qn_L[0]; t1_L = [c.sb("t1%d" % i_, [128, 256], F32) for i_ in range(2)]; t1 = t1_L[0]; t2_L = [c.sb("t2%d" % i_, [128, 256], F32) for i_ in range(2)]; t2 = t2_L[0]
                qr_ = [c.sb("qr%d" % i, [128, 512], BF16) for i in range(2)]
                trs = [c.sb("trs%d" % i, [128, 4, 128], BF16) for i in range(2)]
                vst = [c.sb("vst%d" % i, [128, 8, 65], BF16) for i in range(2)]
                mvst = [c.sb("mvst%d" % i, [128, 4, 129], BF16) for i in range(2)]
                gst = [c.sb("gst%d" % i, [128, 512], BF16) for i in range(2)]
                mg1_L = [c.sb("mg1%d" % i_, [128, 16], F32) for i_ in range(2)]; mg1 = mg1_L[0]; mg2_L = [c.sb("mg2%d" % i_, [128, 8], F32) for i_ in range(2)]; mg2 = mg2_L[0]
                gpre_L = [c.sb("gpre%d" % i_, [128, 512], F32) for i_ in range(2)]; gpre = gpre_L[0]
                for st_ in vst:
                    c.V(lambda e: e.memset(st_.a, 1.0), [], [st_])
                for st_ in mvst:
                    c.V(lambda e: e.memset(st_.a, 1.0), [], [st_])
                chunks = [(0, 512, "Aq"), (512, 256, "Akv"), (768, 512, "Bq"), (1280, 512, "Bk"), (1792, 512, "Bv"),
                          (2304, 512, "Cq"), (2816, 512, "Ck"), (3328, 512, "Cv"), (3840, 512, "Co"), (4352, 16, "Cg")]
                chunks += [(4368 + 512 * m, 512, "Mg%d" % m) for m in range(6)]
                it = 0

                def qknorm(p_, ncol, gidx, dst):
                    nh = ncol // 64
                    c.A(lambda e: e.activation(out=sq.a[:, :ncol], in_=p_.a[:, :ncol], func=AF.Square), [p_], [sq])
                    c.V(lambda e: e.tensor_reduce(out=ssq.a[:, :nh], in_=sq.a[:, :ncol].rearrange("p (h d) -> p h d", d=64), axis=AX.X, op=ALU.add), [sq], [ssq])
                    c.V(lambda e: e.tensor_scalar(out=rq.a[:, :nh], in0=ssq.a[:, :nh], scalar1=1.0 / 64, scalar2=1e-6, op0=ALU.mult, op1=ALU.add), [ssq], [rq])
                    c.A(lambda e: e.activation(out=rq.a[:, :nh], in_=rq.a[:, :nh], func=AF.Sqrt), [rq], [rq])
                    c.V(lambda e: e.reciprocal(out=rq.a[:, :nh], in_=rq.a[:, :nh]), [rq], [rq])
                    c.V(lambda e: e.tensor_tensor(out=dst.rearrange("p (h d) -> p h d", d=64), in0=p_.a[:, :ncol].rearrange("p (h d) -> p h d", d=64),
                                                  in1=rq.a[:, :nh].unsqueeze(2).to_broadcast([128, nh, 64]), op=ALU.mult), [p_, rq], [qn])
                    c.V(lambda e: e.tensor_tensor(out=dst.rearrange("p (h d) -> p h d", d=64), in0=dst.rearrange("p (h d) -> p h d", d=64),
                                                  in1=gqk.a[:, gidx:gidx + 1, :].to_broadcast([128, nh, 64]), op=ALU.mult), [qn, gqk], [qn])

                def rope(src, nh, i, dst4):
                    s3 = src.rearrange("p (h t f) -> p h t f", t=2, f=32)
                    x1 = s3[:, :, 0, :]; x2 = s3[:, :, 1, :]
                    cb = cosb.a[:, i:i + 1, :].to_broadcast([128, nh, 32]); sb_ = sinb.a[:, i:i + 1, :].to_broadcast([128, nh, 32])
                    a1 = t1.a[:, :nh * 32].rearrange("p (h f) -> p h f", f=32); a2 = t2.a[:, :nh * 32].rearrange("p (h f) -> p h f", f=32)
                    c.V(lambda e: e.tensor_tensor(out=a1, in0=x1, in1=cb, op=ALU.mult), [qn, cosb], [t1])
                    c.G(lambda e: e.tensor_tensor(out=a2, in0=x2, in1=sb_, op=ALU.mult), [qn, sinb], [t2])
                    c.V(lambda e: e.tensor_tensor(out=dst4[:, :, 0, :], in0=a1, in1=a2, op=ALU.subtract), [t1, t2], [dstbuf[0]])
                    c.V(lambda e: e.tensor_tensor(out=a1, in0=x2, in1=cb, op=ALU.mult), [qn, cosb], [t1])
                    c.G(lambda e: e.tensor_tensor(out=a2, in0=x1, in1=sb_, op=ALU.mult), [qn, sinb], [t2])
                    c.V(lambda e: e.tensor_tensor(out=dst4[:, :, 1, :], in0=a1, in1=a2, op=ALU.add), [t1, t2], [dstbuf[0]])

                dstbuf = [None]

                def transposes(srcb, nblk, dstD, i, scale_src=None):
                    nonlocal it
                    pt = ptr[it % 2]; ts_ = trs[it % 2]
                    for m in range(nblk):
                        c.mm(lambda e: e.transpose(out=pt.a[:, m, :], in_=srcb.a[:, m * 128:(m + 1) * 128], identity=ident.a), [srcb, ident], [pt], last=(m == nblk - 1))
                    c.A(lambda e: e.copy(out=ts_.a[:, :nblk, :], in_=pt.a[:, :nblk, :]), [pt], [ts_])
                    if nblk == 1:
                        c.dma("sp", dstD.a[:, i * 128:(i + 1) * 128], ts_.a[:, 0, :], dstD, ts_)
                    else:
                        c.dma("sp", dstD.a[:, :, i * 128:(i + 1) * 128], ts_.a[:, :nblk, :], dstD, ts_)

                for ci, (c0, cw, kind) in enumerate(chunks):
                    w_ = wc[ci % 2]
                    c.dma("pool", w_.a[:, :, :cw], w_in.a[l, :, c0:c0 + cw].rearrange("(k p) n -> p k n", p=128), w_, w_in)
                    for i in range(NT):
                        it += 1
                        p_ = pp[it % 2]
                        sq = sq_L[it % 2]; ssq = ssq_L[it % 2]; rq = rq_L[it % 2]; qn = qn_L[it % 2]; t1 = t1_L[it % 2]; t2 = t2_L[it % 2]; mg1 = mg1_L[it % 2]; mg2 = mg2_L[it % 2]; gpre = gpre_L[it % 2]
                        for k in range(8):
                            c.mm(lambda e: e.matmul(p_.a[:, :cw], lhsT=hT.a[:, k, i * 128:(i + 1) * 128], rhs=w_.a[:, k, :cw], start=(k == 0), stop=(k == 7)), [hT, w_], [p_], last=(k == 7))
                        tok = slice(i * 128, (i + 1) * 128)
                        if kind == "Aq":
                            qknorm(p_, 512, 0, qn.a[:, :512])
                            q_ = qr_[it % 2]; dstbuf[0] = q_
                            d5 = q_.a.rearrange("p (m g t f) -> p g m t f", g=2, t=2, f=32)
                            for g in range(2):
                                rope(qn.a[:, g * 256:(g + 1) * 256], 4, i, d5[:, g])
                            transposes(q_, 4, QAT, i)
                        elif kind == "Akv":
                            qknorm(p_, 128, 1, qn.a[:, :128])
                            q_ = qr_[it % 2]; dstbuf[0] = q_
                            rope(qn.a[:, :128], 2, i, q_.a[:, :128].rearrange("p (h t f) -> p h t f", t=2, f=32))
                            transposes(q_, 1, KAT, i)
                            v_ = vst[it % 2]
                            c.A(lambda e: e.copy(out=v_.a[:, 0:2, 0:64], in_=p_.a[:, 128:256].rearrange("p (h d) -> p h d", d=64)), [p_], [v_])
                            c.dma("sp", VA.a[tok], v_.a[:, 0:2, :], VA, v_)
                        elif kind in ("Bq", "Bk"):
                            qknorm(p_, 512, 2 if kind == "Bq" else 3, qn.a[:, :512])
                            q_ = qr_[it % 2]
                            c.A(lambda e: e.copy(out=q_.a, in_=qn.a[:, :512]), [qn], [q_])
                            transposes(q_, 4, QBT if kind == "Bq" else KBT, i)
                        elif kind == "Bv":
                            v_ = vst[it % 2]
                            c.A(lambda e: e.copy(out=v_.a[:, :, 0:64], in_=p_.a.rearrange("p (h d) -> p h d", d=64)), [p_], [v_])
                            c.dma("sp", VB.a[tok], v_.a, VB, v_)
                        elif kind in ("Cq", "Ck"):
                            q_ = qr_[it % 2]
                            c.A(lambda e: e.activation(out=q_.a, in_=p_.a, func=AF.Identity, scale=(1.0 if kind == "Cq" else 128 ** -0.5)), [p_], [q_])
                            if kind == "Ck":
                                c.dma("sp", MK.a[tok], q_.a, MK, q_)
                            transposes(q_, 4, MQT if kind == "Cq" else MKT, i)
                        elif kind == "Cv":
                            v_ = mvst[it % 2]
                            c.A(lambda e: e.copy(out=v_.a[:, :, 0:128], in_=p_.a.rearrange("p (h d) -> p h d", d=128)), [p_], [v_])
                            c.dma("sp", MV.a[tok], v_.a, MV, v_)
                        elif kind == "Co":
                            g_ = gst[it % 2]
                            c.A(lambda e: e.activation(out=g_.a, in_=p_.a, func=AF.Sigmoid), [p_], [g_])
                            c.dma("sp", MO.a[tok], g_.a, MO, g_)
                        elif kind == "Cg":
                            c.V(lambda e: e.tensor_tensor(out=mg1.a, in0=p_.a[:, :16], in1=bml.a, op=ALU.add), [p_, bml], [mg1])
                            fv = mg1.a.rearrange("p (d t h) -> p d t h", d=2, t=2)[:, :, 1, :]
                            m2 = mg2.a.rearrange("p (d h) -> p d h", d=2)
                            c.A(lambda e: e.activation(out=m2, in_=fv, func=AF.Exp, scale=-1.0), [mg1], [mg2])
                            c.A(lambda e: e.activation(out=m2, in_=m2, func=AF.Ln, bias=1.0), [mg2], [mg2])
                            c.V(lambda e: e.tensor_scalar(out=fv, in0=m2, scalar1=-1.0, scalar2=None, op0=ALU.mult), [mg2], [mg1])
                            c.dma("sp", MG.a[tok], mg1.a, MG, mg1)
                        else:
                            m = int(kind[2:])
                            g_ = gst[it % 2]
                            c.V(lambda e: e.tensor_tensor(out=gpre.a, in0=p_.a, in1=bmg.a[:, m * 512:(m + 1) * 512], op=ALU.add), [p_, bmg], [gpre])
                            c.A(lambda e: e.activation(out=g_.a, in_=gpre.a, func=AF.Sigmoid), [gpre], [g_])
                            c.dma("sp", GATES.a[tok, m * 512:(m + 1) * 512], g_.a, GATES, g_)
                c.pop()
                if stop_after == "p1":
                    break

                c.push()
                kat = c.sb("kat", [128, T], BF16, grp="ka"); va = c.sb("va", [128, NT, 2, 65], BF16, grp="ka")
                c.dma("sp", kat.a, KAT.a, kat, KAT)
                c.dma("sp", va.a, VA.a.rearrange("(i p) g d -> p i g d", p=128), va, VA)
                qa = [[c.sb("qa%d_%d" % (i, g), [128, 4, 128], BF16, grp="qa%d" % i) for g in range(2)] for i in range(2)]
                for i in range(2):
                    for g in range(2):
                        c.V(lambda e: e.memset(qa[i][g].a, 0.0), [], [qa[i][g]])
                ND = 2
                pS = [c.ps("pS%d" % i, [128, 1024], F32) for i in range(ND)]
                pe_ = [c.sb("pe%d" % i, [128, 1024], BF16) for i in range(3)]
                pO = [c.ps("pO%d" % i, [128, 512], F32) for i in range(2)]
                pbc = c.ps("pbc", [128, 512], F32)
                ones_f = c.sb("ones_f", [128, 64], F32)
                c.V(lambda e: e.memset(ones_f.a, 1.0), [], [ones_f])
                dn = [c.sb("dn%d" % i, [128, 512], F32) for i in range(2)]
                bcs = [c.sb("bcs%d" % i, [64, 512], F32) for i in range(2)]
                yTa = [c.sb("yTa%d" % i, [64, 512], BF16) for i in range(2)]
                steps = []
                for i in range(first_tile, NT):
                    kts = list(range(0, 2)) if i < 2 else list(range(0, NT))
                    prs = [kts[j:j + 2] for j in range(0, len(kts), 2)]
                    for g in range(2):
                        for pi, pr in enumerate(prs):
                            steps.append((i, g, pr, pi == 0, pi == len(prs) - 1))

                def a_s1(si):
                    i, g, pr, first, last = steps[si]
                    q_ = qa[i % 2][g]
                    if g == 0 and first:
                        for g2 in range(2):
                            c.dma("sp", qa[i % 2][g2].a[g2 * 64:(g2 + 1) * 64], QAT.a[g2 * 64:(g2 + 1) * 64, :, i * 128:(i + 1) * 128], qa[i % 2][g2], QAT)
                    ps_ = pS[si % ND]; e_ = pe_[si % 3]
                    for j, kt in enumerate(pr):
                        c.mm(lambda e: e.matmul(ps_.a[:, j * 512:(j + 1) * 512], lhsT=kat.a[:, kt * 128:(kt + 1) * 128], rhs=q_.a.rearrange("p m t -> p (m t)"), start=True, stop=True), [kat, q_], [ps_], last=(j == len(pr) - 1))
                    c.A(lambda e: e.activation(out=e_.a, in_=ps_.a, func=AF.Exp, scale=0.125), [ps_], [e_])

                def a_s2(si):
                    i, g, pr, first, last = steps[si]
                    e_ = pe_[si % 3]
                    po = pO[g]
                    for j, kt in enumerate(pr):
                        c.mm(lambda e: e.matmul(po.a[0:65, :], lhsT=va.a[:, kt, g, :], rhs=e_.a[:, j * 512:(j + 1) * 512], start=(first and j == 0), stop=(last and j == len(pr) - 1)), [e_, va], [po], last=(j == len(pr) - 1))
                    if not last:
                        return
                    d_ = dn[g]; b_ = bcs[g]; y_ = yTa[g]
                    c.A(lambda e: e.copy(out=d_.a[64:65, :], in_=po.a[64:65, :]), [po], [d_])
                    c.V(lambda e: e.reciprocal(out=d_.a[64:65, :], in_=d_.a[64:65, :]), [d_], [d_])
                    c.mm(lambda e: e.matmul(pbc.a[0:64, :], lhsT=ones_f.a[64:65, :], rhs=d_.a[64:65, :], start=True, stop=True), [ones_f, d_], [pbc])
                    c.A(lambda e: e.copy(out=b_.a, in_=pbc.a[0:64, :]), [pbc], [b_])
                    c.V(lambda e: e.tensor_tensor(out=y_.a, in0=po.a[0:64, :], in1=b_.a, op=ALU.mult), [po, b_], [y_])
                    for m in range(4):
                        c.dma("sp", YAT.a[g * 2 + m // 2, (m % 2) * 64:(m % 2) * 64 + 64, i * 128:(i + 1) * 128], y_.a[:, m * 128:(m + 1) * 128], YAT, y_)

                pipeline(len(steps), a_s1, a_s2, ND, s1_first=True)
                c.pop()
                if stop_after == "p2a":
                    break

                c.push()
                kbt = c.sb("kbt", [128, 4, T], BF16, grp="kb"); vb = c.sb("vb", [128, NT, 8, 65], BF16, grp="kb")
                c.dma("sp", kbt.a, KBT.a, kbt, KBT)
                c.dma("sp", vb.a, VB.a.rearrange("(i p) h d -> p i h d", p=128), vb, VB)
                TP = c.sb("TP", [128, 8, 15, 64], F32, grp="kb")
                c.dma("sp", TP.a, TPD.a, TP, TPD)
                tabI = c.sb("tabI", [128, 5, 8, 128], F32); tabE = c.sb("tabE", [128, 5, 8, 128], F32)

                def build_tab(tab, plan):
                    for di, (kt, blocks) in enumerate(plan):
                        for (a, b2), dr in blocks.items():
                            o = tab.a[a * 64:(a + 1) * 64, di, :, b2 * 64:(b2 + 1) * 64]
                            if dr is None:
                                c.G(lambda e: e.memset(o, NEG), [], [tab])
                            else:
                                c.V(lambda e: e.tensor_copy(out=o, in_=TP.a[a * 64:(a + 1) * 64, :, dr, :]), [TP], [tab])

                build_tab(tabI, na_plan(5))
                qz = [[c.sb("qz%d_%d" % (i, p), [128, 4, 128], BF16, grp="qz%d" % i) for p in range(2)] for i in range(2)]
                for i in range(2):
                    for p in range(2):
                        c.V(lambda e: e.memset(qz[i][p].a, 0.0), [], [qz[i][p]])
                pS = [c.ps("pSb%d" % i, [128, 8, 128], F32) for i in range(2)]
                sS = c.sb("sS", [128, 8, 128], F32)
                pe_ = [c.sb("peb%d" % i, [128, 8, 128], BF16) for i in range(2)]
                pOb = c.ps("pOb", [128, 2, 512], F32)
                pO3 = [pOb.a[:, hh, 0:260].rearrange("p (m d) -> p m d", d=65) for hh in range(2)]
                rd = c.sb("rdb", [128, 4], F32)
                ya = [c.sb("yb%d" % i, [128, 512], BF16) for i in range(2)]
                pY = c.ps("pYb", [128, 4, 128], BF16); yT = c.sb("yTb", [128, 4, 128], BF16)
                steps = []
                for i in range(first_tile, NT):
                    keys = [(0, None, None), (1, None, None)]
                    plan = None
                    if i >= 2:
                        j = i - 2
                        plan = na_plan(j)
                        tab = tabI if 2 <= j <= 29 else tabE
                        keys = [(kt + 2, tab, di) for di, (kt, _) in enumerate(plan)] + keys
                    for ki, (kt, tab, di) in enumerate(keys):
                        steps.append((i, ki, kt, tab, di, len(keys), plan))

                def b_s1(si):
                    i, ki, kt, tab, di, nk, plan = steps[si]
                    qz_ = qz[i % 2]
                    if ki == 0:
                        for p in range(2):
                            c.dma("sp", qz_[p].a[p * 64:(p + 1) * 64], QBT.a[p * 64:(p + 1) * 64, :, i * 128:(i + 1) * 128], qz_[p], QBT)
                        if tab is tabE:
                            build_tab(tabE, plan)
                    ps_ = pS[si % 2]; e_ = pe_[si % 2]
                    for h in range(8):
                        par = h % 2; pr = h // 2
                        c.mm(lambda e: e.matmul(ps_.a[:, h, :], lhsT=kbt.a[:, pr, kt * 128:(kt + 1) * 128], rhs=qz_[par].a[:, pr, :], start=True, stop=True), [kbt, qz_[par]], [ps_], last=(h == 7))
                    if tab is not None:
                        c.V(lambda e: e.scalar_tensor_tensor(out=sS.a, in0=ps_.a, scalar=0.125, in1=tab.a[:, di], op0=ALU.mult, op1=ALU.add), [ps_, tab], [sS])
                        c.A(lambda e: e.activation(out=e_.a, in_=sS.a, func=AF.Exp), [sS], [e_])
                    else:
                        c.A(lambda e: e.activation(out=e_.a, in_=ps_.a, func=AF.Exp, scale=0.125), [ps_], [e_])

                def b_s2(si):
                    i, ki, kt, tab, di, nk, plan = steps[si]
                    e_ = pe_[si % 2]; y_ = ya[i % 2]
                    for h in range(8):
                        c.mm(lambda e: e.matmul(pO3[h // 4][:, h % 4, :], lhsT=e_.a[:, h, :], rhs=vb.a[:, kt, h, :], start=(ki == 0 and h % 4 == 0), stop=(ki == nk - 1)), [e_, vb], [pOb], last=(h == 7))
                    if ki != nk - 1:
                        return
                    for hh in range(2):
                        po3 = pO3[hh]
                        c.V(lambda e: e.reciprocal(out=rd.a, in_=po3[:, :, 64]), [pOb], [rd])
                        c.V(lambda e: e.tensor_tensor(out=y_.a[:, hh * 256:(hh + 1) * 256].rearrange("p (m d) -> p m d", d=64), in0=po3[:, :, 0:64], in1=rd.a.unsqueeze(2).to_broadcast([128, 4, 64]), op=ALU.mult), [pOb, rd], [y_])
                    for m in range(4):
                        c.mm(lambda e: e.transpose(out=pY.a[:, m, :], in_=y_.a[:, m * 128:(m + 1) * 128], identity=ident.a), [y_, ident], [pY], last=(m == 3))
                    c.A(lambda e: e.copy(out=yT.a, in_=pY.a), [pY], [yT])
                    c.dma("sp", YBT.a[:, :, i * 128:(i + 1) * 128].rearrange("m p t -> p m t"), yT.a, YBT, yT)

                pipeline(len(steps), b_s1, b_s2, 2)
                c.pop()
                if stop_after == "p2b":
                    break

                c.push()
                Cst = c.sb("Cst", [128, 4, 129], F32)
                Cb = [c.sb("Cb%d" % i, [128, 4, 129], BF16) for i in range(2)]
                mq = [c.sb("mq%d" % i, [128, 4, 128], BF16, grp="m%d" % i) for i in range(3)]
                mk = [c.sb("mk%d" % i, [128, 4, 128], BF16, grp="m%d" % i) for i in range(3)]
                mkt = [c.sb("mkt%d" % i, [128, 512], BF16, grp="m%d" % i) for i in range(3)]
                mv = [c.sb("mv%d" % i, [128, 4, 129], BF16, grp="m%d" % i) for i in range(3)]
                mg = [c.sb("mg%d" % i, [128, 16], F32, grp="m%d" % i) for i in range(3)]
                mo = [c.sb("mo%d" % i, [128, 512], BF16, grp="m%d" % i) for i in range(3)]
                hs = [c.sb("hs%d" % i, [128, 512], F32, grp="m%d" % i) for i in range(3)]
                pb = c.ps("pb", [128, 4], F32); pbB = c.ps("pbB", [128, 4, 128], F32); pST = c.ps("pST", [128, 4, 128], F32)
                pH = c.ps("pH", [128, 2, 512], F32); pC = c.ps("pC", [128, 2, 512], F32)
                pY = c.ps("pYc", [128, 4, 128], BF16)
                biasc = [c.sb("biasc%d" % i, [128, 4], F32) for i in range(3)]
                gcol = [c.sb("gcol%d" % i, [128, 4], F32) for i in range(3)]
                ebend = [c.sb("ebend%d" % i, [128, 4], F32) for i in range(3)]
                EB_L = [c.sb("EB%d" % i_, [128, 4, 128], F32) for i_ in range(2)]; EB = EB_L[0]
                qs = [c.sb("qs%d" % i, [128, 4, 128], BF16) for i in range(3)]
                DT_L = [c.sb("DT%d" % i_, [128, 4, 128], F32) for i_ in range(2)]; DT = DT_L[0]; DTm_L = [c.sb("DTm%d" % i_, [128, 4, 128], F32) for i_ in range(2)]; DTm = DTm_L[0]
                SD = [c.sb("SD%d" % i, [128, 4, 128], BF16) for i in range(3)]
                kg_L = [c.sb("kg%d" % i_, [128, 4, 128], BF16) for i_ in range(2)]; kg = kg_L[0]
                pCs = [c.sb("pCs%d" % i, [128, 2, 258], F32) for i in range(3)]
                den_L = [c.sb("den%d" % i_, [128, 4], F32) for i_ in range(2)]; den = den_L[0]; hout = [c.sb("hout%d" % i, [128, 512], F32) for i in range(2)]
                hsq_L = [c.sb("hsq%d" % i_, [128, 512], F32) for i_ in range(2)]; hsq = hsq_L[0]; hss_L = [c.sb("hss%d" % i_, [128, 4], F32) for i_ in range(2)]; hss = hss_L[0]; hn_L = [c.sb("hn%d" % i_, [128, 512], F32) for i_ in range(2)]; hn = hn_L[0]
                yc_L = [c.sb("yc%d" % i_, [128, 512], BF16) for i_ in range(2)]; yc = yc_L[0]; yT_L = [c.sb("yTc%d" % i_, [128, 4, 128], BF16) for i_ in range(2)]; yT = yT_L[0]

                def hreg(pt, h):
                    return pt.a[:, h // 2, (h % 2) * 129:(h % 2) * 129 + 129]

                def sreg(pt, h):
                    return pt.a[:, h // 2, (h % 2) * 129:(h % 2) * 129 + 129]

                for d in range(2):
                    order = list(range(NT)) if d == 0 else [1, 0] + list(range(NT - 1, 1, -1))
                    tend = 127 if d == 0 else 0
                    Ud = U.a[:, d, :]
                    c.V(lambda e: e.memset(Cst.a, 0.0), [], [Cst])
                    c.V(lambda e: e.memset(Cb[1].a, 0.0), [], [Cb[1]])

                    def l_s1(n):
                        kg_c = kg_L[n % 2]; EB_c = EB_L[n % 2]; DT_c = DT_L[n % 2]; DTm_c = DTm_L[n % 2]
                        i = order[n]
                        q_ = mq[n % 3]; k_ = mk[n % 3]; kt_ = mkt[n % 3]; v_ = mv[n % 3]; g_ = mg[n % 3]
                        bc_ = biasc[n % 3]; gc_ = gcol[n % 3]; eb_ = ebend[n % 3]; qs_ = qs[n % 3]; SD_ = SD[n % 3]; pcs = pCs[n % 3]
                        tok = slice(i * 128, (i + 1) * 128)
                        need_out = i >= first_tile
                        c.dma("sp", q_.a, MQT.a[:, :, tok], q_, MQT)
                        yield
                        c.dma("sp", k_.a, MKT.a[:, :, tok], k_, MKT)
                        yield
                        c.dma("sp", kt_.a, MK.a[tok], kt_, MK)
                        yield
                        c.dma("sp", v_.a, MV.a[tok], v_, MV)
                        yield
                        c.dma("sp", g_.a, MG.a[tok], g_, MG)
                        yield
                        if need_out and d == 1:
                            c.dma("sp", hs[n % 3].a, HS.a[tok], hs[n % 3], HS)
                            yield
                            c.dma("sp", mo[n % 3].a, MO.a[tok], mo[n % 3], MO)
                            yield
                        gv = g_.a.rearrange("p (d t h) -> p d t h", d=2, t=2)
                        ig = gv[:, d, 0, :]; lf = gv[:, d, 1, :]
                        c.mm(lambda e: e.matmul(pb.a, lhsT=Ud, rhs=lf, start=True, stop=True), [U, g_], [pb])
                        yield
                        for h in range(4):
                            c.mm(lambda e: e.matmul(pbB.a[:, h, :], lhsT=gv[:, d, 1, h:h + 1].to_broadcast([128, 128]), rhs=Ud, start=True, stop=True), [g_, U], [pbB], last=(h == 3))
                            yield
                        if need_out:
                            for h in range(4):
                                c.mm(lambda e: e.matmul(pST.a[:, h, :], lhsT=k_.a[:, h, :], rhs=q_.a[:, h, :], start=True, stop=True), [k_, q_], [pST], last=(h == 3))
                                yield
                        c.V(lambda e: e.tensor_tensor(out=bc_.a, in0=ig, in1=pb.a, op=ALU.subtract), [g_, pb], [bc_])
                        yield
                        c.V(lambda e: e.tensor_tensor(out=gc_.a, in0=pbB.a[:, :, tend], in1=bc_.a, op=ALU.add), [pbB, bc_], [gc_])
                        yield
                        c.A(lambda e: e.activation(out=gc_.a, in_=gc_.a, func=AF.Exp), [gc_], [gc_])
                        yield
                        c.A(lambda e: e.activation(out=eb_.a, in_=pbB.a[:, :, tend], func=AF.Exp), [pbB], [eb_])
                        yield
                        c.V(lambda e: e.tensor_tensor(out=kg_c.a, in0=kt_.a.rearrange("p (h d) -> p h d", d=128), in1=gc_.a.unsqueeze(2).to_broadcast([128, 4, 128]), op=ALU.mult), [kt_, gc_], [kg_c])
                        yield
                        for h in range(4):
                            c.mm(lambda e: e.matmul(hreg(pC, h), lhsT=kg_c.a[:, h, :], rhs=v_.a[:, h, :], start=True, stop=True), [kg_c, v_], [pC], last=(h == 3))
                            yield
                        for hh in range(2):
                            c.A(lambda e: e.copy(out=pcs.a[:, hh, :], in_=pC.a[:, hh, 0:258]), [pC], [pcs])
                            yield
                        if need_out:
                            c.A(lambda e: e.activation(out=EB_c.a, in_=pbB.a, func=AF.Exp), [pbB], [EB_c])
                            yield
                            c.V(lambda e: e.tensor_tensor(out=qs_.a, in0=q_.a, in1=EB_c.a, op=ALU.mult), [q_, EB_c], [qs_])
                            yield
                            for h in range(4):
                                c.A(lambda e: e.activation(out=DT_c.a[:, h, :], in_=pbB.a[:, h, :], func=AF.Exp, bias=bc_.a[:, h:h + 1]), [pbB, bc_], [DT_c])
                                yield
                            c.G(lambda e: e.tensor_tensor(out=DTm_c.a, in0=DT_c.a, in1=U.a[:, d:d + 1, :].to_broadcast([128, 4, 128]), op=ALU.mult), [DT_c, U], [DTm_c])
                            yield
                            c.V(lambda e: e.tensor_tensor(out=SD_.a, in0=DTm_c.a, in1=pST.a, op=ALU.mult), [DTm_c, pST], [SD_])
                            yield

                    def l_s2(n):
                        den_c = den_L[n % 2]; hsq_c = hsq_L[n % 2]; hss_c = hss_L[n % 2]; hn_c = hn_L[n % 2]; yc_c = yc_L[n % 2]; yT_c = yT_L[n % 2]
                        i = order[n]
                        v_ = mv[n % 3]; eb_ = ebend[n % 3]; qs_ = qs[n % 3]; SD_ = SD[n % 3]; pcs = pCs[n % 3]
                        cb_old = Cb[(n + 1) % 2]; cb_new = Cb[n % 2]
                        ho = hout[n % 2]
                        tok = slice(i * 128, (i + 1) * 128)
                        need_out = i >= first_tile
                        if need_out:
                            for h in range(4):
                                c.mm(lambda e: e.matmul(hreg(pH, h), lhsT=qs_.a[:, h, :], rhs=cb_old.a[:, h, :], start=True, stop=False), [qs_, cb_old], [pH], last=False)
                                yield
                                c.mm(lambda e: e.matmul(hreg(pH, h), lhsT=SD_.a[:, h, :], rhs=v_.a[:, h, :], start=False, stop=True), [SD_, v_], [pH], last=(h == 3))
                                yield
                        for h in range(4):
                            c.V(lambda e: e.scalar_tensor_tensor(out=Cst.a[:, h, :], in0=Cst.a[:, h, :], scalar=eb_.a[:, h:h + 1], in1=sreg(pcs, h), op0=ALU.mult, op1=ALU.add), [Cst, eb_, pcs], [Cst])
                            yield
                        c.A(lambda e: e.copy(out=cb_new.a, in_=Cst.a), [Cst], [cb_new])
                        yield
                        if not need_out:
                            return
                        for h in range(4):
                            c.A(lambda e: e.activation(out=den_c.a[:, h:h + 1], in_=hreg(pH, h)[:, 128:129], func=AF.Abs), [pH], [den_c])
                            yield
                        c.V(lambda e: e.tensor_scalar_max(out=den_c.a, in0=den_c.a, scalar1=1.0), [den_c], [den_c])
                        yield
                        c.V(lambda e: e.reciprocal(out=den_c.a, in_=den_c.a), [den_c], [den_c])
                        yield
                        for h in range(4):
                            c.V(lambda e: e.tensor_scalar(out=ho.a[:, h * 128:(h + 1) * 128], in0=hreg(pH, h)[:, 0:128], scalar1=den_c.a[:, h:h + 1], scalar2=None, op0=ALU.mult), [pH, den_c], [ho])
                            yield
                        if d == 0:
                            c.dma("sp", HS.a[tok], ho.a, HS, ho)
                            yield
                            return
                        h_ = hs[n % 3]; o_ = mo[n % 3]
                        c.G(lambda e: e.tensor_tensor(out=ho.a, in0=ho.a, in1=h_.a, op=ALU.add), [ho, h_], [ho])
                        yield
                        c.A(lambda e: e.activation(out=hsq_c.a, in_=ho.a, func=AF.Square), [ho], [hsq_c])
                        yield
                        c.V(lambda e: e.tensor_reduce(out=hss_c.a, in_=hsq_c.a.rearrange("p (h d) -> p h d", d=128), axis=AX.X, op=ALU.add), [hsq_c], [hss_c])
                        yield
                        c.V(lambda e: e.tensor_scalar(out=hss_c.a, in0=hss_c.a, scalar1=1.0 / 128, scalar2=1e-6, op0=ALU.mult, op1=ALU.add), [hss_c], [hss_c])
                        yield
                        c.A(lambda e: e.activation(out=hss_c.a, in_=hss_c.a, func=AF.Sqrt), [hss_c], [hss_c])
                        yield
                        c.V(lambda e: e.reciprocal(out=hss_c.a, in_=hss_c.a), [hss_c], [hss_c])
                        yield
                        c.V(lambda e: e.tensor_tensor(out=hn_c.a.rearrange("p (h d) -> p h d", d=128), in0=ho.a.rearrange("p (h d) -> p h d", d=128), in1=hss_c.a.unsqueeze(2).to_broadcast([128, 4, 128]), op=ALU.mult), [ho, hss_c], [hn_c])
                        yield
                        c.G(lambda e: e.tensor_tensor(out=hn_c.a, in0=hn_c.a, in1=gml.a, op=ALU.mult), [hn_c, gml], [hn_c])
                        yield
                        c.V(lambda e: e.tensor_tensor(out=yc_c.a, in0=hn_c.a, in1=o_.a, op=ALU.mult), [hn_c, o_], [yc_c])
                        yield
                        for m in range(4):
                            c.mm(lambda e: e.transpose(out=pY.a[:, m, :], in_=yc_c.a[:, m * 128:(m + 1) * 128], identity=ident.a), [yc_c, ident], [pY], last=(m == 3))
                            yield
                        c.A(lambda e: e.copy(out=yT_c.a, in_=pY.a), [pY], [yT_c])
                        yield
                        c.dma("sp", YCT.a[:, :, tok].rearrange("m p t -> p m t"), yT_c.a, YCT, yT_c)
                        yield

                    rr(l_s1(0))
                    if NT > 1:
                        rr(l_s1(1))
                    for n in range(NT):
                        rr(l_s2(n), l_s1(n + 2) if n + 2 < NT else None)
                c.pop()
                if stop_after == "p2c":
                    break

                c.push()
                wbr = c.sb("wbr", [128, 3, 4, D], BF16, grp="w3"); wo = c.sb("wo", [128, 8, D], BF16, grp="w3")
                for br in range(3):
                    c.dma("pool", wbr.a[:, br], w_branch.a[l, br].rearrange("(k p) n -> p k n", p=128), wbr, w_branch)
                c.dma("pool", wo.a, w_out.a[l].rearrange("(k p) n -> p k n", p=128), wo, w_out)
                ybr = [c.sb("ybr%d" % i, [128, 3, 4, 128], BF16, grp="l3%d" % i) for i in range(2)]
                gt_ = [c.sb("gt%d" % i, [128, 3072], BF16, grp="l3%d" % i) for i in range(2)]
                xt = [c.sb("x3%d" % i, [128, D], F32, grp="l3%d" % i) for i in range(2)]
                pB = [c.ps("pB%d" % i, [128, 512], F32) for i in range(2)]
                mrg_L = [c.sb("mrg%d" % i_, [128, D], F32) for i_ in range(2)]; mrg = mrg_L[0]; mtmp_L = [c.sb("mtmp%d" % i_, [128, 512], F32) for i_ in range(2)]; mtmp = mtmp_L[0]; mrb_L = [c.sb("mrb%d" % i_, [128, D], BF16) for i_ in range(2)]; mrb = mrb_L[0]
                pT = c.ps("pT3", [128, 8, 128], BF16); mT_L = [c.sb("mT%d" % i_, [128, 8, 128], BF16) for i_ in range(2)]; mT = mT_L[0]
                pYo = c.ps("pYo", [128, D], F32)
                x1 = [c.sb("x1%d" % i, [128, D], F32) for i in range(2)]
                junk_L = [c.sb("junk3%d" % i_, [128, D], F32) for i_ in range(2)]; junk = junk_L[0]; ss_L = [c.sb("ss3%d" % i_, [128, 1], F32) for i_ in range(2)]; ss = ss_L[0]; rstd_L = [c.sb("rstd3%d" % i_, [128, 1], F32) for i_ in range(2)]; rstd = rstd_L[0]
                xnf_L = [c.sb("xnf%d" % i_, [128, D], F32) for i_ in range(2)]; xnf = xnf_L[0]
                pTf = c.ps("pTf", [128, 8, 128], F32)
                h2f_L = [c.sb("h2f%d" % i_, [128, 8, 128], F32) for i_ in range(2)]; h2f = h2f_L[0]; h2b = [c.sb("h2b%d" % i, [128, 8, 128], BF16) for i in range(2)]
                pL = c.ps("pL", [128, 36], F32)
                lg_L = [c.sb("lg%d" % i_, [128, 36], F32) for i_ in range(2)]; lg = lg_L[0]; gmax_L = [c.sb("gmax%d" % i_, [128, 1], F32) for i_ in range(2)]; gmax = gmax_L[0]; ngmax_L = [c.sb("ngmax%d" % i_, [128, 1], F32) for i_ in range(2)]; ngmax = ngmax_L[0]
                eg_L = [c.sb("eg%d" % i_, [128, 4], F32) for i_ in range(2)]; eg = eg_L[0]; sg_L = [c.sb("sg%d" % i_, [128, 1], F32) for i_ in range(2)]; sg = sg_L[0]; ohg_L = [c.sb("ohg%d" % i_, [128, 4], F32) for i_ in range(2)]; ohg = ohg_L[0]
                lem_L = [c.sb("lem%d" % i_, [128, 4, 8], F32) for i_ in range(2)]; lem = lem_L[0]; les_L = [c.sb("les%d" % i_, [128, 8], F32) for i_ in range(2)]; les = les_L[0]; m8_L = [c.sb("m8%d" % i_, [128, 8], F32) for i_ in range(2)]; m8 = m8_L[0]
                nv0_L = [c.sb("nv0%d" % i_, [128, 1], F32) for i_ in range(2)]; nv0 = nv0_L[0]; e8_L = [c.sb("e8%d" % i_, [128, 8], F32) for i_ in range(2)]; e8 = e8_L[0]; mk2_L = [c.sb("mk2%d" % i_, [128, 8], F32) for i_ in range(2)]; mk2 = mk2_L[0]
                w8_L = [c.sb("w8%d" % i_, [128, 8], F32) for i_ in range(2)]; w8 = w8_L[0]; sden_L = [c.sb("sden%d" % i_, [128, 1], F32) for i_ in range(2)]; sden = sden_L[0]
                dw = [c.sb("dw%d" % i, [128, 4, 8], F32) for i in range(2)]
                def genA(i):
                    tok = slice(i * 128, (i + 1) * 128)
                    j3 = 2 if i < 2 else b
                    mrg = mrg_L[i % 2]; mtmp = mtmp_L[i % 2]; mrb = mrb_L[i % 2]; mT = mT_L[i % 2]; junk = junk_L[i % 2]; ss = ss_L[i % 2]; rstd = rstd_L[i % 2]; xnf = xnf_L[i % 2]; h2f = h2f_L[i % 2]; lg = lg_L[i % 2]; gmax = gmax_L[i % 2]; ngmax = ngmax_L[i % 2]; eg = eg_L[i % 2]; sg = sg_L[i % 2]; ohg = ohg_L[i % 2]; lem = lem_L[i % 2]; les = les_L[i % 2]; m8 = m8_L[i % 2]; nv0 = nv0_L[i % 2]; e8 = e8_L[i % 2]; mk2 = mk2_L[i % 2]; w8 = w8_L[i % 2]; sden = sden_L[i % 2]
                    y_ = ybr[i % 2]; g_ = gt_[i % 2]; x_ = xt[i % 2]; xo = x1[i % 2]; hb = h2b[i % 2]; dw_ = dw[i % 2]
                    for br, YT_ in enumerate((YAT, YBT, YCT)):
                        c.dma("sp", y_.a[:, br], YT_.a[:, :, tok].rearrange("m p t -> p m t"), y_, YT_)
                        yield
                    c.dma("sp", g_.a, GATES.a[tok], g_, GATES)
                    yield
                    src, ap = tile_src(l, b, i)
                    c.dma("sp", x_.a, ap, x_, src)
                    yield
                    for half in range(2):
                        cs = slice(half * 512, (half + 1) * 512)
                        for br in range(3):
                            p_ = pB[(half * 3 + br) % 2]
                            for k in range(4):
                                c.mm(lambda e: e.matmul(p_.a, lhsT=y_.a[:, br, k, :], rhs=wbr.a[:, br, k, cs], start=(k == 0), stop=(k == 3)), [y_, wbr], [p_], last=(k == 3))
                                yield
                            gsl = g_.a[:, br * 1024 + half * 512: br * 1024 + (half + 1) * 512]
                            if br == 0:
                                c.V(lambda e: e.tensor_tensor(out=mrg.a[:, cs], in0=p_.a, in1=gsl, op=ALU.mult), [p_, g_], [mrg])
                                yield
                            else:
                                c.V(lambda e: e.tensor_tensor(out=mtmp.a, in0=p_.a, in1=gsl, op=ALU.mult), [p_, g_], [mtmp])
                                yield
                                c.G(lambda e: e.tensor_tensor(out=mrg.a[:, cs], in0=mrg.a[:, cs], in1=mtmp.a, op=ALU.add), [mrg, mtmp], [mrg])
                                yield
                    c.A(lambda e: e.copy(out=mrb.a, in_=mrg.a), [mrg], [mrb])
                    yield
                    for k in range(8):
                        c.mm(lambda e: e.transpose(out=pT.a[:, k, :], in_=mrb.a[:, k * 128:(k + 1) * 128], identity=ident.a), [mrb, ident], [pT], last=(k == 7))
                        yield
                    c.A(lambda e: e.copy(out=mT.a, in_=pT.a), [pT], [mT])
                    yield
                    for half in range(2):
                        cs = slice(half * 512, (half + 1) * 512)
                        for k in range(8):
                            c.mm(lambda e: e.matmul(pYo.a[:, cs], lhsT=mT.a[:, k, :], rhs=wo.a[:, k, cs], start=(k == 0), stop=(k == 7)), [mT, wo], [pYo], last=(k == 7 and half == 1))
                            yield
                    c.V(lambda e: e.tensor_tensor(out=xo.a, in0=pYo.a, in1=GT.a[:, 0, j3, :], op=ALU.mult), [pYo, GT], [xo])
                    yield
                    c.G(lambda e: e.tensor_tensor(out=xo.a, in0=xo.a, in1=x_.a, op=ALU.add), [xo, x_], [xo])
                    yield
                    c.dma("sp", XS.a[b, tok, :], xo.a, XS, xo)
                    yield
                def genB(i):
                    tok = slice(i * 128, (i + 1) * 128)
                    j3 = 2 if i < 2 else b
                    mrg = mrg_L[i % 2]; mtmp = mtmp_L[i % 2]; mrb = mrb_L[i % 2]; mT = mT_L[i % 2]; junk = junk_L[i % 2]; ss = ss_L[i % 2]; rstd = rstd_L[i % 2]; xnf = xnf_L[i % 2]; h2f = h2f_L[i % 2]; lg = lg_L[i % 2]; gmax = gmax_L[i % 2]; ngmax = ngmax_L[i % 2]; eg = eg_L[i % 2]; sg = sg_L[i % 2]; ohg = ohg_L[i % 2]; lem = lem_L[i % 2]; les = les_L[i % 2]; m8 = m8_L[i % 2]; nv0 = nv0_L[i % 2]; e8 = e8_L[i % 2]; mk2 = mk2_L[i % 2]; w8 = w8_L[i % 2]; sden = sden_L[i % 2]
                    y_ = ybr[i % 2]; g_ = gt_[i % 2]; x_ = xt[i % 2]; xo = x1[i % 2]; hb = h2b[i % 2]; dw_ = dw[i % 2]
                    c.V(lambda e: e.memset(ss.a, 0.0), [], [ss])
                    yield
                    c.A(lambda e: e.activation(out=junk.a, in_=xo.a, func=AF.Square, accum_out=ss.a), [xo], [junk, ss])
                    yield
                    c.V(lambda e: e.tensor_scalar(out=rstd.a, in0=ss.a, scalar1=1.0 / D, scalar2=1e-6, op0=ALU.mult, op1=ALU.add), [ss], [rstd])
                    yield
                    c.A(lambda e: e.activation(out=rstd.a, in_=rstd.a, func=AF.Sqrt), [rstd], [rstd])
                    yield
                    c.V(lambda e: e.reciprocal(out=rstd.a, in_=rstd.a), [rstd], [rstd])
                    yield
                    c.V(lambda e: e.tensor_scalar(out=xnf.a, in0=xo.a, scalar1=rstd.a[:, 0:1], scalar2=None, op0=ALU.mult), [xo, rstd], [xnf])
                    yield
                    for k in range(8):
                        c.mm(lambda e: e.transpose(out=pTf.a[:, k, :], in_=xnf.a[:, k * 128:(k + 1) * 128], identity=ident_f.a), [xnf, ident_f], [pTf], last=(k == 7))
                        yield
                    for k in range(8):
                        c.V(lambda e: e.tensor_scalar(out=h2f.a[:, k, :], in0=pTf.a[:, k, :], scalar1=G2.a[:, k, j3:j3 + 1], scalar2=modT.a[:, 3, k, j3:j3 + 1], op0=ALU.mult, op1=ALU.add), [pTf, G2, modT], [h2f])
                        yield
                    c.A(lambda e: e.copy(out=hb.a, in_=h2f.a), [h2f], [hb])
                    yield
                    c.dma("sp", H2T.a[:, :, tok], hb.a, H2T, hb)
                    yield
                    for k in range(8):
                        c.mm(lambda e: e.matmul(pL.a, lhsT=h2f.a[:, k, :], rhs=wrt.a[:, k, :], start=(k == 0), stop=(k == 7)), [h2f, wrt], [pL], last=(k == 7))
                        yield
                    c.V(lambda e: e.tensor_tensor(out=lg.a, in0=pL.a, in1=brt.a, op=ALU.add), [pL, brt], [lg])
                    yield
                    c.V(lambda e: e.tensor_reduce(out=gmax.a, in_=lg.a[:, 0:4], axis=AX.X, op=ALU.max), [lg], [gmax])
                    yield
                    c.V(lambda e: e.tensor_scalar(out=ngmax.a, in0=gmax.a, scalar1=-1.0, scalar2=None, op0=ALU.mult), [gmax], [ngmax])
                    yield
                    c.V(lambda e: e.memset(sg.a, 0.0), [], [sg])
                    yield
                    c.A(lambda e: e.activation(out=eg.a, in_=lg.a[:, 0:4], func=AF.Exp, bias=ngmax.a[:, 0:1], accum_out=sg.a), [lg, ngmax], [eg, sg])
                    yield
                    c.V(lambda e: e.tensor_scalar(out=ohg.a, in0=lg.a[:, 0:4], scalar1=gmax.a[:, 0:1], scalar2=None, op0=ALU.is_ge), [lg, gmax], [ohg])
                    yield
                    c.V(lambda e: e.tensor_tensor(out=lem.a, in0=lg.a[:, 4:36].rearrange("p (g e) -> p g e", e=8), in1=ohg.a.unsqueeze(2).to_broadcast([128, 4, 8]), op=ALU.mult), [lg, ohg], [lem])
                    yield
                    c.V(lambda e: e.tensor_reduce(out=les.a, in_=lem.a.rearrange("p g e -> p e g"), axis=AX.X, op=ALU.add), [lem], [les])
                    yield
                    c.V(lambda e: e.max(out=m8.a, in_=les.a), [les], [m8])
                    yield
                    c.V(lambda e: e.tensor_scalar(out=nv0.a, in0=m8.a[:, 0:1], scalar1=-1.0, scalar2=None, op0=ALU.mult), [m8], [nv0])
                    yield
                    c.A(lambda e: e.activation(out=e8.a, in_=les.a, func=AF.Exp, bias=nv0.a[:, 0:1]), [les, nv0], [e8])
                    yield
                    c.V(lambda e: e.tensor_scalar(out=mk2.a, in0=les.a, scalar1=m8.a[:, 1:2], scalar2=None, op0=ALU.is_ge), [les, m8], [mk2])
                    yield
                    c.V(lambda e: e.tensor_tensor(out=w8.a, in0=e8.a, in1=mk2.a, op=ALU.mult), [e8, mk2], [w8])
                    yield
                    c.V(lambda e: e.tensor_reduce(out=sden.a, in_=w8.a, axis=AX.X, op=ALU.add), [w8], [sden])
                    yield
                    c.V(lambda e: e.tensor_tensor(out=sden.a, in0=sden.a, in1=sg.a, op=ALU.mult), [sden, sg], [sden])
                    yield
                    c.V(lambda e: e.reciprocal(out=sden.a, in_=sden.a), [sden], [sden])
                    yield
                    c.V(lambda e: e.tensor_scalar(out=w8.a, in0=w8.a, scalar1=sden.a[:, 0:1], scalar2=None, op0=ALU.mult), [w8, sden], [w8])
                    yield
                    c.V(lambda e: e.tensor_tensor(out=dw_.a, in0=ohg.a.unsqueeze(2).to_broadcast([128, 4, 8]), in1=w8.a.unsqueeze(1).to_broadcast([128, 4, 8]), op=ALU.mult), [ohg, w8], [dw_])
                    yield
                    c.dma("sp", DW.a[tok].rearrange("p (g e) -> p g e", e=8), dw_.a, DW, dw_)
                    yield
                tl = list(range(first_tile, NT))
                rr(genA(tl[0]))
                for k_ in range(len(tl)):
                    rr(genB(tl[k_]), genA(tl[k_ + 1]) if k_ + 1 < len(tl) else None)
                c.pop()
                if stop_after == "p3a":
                    break

                tiles = list(range(first_tile, NT))
                ng = 2
                per = (len(tiles) + ng - 1) // ng
                for gi in range(ng):
                    grp = tiles[gi * per:(gi + 1) * per]
                    t0 = grp[0]; G_ = len(grp)
                    c.push()
                    h2 = c.sb("h2", [128, 8, G_ * 128], BF16, grp="g4")
                    dwg = c.sb("dwg", [128, G_, 32], F32, grp="g4")
                    acc = c.sb("acc", [128, G_, D], F32)
                    c.dma("sp", h2.a, H2T.a[:, :, t0 * 128:(t0 + G_) * 128], h2, H2T)
                    c.dma("sp", dwg.a, DW.a[t0 * 128:(t0 + G_) * 128].rearrange("(i p) e -> p i e", p=128), dwg, DW)
                    wg = [c.sb("wg%d" % i, [128, 8, 512], BF16, grp="we%d" % i) for i in range(2)]
                    wd = [c.sb("wd%d" % i, [128, 2, D], BF16, grp="we%d" % i) for i in range(2)]
                    pGU = [c.ps("pGU%d" % i, [128, 2, 512], F32) for i in range(2)]
                    sl = [c.sb("sl%d" % i, [128, 512], F32) for i in range(2)]
                    aT = [c.sb("aT%d" % i, [128, 2, 512], BF16) for i in range(2)]
                    pD = [c.ps("pD%d" % i, [128, D], F32) for i in range(2)]
                    xt = [c.sb("x4%d" % i, [128, D], F32) for i in range(2)]
                    quads = [(tq, min(4, G_ - tq)) for tq in range(0, G_, 4)]
                    steps = [(e_, qi) for e_ in range(32) for qi in range(len(quads))]

                    def m_s1(si):
                        e_, qi = steps[si]
                        tq, nt = quads[qi]; N = nt * 128
                        g_ = wg[e_ % 2]; d_ = wd[e_ % 2]; at_ = aT[si % 2]
                        if qi == 0:
                            c.dma("sp", g_.a, WGU.a[e_], g_, WGU)
                            c.dma("sp", d_.a, WDN.a[e_], d_, WDN)
                        for cch in range(2):
                            pg = pGU[cch]; s_ = sl[cch]
                            for which in range(2):
                                col0 = which * 256 + cch * 128
                                for k in range(8):
                                    c.mm(lambda e: e.matmul(pg.a[:, which, :N], lhsT=g_.a[:, k, col0:col0 + 128], rhs=h2.a[:, k, tq * 128:tq * 128 + N], start=(k == 0), stop=(k == 7)), [g_, h2], [pg], last=(k == 7 and which == 1))
                            c.A(lambda e: e.activation(out=s_.a[:, :N], in_=pg.a[:, 0, :N], func=AF.Silu), [pg], [s_])
                            c.V(lambda e: e.tensor_tensor(out=at_.a[:, cch, :N], in0=s_.a[:, :N], in1=pg.a[:, 1, :N], op=ALU.mult), [s_, pg], [at_])

                    def m_s2(si):
                        e_, qi = steps[si]
                        tq, nt = quads[qi]
                        d_ = wd[e_ % 2]; at_ = aT[si % 2]
                        for tj in range(nt):
                            ti = tq + tj
                            pd = pD[tj % 2]
                            for half in range(2):
                                cs = slice(half * 512, (half + 1) * 512)
                                for k in range(2):
                                    c.mm(lambda e: e.matmul(pd.a[:, cs], lhsT=at_.a[:, k, tj * 128:(tj + 1) * 128], rhs=d_.a[:, k, cs], start=(k == 0), stop=(k == 1)), [at_, d_], [pd], last=(k == 1 and half == 1))
                            if e_ == 0:
                                c.V(lambda e: e.tensor_scalar(out=acc.a[:, ti, :], in0=pd.a, scalar1=dwg.a[:, ti, e_:e_ + 1], scalar2=None, op0=ALU.mult), [pd, dwg], [acc])
                            else:
                                c.V(lambda e: e.scalar_tensor_tensor(out=acc.a[:, ti, :], in0=pd.a, scalar=dwg.a[:, ti, e_:e_ + 1], in1=acc.a[:, ti, :], op0=ALU.mult, op1=ALU.add), [pd, dwg, acc], [acc])

                    pipeline(len(steps), m_s1, m_s2, 2)
                    for ti in range(G_):
                        i = t0 + ti
                        j3 = 2 if i < 2 else b
                        x_ = xt[ti % 2]
                        c.dma("sp", x_.a, XS.a[b, i * 128:(i + 1) * 128, :], x_, XS)
                        c.G(lambda e: e.tensor_tensor(out=acc.a[:, ti, :], in0=acc.a[:, ti, :], in1=GT.a[:, 1, j3, :], op=ALU.mult), [acc, GT], [acc])
                        c.V(lambda e: e.tensor_tensor(out=x_.a, in0=x_.a, in1=acc.a[:, ti, :], op=ALU.add), [x_, acc], [x_])
                        if last_layer:
                            c.dma("sp", y_out.a[b, (i - 2) * 128:(i - 1) * 128, :], x_.a, y_out, x_)
                        else:
                            c.dma("sp", XS.a[b, i * 128:(i + 1) * 128, :], x_.a, XS, x_)
                    c.pop()
            if stop_after is not None:
                break
            c.pop()
        c.barrier()
        while len(c.stack) > 1:
            c.stack.pop().__exit__(None, None, None)
        print("instructions:", c.ninst, "sems:", len(c.sem))
    nc._trace = c.trace
    return nc


def host_consts():
    t = np.arange(4096)
    row = (t // 64).astype(np.float32); col = (t % 64).astype(np.float32)
    inv = (10000.0 ** (-np.arange(16, dtype=np.float32) / 16)).astype(np.float32)
    ang = np.concatenate([row[:, None] * inv, col[:, None] * inv], axis=-1).astype(np.float32)
    cos = np.ones((T, 32), np.float32); sin = np.zeros((T, 32), np.float32)
    cos[256:] = np.cos(ang); sin[256:] = np.sin(ang)
    s = np.arange(128)
    u = np.stack([(s[:, None] <= s[None, :]), (s[:, None] >= s[None, :])]).astype(np.float32)
    kc = np.arange(64)[:, None]; qc = np.arange(64)[None, :]
    cs = np.clip(qc - 8, 0, 48)
    valid = (kc >= cs) & (kc < cs + 16)
    cm = np.where(valid, 0.0, NEG).astype(np.float32)
    jd = np.zeros((64, 128), np.float32)
    for kc in range(64):
        jd[63 - kc, kc] = 1.0; jd[63 - kc, 64 + kc] = 1.0
    return {"k_jd": jd, "k_cos": cos, "k_sin": sin, "k_u": u, "k_id": np.eye(128, dtype=np.float32), "k_colmask": np.concatenate([cm, cm], 0)}


def make_in_maps(inputs, nb, cores):
    consts = host_consts()
    maps = []
    for ci in cores:
        m = dict(consts)
        for k, v in inputs.items():
            v = np.ascontiguousarray(v, dtype=np.float32)
            if k in ("x", "c", "ctx"):
                m[k] = np.ascontiguousarray(v[ci * nb:(ci + 1) * nb])
            elif k == "b_mlstm":
                m[k] = v.reshape(2, 16)
            else:
                m[k] = v
        maps.append(m)
    return maps


def kernel(**inputs):
    nb = 2
    nc = build(nb=nb, nl=2)
    in_maps = make_in_maps(inputs, nb, list(range(8)))
    res = run_bass_kernel_spmd(nc, in_maps, core_ids=list(range(8)))
    return np.concatenate([r["y"] for r in res.results], axis=0).astype(np.float32)
```

```python
import numpy as np
import concourse.bass as bass
import concourse.mybir as mybir
from concourse.bass_utils import run_bass_kernel_spmd
from contextlib import ExitStack
import os

F32 = mybir.dt.float32
BF16 = mybir.dt.bfloat16
AF = mybir.ActivationFunctionType
ALU = mybir.AluOpType
AX = mybir.AxisListType

T = 4352
NT = 34
D = 1024
NEG = -1e30
SKIP_SAME = int(os.environ.get("SKIP_SAME", "0"))


class Buf:
    __slots__ = ("name", "w", "r", "dsem", "t", "grp")

    def __init__(self, name, t=None, grp=None):
        self.name = name
        self.grp = grp
        self.w = None
        self.r = {}
        self.dsem = None
        self.t = t

    @property
    def a(self):
        return self.t.ap() if hasattr(self.t, "ap") else self.t[:]


class Ctx:
    def __init__(self, nc, es):
        self.nc = nc
        self.es = es
        self.E = {"pe": nc.tensor, "act": nc.scalar, "dve": nc.vector, "pool": nc.gpsimd, "sp": nc.sync}
        self.sem = {}
        self.ecnt = {}
        for e in ("pe", "act", "dve", "pool"):
            self.sem[e] = es.enter_context(nc.semaphore("c_" + e))
            self.ecnt[e] = 0
        self.seen = {e: {} for e in self.E}
        self.ninst = 0
        self.uid = 0
        self.trace = {e: [] for e in self.E}
        self.shared = set()
        self.stack = [es]
        self.dtot = {}

    def sb(self, name, shape, dt, grp=None):
        self.uid += 1
        return Buf(name, self.stack[-1].enter_context(self.nc.sbuf_tensor("%s_%d" % (name, self.uid), list(shape), dt)), grp)

    def ps(self, name, shape, dt):
        self.uid += 1
        return Buf(name, self.stack[-1].enter_context(self.nc.psum_tensor("%s_%d" % (name, self.uid), list(shape), dt)))

    def dram(self, name, shape, dt, kind="Internal"):
        return Buf(name, self.nc.dram_tensor(name, list(shape), dt, kind=kind))

    def push(self):
        st = ExitStack()
        st.__enter__()
        self.stack.append(st)

    def pop(self):
        self.barrier()
        self.stack.pop().__exit__(None, None, None)

    def barrier(self):
        evs = [(e, self.ecnt[e]) for e in self.ecnt] + list(self.dtot.items())
        for e in self.E:
            self._wait(e, evs)

    def _wait(self, eng, deps):
        need = {}
        for k, v in deps:
            if eng == "pe" and k == "pe":
                continue
            if SKIP_SAME and k == eng and v <= self.ecnt[eng] - SKIP_SAME:
                continue
            if v > need.get(k, 0):
                need[k] = v
        seen = self.seen[eng]
        for k, v in need.items():
            if k in self.shared:
                v = self.dtot[k]
            if seen.get(k, 0) >= v:
                continue
            self.E[eng].wait_ge(self.sem[k], v)
            self.trace[eng].append(("w", k, v))
            self.ninst += 1
            seen[k] = v

    def _deps(self, reads, writes):
        deps = []
        for b in reads:
            if b.w is not None:
                deps.append(b.w)
        for b in writes:
            if b.w is not None:
                deps.append(b.w)
            deps.extend(b.r.items())
        return deps

    def _commit(self, ev, reads, writes):
        k, v = ev
        for b in reads:
            if b.r.get(k, 0) < v:
                b.r[k] = v
        for b in writes:
            b.w = ev
            b.r = {}

    def op(self, eng, f, reads=(), writes=()):
        self._wait(eng, self._deps(reads, writes))
        ins = f(self.E[eng])
        self.ecnt[eng] += 1
        ins.then_inc(self.sem[eng], 1)
        self.trace[eng].append(("i", eng, 1))
        self.ninst += 1
        self._commit((eng, self.ecnt[eng]), reads, writes)
        return ins

    def V(self, f, r=(), w=()):
        return self.op("dve", f, r, w)

    def A(self, f, r=(), w=()):
        return self.op("act", f, r, w)

    def G(self, f, r=(), w=()):
        return self.op("pool", f, r, w)

    def mm(self, f, reads=(), writes=(), last=True):
        self._wait("pe", self._deps(reads, writes))
        ins = f(self.E["pe"])
        self.ninst += 1
        if last:
            self.ecnt["pe"] += 1
            ins.then_inc(self.sem["pe"], 1)
            self.trace["pe"].append(("i", "pe", 1))
            self._commit(("pe", self.ecnt["pe"]), reads, writes)
        else:
            self._commit(("pe", self.ecnt["pe"] + 1), reads, writes)
        return ins

    def dma(self, q, out, in_, dst, src, **kw):
        self._wait(q, self._deps((src,), (dst,)))
        if dst.dsem is None:
            dst.dsem = "d_" + (dst.grp or dst.name)
            if dst.grp:
                self.shared.add(dst.dsem)
            if dst.dsem not in self.sem:
                self.sem[dst.dsem] = self.es.enter_context(self.nc.semaphore(dst.dsem))
                self.dtot[dst.dsem] = 0
        ins = self.E[q].dma_start(out=out, in_=in_, **kw)
        self.dtot[dst.dsem] += 16
        ins.then_inc(self.sem[dst.dsem], 16)
        self.trace[q].append(("i", dst.dsem, 16))
        self.ninst += 1
        self._commit((dst.dsem, self.dtot[dst.dsem]), (src,), (dst,))
        return ins


def pipeline(n, stage1, stage2, depth, s1_first=False):
    for si in range(min(depth, n)):
        stage1(si)
    for si in range(n):
        if s1_first and si + depth < n:
            stage1(si + depth)
        stage2(si)
        if not s1_first and si + depth < n:
            stage1(si + depth)


def rr(*gens):
    gens = [g for g in gens if g is not None]
    while gens:
        for g in list(gens):
            try:
                next(g)
            except StopIteration:
                gens.remove(g)


def na_plan(j):
    plan = []
    for kt in range(32):
        blocks = {}
        anyv = False
        for a in range(2):
            for b in range(2):
                qr = 2 * j + b
                kr = 2 * kt + a
                rs = min(max(qr - 4, 0), 56)
                ok = rs <= kr < rs + 8
                blocks[(a, b)] = (kr - qr + 7) if ok else None
                anyv = anyv or ok
        if anyv:
            plan.append((kt, blocks))
    return plan


def build(nb=2, nl=2, dbg=(), stop_after=None):
    nc = bass.Bass("TRN2", target_bir_lowering=False)
    es = ExitStack()
    with es:
        c = Ctx(nc, es)

        def inp(name, shape):
            return Buf(name, nc.dram_tensor(name, list(shape), F32, kind="ExternalInput"))

        x_in = inp("x", [nb, 4096, D]); ctx_in = inp("ctx", [nb, 256, D]); c_in = inp("c", [nb, D]); cctx_in = inp("c_ctx", [D])
        w_mod = inp("w_mod", [2, D, 6144]); b_mod = inp("b_mod", [2, 6144]); g_norm = inp("g_norm", [2, 2, D])
        w_in = inp("w_in", [2, D, 7440]); b_merge = inp("b_merge", [2, 3, D]); g_qk = inp("g_qk", [2, 4, 64])
        rpb = inp("rpb", [2, 8, 15, 31]); b_mlstm = inp("b_mlstm", [2, 16]); g_ml = inp("g_ml", [2, 512])
        w_branch = inp("w_branch", [2, 3, 512, D]); w_out = inp("w_out", [2, D, D])
        w_group = inp("w_group", [2, D, 4]); b_group = inp("b_group", [2, 4]); w_router = inp("w_router", [2, D, 32]); b_router = inp("b_router", [2, 32])
        w_gate_up = inp("w_gate_up", [2, 32, D, 512]); w_down = inp("w_down", [2, 32, 256, D])
        k_cos = inp("k_cos", [T, 32]); k_sin = inp("k_sin", [T, 32]); k_u = inp("k_u", [2, 128, 128]); k_id = inp("k_id", [128, 128])
        k_colmask = inp("k_colmask", [128, 64]); k_jd = inp("k_jd", [64, 128])
        y_out = c.dram("y", [nb, 4096, D], F32, kind="ExternalOutput")

        def scr(name, shape, dt):
            return c.dram(name, shape, dt, kind=("ExternalOutput" if name in dbg else "Internal"))

        XS = scr("XS", [nb, T, D], F32)
        QAT = scr("QAT", [128, 4, T], BF16); KAT = scr("KAT", [128, T], BF16); VA = scr("VA", [T, 2, 65], BF16)
        QBT = scr("QBT", [128, 4, T], BF16); KBT = scr("KBT", [128, 4, T], BF16); VB = scr("VB", [T, 8, 65], BF16)
        MQT = scr("MQT", [128, 4, T], BF16); MKT = scr("MKT", [128, 4, T], BF16); MK = scr("MK", [T, 512], BF16)
        MV = scr("MV", [T, 4, 129], BF16); MO = scr("MO", [T, 512], BF16); MG = scr("MG", [T, 16], F32)
        GATES = scr("GATES", [T, 3072], BF16)
        YAT = scr("YAT", [4, 128, T], BF16); YBT = scr("YBT", [4, 128, T], BF16); YCT = scr("YCT", [4, 128, T], BF16)
        HS = scr("HS", [T, 512], F32)
        H2T = scr("H2T", [128, 8, T], BF16); DW = scr("DW", [T, 32], F32)
        WGU = scr("WGU", [32, 128, 8, 512], BF16); WDN = scr("WDN", [32, 128, 2, D], BF16)
        MODD = scr("MODD", [2, 3, 6144], F32)
        RPBP = scr("RPBP", [7568], F32); TPD = scr("TPD", [128, 8, 15, 64], F32)

        ident_f = c.sb("ident_f", [128, 128], F32, grp="setup"); ident = c.sb("ident", [128, 128], BF16)
        U = c.sb("U", [128, 2, 128], F32, grp="setup")
        c.dma("sp", ident_f.a, k_id.a, ident_f, k_id)
        c.dma("sp", U.a, k_u.a.rearrange("d s t -> s d t"), U, k_u)
        c.V(lambda e: e.tensor_copy(out=ident.a, in_=ident_f.a), [ident_f], [ident])
        ones_col = c.sb("ones_col", [128, 8], BF16)
        c.V(lambda e: e.memset(ones_col.a, 1.0), [], [ones_col])

        def tile_src(l, b, i):
            if l == 0:
                if i < 2:
                    return ctx_in, ctx_in.a[b, i * 128:(i + 1) * 128, :]
                return x_in, x_in.a[b, (i - 2) * 128:(i - 1) * 128, :]
            return XS, XS.a[b, i * 128:(i + 1) * 128, :]

        for l in range(nl):
            last_layer = (l == nl - 1)
            first_tile = 2 if last_layer else 0
            c.push()
            c.push()
            cT = c.sb("cT", [128, 8, 3], F32, grp="setup"); cTb = c.sb("cTb", [128, 8, 3], BF16)
            c.V(lambda e: e.memset(cT.a, 0.0), [], [cT])
            for b in range(nb):
                c.dma("sp", cT.a[:, :, b], c_in.a[b].rearrange("(k p) -> p k", p=128), cT, c_in, allow_slow_non_contiguous=True)
            c.dma("sp", cT.a[:, :, 2], cctx_in.a.rearrange("(k p) -> p k", p=128), cT, cctx_in, allow_slow_non_contiguous=True)
            c.A(lambda e: e.activation(out=cTb.a, in_=cT.a, func=AF.Silu), [cT], [cTb])
            modrow = c.sb("modrow", [3, 6144], F32)
            bmrow = c.sb("bmrow", [3, 6144], F32, grp="setup")
            c.dma("sp", bmrow.a, b_mod.a[l].partition_broadcast(3), bmrow, b_mod)
            wm = [c.sb("wm%d" % i, [128, 8, 512], BF16) for i in range(2)]
            pmod = [c.ps("pmod%d" % i, [128, 512], F32) for i in range(2)]
            for n in range(12):
                w_ = wm[n % 2]; p_ = pmod[n % 2]
                c.dma("pool", w_.a, w_mod.a[l, :, n * 512:(n + 1) * 512].rearrange("(k p) n -> p k n", p=128), w_, w_mod)
                for k in range(8):
                    c.mm(lambda e: e.matmul(p_.a[0:3, :], lhsT=cTb.a[:, k, :], rhs=w_.a[:, k, :], start=(k == 0), stop=(k == 7)), [cTb, w_], [p_], last=(k == 7))
                c.V(lambda e: e.tensor_tensor(out=modrow.a[:, n * 512:(n + 1) * 512], in0=p_.a[0:3, :], in1=bmrow.a[:, n * 512:(n + 1) * 512], op=ALU.add), [p_, bmrow], [modrow])
            c.dma("sp", MODD.a[l], modrow.a, MODD, modrow)
            c.pop()
            modT = c.sb("modT", [128, 6, 8, 3], F32, grp="setup")
            for s in range(6):
                for j in range(3):
                    c.dma("sp", modT.a[:, s, :, j], MODD.a[l, j, s * 1024:(s + 1) * 1024].rearrange("(k p) -> p k", p=128), modT, MODD, allow_slow_non_contiguous=True)
            gn = c.sb("gn", [128, 2, 8], F32, grp="setup")
            c.dma("sp", gn.a, g_norm.a[l].rearrange("t (k p) -> p t k", p=128), gn, g_norm, allow_slow_non_contiguous=True)
            G1 = c.sb("G1", [128, 8, 3], F32); G2 = c.sb("G2", [128, 8, 3], F32)
            for (Gx, seg, t_) in ((G1, 1, 0), (G2, 4, 1)):
                c.V(lambda e: e.tensor_scalar(out=Gx.a, in0=modT.a[:, seg], scalar1=1.0, scalar2=None, op0=ALU.add), [modT], [Gx])
                c.V(lambda e: e.tensor_tensor(out=Gx.a, in0=Gx.a, in1=gn.a[:, t_, :].unsqueeze(2).to_broadcast([128, 8, 3]), op=ALU.mult), [Gx, gn], [Gx])
            GT = c.sb("GT", [128, 2, 3, D], F32, grp="setup")
            for gi, seg in ((0, 2), (1, 5)):
                for j in range(3):
                    c.dma("sp", GT.a[:, gi, j, :], MODD.a[l, j, seg * 1024:(seg + 1) * 1024].partition_broadcast(128), GT, MODD)
            gqk = c.sb("gqk", [128, 4, 64], F32, grp="setup")
            c.dma("sp", gqk.a, g_qk.a[l].partition_broadcast(128), gqk, g_qk)
            bml = c.sb("bml", [128, 16], F32, grp="setup")
            c.dma("sp", bml.a, b_mlstm.a[l].partition_broadcast(128), bml, b_mlstm)
            gml = c.sb("gml", [128, 512], F32, grp="setup")
            c.dma("sp", gml.a, g_ml.a[l].partition_broadcast(128), gml, g_ml)
            brt = c.sb("brt", [128, 36], F32, grp="setup")
            c.dma("sp", brt.a[:, 0:4], b_group.a[l].partition_broadcast(128), brt, b_group)
            c.dma("sp", brt.a[:, 4:36], b_router.a[l].partition_broadcast(128), brt, b_router)
            wrt = c.sb("wrt", [128, 8, 36], F32, grp="setup")
            c.dma("sp", wrt.a[:, :, 0:4], w_group.a[l].rearrange("(k p) n -> p k n", p=128), wrt, w_group, allow_slow_non_contiguous=True)
            c.dma("sp", wrt.a[:, :, 4:36], w_router.a[l].rearrange("(k p) n -> p k n", p=128), wrt, w_router, allow_slow_non_contiguous=True)
            c.push()
            zt = c.sb("zt", [1, 8192], F32, grp="setup")
            c.V(lambda e: e.memset(zt.a, 0.0), [], [zt])
            c.dma("sp", RPBP.a.rearrange("(o n) -> o n", o=1), zt.a[:, 0:7568], RPBP, zt)
            c.dma("sp", RPBP.a[64:64 + 3720].rearrange("(r j) -> r j", j=31), bass.AP(rpb.t, l * 3720 + 30, [[31, 120], [-1, 31]]), RPBP, rpb, allow_slow_non_contiguous=True)
            TPb = c.sb("TPb", [128, 8, 15, 64], F32, grp="setup")
            cm = c.sb("cm", [128, 64], F32, grp="setup")
            c.dma("sp", cm.a, k_colmask.a, cm, k_colmask)
            TPx = c.sb("TPx", [64, 8, 15, 64], F32, grp="setup")
            for h in range(8):
                src = bass.AP(RPBP.t, 64 - 48 + h * 15 * 31, [[1, 64], [31, 15], [1, 64]])
                c.dma("sp", TPx.a[:, h], src, TPx, RPBP)
            jd = c.sb("jd", [64, 128], F32, grp="setup")
            c.dma("sp", jd.a, k_jd.a, jd, k_jd)
            pJ = [c.ps("pJ%d" % i, [128, 512], F32) for i in range(2)]
            TPx2 = TPx.a.rearrange("p h r q -> p (h r q)")
            TPb2 = TPb.a.rearrange("p h r q -> p (h r) q")
            for n in range(15):
                p_ = pJ[n % 2]
                c.mm(lambda e: e.matmul(p_.a, lhsT=jd.a, rhs=TPx2[:, n * 512:(n + 1) * 512], start=True, stop=True), [jd, TPx], [p_])
                c.V(lambda e: e.tensor_tensor(out=TPb2[:, n * 8:(n + 1) * 8, :], in0=p_.a.rearrange("p (r q) -> p r q", q=64), in1=cm.a.unsqueeze(1).to_broadcast([128, 8, 64]), op=ALU.add), [p_, cm], [TPb])
            c.dma("sp", TPD.a, TPb.a, TPD, TPb)
            c.pop()
            c.push()
            cv = [c.sb("cv%d" % i, [128, 8, 512], BF16, grp="cv%d" % i) for i in range(2)]
            cd = [c.sb("cd%d" % i, [128, 2, D], BF16, grp="cv%d" % i) for i in range(2)]
            for e_ in range(32 if not os.environ.get("SKIP_CONV") else 0):
                a_ = cv[e_ % 2]; d_ = cd[e_ % 2]
                c.dma("pool", a_.a, w_gate_up.a[l, e_].rearrange("(k p) n -> p k n", p=128), a_, w_gate_up)
                c.dma("sp", WGU.a[e_], a_.a, WGU, a_)
                c.dma("pool", d_.a, w_down.a[l, e_].rearrange("(k p) n -> p k n", p=128), d_, w_down)
                c.dma("sp", WDN.a[e_], d_.a, WDN, d_)
            c.pop()
            if stop_after == "mod":
                break

            for b in range(nb):
                c.push()
                hT = c.sb("hT", [128, 8, T], BF16)
                xt = [c.sb("xt%d" % i, [128, D], F32) for i in range(2)]
                junk_L = [c.sb("junk%d" % i_, [128, D], F32) for i_ in range(2)]; junk = junk_L[0]
                ss_L = [c.sb("ss%d" % i_, [128, 1], F32) for i_ in range(2)]; ss = ss_L[0]; rstd_L = [c.sb("rstd%d" % i_, [128, 1], F32) for i_ in range(2)]; rstd = rstd_L[0]
                xn = [c.sb("xn%d" % i, [128, D], BF16) for i in range(2)]
                pT = [c.ps("pT%d" % i, [128, 8, 128], BF16) for i in range(2)]
                for i in range(NT):
                    x_ = xt[i % 2]; n_ = xn[i % 2]; p_ = pT[i % 2]
                    junk = junk_L[i % 2]; ss = ss_L[i % 2]; rstd = rstd_L[i % 2]
                    src, ap = tile_src(l, b, i)
                    j3 = 2 if i < 2 else b
                    c.dma("sp", x_.a, ap, x_, src)
                    c.V(lambda e: e.memset(ss.a, 0.0), [], [ss])
                    c.A(lambda e: e.activation(out=junk.a, in_=x_.a, func=AF.Square, accum_out=ss.a), [x_], [junk, ss])
                    c.V(lambda e: e.tensor_scalar(out=rstd.a, in0=ss.a, scalar1=1.0 / D, scalar2=1e-6, op0=ALU.mult, op1=ALU.add), [ss], [rstd])
                    c.A(lambda e: e.activation(out=rstd.a, in_=rstd.a, func=AF.Sqrt), [rstd], [rstd])
                    c.V(lambda e: e.reciprocal(out=rstd.a, in_=rstd.a), [rstd], [rstd])
                    c.V(lambda e: e.tensor_scalar(out=n_.a, in0=x_.a, scalar1=rstd.a[:, 0:1], scalar2=None, op0=ALU.mult), [x_, rstd], [n_])
                    for k in range(8):
                        c.mm(lambda e: e.transpose(out=p_.a[:, k, :], in_=n_.a[:, k * 128:(k + 1) * 128], identity=ident.a), [n_, ident], [p_], last=(k == 7))
                    for k in range(8):
                        eng = c.A if k % 2 == 0 else None
                        if k % 2 == 0:
                            c.A(lambda e: e.activation(out=hT.a[:, k, i * 128:(i + 1) * 128], in_=p_.a[:, k, :], func=AF.Identity, scale=G1.a[:, k, j3:j3 + 1], bias=modT.a[:, 0, k, j3:j3 + 1]), [p_, G1, modT], [hT])
                        else:
                            c.V(lambda e: e.tensor_scalar(out=hT.a[:, k, i * 128:(i + 1) * 128], in0=p_.a[:, k, :], scalar1=G1.a[:, k, j3:j3 + 1], scalar2=modT.a[:, 0, k, j3:j3 + 1], op0=ALU.mult, op1=ALU.add), [p_, G1, modT], [hT])
                if "HTD" in dbg:
                    HTD = scr("HTD", [128, 8, T], BF16)
                    c.dma("sp", HTD.a, hT.a, HTD, hT)
                cosb = c.sb("cosb", [128, NT, 32], F32, grp="setup"); sinb = c.sb("sinb", [128, NT, 32], F32, grp="setup")
                c.dma("sp", cosb.a, k_cos.a.rearrange("(i p) f -> p i f", p=128), cosb, k_cos)
                c.dma("sp", sinb.a, k_sin.a.rearrange("(i p) f -> p i f", p=128), sinb, k_sin)
                bmg = c.sb("bmg", [128, 3072], F32, grp="setup")
                c.dma("sp", bmg.a, b_merge.a[l].rearrange("t d -> (t d)").partition_broadcast(128), bmg, b_merge)
                wc = [c.sb("wc%d" % i, [128, 8, 512], BF16) for i in range(2)]
                pp = [c.ps("pp%d" % i, [128, 512], F32) for i in range(2)]
                ptr = [c.ps("ptr%d" % i, [128, 4, 128], BF16) for i in range(2)]
                sq_L = [c.sb("sq%d" % i_, [128, 512], F32) for i_ in range(2)]; sq = sq_L[0]; ssq_L = [c.sb("ssq%d" % i_, [128, 8], F32) for i_ in range(2)]; ssq = ssq_L[0]; rq_L = [c.sb("rq%d" % i_, [128, 8], F32) for i_ in range(2)]; rq = rq_L[0]
                qn_L = [c.sb("qn%d" % i_, [128, 512], F32) for i_ in range(2)]; qn = qn_L[0]; t1_L = [c.sb("t1%d" % i_, [128, 256], F32) for i_ in range(2)]; t1 = t1_L[0]; t2_L = [c.sb("t2%d" % i_, [128, 256], F32) for i_ in range(2)]; t2 = t2_L[0]
                qr_ = [c.sb("qr%d" % i, [128, 512], BF16) for i in range(2)]
                trs = [c.sb("trs%d" % i, [128, 4, 128], BF16) for i in range(2)]
                vst = [c.sb("vst%d" % i, [128, 8, 65], BF16) for i in range(2)]
                mvst = [c.sb("mvst%d" % i, [128, 4, 129], BF16) for i in range(2)]
                gst = [c.sb("gst%d" % i, [128, 512], BF16) for i in range(2)]
                mg1_L = [c.sb("mg1%d" % i_, [128, 16], F32) for i_ in range(2)]; mg1 = mg1_L[0]; mg2_L = [c.sb("mg2%d" % i_, [128, 8], F32) for i_ in range(2)]; mg2 = mg2_L[0]
                gpre_L = [c.sb("gpre%d" % i_, [128, 512], F32) for i_ in range(2)]; gpre = gpre_L[0]
                for st_ in vst:
                    c.V(lambda e: e.memset(st_.a, 1.0), [], [st_])
                for st_ in mvst:
                    c.V(lambda e: e.memset(st_.a, 1.0), [], [st_])
                chunks = [(0, 512, "Aq"), (512, 256, "Akv"), (768, 512, "Bq"), (1280, 512, "Bk"), (1792, 512, "Bv"),
                          (2304, 512, "Cq"), (2816, 512, "Ck"), (3328, 512, "Cv"), (3840, 512, "Co"), (4352, 16, "Cg")]
                chunks += [(4368 + 512 * m, 512, "Mg%d" % m) for m in range(6)]
                pp = pp + [c.ps("pp%d" % i, [128, 512], F32) for i in range(2, 4)]

                def mmgen(i, it, cw, w_):
                    p_ = pp[it % 4]
                    for k in range(8):
                        c.mm(lambda e: e.matmul(p_.a[:, :cw], lhsT=hT.a[:, k, i * 128:(i + 1) * 128], rhs=w_.a[:, k, :cw], start=(k == 0), stop=(k == 7)), [hT, w_], [p_], last=(k == 7))
                        yield

                def ptile(i, it, kind):
                    p_ = pp[it % 4]
                    sq = sq_L[it % 2]; ssq = ssq_L[it % 2]; rq = rq_L[it % 2]; qn = qn_L[it % 2]; t1 = t1_L[it % 2]; t2 = t2_L[it % 2]
                    mg1 = mg1_L[it % 2]; mg2 = mg2_L[it % 2]; gpre = gpre_L[it % 2]
                    tok = slice(i * 128, (i + 1) * 128)

                    def qknorm(ncol, gidx, dst):
                        nh = ncol // 64
                        c.A(lambda e: e.activation(out=sq.a[:, :ncol], in_=p_.a[:, :ncol], func=AF.Square), [p_], [sq])
                        yield
                        c.V(lambda e: e.tensor_reduce(out=ssq.a[:, :nh], in_=sq.a[:, :ncol].rearrange("p (h d) -> p h d", d=64), axis=AX.X, op=ALU.add), [sq], [ssq])
                        yield
                        c.V(lambda e: e.tensor_scalar(out=rq.a[:, :nh], in0=ssq.a[:, :nh], scalar1=1.0 / 64, scalar2=1e-6, op0=ALU.mult, op1=ALU.add), [ssq], [rq])
                        yield
                        c.A(lambda e: e.activation(out=rq.a[:, :nh], in_=rq.a[:, :nh], func=AF.Sqrt), [rq], [rq])
                        yield
                        c.V(lambda e: e.reciprocal(out=rq.a[:, :nh], in_=rq.a[:, :nh]), [rq], [rq])
                        yield
                        c.V(lambda e: e.tensor_tensor(out=dst.rearrange("p (h d) -> p h d", d=64), in0=p_.a[:, :ncol].rearrange("p (h d) -> p h d", d=64),
                                                      in1=rq.a[:, :nh].unsqueeze(2).to_broadcast([128, nh, 64]), op=ALU.mult), [p_, rq], [qn])
                        yield
                        c.V(lambda e: e.tensor_tensor(out=dst.rearrange("p (h d) -> p h d", d=64), in0=dst.rearrange("p (h d) -> p h d", d=64),
                                                      in1=gqk.a[:, gidx:gidx + 1, :].to_broadcast([128, nh, 64]), op=ALU.mult), [qn, gqk], [qn])
                        yield

                    def rope(src, nh, dst4, dbuf):
                        s3 = src.rearrange("p (h t f) -> p h t f", t=2, f=32)
                        x1 = s3[:, :, 0, :]; x2 = s3[:, :, 1, :]
                        cb = cosb.a[:, i:i + 1, :].to_broadcast([128, nh, 32]); sb_ = sinb.a[:, i:i + 1, :].to_broadcast([128, nh, 32])
                        a1 = t1.a[:, :nh * 32].rearrange("p (h f) -> p h f", f=32); a2 = t2.a[:, :nh * 32].rearrange("p (h f) -> p h f", f=32)
                        c.V(lambda e: e.tensor_tensor(out=a1, in0=x1, in1=cb, op=ALU.mult), [qn, cosb], [t1])
                        yield
                        c.G(lambda e: e.tensor_tensor(out=a2, in0=x2, in1=sb_, op=ALU.mult), [qn, sinb], [t2])
                        yield
                        c.V(lambda e: e.tensor_tensor(out=dst4[:, :, 0, :], in0=a1, in1=a2, op=ALU.subtract), [t1, t2], [dbuf])
                        yield
                        c.V(lambda e: e.tensor_tensor(out=a1, in0=x2, in1=cb, op=ALU.mult), [qn, cosb], [t1])
                        yield
                        c.G(lambda e: e.tensor_tensor(out=a2, in0=x1, in1=sb_, op=ALU.mult), [qn, sinb], [t2])
                        yield
                        c.V(lambda e: e.tensor_tensor(out=dst4[:, :, 1, :], in0=a1, in1=a2, op=ALU.add), [t1, t2], [dbuf])
                        yield

                    def transposes(srcb, nblk, dstD):
                        pt = ptr[it % 2]; ts_ = trs[it % 2]
                        for m in range(nblk):
                            c.mm(lambda e: e.transpose(out=pt.a[:, m, :], in_=srcb.a[:, m * 128:(m + 1) * 128], identity=ident.a), [srcb, ident], [pt], last=(m == nblk - 1))
                            yield
                        c.A(lambda e: e.copy(out=ts_.a[:, :nblk, :], in_=pt.a[:, :nblk, :]), [pt], [ts_])
                        yield
                        if nblk == 1:
                            c.dma("sp", dstD.a[:, i * 128:(i + 1) * 128], ts_.a[:, 0, :], dstD, ts_)
                        else:
                            c.dma("sp", dstD.a[:, :, i * 128:(i + 1) * 128], ts_.a[:, :nblk, :], dstD, ts_)
                        yield

                    if kind == "Aq":
                        yield from qknorm(512, 0, qn.a[:, :512])
                        q_ = qr_[it % 2]
                        d5 = q_.a.rearrange("p (m g t f) -> p g m t f", g=2, t=2, f=32)
                        for g in range(2):
                            yield from rope(qn.a[:, g * 256:(g + 1) * 256], 4, d5[:, g], q_)
                        yield from transposes(q_, 4, QAT)
                    elif kind == "Akv":
                        yield from qknorm(128, 1, qn.a[:, :128])
                        q_ = qr_[it % 2]
                        yield from rope(qn.a[:, :128], 2, q_.a[:, :128].rearrange("p (h t f) -> p h t f", t=2, f=32), q_)
                        yield from transposes(q_, 1, KAT)
                        v_ = vst[it % 2]
                        c.A(lambda e: e.copy(out=v_.a[:, 0:2, 0:64], in_=p_.a[:, 128:256].rearrange("p (h d) -> p h d", d=64)), [p_], [v_])
                        yield
                        c.dma("sp", VA.a[tok], v_.a[:, 0:2, :], VA, v_)
                        yield
                    elif kind in ("Bq", "Bk"):
                        yield from qknorm(512, 2 if kind == "Bq" else 3, qn.a[:, :512])
                        q_ = qr_[it % 2]
                        c.A(lambda e: e.copy(out=q_.a, in_=qn.a[:, :512]), [qn], [q_])
                        yield
                        yield from transposes(q_, 4, QBT if kind == "Bq" else KBT)
                    elif kind == "Bv":
                        v_ = vst[it % 2]
                        c.A(lambda e: e.copy(out=v_.a[:, :, 0:64], in_=p_.a.rearrange("p (h d) -> p h d", d=64)), [p_], [v_])
                        yield
                        c.dma("sp", VB.a[tok], v_.a, VB, v_)
                        yield
                    elif kind in ("Cq", "Ck"):
                        q_ = qr_[it % 2]
                        c.A(lambda e: e.activation(out=q_.a, in_=p_.a, func=AF.Identity, scale=(1.0 if kind == "Cq" else 128 ** -0.5)), [p_], [q_])
                        yield
                        if kind == "Ck":
                            c.dma("sp", MK.a[tok], q_.a, MK, q_)
                            yield
                        yield from transposes(q_, 4, MQT if kind == "Cq" else MKT)
                    elif kind == "Cv":
                        v_ = mvst[it % 2]
                        c.A(lambda e: e.copy(out=v_.a[:, :, 0:128], in_=p_.a.rearrange("p (h d) -> p h d", d=128)), [p_], [v_])
                        yield
                        c.dma("sp", MV.a[tok], v_.a, MV, v_)
                        yield
                    elif kind == "Co":
                        g_ = gst[it % 2]
                        c.A(lambda e: e.activation(out=g_.a, in_=p_.a, func=AF.Sigmoid), [p_], [g_])
                        yield
                        c.dma("sp", MO.a[tok], g_.a, MO, g_)
                        yield
                    elif kind == "Cg":
                        c.V(lambda e: e.tensor_tensor(out=mg1.a, in0=p_.a[:, :16], in1=bml.a, op=ALU.add), [p_, bml], [mg1])
                        yield
                        fv = mg1.a.rearrange("p (d t h) -> p d t h", d=2, t=2)[:, :, 1, :]
                        m2 = mg2.a.rearrange("p (d h) -> p d h", d=2)
                        c.A(lambda e: e.activation(out=m2, in_=fv, func=AF.Exp, scale=-1.0), [mg1], [mg2])
                        yield
                        c.A(lambda e: e.activation(out=m2, in_=m2, func=AF.Ln, bias=1.0), [mg2], [mg2])
                        yield
                        c.V(lambda e: e.tensor_scalar(out=fv, in0=m2, scalar1=-1.0, scalar2=None, op0=ALU.mult), [mg2], [mg1])
                        yield
                        c.dma("sp", MG.a[tok], mg1.a, MG, mg1)
                        yield
                    else:
                        m = int(kind[2:])
                        g_ = gst[it % 2]
                        c.V(lambda e: e.tensor_tensor(out=gpre.a, in0=p_.a, in1=bmg.a[:, m * 512:(m + 1) * 512], op=ALU.add), [p_, bmg], [gpre])
                        yield
                        c.A(lambda e: e.activation(out=g_.a, in_=gpre.a, func=AF.Sigmoid), [gpre], [g_])
                        yield
                        c.dma("sp", GATES.a[tok, m * 512:(m + 1) * 512], g_.a, GATES, g_)
                        yield

                def wload(ci):
                    c0, cw, kind = chunks[ci]
                    w_ = wc[ci % 2]
                    c.dma("pool", w_.a[:, :, :cw], w_in.a[l, :, c0:c0 + cw].rearrange("(k p) n -> p k n", p=128), w_, w_in)

                items = [(ci, i) for ci in range(len(chunks)) for i in range(NT)]
                wload(0)
                loaded = 1

                def mm_of(n):
                    ci, i = items[n]
                    return mmgen(i, n, chunks[ci][1], wc[ci % 2])

                rr(mm_of(0), mm_of(1))
                for n in range(0, len(items), 2):
                    ci_next = items[min(n + 3, len(items) - 1)][0]
                    while loaded <= min(ci_next + 1, len(chunks) - 1):
                        wload(loaded)
                        loaded += 1
                    gens = [ptile(items[n][1], n, chunks[items[n][0]][2]), ptile(items[n + 1][1], n + 1, chunks[items[n + 1][0]][2])]
                    for m_ in (n + 2, n + 3):
                        if m_ < len(items):
                            gens.append(mm_of(m_))
                    rr(*gens)
                c.pop()
                if stop_after == "p1":
                    break

                c.push()
                kat = c.sb("kat", [128, T], BF16, grp="ka"); va = c.sb("va", [128, NT, 2, 65], BF16, grp="ka")
                c.dma("sp", kat.a, KAT.a, kat, KAT)
                c.dma("sp", va.a, VA.a.rearrange("(i p) g d -> p i g d", p=128), va, VA)
                qa = [[c.sb("qa%d_%d" % (i, g), [128, 4, 128], BF16, grp="qa%d" % i) for g in range(2)] for i in range(2)]
                for i in range(2):
                    for g in range(2):
                        c.V(lambda e: e.memset(qa[i][g].a, 0.0), [], [qa[i][g]])
                ND = 2
                pS = [c.ps("pS%d" % i, [128, 1024], F32) for i in range(ND)]
                pe_ = [c.sb("pe%d" % i, [128, 1024], BF16) for i in range(3)]
                pO = [c.ps("pO%d" % i, [128, 512], F32) for i in range(2)]
                pbc = c.ps("pbc", [128, 512], F32)
                ones_f = c.sb("ones_f", [128, 64], F32)
                c.V(lambda e: e.memset(ones_f.a, 1.0), [], [ones_f])
                dn = [c.sb("dn%d" % i, [128, 512], F32) for i in range(2)]
                bcs = [c.sb("bcs%d" % i, [64, 512], F32) for i in range(2)]
                yTa = [c.sb("yTa%d" % i, [64, 512], BF16) for i in range(2)]
                steps = []
                for i in range(first_tile, NT):
                    kts = list(range(0, 2)) if i < 2 else list(range(0, NT))
                    prs = [kts[j:j + 2] for j in range(0, len(kts), 2)]
                    for g in range(2):
                        for pi, pr in enumerate(prs):
                            steps.append((i, g, pr, pi == 0, pi == len(prs) - 1))

                def a_s1(si):
                    i, g, pr, first, last = steps[si]
                    q_ = qa[i % 2][g]
                    if g == 0 and first:
                        for g2 in range(2):
                            c.dma("sp", qa[i % 2][g2].a[g2 * 64:(g2 + 1) * 64], QAT.a[g2 * 64:(g2 + 1) * 64, :, i * 128:(i + 1) * 128], qa[i % 2][g2], QAT)
                    ps_ = pS[si % ND]; e_ = pe_[si % 3]
                    for j, kt in enumerate(pr):
                        c.mm(lambda e: e.matmul(ps_.a[:, j * 512:(j + 1) * 512], lhsT=kat.a[:, kt * 128:(kt + 1) * 128], rhs=q_.a.rearrange("p m t -> p (m t)"), start=True, stop=True), [kat, q_], [ps_], last=(j == len(pr) - 1))
                    c.A(lambda e: e.activation(out=e_.a, in_=ps_.a, func=AF.Exp, scale=0.125), [ps_], [e_])

                def a_s2(si):
                    i, g, pr, first, last = steps[si]
                    e_ = pe_[si % 3]
                    po = pO[g]
                    for j, kt in enumerate(pr):
                        c.mm(lambda e: e.matmul(po.a[0:65, :], lhsT=va.a[:, kt, g, :], rhs=e_.a[:, j * 512:(j + 1) * 512], start=(first and j == 0), stop=(last and j == len(pr) - 1)), [e_, va], [po], last=(j == len(pr) - 1))
                    if not last:
                        return
                    d_ = dn[g]; b_ = bcs[g]; y_ = yTa[g]
                    c.A(lambda e: e.copy(out=d_.a[64:65, :], in_=po.a[64:65, :]), [po], [d_])
                    c.V(lambda e: e.reciprocal(out=d_.a[64:65, :], in_=d_.a[64:65, :]), [d_], [d_])
                    c.mm(lambda e: e.matmul(pbc.a[0:64, :], lhsT=ones_f.a[64:65, :], rhs=d_.a[64:65, :], start=True, stop=True), [ones_f, d_], [pbc])
                    c.A(lambda e: e.copy(out=b_.a, in_=pbc.a[0:64, :]), [pbc], [b_])
                    c.V(lambda e: e.tensor_tensor(out=y_.a, in0=po.a[0:64, :], in1=b_.a, op=ALU.mult), [po, b_], [y_])
                    for m in range(4):
                        c.dma("sp", YAT.a[g * 2 + m // 2, (m % 2) * 64:(m % 2) * 64 + 64, i * 128:(i + 1) * 128], y_.a[:, m * 128:(m + 1) * 128], YAT, y_)

                pipeline(len(steps), a_s1, a_s2, ND, s1_first=True)
                c.pop()
                if stop_after == "p2a":
                    break

                c.push()
                kbt = c.sb("kbt", [128, 4, T], BF16, grp="kb"); vb = c.sb("vb", [128, NT, 8, 65], BF16, grp="kb")
                c.dma("sp", kbt.a, KBT.a, kbt, KBT)
                c.dma("sp", vb.a, VB.a.rearrange("(i p) h d -> p i h d", p=128), vb, VB)
                TP = c.sb("TP", [128, 8, 15, 64], F32, grp="kb")
                c.dma("sp", TP.a, TPD.a, TP, TPD)
                tabI = c.sb("tabI", [128, 5, 8, 128], F32); tabE = c.sb("tabE", [128, 5, 8, 128], F32)

                def build_tab(tab, plan):
                    for di, (kt, blocks) in enumerate(plan):
                        for (a, b2), dr in blocks.items():
                            o = tab.a[a * 64:(a + 1) * 64, di, :, b2 * 64:(b2 + 1) * 64]
                            if dr is None:
                                c.G(lambda e: e.memset(o, NEG), [], [tab])
                            else:
                                c.V(lambda e: e.tensor_copy(out=o, in_=TP.a[a * 64:(a + 1) * 64, :, dr, :]), [TP], [tab])

                build_tab(tabI, na_plan(5))
                qz = [[c.sb("qz%d_%d" % (i, p), [128, 4, 128], BF16, grp="qz%d" % i) for p in range(2)] for i in range(2)]
                for i in range(2):
                    for p in range(2):
                        c.V(lambda e: e.memset(qz[i][p].a, 0.0), [], [qz[i][p]])
                pS = [c.ps("pSb%d" % i, [128, 8, 128], F32) for i in range(2)]
                sS = c.sb("sS", [128, 8, 128], F32)
                pe_ = [c.sb("peb%d" % i, [128, 8, 128], BF16) for i in range(2)]
                pOb = c.ps("pOb", [128, 2, 512], F32)
                pO3 = [pOb.a[:, hh, 0:260].rearrange("p (m d) -> p m d", d=65) for hh in range(2)]
                rd = c.sb("rdb", [128, 4], F32)
                ya = [c.sb("yb%d" % i, [128, 512], BF16) for i in range(2)]
                pY = c.ps("pYb", [128, 4, 128], BF16); yT = c.sb("yTb", [128, 4, 128], BF16)
                steps = []
                for i in range(first_tile, NT):
                    keys = [(0, None, None), (1, None, None)]
                    plan = None
                    if i >= 2:
                        j = i - 2
                        plan = na_plan(j)
                        tab = tabI if 2 <= j <= 29 else tabE
                        keys = [(kt + 2, tab, di) for di, (kt, _) in enumerate(plan)] + keys
                    for ki, (kt, tab, di) in enumerate(keys):
                        steps.append((i, ki, kt, tab, di, len(keys), plan))

                def b_s1(si):
                    i, ki, kt, tab, di, nk, plan = steps[si]
                    qz_ = qz[i % 2]
                    if ki == 0:
                        for p in range(2):
                            c.dma("sp", qz_[p].a[p * 64:(p + 1) * 64], QBT.a[p * 64:(p + 1) * 64, :, i * 128:(i + 1) * 128], qz_[p], QBT)
                        if tab is tabE:
                            build_tab(tabE, plan)
                    ps_ = pS[si % 2]; e_ = pe_[si % 2]
                    for h in range(8):
                        par = h % 2; pr = h // 2
                        c.mm(lambda e: e.matmul(ps_.a[:, h, :], lhsT=kbt.a[:, pr, kt * 128:(kt + 1) * 128], rhs=qz_[par].a[:, pr, :], start=True, stop=True), [kbt, qz_[par]], [ps_], last=(h == 7))
                    if tab is not None:
                        c.V(lambda e: e.scalar_tensor_tensor(out=sS.a, in0=ps_.a, scalar=0.125, in1=tab.a[:, di], op0=ALU.mult, op1=ALU.add), [ps_, tab], [sS])
                        c.A(lambda e: e.activation(out=e_.a, in_=sS.a, func=AF.Exp), [sS], [e_])
                    else:
                        c.A(lambda e: e.activation(out=e_.a, in_=ps_.a, func=AF.Exp, scale=0.125), [ps_], [e_])

                def b_s2(si):
                    i, ki, kt, tab, di, nk, plan = steps[si]
                    e_ = pe_[si % 2]; y_ = ya[i % 2]
                    for h in range(8):
                        c.mm(lambda e: e.matmul(pO3[h // 4][:, h % 4, :], lhsT=e_.a[:, h, :], rhs=vb.a[:, kt, h, :], start=(ki == 0 and h % 4 == 0), stop=(ki == nk - 1)), [e_, vb], [pOb], last=(h == 7))
                    if ki != nk - 1:
                        return
                    for hh in range(2):
                        po3 = pO3[hh]
                        c.V(lambda e: e.reciprocal(out=rd.a, in_=po3[:, :, 64]), [pOb], [rd])
                        c.V(lambda e: e.tensor_tensor(out=y_.a[:, hh * 256:(hh + 1) * 256].rearrange("p (m d) -> p m d", d=64), in0=po3[:, :, 0:64], in1=rd.a.unsqueeze(2).to_broadcast([128, 4, 64]), op=ALU.mult), [pOb, rd], [y_])
                    for m in range(4):
                        c.mm(lambda e: e.transpose(out=pY.a[:, m, :], in_=y_.a[:, m * 128:(m + 1) * 128], identity=ident.a), [y_, ident], [pY], last=(m == 3))
                    c.A(lambda e: e.copy(out=yT.a, in_=pY.a), [pY], [yT])
                    c.dma("sp", YBT.a[:, :, i * 128:(i + 1) * 128].rearrange("m p t -> p m t"), yT.a, YBT, yT)

                pipeline(len(steps), b_s1, b_s2, 2)
                c.pop()
                if stop_after == "p2b":
                    break

                c.push()
                Cst = c.sb("Cst", [128, 4, 129], F32)
                Cb = [c.sb("Cb%d" % i, [128, 4, 129], BF16) for i in range(2)]
                mq = [c.sb("mq%d" % i, [128, 4, 128], BF16, grp="m%d" % i) for i in range(3)]
                mk = [c.sb("mk%d" % i, [128, 4, 128], BF16, grp="m%d" % i) for i in range(3)]
                mkt = [c.sb("mkt%d" % i, [128, 512], BF16, grp="m%d" % i) for i in range(3)]
                mv = [c.sb("mv%d" % i, [128, 4, 129], BF16, grp="m%d" % i) for i in range(3)]
                mg = [c.sb("mg%d" % i, [128, 16], F32, grp="m%d" % i) for i in range(3)]
                mo = [c.sb("mo%d" % i, [128, 512], BF16, grp="m%d" % i) for i in range(3)]
                hs = [c.sb("hs%d" % i, [128, 512], F32, grp="m%d" % i) for i in range(3)]
                pb = c.ps("pb", [128, 4], F32); pbB = c.ps("pbB", [128, 4, 128], F32); pST = c.ps("pST", [128, 4, 128], F32)
                pH = c.ps("pH", [128, 2, 512], F32); pC = c.ps("pC", [128, 2, 512], F32)
                pY = c.ps("pYc", [128, 4, 128], BF16)
                biasc = [c.sb("biasc%d" % i, [128, 4], F32) for i in range(3)]
                gcol = [c.sb("gcol%d" % i, [128, 4], F32) for i in range(3)]
                ebend = [c.sb("ebend%d" % i, [128, 4], F32) for i in range(3)]
                EB_L = [c.sb("EB%d" % i_, [128, 4, 128], F32) for i_ in range(2)]; EB = EB_L[0]
                qs = [c.sb("qs%d" % i, [128, 4, 128], BF16) for i in range(3)]
                DT_L = [c.sb("DT%d" % i_, [128, 4, 128], F32) for i_ in range(2)]; DT = DT_L[0]; DTm_L = [c.sb("DTm%d" % i_, [128, 4, 128], F32) for i_ in range(2)]; DTm = DTm_L[0]
                SD = [c.sb("SD%d" % i, [128, 4, 128], BF16) for i in range(3)]
                kg_L = [c.sb("kg%d" % i_, [128, 4, 128], BF16) for i_ in range(2)]; kg = kg_L[0]
                pCs = [c.sb("pCs%d" % i, [128, 2, 258], F32) for i in range(3)]
                den_L = [c.sb("den%d" % i_, [128, 4], F32) for i_ in range(2)]; den = den_L[0]; hout = [c.sb("hout%d" % i, [128, 512], F32) for i in range(2)]
                hsq_L = [c.sb("hsq%d" % i_, [128, 512], F32) for i_ in range(2)]; hsq = hsq_L[0]; hss_L = [c.sb("hss%d" % i_, [128, 4], F32) for i_ in range(2)]; hss = hss_L[0]; hn_L = [c.sb("hn%d" % i_, [128, 512], F32) for i_ in range(2)]; hn = hn_L[0]
                yc_L = [c.sb("yc%d" % i_, [128, 512], BF16) for i_ in range(2)]; yc = yc_L[0]; yT_L = [c.sb("yTc%d" % i_, [128, 4, 128], BF16) for i_ in range(2)]; yT = yT_L[0]

                def hreg(pt, h):
                    return pt.a[:, h // 2, (h % 2) * 129:(h % 2) * 129 + 129]

                def sreg(pt, h):
                    return pt.a[:, h // 2, (h % 2) * 129:(h % 2) * 129 + 129]

                for d in range(2):
                    order = list(range(NT)) if d == 0 else [1, 0] + list(range(NT - 1, 1, -1))
                    tend = 127 if d == 0 else 0
                    Ud = U.a[:, d, :]
                    c.V(lambda e: e.memset(Cst.a, 0.0), [], [Cst])
                    c.V(lambda e: e.memset(Cb[1].a, 0.0), [], [Cb[1]])

                    def l_s1(n):
                        kg_c = kg_L[n % 2]; EB_c = EB_L[n % 2]; DT_c = DT_L[n % 2]; DTm_c = DTm_L[n % 2]
                        i = order[n]
                        q_ = mq[n % 3]; k_ = mk[n % 3]; kt_ = mkt[n % 3]; v_ = mv[n % 3]; g_ = mg[n % 3]
                        bc_ = biasc[n % 3]; gc_ = gcol[n % 3]; eb_ = ebend[n % 3]; qs_ = qs[n % 3]; SD_ = SD[n % 3]; pcs = pCs[n % 3]
                        tok = slice(i * 128, (i + 1) * 128)
                        need_out = i >= first_tile
                        c.dma("sp", q_.a, MQT.a[:, :, tok], q_, MQT)
                        yield
                        c.dma("sp", k_.a, MKT.a[:, :, tok], k_, MKT)
                        yield
                        c.dma("sp", kt_.a, MK.a[tok], kt_, MK)
                        yield
                        c.dma("sp", v_.a, MV.a[tok], v_, MV)
                        yield
                        c.dma("sp", g_.a, MG.a[tok], g_, MG)
                        yield
                        if need_out and d == 1:
                            c.dma("sp", hs[n % 3].a, HS.a[tok], hs[n % 3], HS)
                            yield
                            c.dma("sp", mo[n % 3].a, MO.a[tok], mo[n % 3], MO)
                            yield
                        gv = g_.a.rearrange("p (d t h) -> p d t h", d=2, t=2)
                        ig = gv[:, d, 0, :]; lf = gv[:, d, 1, :]
                        c.mm(lambda e: e.matmul(pb.a, lhsT=Ud, rhs=lf, start=True, stop=True), [U, g_], [pb])
                        yield
                        for h in range(4):
                            c.mm(lambda e: e.matmul(pbB.a[:, h, :], lhsT=gv[:, d, 1, h:h + 1].to_broadcast([128, 128]), rhs=Ud, start=True, stop=True), [g_, U], [pbB], last=(h == 3))
                            yield
                        if need_out:
                            for h in range(4):
                                c.mm(lambda e: e.matmul(pST.a[:, h, :], lhsT=k_.a[:, h, :], rhs=q_.a[:, h, :], start=True, stop=True), [k_, q_], [pST], last=(h == 3))
                                yield
                        c.V(lambda e: e.tensor_tensor(out=bc_.a, in0=ig, in1=pb.a, op=ALU.subtract), [g_, pb], [bc_])
                        yield
                        c.V(lambda e: e.tensor_tensor(out=gc_.a, in0=pbB.a[:, :, tend], in1=bc_.a, op=ALU.add), [pbB, bc_], [gc_])
                        yield
                        c.A(lambda e: e.activation(out=gc_.a, in_=gc_.a, func=AF.Exp), [gc_], [gc_])
                        yield
                        c.A(lambda e: e.activation(out=eb_.a, in_=pbB.a[:, :, tend], func=AF.Exp), [pbB], [eb_])
                        yield
                        c.V(lambda e: e.tensor_tensor(out=kg_c.a, in0=kt_.a.rearrange("p (h d) -> p h d", d=128), in1=gc_.a.unsqueeze(2).to_broadcast([128, 4, 128]), op=ALU.mult), [kt_, gc_], [kg_c])
                        yield
                        for h in range(4):
                            c.mm(lambda e: e.matmul(hreg(pC, h), lhsT=kg_c.a[:, h, :], rhs=v_.a[:, h, :], start=True, stop=True), [kg_c, v_], [pC], last=(h == 3))
                            yield
                        for hh in range(2):
                            c.A(lambda e: e.copy(out=pcs.a[:, hh, :], in_=pC.a[:, hh, 0:258]), [pC], [pcs])
                            yield
                        if need_out:
                            c.A(lambda e: e.activation(out=EB_c.a, in_=pbB.a, func=AF.Exp), [pbB], [EB_c])
                            yield
                            c.V(lambda e: e.tensor_tensor(out=qs_.a, in0=q_.a, in1=EB_c.a, op=ALU.mult), [q_, EB_c], [qs_])
                            yield
                            for h in range(4):
                                c.A(lambda e: e.activation(out=DT_c.a[:, h, :], in_=pbB.a[:, h, :], func=AF.Exp, bias=bc_.a[:, h:h + 1]), [pbB, bc_], [DT_c])
                                yield
                            c.G(lambda e: e.tensor_tensor(out=DTm_c.a, in0=DT_c.a, in1=U.a[:, d:d + 1, :].to_broadcast([128, 4, 128]), op=ALU.mult), [DT_c, U], [DTm_c])
                            yield
                            c.V(lambda e: e.tensor_tensor(out=SD_.a, in0=DTm_c.a, in1=pST.a, op=ALU.mult), [DTm_c, pST], [SD_])
                            yield

                    def l_s2(n):
                        den_c = den_L[n % 2]; hsq_c = hsq_L[n % 2]; hss_c = hss_L[n % 2]; hn_c = hn_L[n % 2]; yc_c = yc_L[n % 2]; yT_c = yT_L[n % 2]
                        i = order[n]
                        v_ = mv[n % 3]; eb_ = ebend[n % 3]; qs_ = qs[n % 3]; SD_ = SD[n % 3]; pcs = pCs[n % 3]
                        cb_old = Cb[(n + 1) % 2]; cb_new = Cb[n % 2]
                        ho = hout[n % 2]
                        tok = slice(i * 128, (i + 1) * 128)
                        need_out = i >= first_tile
                        if need_out:
                            for h in range(4):
                                c.mm(lambda e: e.matmul(hreg(pH, h), lhsT=qs_.a[:, h, :], rhs=cb_old.a[:, h, :], start=True, stop=False), [qs_, cb_old], [pH], last=False)
                                yield
                                c.mm(lambda e: e.matmul(hreg(pH, h), lhsT=SD_.a[:, h, :], rhs=v_.a[:, h, :], start=False, stop=True), [SD_, v_], [pH], last=(h == 3))
                                yield
                        for h in range(4):
                            c.V(lambda e: e.scalar_tensor_tensor(out=Cst.a[:, h, :], in0=Cst.a[:, h, :], scalar=eb_.a[:, h:h + 1], in1=sreg(pcs, h), op0=ALU.mult, op1=ALU.add), [Cst, eb_, pcs], [Cst])
                            yield
                        c.A(lambda e: e.copy(out=cb_new.a, in_=Cst.a), [Cst], [cb_new])
                        yield
                        if not need_out:
                            return
                        for h in range(4):
                            c.A(lambda e: e.activation(out=den_c.a[:, h:h + 1], in_=hreg(pH, h)[:, 128:129], func=AF.Abs), [pH], [den_c])
                            yield
                        c.V(lambda e: e.tensor_scalar_max(out=den_c.a, in0=den_c.a, scalar1=1.0), [den_c], [den_c])
                        yield
                        c.V(lambda e: e.reciprocal(out=den_c.a, in_=den_c.a), [den_c], [den_c])
                        yield
                        for h in range(4):
                            c.V(lambda e: e.tensor_scalar(out=ho.a[:, h * 128:(h + 1) * 128], in0=hreg(pH, h)[:, 0:128], scalar1=den_c.a[:, h:h + 1], scalar2=None, op0=ALU.mult), [pH, den_c], [ho])
                            yield
                        if d == 0:
                            c.dma("sp", HS.a[tok], ho.a, HS, ho)
                            yield
                            return
                        h_ = hs[n % 3]; o_ = mo[n % 3]
                        c.G(lambda e: e.tensor_tensor(out=ho.a, in0=ho.a, in1=h_.a, op=ALU.add), [ho, h_], [ho])
                        yield
                        c.A(lambda e: e.activation(out=hsq_c.a, in_=ho.a, func=AF.Square), [ho], [hsq_c])
                        yield
                        c.V(lambda e: e.tensor_reduce(out=hss_c.a, in_=hsq_c.a.rearrange("p (h d) -> p h d", d=128), axis=AX.X, op=ALU.add), [hsq_c], [hss_c])
                        yield
                        c.V(lambda e: e.tensor_scalar(out=hss_c.a, in0=hss_c.a, scalar1=1.0 / 128, scalar2=1e-6, op0=ALU.mult, op1=ALU.add), [hss_c], [hss_c])
                        yield
                        c.A(lambda e: e.activation(out=hss_c.a, in_=hss_c.a, func=AF.Sqrt), [hss_c], [hss_c])
                        yield
                        c.V(lambda e: e.reciprocal(out=hss_c.a, in_=hss_c.a), [hss_c], [hss_c])
                        yield
                        c.V(lambda e: e.tensor_tensor(out=hn_c.a.rearrange("p (h d) -> p h d", d=128), in0=ho.a.rearrange("p (h d) -> p h d", d=128), in1=hss_c.a.unsqueeze(2).to_broadcast([128, 4, 128]), op=ALU.mult), [ho, hss_c], [hn_c])
                        yield
                        c.G(lambda e: e.tensor_tensor(out=hn_c.a, in0=hn_c.a, in1=gml.a, op=ALU.mult), [hn_c, gml], [hn_c])
                        yield
                        c.V(lambda e: e.tensor_tensor(out=yc_c.a, in0=hn_c.a, in1=o_.a, op=ALU.mult), [hn_c, o_], [yc_c])
                        yield
                        for m in range(4):
                            c.mm(lambda e: e.transpose(out=pY.a[:, m, :], in_=yc_c.a[:, m * 128:(m + 1) * 128], identity=ident.a), [yc_c, ident], [pY], last=(m == 3))
                            yield
                        c.A(lambda e: e.copy(out=yT_c.a, in_=pY.a), [pY], [yT_c])
                        yield
                        c.dma("sp", YCT.a[:, :, tok].rearrange("m p t -> p m t"), yT_c.a, YCT, yT_c)
                        yield

                    rr(l_s1(0))
                    if NT > 1:
                        rr(l_s1(1))
                    for n in range(NT):
                        rr(l_s2(n), l_s1(n + 2) if n + 2 < NT else None)
                c.pop()
                if stop_after == "p2c":
                    break

                c.push()
                wbr = c.sb("wbr", [128, 3, 4, D], BF16, grp="w3"); wo = c.sb("wo", [128, 8, D], BF16, grp="w3")
                for br in range(3):
                    c.dma("pool", wbr.a[:, br], w_branch.a[l, br].rearrange("(k p) n -> p k n", p=128), wbr, w_branch)
                c.dma("pool", wo.a, w_out.a[l].rearrange("(k p) n -> p k n", p=128), wo, w_out)
                ybr = [c.sb("ybr%d" % i, [128, 3, 4, 128], BF16, grp="l3%d" % i) for i in range(2)]
                gt_ = [c.sb("gt%d" % i, [128, 3072], BF16, grp="l3%d" % i) for i in range(2)]
                xt = [c.sb("x3%d" % i, [128, D], F32, grp="l3%d" % i) for i in range(2)]
                pB = [c.ps("pB%d" % i, [128, 512], F32) for i in range(2)]
                mrg_L = [c.sb("mrg%d" % i_, [128, D], F32) for i_ in range(2)]; mrg = mrg_L[0]; mtmp_L = [c.sb("mtmp%d" % i_, [128, 512], F32) for i_ in range(2)]; mtmp = mtmp_L[0]; mrb_L = [c.sb("mrb%d" % i_, [128, D], BF16) for i_ in range(2)]; mrb = mrb_L[0]
                pT = c.ps("pT3", [128, 8, 128], BF16); mT_L = [c.sb("mT%d" % i_, [128, 8, 128], BF16) for i_ in range(2)]; mT = mT_L[0]
                pYo = c.ps("pYo", [128, D], F32)
                x1 = [c.sb("x1%d" % i, [128, D], F32) for i in range(2)]
                junk_L = [c.sb("junk3%d" % i_, [128, D], F32) for i_ in range(2)]; junk = junk_L[0]; ss_L = [c.sb("ss3%d" % i_, [128, 1], F32) for i_ in range(2)]; ss = ss_L[0]; rstd_L = [c.sb("rstd3%d" % i_, [128, 1], F32) for i_ in range(2)]; rstd = rstd_L[0]
                xnf_L = [c.sb("xnf%d" % i_, [128, D], F32) for i_ in range(2)]; xnf = xnf_L[0]
                pTf = c.ps("pTf", [128, 8, 128], F32)
                h2f_L = [c.sb("h2f%d" % i_, [128, 8, 128], F32) for i_ in range(2)]; h2f = h2f_L[0]; h2b = [c.sb("h2b%d" % i, [128, 8, 128], BF16) for i in range(2)]
                pL = c.ps("pL", [128, 36], F32)
                lg_L = [c.sb("lg%d" % i_, [128, 36], F32) for i_ in range(2)]; lg = lg_L[0]; gmax_L = [c.sb("gmax%d" % i_, [128, 1], F32) for i_ in range(2)]; gmax = gmax_L[0]; ngmax_L = [c.sb("ngmax%d" % i_, [128, 1], F32) for i_ in range(2)]; ngmax = ngmax_L[0]
                eg_L = [c.sb("eg%d" % i_, [128, 4], F32) for i_ in range(2)]; eg = eg_L[0]; sg_L = [c.sb("sg%d" % i_, [128, 1], F32) for i_ in range(2)]; sg = sg_L[0]; ohg_L = [c.sb("ohg%d" % i_, [128, 4], F32) for i_ in range(2)]; ohg = ohg_L[0]
                lem_L = [c.sb("lem%d" % i_, [128, 4, 8], F32) for i_ in range(2)]; lem = lem_L[0]; les_L = [c.sb("les%d" % i_, [128, 8], F32) for i_ in range(2)]; les = les_L[0]; m8_L = [c.sb("m8%d" % i_, [128, 8], F32) for i_ in range(2)]; m8 = m8_L[0]
                nv0_L = [c.sb("nv0%d" % i_, [128, 1], F32) for i_ in range(2)]; nv0 = nv0_L[0]; e8_L = [c.sb("e8%d" % i_, [128, 8], F32) for i_ in range(2)]; e8 = e8_L[0]; mk2_L = [c.sb("mk2%d" % i_, [128, 8], F32) for i_ in range(2)]; mk2 = mk2_L[0]
                w8_L = [c.sb("w8%d" % i_, [128, 8], F32) for i_ in range(2)]; w8 = w8_L[0]; sden_L = [c.sb("sden%d" % i_, [128, 1], F32) for i_ in range(2)]; sden = sden_L[0]
                dw = [c.sb("dw%d" % i, [128, 4, 8], F32) for i in range(2)]
                def genA(i):
                    tok = slice(i * 128, (i + 1) * 128)
                    j3 = 2 if i < 2 else b
                    mrg = mrg_L[i % 2]; mtmp = mtmp_L[i % 2]; mrb = mrb_L[i % 2]; mT = mT_L[i % 2]; junk = junk_L[i % 2]; ss = ss_L[i % 2]; rstd = rstd_L[i % 2]; xnf = xnf_L[i % 2]; h2f = h2f_L[i % 2]; lg = lg_L[i % 2]; gmax = gmax_L[i % 2]; ngmax = ngmax_L[i % 2]; eg = eg_L[i % 2]; sg = sg_L[i % 2]; ohg = ohg_L[i % 2]; lem = lem_L[i % 2]; les = les_L[i % 2]; m8 = m8_L[i % 2]; nv0 = nv0_L[i % 2]; e8 = e8_L[i % 2]; mk2 = mk2_L[i % 2]; w8 = w8_L[i % 2]; sden = sden_L[i % 2]
                    y_ = ybr[i % 2]; g_ = gt_[i % 2]; x_ = xt[i % 2]; xo = x1[i % 2]; hb = h2b[i % 2]; dw_ = dw[i % 2]
                    for br, YT_ in enumerate((YAT, YBT, YCT)):
                        c.dma("sp", y_.a[:, br], YT_.a[:, :, tok].rearrange("m p t -> p m t"), y_, YT_)
                        yield
                    c.dma("sp", g_.a, GATES.a[tok], g_, GATES)
                    yield
                    src, ap = tile_src(l, b, i)
                    c.dma("sp", x_.a, ap, x_, src)
                    yield
                    for half in range(2):
                        cs = slice(half * 512, (half + 1) * 512)
                        for br in range(3):
                            p_ = pB[(half * 3 + br) % 2]
                            for k in range(4):
                                c.mm(lambda e: e.matmul(p_.a, lhsT=y_.a[:, br, k, :], rhs=wbr.a[:, br, k, cs], start=(k == 0), stop=(k == 3)), [y_, wbr], [p_], last=(k == 3))
                                yield
                            gsl = g_.a[:, br * 1024 + half * 512: br * 1024 + (half + 1) * 512]
                            if br == 0:
                                c.V(lambda e: e.tensor_tensor(out=mrg.a[:, cs], in0=p_.a, in1=gsl, op=ALU.mult), [p_, g_], [mrg])
                                yield
                            else:
                                c.V(lambda e: e.tensor_tensor(out=mtmp.a, in0=p_.a, in1=gsl, op=ALU.mult), [p_, g_], [mtmp])
                                yield
                                c.G(lambda e: e.tensor_tensor(out=mrg.a[:, cs], in0=mrg.a[:, cs], in1=mtmp.a, op=ALU.add), [mrg, mtmp], [mrg])
                                yield
                    c.A(lambda e: e.copy(out=mrb.a, in_=mrg.a), [mrg], [mrb])
                    yield
                    for k in range(8):
                        c.mm(lambda e: e.transpose(out=pT.a[:, k, :], in_=mrb.a[:, k * 128:(k + 1) * 128], identity=ident.a), [mrb, ident], [pT], last=(k == 7))
                        yield
                    c.A(lambda e: e.copy(out=mT.a, in_=pT.a), [pT], [mT])
                    yield
                    for half in range(2):
                        cs = slice(half * 512, (half + 1) * 512)
                        for k in range(8):
                            c.mm(lambda e: e.matmul(pYo.a[:, cs], lhsT=mT.a[:, k, :], rhs=wo.a[:, k, cs], start=(k == 0), stop=(k == 7)), [mT, wo], [pYo], last=(k == 7 and half == 1))
                            yield
                    c.V(lambda e: e.tensor_tensor(out=xo.a, in0=pYo.a, in1=GT.a[:, 0, j3, :], op=ALU.mult), [pYo, GT], [xo])
                    yield
                    c.G(lambda e: e.tensor_tensor(out=xo.a, in0=xo.a, in1=x_.a, op=ALU.add), [xo, x_], [xo])
                    yield
                    c.dma("sp", XS.a[b, tok, :], xo.a, XS, xo)
                    yield
                def genB(i):
                    tok = slice(i * 128, (i + 1) * 128)
                    j3 = 2 if i < 2 else b
                    mrg = mrg_L[i % 2]; mtmp = mtmp_L[i % 2]; mrb = mrb_L[i % 2]; mT = mT_L[i % 2]; junk = junk_L[i % 2]; ss = ss_L[i % 2]; rstd = rstd_L[i % 2]; xnf = xnf_L[i % 2]; h2f = h2f_L[i % 2]; lg = lg_L[i % 2]; gmax = gmax_L[i % 2]; ngmax = ngmax_L[i % 2]; eg = eg_L[i % 2]; sg = sg_L[i % 2]; ohg = ohg_L[i % 2]; lem = lem_L[i % 2]; les = les_L[i % 2]; m8 = m8_L[i % 2]; nv0 = nv0_L[i % 2]; e8 = e8_L[i % 2]; mk2 = mk2_L[i % 2]; w8 = w8_L[i % 2]; sden = sden_L[i % 2]
                    y_ = ybr[i % 2]; g_ = gt_[i % 2]; x_ = xt[i % 2]; xo = x1[i % 2]; hb = h2b[i % 2]; dw_ = dw[i % 2]
                    c.V(lambda e: e.memset(ss.a, 0.0), [], [ss])
                    yield
                    c.A(lambda e: e.activation(out=junk.a, in_=xo.a, func=AF.Square, accum_out=ss.a), [xo], [junk, ss])
                    yield
                    c.V(lambda e: e.tensor_scalar(out=rstd.a, in0=ss.a, scalar1=1.0 / D, scalar2=1e-6, op0=ALU.mult, op1=ALU.add), [ss], [rstd])
                    yield
                    c.A(lambda e: e.activation(out=rstd.a, in_=rstd.a, func=AF.Sqrt), [rstd], [rstd])
                    yield
                    c.V(lambda e: e.reciprocal(out=rstd.a, in_=rstd.a), [rstd], [rstd])
                    yield
                    c.V(lambda e: e.tensor_scalar(out=xnf.a, in0=xo.a, scalar1=rstd.a[:, 0:1], scalar2=None, op0=ALU.mult), [xo, rstd], [xnf])
                    yield
                    for k in range(8):
                        c.mm(lambda e: e.transpose(out=pTf.a[:, k, :], in_=xnf.a[:, k * 128:(k + 1) * 128], identity=ident_f.a), [xnf, ident_f], [pTf], last=(k == 7))
                        yield
                    for k in range(8):
                        c.V(lambda e: e.tensor_scalar(out=h2f.a[:, k, :], in0=pTf.a[:, k, :], scalar1=G2.a[:, k, j3:j3 + 1], scalar2=modT.a[:, 3, k, j3:j3 + 1], op0=ALU.mult, op1=ALU.add), [pTf, G2, modT], [h2f])
                        yield
                    c.A(lambda e: e.copy(out=hb.a, in_=h2f.a), [h2f], [hb])
                    yield
                    c.dma("sp", H2T.a[:, :, tok], hb.a, H2T, hb)
                    yield
                    for k in range(8):
                        c.mm(lambda e: e.matmul(pL.a, lhsT=h2f.a[:, k, :], rhs=wrt.a[:, k, :], start=(k == 0), stop=(k == 7)), [h2f, wrt], [pL], last=(k == 7))
                        yield
                    c.V(lambda e: e.tensor_tensor(out=lg.a, in0=pL.a, in1=brt.a, op=ALU.add), [pL, brt], [lg])
                    yield
                    c.V(lambda e: e.tensor_reduce(out=gmax.a, in_=lg.a[:, 0:4], axis=AX.X, op=ALU.max), [lg], [gmax])
                    yield
                    c.V(lambda e: e.tensor_scalar(out=ngmax.a, in0=gmax.a, scalar1=-1.0, scalar2=None, op0=ALU.mult), [gmax], [ngmax])
                    yield
                    c.V(lambda e: e.memset(sg.a, 0.0), [], [sg])
                    yield
                    c.A(lambda e: e.activation(out=eg.a, in_=lg.a[:, 0:4], func=AF.Exp, bias=ngmax.a[:, 0:1], accum_out=sg.a), [lg, ngmax], [eg, sg])
                    yield
                    c.V(lambda e: e.tensor_scalar(out=ohg.a, in0=lg.a[:, 0:4], scalar1=gmax.a[:, 0:1], scalar2=None, op0=ALU.is_ge), [lg, gmax], [ohg])
                    yield
                    c.V(lambda e: e.tensor_tensor(out=lem.a, in0=lg.a[:, 4:36].rearrange("p (g e) -> p g e", e=8), in1=ohg.a.unsqueeze(2).to_broadcast([128, 4, 8]), op=ALU.mult), [lg, ohg], [lem])
                    yield
                    c.V(lambda e: e.tensor_reduce(out=les.a, in_=lem.a.rearrange("p g e -> p e g"), axis=AX.X, op=ALU.add), [lem], [les])
                    yield
                    c.V(lambda e: e.max(out=m8.a, in_=les.a), [les], [m8])
                    yield
                    c.V(lambda e: e.tensor_scalar(out=nv0.a, in0=m8.a[:, 0:1], scalar1=-1.0, scalar2=None, op0=ALU.mult), [m8], [nv0])
                    yield
                    c.A(lambda e: e.activation(out=e8.a, in_=les.a, func=AF.Exp, bias=nv0.a[:, 0:1]), [les, nv0], [e8])
                    yield
                    c.V(lambda e: e.tensor_scalar(out=mk2.a, in0=les.a, scalar1=m8.a[:, 1:2], scalar2=None, op0=ALU.is_ge), [les, m8], [mk2])
                    yield
                    c.V(lambda e: e.tensor_tensor(out=w8.a, in0=e8.a, in1=mk2.a, op=ALU.mult), [e8, mk2], [w8])
                    yield
                    c.V(lambda e: e.tensor_reduce(out=sden.a, in_=w8.a, axis=AX.X, op=ALU.add), [w8], [sden])
                    yield
                    c.V(lambda e: e.tensor_tensor(out=sden.a, in0=sden.a, in1=sg.a, op=ALU.mult), [sden, sg], [sden])
                    yield
                    c.V(lambda e: e.reciprocal(out=sden.a, in_=sden.a), [sden], [sden])
                    yield
                    c.V(lambda e: e.tensor_scalar(out=w8.a, in0=w8.a, scalar1=sden.a[:, 0:1], scalar2=None, op0=ALU.mult), [w8, sden], [w8])
                    yield
                    c.V(lambda e: e.tensor_tensor(out=dw_.a, in0=ohg.a.unsqueeze(2).to_broadcast([128, 4, 8]), in1=w8.a.unsqueeze(1).to_broadcast([128, 4, 8]), op=ALU.mult), [ohg, w8], [dw_])
                    yield
                    c.dma("sp", DW.a[tok].rearrange("p (g e) -> p g e", e=8), dw_.a, DW, dw_)
                    yield
                tl = list(range(first_tile, NT))
                rr(genA(tl[0]))
                for k_ in range(len(tl)):
                    rr(genB(tl[k_]), genA(tl[k_ + 1]) if k_ + 1 < len(tl) else None)
                c.pop()
                if stop_after == "p3a":
                    break

                tiles = list(range(first_tile, NT))
                ng = 2
                per = (len(tiles) + ng - 1) // ng
                for gi in range(ng):
                    grp = tiles[gi * per:(gi + 1) * per]
                    t0 = grp[0]; G_ = len(grp)
                    c.push()
                    h2 = c.sb("h2", [128, 8, G_ * 128], BF16, grp="g4")
                    dwg = c.sb("dwg", [128, G_, 32], F32, grp="g4")
                    acc = c.sb("acc", [128, G_, D], F32)
                    c.dma("sp", h2.a, H2T.a[:, :, t0 * 128:(t0 + G_) * 128], h2, H2T)
                    c.dma("sp", dwg.a, DW.a[t0 * 128:(t0 + G_) * 128].rearrange("(i p) e -> p i e", p=128), dwg, DW)
                    wg = [c.sb("wg%d" % i, [128, 8, 512], BF16, grp="we%d" % i) for i in range(2)]
                    wd = [c.sb("wd%d" % i, [128, 2, D], BF16, grp="we%d" % i) for i in range(2)]
                    pGU = [c.ps("pGU%d" % i, [128, 2, 512], F32) for i in range(2)]
                    sl = [c.sb("sl%d" % i, [128, 512], F32) for i in range(2)]
                    aT = [c.sb("aT%d" % i, [128, 2, 512], BF16) for i in range(2)]
                    pD = [c.ps("pD%d" % i, [128, D], F32) for i in range(2)]
                    xt = [c.sb("x4%d" % i, [128, D], F32) for i in range(2)]
                    quads = [(tq, min(4, G_ - tq)) for tq in range(0, G_, 4)]
                    steps = [(e_, qi) for e_ in range(32) for qi in range(len(quads))]

                    def m_s1(si):
                        e_, qi = steps[si]
                        tq, nt = quads[qi]; N = nt * 128
                        g_ = wg[e_ % 2]; d_ = wd[e_ % 2]; at_ = aT[si % 2]
                        if qi == 0:
                            c.dma("sp", g_.a, WGU.a[e_], g_, WGU)
                            c.dma("sp", d_.a, WDN.a[e_], d_, WDN)
                        for cch in range(2):
                            pg = pGU[cch]; s_ = sl[cch]
                            for which in range(2):
                                col0 = which * 256 + cch * 128
                                for k in range(8):
                                    c.mm(lambda e: e.matmul(pg.a[:, which, :N], lhsT=g_.a[:, k, col0:col0 + 128], rhs=h2.a[:, k, tq * 128:tq * 128 + N], start=(k == 0), stop=(k == 7)), [g_, h2], [pg], last=(k == 7 and which == 1))
                            c.A(lambda e: e.activation(out=s_.a[:, :N], in_=pg.a[:, 0, :N], func=AF.Silu), [pg], [s_])
                            c.V(lambda e: e.tensor_tensor(out=at_.a[:, cch, :N], in0=s_.a[:, :N], in1=pg.a[:, 1, :N], op=ALU.mult), [s_, pg], [at_])

                    def m_s2(si):
                        e_, qi = steps[si]
                        tq, nt = quads[qi]
                        d_ = wd[e_ % 2]; at_ = aT[si % 2]
                        for tj in range(nt):
                            ti = tq + tj
                            pd = pD[tj % 2]
                            for half in range(2):
                                cs = slice(half * 512, (half + 1) * 512)
                                for k in range(2):
                                    c.mm(lambda e: e.matmul(pd.a[:, cs], lhsT=at_.a[:, k, tj * 128:(tj + 1) * 128], rhs=d_.a[:, k, cs], start=(k == 0), stop=(k == 1)), [at_, d_], [pd], last=(k == 1 and half == 1))
                            if e_ == 0:
                                c.V(lambda e: e.tensor_scalar(out=acc.a[:, ti, :], in0=pd.a, scalar1=dwg.a[:, ti, e_:e_ + 1], scalar2=None, op0=ALU.mult), [pd, dwg], [acc])
                            else:
                                c.V(lambda e: e.scalar_tensor_tensor(out=acc.a[:, ti, :], in0=pd.a, scalar=dwg.a[:, ti, e_:e_ + 1], in1=acc.a[:, ti, :], op0=ALU.mult, op1=ALU.add), [pd, dwg, acc], [acc])

                    pipeline(len(steps), m_s1, m_s2, 2)
                    for ti in range(G_):
                        i = t0 + ti
                        j3 = 2 if i < 2 else b
                        x_ = xt[ti % 2]
                        c.dma("sp", x_.a, XS.a[b, i * 128:(i + 1) * 128, :], x_, XS)
                        c.G(lambda e: e.tensor_tensor(out=acc.a[:, ti, :], in0=acc.a[:, ti, :], in1=GT.a[:, 1, j3, :], op=ALU.mult), [acc, GT], [acc])
                        c.V(lambda e: e.tensor_tensor(out=x_.a, in0=x_.a, in1=acc.a[:, ti, :], op=ALU.add), [x_, acc], [x_])
                        if last_layer:
                            c.dma("sp", y_out.a[b, (i - 2) * 128:(i - 1) * 128, :], x_.a, y_out, x_)
                        else:
                            c.dma("sp", XS.a[b, i * 128:(i + 1) * 128, :], x_.a, XS, x_)
                    c.pop()
            if stop_after is not None:
                break
            c.pop()
        c.barrier()
        while len(c.stack) > 1:
            c.stack.pop().__exit__(None, None, None)
        print("instructions:", c.ninst, "sems:", len(c.sem))
    nc._trace = c.trace
    return nc


def host_consts():
    t = np.arange(4096)
    row = (t // 64).astype(np.float32); col = (t % 64).astype(np.float32)
    inv = (10000.0 ** (-np.arange(16, dtype=np.float32) / 16)).astype(np.float32)
    ang = np.concatenate([row[:, None] * inv, col[:, None] * inv], axis=-1).astype(np.float32)
    cos = np.ones((T, 32), np.float32); sin = np.zeros((T, 32), np.float32)
    cos[256:] = np.cos(ang); sin[256:] = np.sin(ang)
    s = np.arange(128)
    u = np.stack([(s[:, None] <= s[None, :]), (s[:, None] >= s[None, :])]).astype(np.float32)
    kc = np.arange(64)[:, None]; qc = np.arange(64)[None, :]
    cs = np.clip(qc - 8, 0, 48)
    valid = (kc >= cs) & (kc < cs + 16)
    cm = np.where(valid, 0.0, NEG).astype(np.float32)
    jd = np.zeros((64, 128), np.float32)
    for kc in range(64):
        jd[63 - kc, kc] = 1.0; jd[63 - kc, 64 + kc] = 1.0
    return {"k_jd": jd, "k_cos": cos, "k_sin": sin, "k_u": u, "k_id": np.eye(128, dtype=np.float32), "k_colmask": np.concatenate([cm, cm], 0)}


def make_in_maps(inputs, nb, cores):
    consts = host_consts()
    maps = []
    for ci in cores:
        m = dict(consts)
        for k, v in inputs.items():
            v = np.ascontiguousarray(v, dtype=np.float32)
            if k in ("x", "c", "ctx"):
                m[k] = np.ascontiguousarray(v[ci * nb:(ci + 1) * nb])
            elif k == "b_mlstm":
                m[k] = v.reshape(2, 16)
            else:
                m[k] = v
        maps.append(m)
    return maps


def kernel(**inputs):
    nb = 2
    nc = build(nb=nb, nl=2)
    in_maps = make_in_maps(inputs, nb, list(range(8)))
    res = run_bass_kernel_spmd(nc, in_maps, core_ids=list(range(8)))
    return np.concatenate([r["y"] for r in res.results], axis=0).astype(np.float32)
```

```python
import numpy as np
import concourse.bass as bass
import concourse.mybir as mybir
from concourse.bass_utils import run_bass_kernel_spmd
from contextlib import ExitStack
import os

F32 = mybir.dt.float32
BF16 = mybir.dt.bfloat16
AF = mybir.ActivationFunctionType
ALU = mybir.AluOpType
AX = mybir.AxisListType

T = 4352
NT = 34
D = 1024
NEG = -1e30
SKIP_SAME = int(os.environ.get("SKIP_SAME", "0"))


class Buf:
    __slots__ = ("name", "w", "r", "dsem", "t", "grp")

    def __init__(self, name, t=None, grp=None):
        self.name = name
        self.grp = grp
        self.w = None
        self.r = {}
        self.dsem = None
        self.t = t

    @property
    def a(self):
        return self.t.ap() if hasattr(self.t, "ap") else self.t[:]


class Ctx:
    def __init__(self, nc, es):
        self.nc = nc
        self.es = es
        self.E = {"pe": nc.tensor, "act": nc.scalar, "dve": nc.vector, "pool": nc.gpsimd, "sp": nc.sync}
        self.sem = {}
        self.ecnt = {}
        for e in ("pe", "act", "dve", "pool"):
            self.sem[e] = es.enter_context(nc.semaphore("c_" + e))
            self.ecnt[e] = 0
        self.seen = {e: {} for e in self.E}
        self.ninst = 0
        self.uid = 0
        self.trace = {e: [] for e in self.E}
        self.shared = set()
        self.stack = [es]
        self.dtot = {}

    def sb(self, name, shape, dt, grp=None):
        self.uid += 1
        return Buf(name, self.stack[-1].enter_context(self.nc.sbuf_tensor("%s_%d" % (name, self.uid), list(shape), dt)), grp)

    def ps(self, name, shape, dt):
        self.uid += 1
        return Buf(name, self.stack[-1].enter_context(self.nc.psum_tensor("%s_%d" % (name, self.uid), list(shape), dt)))

    def dram(self, name, shape, dt, kind="Internal"):
        return Buf(name, self.nc.dram_tensor(name, list(shape), dt, kind=kind))

    def push(self):
        st = ExitStack()
        st.__enter__()
        self.stack.append(st)

    def pop(self):
        self.barrier()
        self.stack.pop().__exit__(None, None, None)

    def barrier(self):
        evs = [(e, self.ecnt[e]) for e in self.ecnt] + list(self.dtot.items())
        for e in self.E:
            self._wait(e, evs)

    def _wait(self, eng, deps):
        need = {}
        for k, v in deps:
            if eng == "pe" and k == "pe":
                continue
            if SKIP_SAME and k == eng and v <= self.ecnt[eng] - SKIP_SAME:
                continue
            if v > need.get(k, 0):
                need[k] = v
        seen = self.seen[eng]
        for k, v in need.items():
            if k in self.shared:
                v = self.dtot[k]
            if seen.get(k, 0) >= v:
                continue
            self.E[eng].wait_ge(self.sem[k], v)
            self.trace[eng].append(("w", k, v))
            self.ninst += 1
            seen[k] = v

    def _deps(self, reads, writes):
        deps = []
        for b in reads:
            if b.w is not None:
                deps.append(b.w)
        for b in writes:
            if b.w is not None:
                deps.append(b.w)
            deps.extend(b.r.items())
        return deps

    def _commit(self, ev, reads, writes):
        k, v = ev
        for b in reads:
            if b.r.get(k, 0) < v:
                b.r[k] = v
        for b in writes:
            b.w = ev
            b.r = {}

    def op(self, eng, f, reads=(), writes=()):
        self._wait(eng, self._deps(reads, writes))
        ins = f(self.E[eng])
        self.ecnt[eng] += 1
        ins.then_inc(self.sem[eng], 1)
        self.trace[eng].append(("i", eng, 1))
        self.ninst += 1
        self._commit((eng, self.ecnt[eng]), reads, writes)
        return ins

    def V(self, f, r=(), w=()):
        return self.op("dve", f, r, w)

    def A(self, f, r=(), w=()):
        return self.op("act", f, r, w)

    def G(self, f, r=(), w=()):
        return self.op("pool", f, r, w)

    def mm(self, f, reads=(), writes=(), last=True):
        self._wait("pe", self._deps(reads, writes))
        ins = f(self.E["pe"])
        self.ninst += 1
        if last:
            self.ecnt["pe"] += 1
            ins.then_inc(self.sem["pe"], 1)
            self.trace["pe"].append(("i", "pe", 1))
            self._commit(("pe", self.ecnt["pe"]), reads, writes)
        else:
            self._commit(("pe", self.ecnt["pe"] + 1), reads, writes)
        return ins

    def dma(self, q, out, in_, dst, src, **kw):
        self._wait(q, self._deps((src,), (dst,)))
        if dst.dsem is None:
            dst.dsem = "d_" + (dst.grp or dst.name)
            if dst.grp:
                self.shared.add(dst.dsem)
            if dst.dsem not in self.sem:
                self.sem[dst.dsem] = self.es.enter_context(self.nc.semaphore(dst.dsem))
                self.dtot[dst.dsem] = 0
        ins = self.E[q].dma_start(out=out, in_=in_, **kw)
        self.dtot[dst.dsem] += 16
        ins.then_inc(self.sem[dst.dsem], 16)
        self.trace[q].append(("i", dst.dsem, 16))
        self.ninst += 1
        self._commit((dst.dsem, self.dtot[dst.dsem]), (src,), (dst,))
        return ins


def pipeline(n, stage1, stage2, depth, s1_first=False):
    for si in range(min(depth, n)):
        stage1(si)
    for si in range(n):
        if s1_first and si + depth < n:
            stage1(si + depth)
        stage2(si)
        if not s1_first and si + depth < n:
            stage1(si + depth)


def rr(*gens):
    gens = [g for g in gens if g is not None]
    while gens:
        for g in list(gens):
            try:
                next(g)
            except StopIteration:
                gens.remove(g)


def na_plan(j):
    plan = []
    for kt in range(32):
        blocks = {}
        anyv = False
        for a in range(2):
            for b in range(2):
                qr = 2 * j + b
                kr = 2 * kt + a
                rs = min(max(qr - 4, 0), 56)
                ok = rs <= kr < rs + 8
                blocks[(a, b)] = (kr - qr + 7) if ok else None
                anyv = anyv or ok
        if anyv:
            plan.append((kt, blocks))
    return plan


def build(nb=2, nl=2, dbg=(), stop_after=None):
    nc = bass.Bass("TRN2", target_bir_lowering=False)
    es = ExitStack()
    with es:
        c = Ctx(nc, es)

        def inp(name, shape):
            return Buf(name, nc.dram_tensor(name, list(shape), F32, kind="ExternalInput"))

        x_in = inp("x", [nb, 4096, D]); ctx_in = inp("ctx", [nb, 256, D]); c_in = inp("c", [nb, D]); cctx_in = inp("c_ctx", [D])
        w_mod = inp("w_mod", [2, D, 6144]); b_mod = inp("b_mod", [2, 6144]); g_norm = inp("g_norm", [2, 2, D])
        w_in = inp("w_in", [2, D, 7440]); b_merge = inp("b_merge", [2, 3, D]); g_qk = inp("g_qk", [2, 4, 64])
        rpb = inp("rpb", [2, 8, 15, 31]); b_mlstm = inp("b_mlstm", [2, 16]); g_ml = inp("g_ml", [2, 512])
        w_branch = inp("w_branch", [2, 3, 512, D]); w_out = inp("w_out", [2, D, D])
        w_group = inp("w_group", [2, D, 4]); b_group = inp("b_group", [2, 4]); w_router = inp("w_router", [2, D, 32]); b_router = inp("b_router", [2, 32])
        w_gate_up = inp("w_gate_up", [2, 32, D, 512]); w_down = inp("w_down", [2, 32, 256, D])
        k_cos = inp("k_cos", [T, 32]); k_sin = inp("k_sin", [T, 32]); k_u = inp("k_u", [2, 128, 128]); k_id = inp("k_id", [128, 128])
        k_colmask = inp("k_colmask", [128, 64]); k_jd = inp("k_jd", [64, 128])
        y_out = c.dram("y", [nb, 4096, D], F32, kind="ExternalOutput")

        def scr(name, shape, dt):
            return c.dram(name, shape, dt, kind=("ExternalOutput" if name in dbg else "Internal"))

        XS = scr("XS", [nb, T, D], F32)
        QAT = scr("QAT", [128, 4, T], BF16); KAT = scr("KAT", [128, T], BF16); VA = scr("VA", [T, 2, 65], BF16)
        QBT = scr("QBT", [128, 4, T], BF16); KBT = scr("KBT", [128, 4, T], BF16); VB = scr("VB", [T, 8, 65], BF16)
        MQT = scr("MQT", [128, 4, T], BF16); MKT = scr("MKT", [128, 4, T], BF16); MK = scr("MK", [T, 512], BF16)
        MV = scr("MV", [T, 4, 129], BF16); MO = scr("MO", [T, 512], BF16); MG = scr("MG", [T, 16], F32)
        GATES = scr("GATES", [T, 3072], BF16)
        YAT = scr("YAT", [4, 128, T], BF16); YBT = scr("YBT", [4, 128, T], BF16); YCT = scr("YCT", [4, 128, T], BF16)
        HS = scr("HS", [T, 512], F32)
        H2T = scr("H2T", [128, 8, T], BF16); DW = scr("DW", [T, 32], F32)
        WGU = scr("WGU", [32, 128, 8, 512], BF16); WDN = scr("WDN", [32, 128, 2, D], BF16)
        MODD = scr("MODD", [2, 3, 6144], F32)
        RPBP = scr("RPBP", [7568], F32); TPD = scr("TPD", [128, 8, 15, 64], F32)

        ident_f = c.sb("ident_f", [128, 128], F32, grp="setup"); ident = c.sb("ident", [128, 128], BF16)
        U = c.sb("U", [128, 2, 128], F32, grp="setup")
        c.dma("sp", ident_f.a, k_id.a, ident_f, k_id)
        c.dma("sp", U.a, k_u.a.rearrange("d s t -> s d t"), U, k_u)
        c.V(lambda e: e.tensor_copy(out=ident.a, in_=ident_f.a), [ident_f], [ident])
        ones_col = c.sb("ones_col", [128, 8], BF16)
        c.V(lambda e: e.memset(ones_col.a, 1.0), [], [ones_col])

        def tile_src(l, b, i):
            if l == 0:
                if i < 2:
                    return ctx_in, ctx_in.a[b, i * 128:(i + 1) * 128, :]
                return x_in, x_in.a[b, (i - 2) * 128:(i - 1) * 128, :]
            return XS, XS.a[b, i * 128:(i + 1) * 128, :]

        for l in range(nl):
            last_layer = (l == nl - 1)
            first_tile = 2 if last_layer else 0
            c.push()
            c.push()
            cT = c.sb("cT", [128, 8, 3], F32, grp="setup"); cTb = c.sb("cTb", [128, 8, 3], BF16)
            c.V(lambda e: e.memset(cT.a, 0.0), [], [cT])
            for b in range(nb):
                c.dma("sp", cT.a[:, :, b], c_in.a[b].rearrange("(k p) -> p k", p=128), cT, c_in, allow_slow_non_contiguous=True)
            c.dma("sp", cT.a[:, :, 2], cctx_in.a.rearrange("(k p) -> p k", p=128), cT, cctx_in, allow_slow_non_contiguous=True)
            c.A(lambda e: e.activation(out=cTb.a, in_=cT.a, func=AF.Silu), [cT], [cTb])
            modrow = c.sb("modrow", [3, 6144], F32)
            bmrow = c.sb("bmrow", [3, 6144], F32, grp="setup")
            c.dma("sp", bmrow.a, b_mod.a[l].partition_broadcast(3), bmrow, b_mod)
            wm = [c.sb("wm%d" % i, [128, 8, 512], BF16) for i in range(2)]
            pmod = [c.ps("pmod%d" % i, [128, 512], F32) for i in range(2)]
            for n in range(12):
                w_ = wm[n % 2]; p_ = pmod[n % 2]
                c.dma("pool", w_.a, w_mod.a[l, :, n * 512:(n + 1) * 512].rearrange("(k p) n -> p k n", p=128), w_, w_mod)
                for k in range(8):
                    c.mm(lambda e: e.matmul(p_.a[0:3, :], lhsT=cTb.a[:, k, :], rhs=w_.a[:, k, :], start=(k == 0), stop=(k == 7)), [cTb, w_], [p_], last=(k == 7))
                c.V(lambda e: e.tensor_tensor(out=modrow.a[:, n * 512:(n + 1) * 512], in0=p_.a[0:3, :], in1=bmrow.a[:, n * 512:(n + 1) * 512], op=ALU.add), [p_, bmrow], [modrow])
            c.dma("sp", MODD.a[l], modrow.a, MODD, modrow)
            c.pop()
            modT = c.sb("modT", [128, 6, 8, 3], F32, grp="setup")
            for s in range(6):
                for j in range(3):
                    c.dma("sp", modT.a[:, s, :, j], MODD.a[l, j, s * 1024:(s + 1) * 1024].rearrange("(k p) -> p k", p=128), modT, MODD, allow_slow_non_contiguous=True)
            gn = c.sb("gn", [128, 2, 8], F32, grp="setup")
            c.dma("sp", gn.a, g_norm.a[l].rearrange("t (k p) -> p t k", p=128), gn, g_norm, allow_slow_non_contiguous=True)
            G1 = c.sb("G1", [128, 8, 3], F32); G2 = c.sb("G2", [128, 8, 3], F32)
            for (Gx, seg, t_) in ((G1, 1, 0), (G2, 4, 1)):
                c.V(lambda e: e.tensor_scalar(out=Gx.a, in0=modT.a[:, seg], scalar1=1.0, scalar2=None, op0=ALU.add), [modT], [Gx])
                c.V(lambda e: e.tensor_tensor(out=Gx.a, in0=Gx.a, in1=gn.a[:, t_, :].unsqueeze(2).to_broadcast([128, 8, 3]), op=ALU.mult), [Gx, gn], [Gx])
            GT = c.sb("GT", [128, 2, 3, D], F32, grp="setup")
            for gi, seg in ((0, 2), (1, 5)):
                for j in range(3):
                    c.dma("sp", GT.a[:, gi, j, :], MODD.a[l, j, seg * 1024:(seg + 1) * 1024].partition_broadcast(128), GT, MODD)
            gqk = c.sb("gqk", [128, 4, 64], F32, grp="setup")
            c.dma("sp", gqk.a, g_qk.a[l].partition_broadcast(128), gqk, g_qk)
            bml = c.sb("bml", [128, 16], F32, grp="setup")
            c.dma("sp", bml.a, b_mlstm.a[l].partition_broadcast(128), bml, b_mlstm)
            gml = c.sb("gml", [128, 512], F32, grp="setup")
            c.dma("sp", gml.a, g_ml.a[l].partition_broadcast(128), gml, g_ml)
            brt = c.sb("brt", [128, 36], F32, grp="setup")
            c.dma("sp", brt.a[:, 0:4], b_group.a[l].partition_broadcast(128), brt, b_group)
            c.dma("sp", brt.a[:, 4:36], b_router.a[l].partition_broadcast(128), brt, b_router)
            wrt = c.sb("wrt", [128, 8, 36], F32, grp="setup")
            c.dma("sp", wrt.a[:, :, 0:4], w_group.a[l].rearrange("(k p) n -> p k n", p=128), wrt, w_group, allow_slow_non_contiguous=True)
            c.dma("sp", wrt.a[:, :, 4:36], w_router.a[l].rearrange("(k p) n -> p k n", p=128), wrt, w_router, allow_slow_non_contiguous=True)
            c.push()
            zt = c.sb("zt", [1, 8192], F32, grp="setup")
            c.V(lambda e: e.memset(zt.a, 0.0), [], [zt])
            c.dma("sp", RPBP.a.rearrange("(o n) -> o n", o=1), zt.a[:, 0:7568], RPBP, zt)
            c.dma("sp", RPBP.a[64:64 + 3720].rearrange("(r j) -> r j", j=31), bass.AP(rpb.t, l * 3720 + 30, [[31, 120], [-1, 31]]), RPBP, rpb, allow_slow_non_contiguous=True)
            TPb = c.sb("TPb", [128, 8, 15, 64], F32, grp="setup")
            cm = c.sb("cm", [128, 64], F32, grp="setup")
            c.dma("sp", cm.a, k_colmask.a, cm, k_colmask)
            TPx = c.sb("TPx", [64, 8, 15, 64], F32, grp="setup")
            for h in range(8):
                src = bass.AP(RPBP.t, 64 - 48 + h * 15 * 31, [[1, 64], [31, 15], [1, 64]])
                c.dma("sp", TPx.a[:, h], src, TPx, RPBP)
            jd = c.sb("jd", [64, 128], F32, grp="setup")
            c.dma("sp", jd.a, k_jd.a, jd, k_jd)
            pJ = [c.ps("pJ%d" % i, [128, 512], F32) for i in range(2)]
            TPx2 = TPx.a.rearrange("p h r q -> p (h r q)")
            TPb2 = TPb.a.rearrange("p h r q -> p (h r) q")
            for n in range(15):
                p_ = pJ[n % 2]
                c.mm(lambda e: e.matmul(p_.a, lhsT=jd.a, rhs=TPx2[:, n * 512:(n + 1) * 512], start=True, stop=True), [jd, TPx], [p_])
                c.V(lambda e: e.tensor_tensor(out=TPb2[:, n * 8:(n + 1) * 8, :], in0=p_.a.rearrange("p (r q) -> p r q", q=64), in1=cm.a.unsqueeze(1).to_broadcast([128, 8, 64]), op=ALU.add), [p_, cm], [TPb])
            c.dma("sp", TPD.a, TPb.a, TPD, TPb)
            c.pop()
            c.push()
            cv = [c.sb("cv%d" % i, [128, 8, 512], BF16, grp="cv%d" % i) for i in range(2)]
            cd = [c.sb("cd%d" % i, [128, 2, D], BF16, grp="cv%d" % i) for i in range(2)]
            for e_ in range(32 if not os.environ.get("SKIP_CONV") else 0):
                a_ = cv[e_ % 2]; d_ = cd[e_ % 2]
                c.dma("pool", a_.a, w_gate_up.a[l, e_].rearrange("(k p) n -> p k n", p=128), a_, w_gate_up)
                c.dma("sp", WGU.a[e_], a_.a, WGU, a_)
                c.dma("pool", d_.a, w_down.a[l, e_].rearrange("(k p) n -> p k n", p=128), d_, w_down)
                c.dma("sp", WDN.a[e_], d_.a, WDN, d_)
            c.pop()
            if stop_after == "mod":
                break

            for b in range(nb):
                c.push()
                hT = c.sb("hT", [128, 8, T], BF16)
                xt = [c.sb("xt%d" % i, [128, D], F32) for i in range(2)]
                junk_L = [c.sb("junk%d" % i_, [128, D], F32) for i_ in range(2)]; junk = junk_L[0]
                ss_L = [c.sb("ss%d" % i_, [128, 1], F32) for i_ in range(2)]; ss = ss_L[0]; rstd_L = [c.sb("rstd%d" % i_, [128, 1], F32) for i_ in range(2)]; rstd = rstd_L[0]
                xn = [c.sb("xn%d" % i, [128, D], BF16) for i in range(2)]
                pT = [c.ps("pT%d" % i, [128, 8, 128], BF16) for i in range(2)]
                for i in range(NT):
                    x_ = xt[i % 2]; n_ = xn[i % 2]; p_ = pT[i % 2]
                    junk = junk_L[i % 2]; ss = ss_L[i % 2]; rstd = rstd_L[i % 2]
                    src, ap = tile_src(l, b, i)
                    j3 = 2 if i < 2 else b
                    c.dma("sp", x_.a, ap, x_, src)
                    c.V(lambda e: e.memset(ss.a, 0.0), [], [ss])
                    c.A(lambda e: e.activation(out=junk.a, in_=x_.a, func=AF.Square, accum_out=ss.a), [x_], [junk, ss])
                    c.V(lambda e: e.tensor_scalar(out=rstd.a, in0=ss.a, scalar1=1.0 / D, scalar2=1e-6, op0=ALU.mult, op1=ALU.add), [ss], [rstd])
                    c.A(lambda e: e.activation(out=rstd.a, in_=rstd.a, func=AF.Sqrt), [rstd], [rstd])
                    c.V(lambda e: e.reciprocal(out=rstd.a, in_=rstd.a), [rstd], [rstd])
                    c.V(lambda e: e.tensor_scalar(out=n_.a, in0=x_.a, scalar1=rstd.a[:, 0:1], scalar2=None, op0=ALU.mult), [x_, rstd], [n_])
                    for k in range(8):
                        c.mm(lambda e: e.transpose(out=p_.a[:, k, :], in_=n_.a[:, k * 128:(k + 1) * 128], identity=ident.a), [n_, ident], [p_], last=(k == 7))
                    for k in range(8):
                        eng = c.A if k % 2 == 0 else None
                        if k % 2 == 0:
                            c.A(lambda e: e.activation(out=hT.a[:, k, i * 128:(i + 1) * 128], in_=p_.a[:, k, :], func=AF.Identity, scale=G1.a[:, k, j3:j3 + 1], bias=modT.a[:, 0, k, j3:j3 + 1]), [p_, G1, modT], [hT])
                        else:
                            c.V(lambda e: e.tensor_scalar(out=hT.a[:, k, i * 128:(i + 1) * 128], in0=p_.a[:, k, :], scalar1=G1.a[:, k, j3:j3 + 1], scalar2=modT.a[:, 0, k, j3:j3 + 1], op0=ALU.mult, op1=ALU.add), [p_, G1, modT], [hT])
                if "HTD" in dbg:
                    HTD = scr("HTD", [128, 8, T], BF16)
                    c.dma("sp", HTD.a, hT.a, HTD, hT)
                cosb = c.sb("cosb", [128, NT, 32], F32, grp="setup"); sinb = c.sb("sinb", [128, NT, 32], F32, grp="setup")
                c.dma("sp", cosb.a, k_cos.a.rearrange("(i p) f -> p i f", p=128), cosb, k_cos)
                c.dma("sp", sinb.a, k_sin.a.rearrange("(i p) f -> p i f", p=128), sinb, k_sin)
                bmg = c.sb("bmg", [128, 3072], F32, grp="setup")
                c.dma("sp", bmg.a, b_merge.a[l].rearrange("t d -> (t d)").partition_broadcast(128), bmg, b_merge)
                wc = [c.sb("wc%d" % i, [128, 8, 512], BF16) for i in range(2)]
                pp = [c.ps("pp%d" % i, [128, 512], F32) for i in range(2)]
                ptr = [c.ps("ptr%d" % i, [128, 4, 128], BF16) for i in range(2)]
                sq_L = [c.sb("sq%d" % i_, [128, 512], F32) for i_ in range(2)]; sq = sq_L[0]; ssq_L = [c.sb("ssq%d" % i_, [128, 8], F32) for i_ in range(2)]; ssq = ssq_L[0]; rq_L = [c.sb("rq%d" % i_, [128, 8], F32) for i_ in range(2)]; rq = rq_L[0]
                qn_L = [c.sb("qn%d" % i_, [128, 512], F32) for i_ in range(2)]; qn = qn_L[0]; t1_L = [c.sb("t1%d" % i_, [128, 256], F32) for i_ in range(2)]; t1 = t1_L[0]; t2_L = [c.sb("t2%d" % i_, [128, 256], F32) for i_ in range(2)]; t2 = t2_L[0]
                qr_ = [c.sb("qr%d" % i, [128, 512], BF16) for i in range(2)]
                trs = [c.sb("trs%d" % i, [128, 4, 128], BF16) for i in range(2)]
                vst = [c.sb("vst%d" % i, [128, 8, 65], BF16) for i in range(2)]
                mvst = [c.sb("mvst%d" % i, [128, 4, 129], BF16) for i in range(2)]
                gst = [c.sb("gst%d" % i, [128, 512], BF16) for i in range(2)]
                mg1_L = [c.sb("mg1%d" % i_, [128, 16], F32) for i_ in range(2)]; mg1 = mg1_L[0]; mg2_L = [c.sb("mg2%d" % i_, [128, 8], F32) for i_ in range(2)]; mg2 = mg2_L[0]
                gpre_L = [c.sb("gpre%d" % i_, [128, 512], F32) for i_ in range(2)]; gpre = gpre_L[0]
                for st_ in vst:
                    c.V(lambda e: e.memset(st_.a, 1.0), [], [st_])
                for st_ in mvst:
                    c.V(lambda e: e.memset(st_.a, 1.0), [], [st_])
                chunks = [(0, 512, "Aq"), (512, 256, "Akv"), (768, 512, "Bq"), (1280, 512, "Bk"), (1792, 512, "Bv"),
                          (2304, 512, "Cq"), (2816, 512, "Ck"), (3328, 512, "Cv"), (3840, 512, "Co"), (4352, 16, "Cg")]
                chunks += [(4368 + 512 * m, 512, "Mg%d" % m) for m in range(6)]
                pp = pp + [c.ps("pp%d" % i, [128, 512], F32) for i in range(2, 4)]

                def mmgen(i, it, cw, w_):
                    p_ = pp[it % 4]
                    for k in range(8):
                        c.mm(lambda e: e.matmul(p_.a[:, :cw], lhsT=hT.a[:, k, i * 128:(i + 1) * 128], rhs=w_.a[:, k, :cw], start=(k == 0), stop=(k == 7)), [hT, w_], [p_], last=(k == 7))
                        yield

                def ptile(i, it, kind):
                    p_ = pp[it % 4]
                    sq = sq_L[it % 2]; ssq = ssq_L[it % 2]; rq = rq_L[it % 2]; qn = qn_L[it % 2]; t1 = t1_L[it % 2]; t2 = t2_L[it % 2]
                    mg1 = mg1_L[it % 2]; mg2 = mg2_L[it % 2]; gpre = gpre_L[it % 2]
                    tok = slice(i * 128, (i + 1) * 128)

                    def qknorm(ncol, gidx, dst):
                        nh = ncol // 64
                        c.A(lambda e: e.activation(out=sq.a[:, :ncol], in_=p_.a[:, :ncol], func=AF.Square), [p_], [sq])
                        yield
                        c.V(lambda e: e.tensor_reduce(out=ssq.a[:, :nh], in_=sq.a[:, :ncol].rearrange("p (h d) -> p h d", d=64), axis=AX.X, op=ALU.add), [sq], [ssq])
                        yield
                        c.V(lambda e: e.tensor_scalar(out=rq.a[:, :nh], in0=ssq.a[:, :nh], scalar1=1.0 / 64, scalar2=1e-6, op0=ALU.mult, op1=ALU.add), [ssq], [rq])
                        yield
                        c.A(lambda e: e.activation(out=rq.a[:, :nh], in_=rq.a[:, :nh], func=AF.Sqrt), [rq], [rq])
                        yield
                        c.V(lambda e: e.reciprocal(out=rq.a[:, :nh], in_=rq.a[:, :nh]), [rq], [rq])
                        yield
                        c.V(lambda e: e.tensor_tensor(out=dst.rearrange("p (h d) -> p h d", d=64), in0=p_.a[:, :ncol].rearrange("p (h d) -> p h d", d=64),
                                                      in1=rq.a[:, :nh].unsqueeze(2).to_broadcast([128, nh, 64]), op=ALU.mult), [p_, rq], [qn])
                        yield
                        c.V(lambda e: e.tensor_tensor(out=dst.rearrange("p (h d) -> p h d", d=64), in0=dst.rearrange("p (h d) -> p h d", d=64),
                                                      in1=gqk.a[:, gidx:gidx + 1, :].to_broadcast([128, nh, 64]), op=ALU.mult), [qn, gqk], [qn])
                        yield

                    def rope(src, nh, dst4, dbuf):
                        s3 = src.rearrange("p (h t f) -> p h t f", t=2, f=32)
                        x1 = s3[:, :, 0, :]; x2 = s3[:, :, 1, :]
                        cb = cosb.a[:, i:i + 1, :].to_broadcast([128, nh, 32]); sb_ = sinb.a[:, i:i + 1, :].to_broadcast([128, nh, 32])
                        a1 = t1.a[:, :nh * 32].rearrange("p (h f) -> p h f", f=32); a2 = t2.a[:, :nh * 32].rearrange("p (h f) -> p h f", f=32)
                        c.V(lambda e: e.tensor_tensor(out=a1, in0=x1, in1=cb, op=ALU.mult), [qn, cosb], [t1])
                        yield
                        c.G(lambda e: e.tensor_tensor(out=a2, in0=x2, in1=sb_, op=ALU.mult), [qn, sinb], [t2])
                        yield
                        c.V(lambda e: e.tensor_tensor(out=dst4[:, :, 0, :], in0=a1, in1=a2, op=ALU.subtract), [t1, t2], [dbuf])
                        yield
                        c.V(lambda e: e.tensor_tensor(out=a1, in0=x2, in1=cb, op=ALU.mult), [qn, cosb], [t1])
                        yield
                        c.G(lambda e: e.tensor_tensor(out=a2, in0=x1, in1=sb_, op=ALU.mult), [qn, sinb], [t2])
                        yield
                        c.V(lambda e: e.tensor_tensor(out=dst4[:, :, 1, :], in0=a1, in1=a2, op=ALU.add), [t1, t2], [dbuf])
                        yield

                    def transposes(srcb, nblk, dstD):
                        pt = ptr[it % 2]; ts_ = trs[it % 2]
                        for m in range(nblk):
                            c.mm(lambda e: e.transpose(out=pt.a[:, m, :], in_=srcb.a[:, m * 128:(m + 1) * 128], identity=ident.a), [srcb, ident], [pt], last=(m == nblk - 1))
                            yield
                        c.A(lambda e: e.copy(out=ts_.a[:, :nblk, :], in_=pt.a[:, :nblk, :]), [pt], [ts_])
                        yield
                        if nblk == 1:
                            c.dma("sp", dstD.a[:, i * 128:(i + 1) * 128], ts_.a[:, 0, :], dstD, ts_)
                        else:
                            c.dma("sp", dstD.a[:, :, i * 128:(i + 1) * 128], ts_.a[:, :nblk, :], dstD, ts_)
                        yield

                    if kind == "Aq":
                        yield from qknorm(512, 0, qn.a[:, :512])
                        q_ = qr_[it % 2]
                        d5 = q_.a.rearrange("p (m g t f) -> p g m t f", g=2, t=2, f=32)
                        for g in range(2):
                            yield from rope(qn.a[:, g * 256:(g + 1) * 256], 4, d5[:, g], q_)
                        yield from transposes(q_, 4, QAT)
                    elif kind == "Akv":
                        yield from qknorm(128, 1, qn.a[:, :128])
                        q_ = qr_[it % 2]
                        yield from rope(qn.a[:, :128], 2, q_.a[:, :128].rearrange("p (h t f) -> p h t f", t=2, f=32), q_)
                        yield from transposes(q_, 1, KAT)
                        v_ = vst[it % 2]
                        c.A(lambda e: e.copy(out=v_.a[:, 0:2, 0:64], in_=p_.a[:, 128:256].rearrange("p (h d) -> p h d", d=64)), [p_], [v_])
                        yield
                        c.dma("sp", VA.a[tok], v_.a[:, 0:2, :], VA, v_)
                        yield
                    elif kind in ("Bq", "Bk"):
                        yield from qknorm(512, 2 if kind == "Bq" else 3, qn.a[:, :512])
                        q_ = qr_[it % 2]
                        c.A(lambda e: e.copy(out=q_.a, in_=qn.a[:, :512]), [qn], [q_])
                        yield
                        yield from transposes(q_, 4, QBT if kind == "Bq" else KBT)
                    elif kind == "Bv":
                        v_ = vst[it % 2]
                        c.A(lambda e: e.copy(out=v_.a[:, :, 0:64], in_=p_.a.rearrange("p (h d) -> p h d", d=64)), [p_], [v_])
                        yield
                        c.dma("sp", VB.a[tok], v_.a, VB, v_)
                        yield
                    elif kind in ("Cq", "Ck"):
                        q_ = qr_[it % 2]
                        c.A(lambda e: e.activation(out=q_.a, in_=p_.a, func=AF.Identity, scale=(1.0 if kind == "Cq" else 128 ** -0.5)), [p_], [q_])
                        yield
                        if kind == "Ck":
                            c.dma("sp", MK.a[tok], q_.a, MK, q_)
                            yield
                        yield from transposes(q_, 4, MQT if kind == "Cq" else MKT)
                    elif kind == "Cv":
                        v_ = mvst[it % 2]
                        c.A(lambda e: e.copy(out=v_.a[:, :, 0:128], in_=p_.a.rearrange("p (h d) -> p h d", d=128)), [p_], [v_])
                        yield
                        c.dma("sp", MV.a[tok], v_.a, MV, v_)
                        yield
                    elif kind == "Co":
                        g_ = gst[it % 2]
                        c.A(lambda e: e.activation(out=g_.a, in_=p_.a, func=AF.Sigmoid), [p_], [g_])
                        yield
                        c.dma("sp", MO.a[tok], g_.a, MO, g_)
                        yield
                    elif kind == "Cg":
                        c.V(lambda e: e.tensor_tensor(out=mg1.a, in0=p_.a[:, :16], in1=bml.a, op=ALU.add), [p_, bml], [mg1])
                        yield
                        fv = mg1.a.rearrange("p (d t h) -> p d t h", d=2, t=2)[:, :, 1, :]
                        m2 = mg2.a.rearrange("p (d h) -> p d h", d=2)
                        c.A(lambda e: e.activation(out=m2, in_=fv, func=AF.Exp, scale=-1.0), [mg1], [mg2])
                        yield
                        c.A(lambda e: e.activation(out=m2, in_=m2, func=AF.Ln, bias=1.0), [mg2], [mg2])
                        yield
                        c.V(lambda e: e.tensor_scalar(out=fv, in0=m2, scalar1=-1.0, scalar2=None, op0=ALU.mult), [mg2], [mg1])
                        yield
                        c.dma("sp", MG.a[tok], mg1.a, MG, mg1)
                        yield
                    else:
                        m = int(kind[2:])
                        g_ = gst[it % 2]
                        c.V(lambda e: e.tensor_tensor(out=gpre.a, in0=p_.a, in1=bmg.a[:, m * 512:(m + 1) * 512], op=ALU.add), [p_, bmg], [gpre])
                        yield
                        c.A(lambda e: e.activation(out=g_.a, in_=gpre.a, func=AF.Sigmoid), [gpre], [g_])
                        yield
                        c.dma("sp", GATES.a[tok, m * 512:(m + 1) * 512], g_.a, GATES, g_)
                        yield

                def wload(ci):
                    c0, cw, kind = chunks[ci]
                    w_ = wc[ci % 2]
                    c.dma("pool", w_.a[:, :, :cw], w_in.a[l, :, c0:c0 + cw].rearrange("(k p) n -> p k n", p=128), w_, w_in)

                items = [(ci, i) for ci in range(len(chunks)) for i in range(NT)]
                wload(0)
                loaded = 1

                def mm_of(n):
                    ci, i = items[n]
                    return mmgen(i, n, chunks[ci][1], wc[ci % 2])

                rr(mm_of(0), mm_of(1))
                for n in range(0, len(items), 2):
                    ci_next = items[min(n + 3, len(items) - 1)][0]
                    while loaded <= min(ci_next + 1, len(chunks) - 1):
                        wload(loaded)
                        loaded += 1
                    gens = [ptile(items[n][1], n, chunks[items[n][0]][2]), ptile(items[n + 1][1], n + 1, chunks[items[n + 1][0]][2])]
                    for m_ in (n + 2, n + 3):
                        if m_ < len(items):
                            gens.append(mm_of(m_))
                    rr(*gens)
                c.pop()
                if stop_after == "p1":
                    break

                c.push()
                kat = c.sb("kat", [128, T], BF16, grp="ka"); va = c.sb("va", [128, NT, 2, 65], BF16, grp="ka")
                c.dma("sp", kat.a, KAT.a, kat, KAT)
                c.dma("sp", va.a, VA.a.rearrange("(i p) g d -> p i g d", p=128), va, VA)
                qa = [[c.sb("qa%d_%d" % (i, g), [128, 4, 128], BF16, grp="qa%d" % i) for g in range(2)] for i in range(2)]
                for i in range(2):
                    for g in range(2):
                        c.V(lambda e: e.memset(qa[i][g].a, 0.0), [], [qa[i][g]])
                ND = 2
                pS = [c.ps("pS%d" % i, [128, 1024], F32) for i in range(ND)]
                pe_ = [c.sb("pe%d" % i, [128, 1024], BF16) for i in range(3)]
                pO = [c.ps("pO%d" % i, [128, 512], F32) for i in range(2)]
                pbc = c.ps("pbc", [128, 512], F32)
                ones_f = c.sb("ones_f", [128, 64], F32)
                c.V(lambda e: e.memset(ones_f.a, 1.0), [], [ones_f])
                dn = [c.sb("dn%d" % i, [128, 512], F32) for i in range(2)]
                bcs = [c.sb("bcs%d" % i, [64, 512], F32) for i in range(2)]
                yTa = [c.sb("yTa%d" % i, [64, 512], BF16) for i in range(2)]
                steps = []
                for i in range(first_tile, NT):
                    kts = list(range(0, 2)) if i < 2 else list(range(0, NT))
                    prs = [kts[j:j + 2] for j in range(0, len(kts), 2)]
                    for g in range(2):
                        for pi, pr in enumerate(prs):
                            steps.append((i, g, pr, pi == 0, pi == len(prs) - 1))

                def a_s1(si):
                    i, g, pr, first, last = steps[si]
                    q_ = qa[i % 2][g]
                    if g == 0 and first:
                        for g2 in range(2):
                            c.dma("sp", qa[i % 2][g2].a[g2 * 64:(g2 + 1) * 64], QAT.a[g2 * 64:(g2 + 1) * 64, :, i * 128:(i + 1) * 128], qa[i % 2][g2], QAT)
                            yield
                    ps_ = pS[si % ND]; e_ = pe_[si % 3]
                    for j, kt in enumerate(pr):
                        c.mm(lambda e: e.matmul(ps_.a[:, j * 512:(j + 1) * 512], lhsT=kat.a[:, kt * 128:(kt + 1) * 128], rhs=q_.a.rearrange("p m t -> p (m t)"), start=True, stop=True), [kat, q_], [ps_], last=(j == len(pr) - 1))
                        yield
                    c.A(lambda e: e.activation(out=e_.a, in_=ps_.a, func=AF.Exp, scale=0.125), [ps_], [e_])
                    yield

                def a_s2(si):
                    i, g, pr, first, last = steps[si]
                    e_ = pe_[si % 3]
                    po = pO[g]
                    for j, kt in enumerate(pr):
                        c.mm(lambda e: e.matmul(po.a[0:65, :], lhsT=va.a[:, kt, g, :], rhs=e_.a[:, j * 512:(j + 1) * 512], start=(first and j == 0), stop=(last and j == len(pr) - 1)), [e_, va], [po], last=(j == len(pr) - 1))
                        yield
                    if not last:
                        return
                    d_ = dn[g]; b_ = bcs[g]; y_ = yTa[g]
                    c.A(lambda e: e.copy(out=d_.a[64:65, :], in_=po.a[64:65, :]), [po], [d_])
                    yield
                    c.V(lambda e: e.reciprocal(out=d_.a[64:65, :], in_=d_.a[64:65, :]), [d_], [d_])
                    yield
                    c.mm(lambda e: e.matmul(pbc.a[0:64, :], lhsT=ones_f.a[64:65, :], rhs=d_.a[64:65, :], start=True, stop=True), [ones_f, d_], [pbc])
                    yield
                    c.A(lambda e: e.copy(out=b_.a, in_=pbc.a[0:64, :]), [pbc], [b_])
                    yield
                    c.V(lambda e: e.tensor_tensor(out=y_.a, in0=po.a[0:64, :], in1=b_.a, op=ALU.mult), [po, b_], [y_])
                    yield
                    for m in range(4):
                        c.dma("sp", YAT.a[g * 2 + m // 2, (m % 2) * 64:(m % 2) * 64 + 64, i * 128:(i + 1) * 128], y_.a[:, m * 128:(m + 1) * 128], YAT, y_)
                        yield

                ns_ = len(steps)
                rr(a_s1(0))
                if ns_ > 1:
                    rr(a_s1(1))
                for si in range(ns_):
                    if si + 2 < ns_:
                        rr(a_s1(si + 2))
                    rr(a_s2(si))
                c.pop()
                if stop_after == "p2a":
                    break

                c.push()
                kbt = c.sb("kbt", [128, 4, T], BF16, grp="kb"); vb = c.sb("vb", [128, NT, 8, 65], BF16, grp="kb")
                c.dma("sp", kbt.a, KBT.a, kbt, KBT)
                c.dma("sp", vb.a, VB.a.rearrange("(i p) h d -> p i h d", p=128), vb, VB)
                TP = c.sb("TP", [128, 8, 15, 64], F32, grp="kb")
                c.dma("sp", TP.a, TPD.a, TP, TPD)
                tabI = c.sb("tabI", [128, 5, 8, 128], F32); tabE = c.sb("tabE", [128, 5, 8, 128], F32)

                def build_tab(tab, plan):
                    for di, (kt, blocks) in enumerate(plan):
                        for (a, b2), dr in blocks.items():
                            o = tab.a[a * 64:(a + 1) * 64, di, :, b2 * 64:(b2 + 1) * 64]
                            if dr is None:
                                c.G(lambda e: e.memset(o, NEG), [], [tab])
                            else:
                                c.V(lambda e: e.tensor_copy(out=o, in_=TP.a[a * 64:(a + 1) * 64, :, dr, :]), [TP], [tab])

                build_tab(tabI, na_plan(5))
                qz = [[c.sb("qz%d_%d" % (i, p), [128, 4, 128], BF16, grp="qz%d" % i) for p in range(2)] for i in range(2)]
                for i in range(2):
                    for p in range(2):
                        c.V(lambda e: e.memset(qz[i][p].a, 0.0), [], [qz[i][p]])
                pS = [c.ps("pSb%d" % i, [128, 8, 128], F32) for i in range(2)]
                sS = c.sb("sS", [128, 8, 128], F32)
                pe_ = [c.sb("peb%d" % i, [128, 8, 128], BF16) for i in range(2)]
                pOb = c.ps("pOb", [128, 2, 512], F32)
                pO3 = [pOb.a[:, hh, 0:260].rearrange("p (m d) -> p m d", d=65) for hh in range(2)]
                rd = c.sb("rdb", [128, 4], F32)
                ya = [c.sb("yb%d" % i, [128, 512], BF16) for i in range(2)]
                pY = c.ps("pYb", [128, 4, 128], BF16); yT = c.sb("yTb", [128, 4, 128], BF16)
                steps = []
                for i in range(first_tile, NT):
                    keys = [(0, None, None), (1, None, None)]
                    plan = None
                    if i >= 2:
                        j = i - 2
                        plan = na_plan(j)
                        tab = tabI if 2 <= j <= 29 else tabE
                        keys = [(kt + 2, tab, di) for di, (kt, _) in enumerate(plan)] + keys
                    for ki, (kt, tab, di) in enumerate(keys):
                        steps.append((i, ki, kt, tab, di, len(keys), plan))

                def b_s1(si):
                    i, ki, kt, tab, di, nk, plan = steps[si]
                    qz_ = qz[i % 2]
                    if ki == 0:
                        for p in range(2):
                            c.dma("sp", qz_[p].a[p * 64:(p + 1) * 64], QBT.a[p * 64:(p + 1) * 64, :, i * 128:(i + 1) * 128], qz_[p], QBT)
                        if tab is tabE:
                            build_tab(tabE, plan)
                    ps_ = pS[si % 2]; e_ = pe_[si % 2]
                    for h in range(8):
                        par = h % 2; pr = h // 2
                        c.mm(lambda e: e.matmul(ps_.a[:, h, :], lhsT=kbt.a[:, pr, kt * 128:(kt + 1) * 128], rhs=qz_[par].a[:, pr, :], start=True, stop=True), [kbt, qz_[par]], [ps_], last=(h == 7))
                    if tab is not None:
                        c.V(lambda e: e.scalar_tensor_tensor(out=sS.a, in0=ps_.a, scalar=0.125, in1=tab.a[:, di], op0=ALU.mult, op1=ALU.add), [ps_, tab], [sS])
                        c.A(lambda e: e.activation(out=e_.a, in_=sS.a, func=AF.Exp), [sS], [e_])
                    else:
                        c.A(lambda e: e.activation(out=e_.a, in_=ps_.a, func=AF.Exp, scale=0.125), [ps_], [e_])

                def b_s2(si):
                    i, ki, kt, tab, di, nk, plan = steps[si]
                    e_ = pe_[si % 2]; y_ = ya[i % 2]
                    for h in range(8):
                        c.mm(lambda e: e.matmul(pO3[h // 4][:, h % 4, :], lhsT=e_.a[:, h, :], rhs=vb.a[:, kt, h, :], start=(ki == 0 and h % 4 == 0), stop=(ki == nk - 1)), [e_, vb], [pOb], last=(h == 7))
                    if ki != nk - 1:
                        return
                    for hh in range(2):
                        po3 = pO3[hh]
                        c.V(lambda e: e.reciprocal(out=rd.a, in_=po3[:, :, 64]), [pOb], [rd])
                        c.V(lambda e: e.tensor_tensor(out=y_.a[:, hh * 256:(hh + 1) * 256].rearrange("p (m d) -> p m d", d=64), in0=po3[:, :, 0:64], in1=rd.a.unsqueeze(2).to_broadcast([128, 4, 64]), op=ALU.mult), [pOb, rd], [y_])
                    for m in range(4):
                        c.mm(lambda e: e.transpose(out=pY.a[:, m, :], in_=y_.a[:, m * 128:(m + 1) * 128], identity=ident.a), [y_, ident], [pY], last=(m == 3))
                    c.A(lambda e: e.copy(out=yT.a, in_=pY.a), [pY], [yT])
                    c.dma("sp", YBT.a[:, :, i * 128:(i + 1) * 128].rearrange("m p t -> p m t"), yT.a, YBT, yT)

                pipeline(len(steps), b_s1, b_s2, 2)
                c.pop()
                if stop_after == "p2b":
                    break

                c.push()
                Cst = c.sb("Cst", [128, 4, 129], F32)
                Cb = [c.sb("Cb%d" % i, [128, 4, 129], BF16) for i in range(2)]
                mq = [c.sb("mq%d" % i, [128, 4, 128], BF16, grp="m%d" % i) for i in range(3)]
                mk = [c.sb("mk%d" % i, [128, 4, 128], BF16, grp="m%d" % i) for i in range(3)]
                mkt = [c.sb("mkt%d" % i, [128, 512], BF16, grp="m%d" % i) for i in range(3)]
                mv = [c.sb("mv%d" % i, [128, 4, 129], BF16, grp="m%d" % i) for i in range(3)]
                mg = [c.sb("mg%d" % i, [128, 16], F32, grp="m%d" % i) for i in range(3)]
                mo = [c.sb("mo%d" % i, [128, 512], BF16, grp="m%d" % i) for i in range(3)]
                hs = [c.sb("hs%d" % i, [128, 512], F32, grp="m%d" % i) for i in range(3)]
                pb = c.ps("pb", [128, 4], F32); pbB = c.ps("pbB", [128, 4, 128], F32); pST = c.ps("pST", [128, 4, 128], F32)
                pH = c.ps("pH", [128, 2, 512], F32); pC = c.ps("pC", [128, 2, 512], F32)
                pY = c.ps("pYc", [128, 4, 128], BF16)
                biasc = [c.sb("biasc%d" % i, [128, 4], F32) for i in range(3)]
                gcol = [c.sb("gcol%d" % i, [128, 4], F32) for i in range(3)]
                ebend = [c.sb("ebend%d" % i, [128, 4], F32) for i in range(3)]
                EB_L = [c.sb("EB%d" % i_, [128, 4, 128], F32) for i_ in range(2)]; EB = EB_L[0]
                qs = [c.sb("qs%d" % i, [128, 4, 128], BF16) for i in range(3)]
                DT_L = [c.sb("DT%d" % i_, [128, 4, 128], F32) for i_ in range(2)]; DT = DT_L[0]; DTm_L = [c.sb("DTm%d" % i_, [128, 4, 128], F32) for i_ in range(2)]; DTm = DTm_L[0]
                SD = [c.sb("SD%d" % i, [128, 4, 128], BF16) for i in range(3)]
                kg_L = [c.sb("kg%d" % i_, [128, 4, 128], BF16) for i_ in range(2)]; kg = kg_L[0]
                pCs = [c.sb("pCs%d" % i, [128, 2, 258], F32) for i in range(3)]
                den_L = [c.sb("den%d" % i_, [128, 4], F32) for i_ in range(2)]; den = den_L[0]; hout = [c.sb("hout%d" % i, [128, 512], F32) for i in range(2)]
                hsq_L = [c.sb("hsq%d" % i_, [128, 512], F32) for i_ in range(2)]; hsq = hsq_L[0]; hss_L = [c.sb("hss%d" % i_, [128, 4], F32) for i_ in range(2)]; hss = hss_L[0]; hn_L = [c.sb("hn%d" % i_, [128, 512], F32) for i_ in range(2)]; hn = hn_L[0]
                yc_L = [c.sb("yc%d" % i_, [128, 512], BF16) for i_ in range(2)]; yc = yc_L[0]; yT_L = [c.sb("yTc%d" % i_, [128, 4, 128], BF16) for i_ in range(2)]; yT = yT_L[0]

                def hreg(pt, h):
                    return pt.a[:, h // 2, (h % 2) * 129:(h % 2) * 129 + 129]

                def sreg(pt, h):
                    return pt.a[:, h // 2, (h % 2) * 129:(h % 2) * 129 + 129]

                for d in range(2):
                    order = list(range(NT)) if d == 0 else [1, 0] + list(range(NT - 1, 1, -1))
                    tend = 127 if d == 0 else 0
                    Ud = U.a[:, d, :]
                    c.V(lambda e: e.memset(Cst.a, 0.0), [], [Cst])
                    c.V(lambda e: e.memset(Cb[1].a, 0.0), [], [Cb[1]])

                    def l_s1(n):
                        kg_c = kg_L[n % 2]; EB_c = EB_L[n % 2]; DT_c = DT_L[n % 2]; DTm_c = DTm_L[n % 2]
                        i = order[n]
                        q_ = mq[n % 3]; k_ = mk[n % 3]; kt_ = mkt[n % 3]; v_ = mv[n % 3]; g_ = mg[n % 3]
                        bc_ = biasc[n % 3]; gc_ = gcol[n % 3]; eb_ = ebend[n % 3]; qs_ = qs[n % 3]; SD_ = SD[n % 3]; pcs = pCs[n % 3]
                        tok = slice(i * 128, (i + 1) * 128)
                        need_out = i >= first_tile
                        c.dma("sp", q_.a, MQT.a[:, :, tok], q_, MQT)
                        yield
                        c.dma("sp", k_.a, MKT.a[:, :, tok], k_, MKT)
                        yield
                        c.dma("sp", kt_.a, MK.a[tok], kt_, MK)
                        yield
                        c.dma("sp", v_.a, MV.a[tok], v_, MV)
                        yield
                        c.dma("sp", g_.a, MG.a[tok], g_, MG)
                        yield
                        if need_out and d == 1:
                            c.dma("sp", hs[n % 3].a, HS.a[tok], hs[n % 3], HS)
                            yield
                            c.dma("sp", mo[n % 3].a, MO.a[tok], mo[n % 3], MO)
                            yield
                        gv = g_.a.rearrange("p (d t h) -> p d t h", d=2, t=2)
                        ig = gv[:, d, 0, :]; lf = gv[:, d, 1, :]
                        c.mm(lambda e: e.matmul(pb.a, lhsT=Ud, rhs=lf, start=True, stop=True), [U, g_], [pb])
                        yield
                        for h in range(4):
                            c.mm(lambda e: e.matmul(pbB.a[:, h, :], lhsT=gv[:, d, 1, h:h + 1].to_broadcast([128, 128]), rhs=Ud, start=True, stop=True), [g_, U], [pbB], last=(h == 3))
                            yield
                        if need_out:
                            for h in range(4):
                                c.mm(lambda e: e.matmul(pST.a[:, h, :], lhsT=k_.a[:, h, :], rhs=q_.a[:, h, :], start=True, stop=True), [k_, q_], [pST], last=(h == 3))
                                yield
                        c.V(lambda e: e.tensor_tensor(out=bc_.a, in0=ig, in1=pb.a, op=ALU.subtract), [g_, pb], [bc_])
                        yield
                        c.V(lambda e: e.tensor_tensor(out=gc_.a, in0=pbB.a[:, :, tend], in1=bc_.a, op=ALU.add), [pbB, bc_], [gc_])
                        yield
                        c.A(lambda e: e.activation(out=gc_.a, in_=gc_.a, func=AF.Exp), [gc_], [gc_])
                        yield
                        c.A(lambda e: e.activation(out=eb_.a, in_=pbB.a[:, :, tend], func=AF.Exp), [pbB], [eb_])
                        yield
                        c.V(lambda e: e.tensor_tensor(out=kg_c.a, in0=kt_.a.rearrange("p (h d) -> p h d", d=128), in1=gc_.a.unsqueeze(2).to_broadcast([128, 4, 128]), op=ALU.mult), [kt_, gc_], [kg_c])
                        yield
                        for h in range(4):
                            c.mm(lambda e: e.matmul(hreg(pC, h), lhsT=kg_c.a[:, h, :], rhs=v_.a[:, h, :], start=True, stop=True), [kg_c, v_], [pC], last=(h == 3))
                            yield
                        for hh in range(2):
                            c.A(lambda e: e.copy(out=pcs.a[:, hh, :], in_=pC.a[:, hh, 0:258]), [pC], [pcs])
                            yield
                        if need_out:
                            c.A(lambda e: e.activation(out=EB_c.a, in_=pbB.a, func=AF.Exp), [pbB], [EB_c])
                            yield
                            c.V(lambda e: e.tensor_tensor(out=qs_.a, in0=q_.a, in1=EB_c.a, op=ALU.mult), [q_, EB_c], [qs_])
                            yield
                            for h in range(4):
                                c.A(lambda e: e.activation(out=DT_c.a[:, h, :], in_=pbB.a[:, h, :], func=AF.Exp, bias=bc_.a[:, h:h + 1]), [pbB, bc_], [DT_c])
                                yield
                            c.G(lambda e: e.tensor_tensor(out=DTm_c.a, in0=DT_c.a, in1=U.a[:, d:d + 1, :].to_broadcast([128, 4, 128]), op=ALU.mult), [DT_c, U], [DTm_c])
                            yield
                            c.V(lambda e: e.tensor_tensor(out=SD_.a, in0=DTm_c.a, in1=pST.a, op=ALU.mult), [DTm_c, pST], [SD_])
                            yield

                    def l_s2(n):
                        den_c = den_L[n % 2]; hsq_c = hsq_L[n % 2]; hss_c = hss_L[n % 2]; hn_c = hn_L[n % 2]; yc_c = yc_L[n % 2]; yT_c = yT_L[n % 2]
                        i = order[n]
                        v_ = mv[n % 3]; eb_ = ebend[n % 3]; qs_ = qs[n % 3]; SD_ = SD[n % 3]; pcs = pCs[n % 3]
                        cb_old = Cb[(n + 1) % 2]; cb_new = Cb[n % 2]
                        ho = hout[n % 2]
                        tok = slice(i * 128, (i + 1) * 128)
                        need_out = i >= first_tile
                        if need_out:
                            for h in range(4):
                                c.mm(lambda e: e.matmul(hreg(pH, h), lhsT=qs_.a[:, h, :], rhs=cb_old.a[:, h, :], start=True, stop=False), [qs_, cb_old], [pH], last=False)
                                yield
                                c.mm(lambda e: e.matmul(hreg(pH, h), lhsT=SD_.a[:, h, :], rhs=v_.a[:, h, :], start=False, stop=True), [SD_, v_], [pH], last=(h == 3))
                                yield
                        for h in range(4):
                            c.V(lambda e: e.scalar_tensor_tensor(out=Cst.a[:, h, :], in0=Cst.a[:, h, :], scalar=eb_.a[:, h:h + 1], in1=sreg(pcs, h), op0=ALU.mult, op1=ALU.add), [Cst, eb_, pcs], [Cst])
                            yield
                        c.A(lambda e: e.copy(out=cb_new.a, in_=Cst.a), [Cst], [cb_new])
                        yield
                        if not need_out:
                            return
                        for h in range(4):
                            c.A(lambda e: e.activation(out=den_c.a[:, h:h + 1], in_=hreg(pH, h)[:, 128:129], func=AF.Abs), [pH], [den_c])
                            yield
                        c.V(lambda e: e.tensor_scalar_max(out=den_c.a, in0=den_c.a, scalar1=1.0), [den_c], [den_c])
                        yield
                        c.V(lambda e: e.reciprocal(out=den_c.a, in_=den_c.a), [den_c], [den_c])
                        yield
                        for h in range(4):
                            c.V(lambda e: e.tensor_scalar(out=ho.a[:, h * 128:(h + 1) * 128], in0=hreg(pH, h)[:, 0:128], scalar1=den_c.a[:, h:h + 1], scalar2=None, op0=ALU.mult), [pH, den_c], [ho])
                            yield
                        if d == 0:
                            c.dma("sp", HS.a[tok], ho.a, HS, ho)
                            yield
                            return
                        h_ = hs[n % 3]; o_ = mo[n % 3]
                        c.G(lambda e: e.tensor_tensor(out=ho.a, in0=ho.a, in1=h_.a, op=ALU.add), [ho, h_], [ho])
                        yield
                        c.A(lambda e: e.activation(out=hsq_c.a, in_=ho.a, func=AF.Square), [ho], [hsq_c])
                        yield
                        c.V(lambda e: e.tensor_reduce(out=hss_c.a, in_=hsq_c.a.rearrange("p (h d) -> p h d", d=128), axis=AX.X, op=ALU.add), [hsq_c], [hss_c])
                        yield
                        c.V(lambda e: e.tensor_scalar(out=hss_c.a, in0=hss_c.a, scalar1=1.0 / 128, scalar2=1e-6, op0=ALU.mult, op1=ALU.add), [hss_c], [hss_c])
                        yield
                        c.A(lambda e: e.activation(out=hss_c.a, in_=hss_c.a, func=AF.Sqrt), [hss_c], [hss_c])
                        yield
                        c.V(lambda e: e.reciprocal(out=hss_c.a, in_=hss_c.a), [hss_c], [hss_c])
                        yield
                        c.V(lambda e: e.tensor_tensor(out=hn_c.a.rearrange("p (h d) -> p h d", d=128), in0=ho.a.rearrange("p (h d) -> p h d", d=128), in1=hss_c.a.unsqueeze(2).to_broadcast([128, 4, 128]), op=ALU.mult), [ho, hss_c], [hn_c])
                        yield
                        c.G(lambda e: e.tensor_tensor(out=hn_c.a, in0=hn_c.a, in1=gml.a, op=ALU.mult), [hn_c, gml], [hn_c])
                        yield
                        c.V(lambda e: e.tensor_tensor(out=yc_c.a, in0=hn_c.a, in1=o_.a, op=ALU.mult), [hn_c, o_], [yc_c])
                        yield
                        for m in range(4):
                            c.mm(lambda e: e.transpose(out=pY.a[:, m, :], in_=yc_c.a[:, m * 128:(m + 1) * 128], identity=ident.a), [yc_c, ident], [pY], last=(m == 3))
                            yield
                        c.A(lambda e: e.copy(out=yT_c.a, in_=pY.a), [pY], [yT_c])
                        yield
                        c.dma("sp", YCT.a[:, :, tok].rearrange("m p t -> p m t"), yT_c.a, YCT, yT_c)
                        yield

                    rr(l_s1(0))
                    if NT > 1:
                        rr(l_s1(1))
                    for n in range(NT):
                        rr(l_s2(n), l_s1(n + 2) if n + 2 < NT else None)
                c.pop()
                if stop_after == "p2c":
                    break

                c.push()
                wbr = c.sb("wbr", [128, 3, 4, D], BF16, grp="w3"); wo = c.sb("wo", [128, 8, D], BF16, grp="w3")
                for br in range(3):
                    c.dma("pool", wbr.a[:, br], w_branch.a[l, br].rearrange("(k p) n -> p k n", p=128), wbr, w_branch)
                c.dma("pool", wo.a, w_out.a[l].rearrange("(k p) n -> p k n", p=128), wo, w_out)
                ybr = [c.sb("ybr%d" % i, [128, 3, 4, 128], BF16, grp="l3%d" % i) for i in range(2)]
                gt_ = [c.sb("gt%d" % i, [128, 3072], BF16, grp="l3%d" % i) for i in range(2)]
                xt = [c.sb("x3%d" % i, [128, D], F32, grp="l3%d" % i) for i in range(2)]
                pB = [c.ps("pB%d" % i, [128, 512], F32) for i in range(2)]
                mrg_L = [c.sb("mrg%d" % i_, [128, D], F32) for i_ in range(2)]; mrg = mrg_L[0]; mtmp_L = [c.sb("mtmp%d" % i_, [128, 512], F32) for i_ in range(2)]; mtmp = mtmp_L[0]; mrb_L = [c.sb("mrb%d" % i_, [128, D], BF16) for i_ in range(2)]; mrb = mrb_L[0]
                pT = c.ps("pT3", [128, 8, 128], BF16); mT_L = [c.sb("mT%d" % i_, [128, 8, 128], BF16) for i_ in range(2)]; mT = mT_L[0]
                pYo = c.ps("pYo", [128, D], F32)
                x1 = [c.sb("x1%d" % i, [128, D], F32) for i in range(2)]
                junk_L = [c.sb("junk3%d" % i_, [128, D], F32) for i_ in range(2)]; junk = junk_L[0]; ss_L = [c.sb("ss3%d" % i_, [128, 1], F32) for i_ in range(2)]; ss = ss_L[0]; rstd_L = [c.sb("rstd3%d" % i_, [128, 1], F32) for i_ in range(2)]; rstd = rstd_L[0]
                xnf_L = [c.sb("xnf%d" % i_, [128, D], F32) for i_ in range(2)]; xnf = xnf_L[0]
                pTf = c.ps("pTf", [128, 8, 128], F32)
                h2f_L = [c.sb("h2f%d" % i_, [128, 8, 128], F32) for i_ in range(2)]; h2f = h2f_L[0]; h2b = [c.sb("h2b%d" % i, [128, 8, 128], BF16) for i in range(2)]
                pL = c.ps("pL", [128, 36], F32)
                lg_L = [c.sb("lg%d" % i_, [128, 36], F32) for i_ in range(2)]; lg = lg_L[0]; gmax_L = [c.sb("gmax%d" % i_, [128, 1], F32) for i_ in range(2)]; gmax = gmax_L[0]; ngmax_L = [c.sb("ngmax%d" % i_, [128, 1], F32) for i_ in range(2)]; ngmax = ngmax_L[0]
                eg_L = [c.sb("eg%d" % i_, [128, 4], F32) for i_ in range(2)]; eg = eg_L[0]; sg_L = [c.sb("sg%d" % i_, [128, 1], F32) for i_ in range(2)]; sg = sg_L[0]; ohg_L = [c.sb("ohg%d" % i_, [128, 4], F32) for i_ in range(2)]; ohg = ohg_L[0]
                lem_L = [c.sb("lem%d" % i_, [128, 4, 8], F32) for i_ in range(2)]; lem = lem_L[0]; les_L = [c.sb("les%d" % i_, [128, 8], F32) for i_ in range(2)]; les = les_L[0]; m8_L = [c.sb("m8%d" % i_, [128, 8], F32) for i_ in range(2)]; m8 = m8_L[0]
                nv0_L = [c.sb("nv0%d" % i_, [128, 1], F32) for i_ in range(2)]; nv0 = nv0_L[0]; e8_L = [c.sb("e8%d" % i_, [128, 8], F32) for i_ in range(2)]; e8 = e8_L[0]; mk2_L = [c.sb("mk2%d" % i_, [128, 8], F32) for i_ in range(2)]; mk2 = mk2_L[0]
                w8_L = [c.sb("w8%d" % i_, [128, 8], F32) for i_ in range(2)]; w8 = w8_L[0]; sden_L = [c.sb("sden%d" % i_, [128, 1], F32) for i_ in range(2)]; sden = sden_L[0]
                dw = [c.sb("dw%d" % i, [128, 4, 8], F32) for i in range(2)]
                def genA(i):
                    tok = slice(i * 128, (i + 1) * 128)
                    j3 = 2 if i < 2 else b
                    mrg = mrg_L[i % 2]; mtmp = mtmp_L[i % 2]; mrb = mrb_L[i % 2]; mT = mT_L[i % 2]; junk = junk_L[i % 2]; ss = ss_L[i % 2]; rstd = rstd_L[i % 2]; xnf = xnf_L[i % 2]; h2f = h2f_L[i % 2]; lg = lg_L[i % 2]; gmax = gmax_L[i % 2]; ngmax = ngmax_L[i % 2]; eg = eg_L[i % 2]; sg = sg_L[i % 2]; ohg = ohg_L[i % 2]; lem = lem_L[i % 2]; les = les_L[i % 2]; m8 = m8_L[i % 2]; nv0 = nv0_L[i % 2]; e8 = e8_L[i % 2]; mk2 = mk2_L[i % 2]; w8 = w8_L[i % 2]; sden = sden_L[i % 2]
                    y_ = ybr[i % 2]; g_ = gt_[i % 2]; x_ = xt[i % 2]; xo = x1[i % 2]; hb = h2b[i % 2]; dw_ = dw[i % 2]
                    for br, YT_ in enumerate((YAT, YBT, YCT)):
                        c.dma("sp", y_.a[:, br], YT_.a[:, :, tok].rearrange("m p t -> p m t"), y_, YT_)
                        yield
                    c.dma("sp", g_.a, GATES.a[tok], g_, GATES)
                    yield
                    src, ap = tile_src(l, b, i)
                    c.dma("sp", x_.a, ap, x_, src)
                    yield
                    for half in range(2):
                        cs = slice(half * 512, (half + 1) * 512)
                        for br in range(3):
                            p_ = pB[(half * 3 + br) % 2]
                            for k in range(4):
                                c.mm(lambda e: e.matmul(p_.a, lhsT=y_.a[:, br, k, :], rhs=wbr.a[:, br, k, cs], start=(k == 0), stop=(k == 3)), [y_, wbr], [p_], last=(k == 3))
                                yield
                            gsl = g_.a[:, br * 1024 + half * 512: br * 1024 + (half + 1) * 512]
                            if br == 0:
                                c.V(lambda e: e.tensor_tensor(out=mrg.a[:, cs], in0=p_.a, in1=gsl, op=ALU.mult), [p_, g_], [mrg])
                                yield
                            else:
                                c.V(lambda e: e.tensor_tensor(out=mtmp.a, in0=p_.a, in1=gsl, op=ALU.mult), [p_, g_], [mtmp])
                                yield
                                c.G(lambda e: e.tensor_tensor(out=mrg.a[:, cs], in0=mrg.a[:, cs], in1=mtmp.a, op=ALU.add), [mrg, mtmp], [mrg])
                                yield
                    c.A(lambda e: e.copy(out=mrb.a, in_=mrg.a), [mrg], [mrb])
                    yield
                    for k in range(8):
                        c.mm(lambda e: e.transpose(out=pT.a[:, k, :], in_=mrb.a[:, k * 128:(k + 1) * 128], identity=ident.a), [mrb, ident], [pT], last=(k == 7))
                        yield
                    c.A(lambda e: e.copy(out=mT.a, in_=pT.a), [pT], [mT])
                    yield
                    for half in range(2):
                        cs = slice(half * 512, (half + 1) * 512)
                        for k in range(8):
                            c.mm(lambda e: e.matmul(pYo.a[:, cs], lhsT=mT.a[:, k, :], rhs=wo.a[:, k, cs], start=(k == 0), stop=(k == 7)), [mT, wo], [pYo], last=(k == 7 and half == 1))
                            yield
                    c.V(lambda e: e.tensor_tensor(out=xo.a, in0=pYo.a, in1=GT.a[:, 0, j3, :], op=ALU.mult), [pYo, GT], [xo])
                    yield
                    c.G(lambda e: e.tensor_tensor(out=xo.a, in0=xo.a, in1=x_.a, op=ALU.add), [xo, x_], [xo])
                    yield
                    c.dma("sp", XS.a[b, tok, :], xo.a, XS, xo)
                    yield
                def genB(i):
                    tok = slice(i * 128, (i + 1) * 128)
                    j3 = 2 if i < 2 else b
                    mrg = mrg_L[i % 2]; mtmp = mtmp_L[i % 2]; mrb = mrb_L[i % 2]; mT = mT_L[i % 2]; junk = junk_L[i % 2]; ss = ss_L[i % 2]; rstd = rstd_L[i % 2]; xnf = xnf_L[i % 2]; h2f = h2f_L[i % 2]; lg = lg_L[i % 2]; gmax = gmax_L[i % 2]; ngmax = ngmax_L[i % 2]; eg = eg_L[i % 2]; sg = sg_L[i % 2]; ohg = ohg_L[i % 2]; lem = lem_L[i % 2]; les = les_L[i % 2]; m8 = m8_L[i % 2]; nv0 = nv0_L[i % 2]; e8 = e8_L[i % 2]; mk2 = mk2_L[i % 2]; w8 = w8_L[i % 2]; sden = sden_L[i % 2]
                    y_ = ybr[i % 2]; g_ = gt_[i % 2]; x_ = xt[i % 2]; xo = x1[i % 2]; hb = h2b[i % 2]; dw_ = dw[i % 2]
                    c.V(lambda e: e.memset(ss.a, 0.0), [], [ss])
                    yield
                    c.A(lambda e: e.activation(out=junk.a, in_=xo.a, func=AF.Square, accum_out=ss.a), [xo], [junk, ss])
                    yield
                    c.V(lambda e: e.tensor_scalar(out=rstd.a, in0=ss.a, scalar1=1.0 / D, scalar2=1e-6, op0=ALU.mult, op1=ALU.add), [ss], [rstd])
                    yield
                    c.A(lambda e: e.activation(out=rstd.a, in_=rstd.a, func=AF.Sqrt), [rstd], [rstd])
                    yield
                    c.V(lambda e: e.reciprocal(out=rstd.a, in_=rstd.a), [rstd], [rstd])
                    yield
                    c.V(lambda e: e.tensor_scalar(out=xnf.a, in0=xo.a, scalar1=rstd.a[:, 0:1], scalar2=None, op0=ALU.mult), [xo, rstd], [xnf])
                    yield
                    for k in range(8):
                        c.mm(lambda e: e.transpose(out=pTf.a[:, k, :], in_=xnf.a[:, k * 128:(k + 1) * 128], identity=ident_f.a), [xnf, ident_f], [pTf], last=(k == 7))
                        yield
                    for k in range(8):
                        c.V(lambda e: e.tensor_scalar(out=h2f.a[:, k, :], in0=pTf.a[:, k, :], scalar1=G2.a[:, k, j3:j3 + 1], scalar2=modT.a[:, 3, k, j3:j3 + 1], op0=ALU.mult, op1=ALU.add), [pTf, G2, modT], [h2f])
                        yield
                    c.A(lambda e: e.copy(out=hb.a, in_=h2f.a), [h2f], [hb])
                    yield
                    c.dma("sp", H2T.a[:, :, tok], hb.a, H2T, hb)
                    yield
                    for k in range(8):
                        c.mm(lambda e: e.matmul(pL.a, lhsT=h2f.a[:, k, :], rhs=wrt.a[:, k, :], start=(k == 0), stop=(k == 7)), [h2f, wrt], [pL], last=(k == 7))
                        yield
                    c.V(lambda e: e.tensor_tensor(out=lg.a, in0=pL.a, in1=brt.a, op=ALU.add), [pL, brt], [lg])
                    yield
                    c.V(lambda e: e.tensor_reduce(out=gmax.a, in_=lg.a[:, 0:4], axis=AX.X, op=ALU.max), [lg], [gmax])
                    yield
                    c.V(lambda e: e.tensor_scalar(out=ngmax.a, in0=gmax.a, scalar1=-1.0, scalar2=None, op0=ALU.mult), [gmax], [ngmax])
                    yield
                    c.V(lambda e: e.memset(sg.a, 0.0), [], [sg])
                    yield
                    c.A(lambda e: e.activation(out=eg.a, in_=lg.a[:, 0:4], func=AF.Exp, bias=ngmax.a[:, 0:1], accum_out=sg.a), [lg, ngmax], [eg, sg])
                    yield
                    c.V(lambda e: e.tensor_scalar(out=ohg.a, in0=lg.a[:, 0:4], scalar1=gmax.a[:, 0:1], scalar2=None, op0=ALU.is_ge), [lg, gmax], [ohg])
                    yield
                    c.V(lambda e: e.tensor_tensor(out=lem.a, in0=lg.a[:, 4:36].rearrange("p (g e) -> p g e", e=8), in1=ohg.a.unsqueeze(2).to_broadcast([128, 4, 8]), op=ALU.mult), [lg, ohg], [lem])
                    yield
                    c.V(lambda e: e.tensor_reduce(out=les.a, in_=lem.a.rearrange("p g e -> p e g"), axis=AX.X, op=ALU.add), [lem], [les])
                    yield
                    c.V(lambda e: e.max(out=m8.a, in_=les.a), [les], [m8])
                    yield
                    c.V(lambda e: e.tensor_scalar(out=nv0.a, in0=m8.a[:, 0:1], scalar1=-1.0, scalar2=None, op0=ALU.mult), [m8], [nv0])
                    yield
                    c.A(lambda e: e.activation(out=e8.a, in_=les.a, func=AF.Exp, bias=nv0.a[:, 0:1]), [les, nv0], [e8])
                    yield
                    c.V(lambda e: e.tensor_scalar(out=mk2.a, in0=les.a, scalar1=m8.a[:, 1:2], scalar2=None, op0=ALU.is_ge), [les, m8], [mk2])
                    yield
                    c.V(lambda e: e.tensor_tensor(out=w8.a, in0=e8.a, in1=mk2.a, op=ALU.mult), [e8, mk2], [w8])
                    yield
                    c.V(lambda e: e.tensor_reduce(out=sden.a, in_=w8.a, axis=AX.X, op=ALU.add), [w8], [sden])
                    yield
                    c.V(lambda e: e.tensor_tensor(out=sden.a, in0=sden.a, in1=sg.a, op=ALU.mult), [sden, sg], [sden])
                    yield
                    c.V(lambda e: e.reciprocal(out=sden.a, in_=sden.a), [sden], [sden])
                    yield
                    c.V(lambda e: e.tensor_scalar(out=w8.a, in0=w8.a, scalar1=sden.a[:, 0:1], scalar2=None, op0=ALU.mult), [w8, sden], [w8])
                    yield
                    c.V(lambda e: e.tensor_tensor(out=dw_.a, in0=ohg.a.unsqueeze(2).to_broadcast([128, 4, 8]), in1=w8.a.unsqueeze(1).to_broadcast([128, 4, 8]), op=ALU.mult), [ohg, w8], [dw_])
                    yield
                    c.dma("sp", DW.a[tok].rearrange("p (g e) -> p g e", e=8), dw_.a, DW, dw_)
                    yield
                tl = list(range(first_tile, NT))
                rr(genA(tl[0]))
                for k_ in range(len(tl)):
                    rr(genB(tl[k_]), genA(tl[k_ + 1]) if k_ + 1 < len(tl) else None)
                c.pop()
                if stop_after == "p3a":
                    break

                tiles = list(range(first_tile, NT))
                ng = 2
                per = (len(tiles) + ng - 1) // ng
                for gi in range(ng):
                    grp = tiles[gi * per:(gi + 1) * per]
                    t0 = grp[0]; G_ = len(grp)
                    c.push()
                    h2 = c.sb("h2", [128, 8, G_ * 128], BF16, grp="g4")
                    dwg = c.sb("dwg", [128, G_, 32], F32, grp="g4")
                    acc = c.sb("acc", [128, G_, D], F32)
                    c.dma("sp", h2.a, H2T.a[:, :, t0 * 128:(t0 + G_) * 128], h2, H2T)
                    c.dma("sp", dwg.a, DW.a[t0 * 128:(t0 + G_) * 128].rearrange("(i p) e -> p i e", p=128), dwg, DW)
                    wg = [c.sb("wg%d" % i, [128, 8, 512], BF16, grp="we%d" % i) for i in range(2)]
                    wd = [c.sb("wd%d" % i, [128, 2, D], BF16, grp="we%d" % i) for i in range(2)]
                    pGU = [c.ps("pGU%d" % i, [128, 2, 512], F32) for i in range(2)]
                    sl = [c.sb("sl%d" % i, [128, 512], F32) for i in range(2)]
                    aT = [c.sb("aT%d" % i, [128, 2, 512], BF16) for i in range(3)]
                    pD = [c.ps("pD%d" % i, [128, D], F32) for i in range(2)]
                    xt = [c.sb("x4%d" % i, [128, D], F32) for i in range(2)]
                    quads = [(tq, min(4, G_ - tq)) for tq in range(0, G_, 4)]
                    steps = [(e_, qi) for e_ in range(32) for qi in range(len(quads))]

                    def m_s1(si):
                        e_, qi = steps[si]
                        tq, nt = quads[qi]; N = nt * 128
                        g_ = wg[e_ % 2]; d_ = wd[e_ % 2]; at_ = aT[si % 3]
                        if qi == 0:
                            c.dma("sp", g_.a, WGU.a[e_], g_, WGU)
                            yield
                            c.dma("sp", d_.a, WDN.a[e_], d_, WDN)
                            yield
                        for cch in range(2):
                            pg = pGU[cch]; s_ = sl[cch]
                            for which in range(2):
                                col0 = which * 256 + cch * 128
                                for k in range(8):
                                    c.mm(lambda e: e.matmul(pg.a[:, which, :N], lhsT=g_.a[:, k, col0:col0 + 128], rhs=h2.a[:, k, tq * 128:tq * 128 + N], start=(k == 0), stop=(k == 7)), [g_, h2], [pg], last=(k == 7 and which == 1))
                                    yield
                            c.A(lambda e: e.activation(out=s_.a[:, :N], in_=pg.a[:, 0, :N], func=AF.Silu), [pg], [s_])
                            yield
                            c.V(lambda e: e.tensor_tensor(out=at_.a[:, cch, :N], in0=s_.a[:, :N], in1=pg.a[:, 1, :N], op=ALU.mult), [s_, pg], [at_])
                            yield

                    def m_s2(si):
                        e_, qi = steps[si]
                        tq, nt = quads[qi]
                        d_ = wd[e_ % 2]; at_ = aT[si % 3]
                        for tj in range(nt):
                            ti = tq + tj
                            pd = pD[tj % 2]
                            for half in range(2):
                                cs = slice(half * 512, (half + 1) * 512)
                                for k in range(2):
                                    c.mm(lambda e: e.matmul(pd.a[:, cs], lhsT=at_.a[:, k, tj * 128:(tj + 1) * 128], rhs=d_.a[:, k, cs], start=(k == 0), stop=(k == 1)), [at_, d_], [pd], last=(k == 1 and half == 1))
                                    yield
                            if e_ == 0:
                                c.V(lambda e: e.tensor_scalar(out=acc.a[:, ti, :], in0=pd.a, scalar1=dwg.a[:, ti, e_:e_ + 1], scalar2=None, op0=ALU.mult), [pd, dwg], [acc])
                                yield
                            else:
                                c.V(lambda e: e.scalar_tensor_tensor(out=acc.a[:, ti, :], in0=pd.a, scalar=dwg.a[:, ti, e_:e_ + 1], in1=acc.a[:, ti, :], op0=ALU.mult, op1=ALU.add), [pd, dwg, acc], [acc])
                                yield

                    ns_ = len(steps)
                    rr(m_s1(0))
                    if ns_ > 1:
                        rr(m_s1(1))
                    for si in range(ns_):
                        rr(m_s2(si), m_s1(si + 2) if si + 2 < ns_ else None)
                    for ti in range(G_):
                        i = t0 + ti
                        j3 = 2 if i < 2 else b
                        x_ = xt[ti % 2]
                        c.dma("sp", x_.a, XS.a[b, i * 128:(i + 1) * 128, :], x_, XS)
                        c.G(lambda e: e.tensor_tensor(out=acc.a[:, ti, :], in0=acc.a[:, ti, :], in1=GT.a[:, 1, j3, :], op=ALU.mult), [acc, GT], [acc])
                        c.V(lambda e: e.tensor_tensor(out=x_.a, in0=x_.a, in1=acc.a[:, ti, :], op=ALU.add), [x_, acc], [x_])
                        if last_layer:
                            c.dma("sp", y_out.a[b, (i - 2) * 128:(i - 1) * 128, :], x_.a, y_out, x_)
                        else:
                            c.dma("sp", XS.a[b, i * 128:(i + 1) * 128, :], x_.a, XS, x_)
                    c.pop()
            if stop_after is not None:
                break
            c.pop()
        c.barrier()
        while len(c.stack) > 1:
            c.stack.pop().__exit__(None, None, None)
        print("instructions:", c.ninst, "sems:", len(c.sem))
    nc._trace = c.trace
    return nc


def host_consts():
    t = np.arange(4096)
    row = (t // 64).astype(np.float32); col = (t % 64).astype(np.float32)
    inv = (10000.0 ** (-np.arange(16, dtype=np.float32) / 16)).astype(np.float32)
    ang = np.concatenate([row[:, None] * inv, col[:, None] * inv], axis=-1).astype(np.float32)
    cos = np.ones((T, 32), np.float32); sin = np.zeros((T, 32), np.float32)
    cos[256:] = np.cos(ang); sin[256:] = np.sin(ang)
    s = np.arange(128)
    u = np.stack([(s[:, None] <= s[None, :]), (s[:, None] >= s[None, :])]).astype(np.float32)
    kc = np.arange(64)[:, None]; qc = np.arange(64)[None, :]
    cs = np.clip(qc - 8, 0, 48)
    valid = (kc >= cs) & (kc < cs + 16)
    cm = np.where(valid, 0.0, NEG).astype(np.float32)
    jd = np.zeros((64, 128), np.float32)
    for kc in range(64):
        jd[63 - kc, kc] = 1.0; jd[63 - kc, 64 + kc] = 1.0
    return {"k_jd": jd, "k_cos": cos, "k_sin": sin, "k_u": u, "k_id": np.eye(128, dtype=np.float32), "k_colmask": np.concatenate([cm, cm], 0)}


def make_in_maps(inputs, nb, cores):
    consts = host_consts()
    maps = []
    for ci in cores:
        m = dict(consts)
        for k, v in inputs.items():
            v = np.ascontiguousarray(v, dtype=np.float32)
            if k in ("x", "c", "ctx"):
                m[k] = np.ascontiguousarray(v[ci * nb:(ci + 1) * nb])
            elif k == "b_mlstm":
                m[k] = v.reshape(2, 16)
            else:
                m[k] = v
        maps.append(m)
    return maps


def kernel(**inputs):
    nb = 2
    nc = build(nb=nb, nl=2)
    in_maps = make_in_maps(inputs, nb, list(range(8)))
    res = run_bass_kernel_spmd(nc, in_maps, core_ids=list(range(8)))
    return np.concatenate([r["y"] for r in res.results], axis=0).astype(np.float32)
```

```python
import numpy as np
import concourse.bass as bass
import concourse.mybir as mybir
from concourse.bass_utils import run_bass_kernel_spmd
from contextlib import ExitStack
import os

F32 = mybir.dt.float32
BF16 = mybir.dt.bfloat16
AF = mybir.ActivationFunctionType
ALU = mybir.AluOpType
AX = mybir.AxisListType

T = 4352
NT = 34
D = 1024
NEG = -1e30
SKIP_SAME = int(os.environ.get("SKIP_SAME", "0"))


class Buf:
    __slots__ = ("name", "w", "r", "dsem", "t", "grp")

    def __init__(self, name, t=None, grp=None):
        self.name = name
        self.grp = grp
        self.w = None
        self.r = {}
        self.dsem = None
        self.t = t

    @property
    def a(self):
        return self.t.ap() if hasattr(self.t, "ap") else self.t[:]


class Ctx:
    def __init__(self, nc, es):
        self.nc = nc
        self.es = es
        self.E = {"pe": nc.tensor, "act": nc.scalar, "dve": nc.vector, "pool": nc.gpsimd, "sp": nc.sync}
        self.sem = {}
        self.ecnt = {}
        for e in ("pe", "act", "dve", "pool"):
            self.sem[e] = es.enter_context(nc.semaphore("c_" + e))
            self.ecnt[e] = 0
        self.seen = {e: {} for e in self.E}
        self.ninst = 0
        self.uid = 0
        self.trace = {e: [] for e in self.E}
        self.shared = set()
        self.stack = [es]
        self.dtot = {}

    def sb(self, name, shape, dt, grp=None):
        self.uid += 1
        return Buf(name, self.stack[-1].enter_context(self.nc.sbuf_tensor("%s_%d" % (name, self.uid), list(shape), dt)), grp)

    def ps(self, name, shape, dt):
        self.uid += 1
        return Buf(name, self.stack[-1].enter_context(self.nc.psum_tensor("%s_%d" % (name, self.uid), list(shape), dt)))

    def dram(self, name, shape, dt, kind="Internal"):
        return Buf(name, self.nc.dram_tensor(name, list(shape), dt, kind=kind))

    def push(self):
        st = ExitStack()
        st.__enter__()
        self.stack.append(st)

    def pop(self):
        self.barrier()
        self.stack.pop().__exit__(None, None, None)

    def barrier(self):
        evs = [(e, self.ecnt[e]) for e in self.ecnt] + list(self.dtot.items())
        for e in self.E:
            self._wait(e, evs)

    def _wait(self, eng, deps):
        need = {}
        for k, v in deps:
            if eng == "pe" and k == "pe":
                continue
            if SKIP_SAME and k == eng and v <= self.ecnt[eng] - SKIP_SAME:
                continue
            if v > need.get(k, 0):
                need[k] = v
        seen = self.seen[eng]
        for k, v in need.items():
            if k in self.shared:
                v = self.dtot[k]
            if seen.get(k, 0) >= v:
                continue
            self.E[eng].wait_ge(self.sem[k], v)
            self.trace[eng].append(("w", k, v))
            self.ninst += 1
            seen[k] = v

    def _deps(self, reads, writes):
        deps = []
        for b in reads:
            if b.w is not None:
                deps.append(b.w)
        for b in writes:
            if b.w is not None:
                deps.append(b.w)
            deps.extend(b.r.items())
        return deps

    def _commit(self, ev, reads, writes):
        k, v = ev
        for b in reads:
            if b.r.get(k, 0) < v:
                b.r[k] = v
        for b in writes:
            b.w = ev
            b.r = {}

    def op(self, eng, f, reads=(), writes=()):
        self._wait(eng, self._deps(reads, writes))
        ins = f(self.E[eng])
        self.ecnt[eng] += 1
        ins.then_inc(self.sem[eng], 1)
        self.trace[eng].append(("i", eng, 1))
        self.ninst += 1
        self._commit((eng, self.ecnt[eng]), reads, writes)
        return ins

    def V(self, f, r=(), w=()):
        return self.op("dve", f, r, w)

    def A(self, f, r=(), w=()):
        return self.op("act", f, r, w)

    def G(self, f, r=(), w=()):
        return self.op("pool", f, r, w)

    def mm(self, f, reads=(), writes=(), last=True):
        self._wait("pe", self._deps(reads, writes))
        ins = f(self.E["pe"])
        self.ninst += 1
        if last:
            self.ecnt["pe"] += 1
            ins.then_inc(self.sem["pe"], 1)
            self.trace["pe"].append(("i", "pe", 1))
            self._commit(("pe", self.ecnt["pe"]), reads, writes)
        else:
            self._commit(("pe", self.ecnt["pe"] + 1), reads, writes)
        return ins

    def dma(self, q, out, in_, dst, src, **kw):
        self._wait(q, self._deps((src,), (dst,)))
        if dst.dsem is None:
            dst.dsem = "d_" + (dst.grp or dst.name)
            if dst.grp:
                self.shared.add(dst.dsem)
            if dst.dsem not in self.sem:
                self.sem[dst.dsem] = self.es.enter_context(self.nc.semaphore(dst.dsem))
                self.dtot[dst.dsem] = 0
        ins = self.E[q].dma_start(out=out, in_=in_, **kw)
        self.dtot[dst.dsem] += 16
        ins.then_inc(self.sem[dst.dsem], 16)
        self.trace[q].append(("i", dst.dsem, 16))
        self.ninst += 1
        self._commit((dst.dsem, self.dtot[dst.dsem]), (src,), (dst,))
        return ins


def pipeline(n, stage1, stage2, depth, s1_first=False):
    for si in range(min(depth, n)):
        stage1(si)
    for si in range(n):
        if s1_first and si + depth < n:
            stage1(si + depth)
        stage2(si)
        if not s1_first and si + depth < n:
            stage1(si + depth)


def rr(*gens):
    gens = [g for g in gens if g is not None]
    while gens:
        for g in list(gens):
            try:
                next(g)
            except StopIteration:
                gens.remove(g)


def na_plan(j):
    plan = []
    for kt in range(32):
        blocks = {}
        anyv = False
        for a in range(2):
            for b in range(2):
                qr = 2 * j + b
                kr = 2 * kt + a
                rs = min(max(qr - 4, 0), 56)
                ok = rs <= kr < rs + 8
                blocks[(a, b)] = (kr - qr + 7) if ok else None
                anyv = anyv or ok
        if anyv:
            plan.append((kt, blocks))
    return plan


def build(nb=2, nl=2, dbg=(), stop_after=None):
    nc = bass.Bass("TRN2", target_bir_lowering=False)
    es = ExitStack()
    with es:
        c = Ctx(nc, es)

        def inp(name, shape):
            return Buf(name, nc.dram_tensor(name, list(shape), F32, kind="ExternalInput"))

        x_in = inp("x", [nb, 4096, D]); ctx_in = inp("ctx", [nb, 256, D]); c_in = inp("c", [nb, D]); cctx_in = inp("c_ctx", [D])
        w_mod = inp("w_mod", [2, D, 6144]); b_mod = inp("b_mod", [2, 6144]); g_norm = inp("g_norm", [2, 2, D])
        w_in = inp("w_in", [2, D, 7440]); b_merge = inp("b_merge", [2, 3, D]); g_qk = inp("g_qk", [2, 4, 64])
        rpb = inp("rpb", [2, 8, 15, 31]); b_mlstm = inp("b_mlstm", [2, 16]); g_ml = inp("g_ml", [2, 512])
        w_branch = inp("w_branch", [2, 3, 512, D]); w_out = inp("w_out", [2, D, D])
        w_group = inp("w_group", [2, D, 4]); b_group = inp("b_group", [2, 4]); w_router = inp("w_router", [2, D, 32]); b_router = inp("b_router", [2, 32])
        w_gate_up = inp("w_gate_up", [2, 32, D, 512]); w_down = inp("w_down", [2, 32, 256, D])
        k_cos = inp("k_cos", [T, 32]); k_sin = inp("k_sin", [T, 32]); k_u = inp("k_u", [2, 128, 128]); k_id = inp("k_id", [128, 128])
        k_colmask = inp("k_colmask", [128, 64]); k_jd = inp("k_jd", [64, 128])
        y_out = c.dram("y", [nb, 4096, D], F32, kind="ExternalOutput")

        def scr(name, shape, dt):
            return c.dram(name, shape, dt, kind=("ExternalOutput" if name in dbg else "Internal"))

        XS = scr("XS", [nb, T, D], F32)
        QAT = scr("QAT", [128, 4, T], BF16); KAT = scr("KAT", [128, T], BF16); VA = scr("VA", [T, 2, 65], BF16)
        QBT = scr("QBT", [128, 4, T], BF16); KBT = scr("KBT", [128, 4, T], BF16); VB = scr("VB", [T, 8, 65], BF16)
        MQT = scr("MQT", [128, 4, T], BF16); MKT = scr("MKT", [128, 4, T], BF16); MK = scr("MK", [T, 512], BF16)
        MV = scr("MV", [T, 4, 129], BF16); MO = scr("MO", [T, 512], BF16); MG = scr("MG", [T, 16], F32)
        GATES = scr("GATES", [T, 3072], BF16)
        YAT = scr("YAT", [4, 128, T], BF16); YBT = scr("YBT", [4, 128, T], BF16); YCT = scr("YCT", [4, 128, T], BF16)
        HS = scr("HS", [T, 512], F32)
        H2T = scr("H2T", [128, 8, T], BF16); DW = scr("DW", [T, 32], F32)
        WGU = scr("WGU", [32, 128, 8, 512], BF16); WDN = scr("WDN", [32, 128, 2, D], BF16)
        MODD = scr("MODD", [2, 3, 6144], F32)
        RPBP = scr("RPBP", [7568], F32); TPD = scr("TPD", [128, 8, 15, 64], F32)

        ident_f = c.sb("ident_f", [128, 128], F32, grp="setup"); ident = c.sb("ident", [128, 128], BF16)
        U = c.sb("U", [128, 2, 128], F32, grp="setup")
        c.dma("sp", ident_f.a, k_id.a, ident_f, k_id)
        c.dma("sp", U.a, k_u.a.rearrange("d s t -> s d t"), U, k_u)
        c.V(lambda e: e.tensor_copy(out=ident.a, in_=ident_f.a), [ident_f], [ident])
        ones_col = c.sb("ones_col", [128, 8], BF16)
        c.V(lambda e: e.memset(ones_col.a, 1.0), [], [ones_col])

        def tile_src(l, b, i):
            if l == 0:
                if i < 2:
                    return ctx_in, ctx_in.a[b, i * 128:(i + 1) * 128, :]
                return x_in, x_in.a[b, (i - 2) * 128:(i - 1) * 128, :]
            return XS, XS.a[b, i * 128:(i + 1) * 128, :]

        for l in range(nl):
            last_layer = (l == nl - 1)
            first_tile = 2 if last_layer else 0
            c.push()
            c.push()
            cT = c.sb("cT", [128, 8, 3], F32, grp="setup"); cTb = c.sb("cTb", [128, 8, 3], BF16)
            c.V(lambda e: e.memset(cT.a, 0.0), [], [cT])
            for b in range(nb):
                c.dma("sp", cT.a[:, :, b], c_in.a[b].rearrange("(k p) -> p k", p=128), cT, c_in, allow_slow_non_contiguous=True)
            c.dma("sp", cT.a[:, :, 2], cctx_in.a.rearrange("(k p) -> p k", p=128), cT, cctx_in, allow_slow_non_contiguous=True)
            c.A(lambda e: e.activation(out=cTb.a, in_=cT.a, func=AF.Silu), [cT], [cTb])
            modrow = c.sb("modrow", [3, 6144], F32)
            bmrow = c.sb("bmrow", [3, 6144], F32, grp="setup")
            c.dma("sp", bmrow.a, b_mod.a[l].partition_broadcast(3), bmrow, b_mod)
            wm = [c.sb("wm%d" % i, [128, 8, 512], BF16) for i in range(2)]
            pmod = [c.ps("pmod%d" % i, [128, 512], F32) for i in range(2)]
            for n in range(12):
                w_ = wm[n % 2]; p_ = pmod[n % 2]
                c.dma("pool", w_.a, w_mod.a[l, :, n * 512:(n + 1) * 512].rearrange("(k p) n -> p k n", p=128), w_, w_mod)
                for k in range(8):
                    c.mm(lambda e: e.matmul(p_.a[0:3, :], lhsT=cTb.a[:, k, :], rhs=w_.a[:, k, :], start=(k == 0), stop=(k == 7)), [cTb, w_], [p_], last=(k == 7))
                c.V(lambda e: e.tensor_tensor(out=modrow.a[:, n * 512:(n + 1) * 512], in0=p_.a[0:3, :], in1=bmrow.a[:, n * 512:(n + 1) * 512], op=ALU.add), [p_, bmrow], [modrow])
            c.dma("sp", MODD.a[l], modrow.a, MODD, modrow)
            c.pop()
            modT = c.sb("modT", [128, 6, 8, 3], F32, grp="setup")
            for s in range(6):
                for j in range(3):
                    c.dma("sp", modT.a[:, s, :, j], MODD.a[l, j, s * 1024:(s + 1) * 1024].rearrange("(k p) -> p k", p=128), modT, MODD, allow_slow_non_contiguous=True)
            gn = c.sb("gn", [128, 2, 8], F32, grp="setup")
            c.dma("sp", gn.a, g_norm.a[l].rearrange("t (k p) -> p t k", p=128), gn, g_norm, allow_slow_non_contiguous=True)
            G1 = c.sb("G1", [128, 8, 3], F32); G2 = c.sb("G2", [128, 8, 3], F32)
            for (Gx, seg, t_) in ((G1, 1, 0), (G2, 4, 1)):
                c.V(lambda e: e.tensor_scalar(out=Gx.a, in0=modT.a[:, seg], scalar1=1.0, scalar2=None, op0=ALU.add), [modT], [Gx])
                c.V(lambda e: e.tensor_tensor(out=Gx.a, in0=Gx.a, in1=gn.a[:, t_, :].unsqueeze(2).to_broadcast([128, 8, 3]), op=ALU.mult), [Gx, gn], [Gx])
            GT = c.sb("GT", [128, 2, 3, D], F32, grp="setup")
            for gi, seg in ((0, 2), (1, 5)):
                for j in range(3):
                    c.dma("sp", GT.a[:, gi, j, :], MODD.a[l, j, seg * 1024:(seg + 1) * 1024].partition_broadcast(128), GT, MODD)
            gqk = c.sb("gqk", [128, 4, 64], F32, grp="setup")
            c.dma("sp", gqk.a, g_qk.a[l].partition_broadcast(128), gqk, g_qk)
            bml = c.sb("bml", [128, 16], F32, grp="setup")
            c.dma("sp", bml.a, b_mlstm.a[l].partition_broadcast(128), bml, b_mlstm)
            gml = c.sb("gml", [128, 512], F32, grp="setup")
            c.dma("sp", gml.a, g_ml.a[l].partition_broadcast(128), gml, g_ml)
            brt = c.sb("brt", [128, 36], F32, grp="setup")
            c.dma("sp", brt.a[:, 0:4], b_group.a[l].partition_broadcast(128), brt, b_group)
            c.dma("sp", brt.a[:, 4:36], b_router.a[l].partition_broadcast(128), brt, b_router)
            wrt = c.sb("wrt", [128, 8, 36], F32, grp="setup")
            c.dma("sp", wrt.a[:, :, 0:4], w_group.a[l].rearrange("(k p) n -> p k n", p=128), wrt, w_group, allow_slow_non_contiguous=True)
            c.dma("sp", wrt.a[:, :, 4:36], w_router.a[l].rearrange("(k p) n -> p k n", p=128), wrt, w_router, allow_slow_non_contiguous=True)
            c.push()
            zt = c.sb("zt", [1, 8192], F32, grp="setup")
            c.V(lambda e: e.memset(zt.a, 0.0), [], [zt])
            c.dma("sp", RPBP.a.rearrange("(o n) -> o n", o=1), zt.a[:, 0:7568], RPBP, zt)
            c.dma("sp", RPBP.a[64:64 + 3720].rearrange("(r j) -> r j", j=31), bass.AP(rpb.t, l * 3720 + 30, [[31, 120], [-1, 31]]), RPBP, rpb, allow_slow_non_contiguous=True)
            TPb = c.sb("TPb", [128, 8, 15, 64], F32, grp="setup")
            cm = c.sb("cm", [128, 64], F32, grp="setup")
            c.dma("sp", cm.a, k_colmask.a, cm, k_colmask)
            TPx = c.sb("TPx", [64, 8, 15, 64], F32, grp="setup")
            for h in range(8):
                src = bass.AP(RPBP.t, 64 - 48 + h * 15 * 31, [[1, 64], [31, 15], [1, 64]])
                c.dma("sp", TPx.a[:, h], src, TPx, RPBP)
            jd = c.sb("jd", [64, 128], F32, grp="setup")
            c.dma("sp", jd.a, k_jd.a, jd, k_jd)
            pJ = [c.ps("pJ%d" % i, [128, 512], F32) for i in range(2)]
            TPx2 = TPx.a.rearrange("p h r q -> p (h r q)")
            TPb2 = TPb.a.rearrange("p h r q -> p (h r) q")
            for n in range(15):
                p_ = pJ[n % 2]
                c.mm(lambda e: e.matmul(p_.a, lhsT=jd.a, rhs=TPx2[:, n * 512:(n + 1) * 512], start=True, stop=True), [jd, TPx], [p_])
                c.V(lambda e: e.tensor_tensor(out=TPb2[:, n * 8:(n + 1) * 8, :], in0=p_.a.rearrange("p (r q) -> p r q", q=64), in1=cm.a.unsqueeze(1).to_broadcast([128, 8, 64]), op=ALU.add), [p_, cm], [TPb])
            c.dma("sp", TPD.a, TPb.a, TPD, TPb)
            c.pop()
            c.push()
            cv = [c.sb("cv%d" % i, [128, 8, 512], BF16, grp="cv%d" % i) for i in range(2)]
            cd = [c.sb("cd%d" % i, [128, 2, D], BF16, grp="cv%d" % i) for i in range(2)]
            for e_ in range(32 if not os.environ.get("SKIP_CONV") else 0):
                a_ = cv[e_ % 2]; d_ = cd[e_ % 2]
                c.dma("pool", a_.a, w_gate_up.a[l, e_].rearrange("(k p) n -> p k n", p=128), a_, w_gate_up)
                c.dma("sp", WGU.a[e_], a_.a, WGU, a_)
                c.dma("pool", d_.a, w_down.a[l, e_].rearrange("(k p) n -> p k n", p=128), d_, w_down)
                c.dma("sp", WDN.a[e_], d_.a, WDN, d_)
            c.pop()
            if stop_after == "mod":
                break

            for b in range(nb):
                c.push()
                hT = c.sb("hT", [128, 8, T], BF16)
                xt = [c.sb("xt%d" % i, [128, D], F32) for i in range(2)]
                junk_L = [c.sb("junk%d" % i_, [128, D], F32) for i_ in range(2)]; junk = junk_L[0]
                ss_L = [c.sb("ss%d" % i_, [128, 1], F32) for i_ in range(2)]; ss = ss_L[0]; rstd_L = [c.sb("rstd%d" % i_, [128, 1], F32) for i_ in range(2)]; rstd = rstd_L[0]
                xn = [c.sb("xn%d" % i, [128, D], BF16) for i in range(2)]
                pT = [c.ps("pT%d" % i, [128, 8, 128], BF16) for i in range(2)]
                for i in range(NT):
                    x_ = xt[i % 2]; n_ = xn[i % 2]; p_ = pT[i % 2]
                    junk = junk_L[i % 2]; ss = ss_L[i % 2]; rstd = rstd_L[i % 2]
                    src, ap = tile_src(l, b, i)
                    j3 = 2 if i < 2 else b
                    c.dma("sp", x_.a, ap, x_, src)
                    c.V(lambda e: e.memset(ss.a, 0.0), [], [ss])
                    c.A(lambda e: e.activation(out=junk.a, in_=x_.a, func=AF.Square, accum_out=ss.a), [x_], [junk, ss])
                    c.V(lambda e: e.tensor_scalar(out=rstd.a, in0=ss.a, scalar1=1.0 / D, scalar2=1e-6, op0=ALU.mult, op1=ALU.add), [ss], [rstd])
                    c.A(lambda e: e.activation(out=rstd.a, in_=rstd.a, func=AF.Sqrt), [rstd], [rstd])
                    c.V(lambda e: e.reciprocal(out=rstd.a, in_=rstd.a), [rstd], [rstd])
                    c.V(lambda e: e.tensor_scalar(out=n_.a, in0=x_.a, scalar1=rstd.a[:, 0:1], scalar2=None, op0=ALU.mult), [x_, rstd], [n_])
                    for k in range(8):
                        c.mm(lambda e: e.transpose(out=p_.a[:, k, :], in_=n_.a[:, k * 128:(k + 1) * 128], identity=ident.a), [n_, ident], [p_], last=(k == 7))
                    for k in range(8):
                        eng = c.A if k % 2 == 0 else None
                        if k % 2 == 0:
                            c.A(lambda e: e.activation(out=hT.a[:, k, i * 128:(i + 1) * 128], in_=p_.a[:, k, :], func=AF.Identity, scale=G1.a[:, k, j3:j3 + 1], bias=modT.a[:, 0, k, j3:j3 + 1]), [p_, G1, modT], [hT])
                        else:
                            c.V(lambda e: e.tensor_scalar(out=hT.a[:, k, i * 128:(i + 1) * 128], in0=p_.a[:, k, :], scalar1=G1.a[:, k, j3:j3 + 1], scalar2=modT.a[:, 0, k, j3:j3 + 1], op0=ALU.mult, op1=ALU.add), [p_, G1, modT], [hT])
                if "HTD" in dbg:
                    HTD = scr("HTD", [128, 8, T], BF16)
                    c.dma("sp", HTD.a, hT.a, HTD, hT)
                cosb = c.sb("cosb", [128, NT, 32], F32, grp="setup"); sinb = c.sb("sinb", [128, NT, 32], F32, grp="setup")
                c.dma("sp", cosb.a, k_cos.a.rearrange("(i p) f -> p i f", p=128), cosb, k_cos)
                c.dma("sp", sinb.a, k_sin.a.rearrange("(i p) f -> p i f", p=128), sinb, k_sin)
                bmg = c.sb("bmg", [128, 3072], F32, grp="setup")
                c.dma("sp", bmg.a, b_merge.a[l].rearrange("t d -> (t d)").partition_broadcast(128), bmg, b_merge)
                wc = [c.sb("wc%d" % i, [128, 8, 512], BF16) for i in range(2)]
                pp = [c.ps("pp%d" % i, [128, 512], F32) for i in range(2)]
                ptr = [c.ps("ptr%d" % i, [128, 4, 128], BF16) for i in range(2)]
                sq_L = [c.sb("sq%d" % i_, [128, 512], F32) for i_ in range(2)]; sq = sq_L[0]; ssq_L = [c.sb("ssq%d" % i_, [128, 8], F32) for i_ in range(2)]; ssq = ssq_L[0]; rq_L = [c.sb("rq%d" % i_, [128, 8], F32) for i_ in range(2)]; rq = rq_L[0]
                qn_L = [c.sb("qn%d" % i_, [128, 512], F32) for i_ in range(2)]; qn = qn_L[0]; t1_L = [c.sb("t1%d" % i_, [128, 256], F32) for i_ in range(2)]; t1 = t1_L[0]; t2_L = [c.sb("t2%d" % i_, [128, 256], F32) for i_ in range(2)]; t2 = t2_L[0]
                qr_ = [c.sb("qr%d" % i, [128, 512], BF16) for i in range(2)]
                trs = [c.sb("trs%d" % i, [128, 4, 128], BF16) for i in range(2)]
                vst = [c.sb("vst%d" % i, [128, 8, 65], BF16) for i in range(2)]
                mvst = [c.sb("mvst%d" % i, [128, 4, 129], BF16) for i in range(2)]
                gst = [c.sb("gst%d" % i, [128, 512], BF16) for i in range(2)]
                mg1_L = [c.sb("mg1%d" % i_, [128, 16], F32) for i_ in range(2)]; mg1 = mg1_L[0]; mg2_L = [c.sb("mg2%d" % i_, [128, 8], F32) for i_ in range(2)]; mg2 = mg2_L[0]
                gpre_L = [c.sb("gpre%d" % i_, [128, 512], F32) for i_ in range(2)]; gpre = gpre_L[0]
                for st_ in vst:
                    c.V(lambda e: e.memset(st_.a, 1.0), [], [st_])
                for st_ in mvst:
                    c.V(lambda e: e.memset(st_.a, 1.0), [], [st_])
                chunks = [(0, 512, "Aq"), (512, 256, "Akv"), (768, 512, "Bq"), (1280, 512, "Bk"), (1792, 512, "Bv"),
                          (2304, 512, "Cq"), (2816, 512, "Ck"), (3328, 512, "Cv"), (3840, 512, "Co"), (4352, 16, "Cg")]
                chunks += [(4368 + 512 * m, 512, "Mg%d" % m) for m in range(6)]
                pp = pp + [c.ps("pp%d" % i, [128, 512], F32) for i in range(2, 4)]

                def mmgen(i, it, cw, w_):
                    p_ = pp[it % 4]
                    for k in range(8):
                        c.mm(lambda e: e.matmul(p_.a[:, :cw], lhsT=hT.a[:, k, i * 128:(i + 1) * 128], rhs=w_.a[:, k, :cw], start=(k == 0), stop=(k == 7)), [hT, w_], [p_], last=(k == 7))
                        yield

                def ptile(i, it, kind):
                    p_ = pp[it % 4]
                    sq = sq_L[it % 2]; ssq = ssq_L[it % 2]; rq = rq_L[it % 2]; qn = qn_L[it % 2]; t1 = t1_L[it % 2]; t2 = t2_L[it % 2]
                    mg1 = mg1_L[it % 2]; mg2 = mg2_L[it % 2]; gpre = gpre_L[it % 2]
                    tok = slice(i * 128, (i + 1) * 128)

                    def qknorm(ncol, gidx, dst):
                        nh = ncol // 64
                        c.A(lambda e: e.activation(out=sq.a[:, :ncol], in_=p_.a[:, :ncol], func=AF.Square), [p_], [sq])
                        yield
                        c.V(lambda e: e.tensor_reduce(out=ssq.a[:, :nh], in_=sq.a[:, :ncol].rearrange("p (h d) -> p h d", d=64), axis=AX.X, op=ALU.add), [sq], [ssq])
                        yield
                        c.V(lambda e: e.tensor_scalar(out=rq.a[:, :nh], in0=ssq.a[:, :nh], scalar1=1.0 / 64, scalar2=1e-6, op0=ALU.mult, op1=ALU.add), [ssq], [rq])
                        yield
                        c.A(lambda e: e.activation(out=rq.a[:, :nh], in_=rq.a[:, :nh], func=AF.Sqrt), [rq], [rq])
                        yield
                        c.V(lambda e: e.reciprocal(out=rq.a[:, :nh], in_=rq.a[:, :nh]), [rq], [rq])
                        yield
                        c.V(lambda e: e.tensor_tensor(out=dst.rearrange("p (h d) -> p h d", d=64), in0=p_.a[:, :ncol].rearrange("p (h d) -> p h d", d=64),
                                                      in1=rq.a[:, :nh].unsqueeze(2).to_broadcast([128, nh, 64]), op=ALU.mult), [p_, rq], [qn])
                        yield
                        c.V(lambda e: e.tensor_tensor(out=dst.rearrange("p (h d) -> p h d", d=64), in0=dst.rearrange("p (h d) -> p h d", d=64),
                                                      in1=gqk.a[:, gidx:gidx + 1, :].to_broadcast([128, nh, 64]), op=ALU.mult), [qn, gqk], [qn])
                        yield

                    def rope(src, nh, dst4, dbuf):
                        s3 = src.rearrange("p (h t f) -> p h t f", t=2, f=32)
                        x1 = s3[:, :, 0, :]; x2 = s3[:, :, 1, :]
                        cb = cosb.a[:, i:i + 1, :].to_broadcast([128, nh, 32]); sb_ = sinb.a[:, i:i + 1, :].to_broadcast([128, nh, 32])
                        a1 = t1.a[:, :nh * 32].rearrange("p (h f) -> p h f", f=32); a2 = t2.a[:, :nh * 32].rearrange("p (h f) -> p h f", f=32)
                        c.V(lambda e: e.tensor_tensor(out=a1, in0=x1, in1=cb, op=ALU.mult), [qn, cosb], [t1])
                        yield
                        c.V(lambda e: e.tensor_tensor(out=a2, in0=x2, in1=sb_, op=ALU.mult), [qn, sinb], [t2])
                        yield
                        c.V(lambda e: e.tensor_tensor(out=dst4[:, :, 0, :], in0=a1, in1=a2, op=ALU.subtract), [t1, t2], [dbuf])
                        yield
                        c.V(lambda e: e.tensor_tensor(out=a1, in0=x2, in1=cb, op=ALU.mult), [qn, cosb], [t1])
                        yield
                        c.V(lambda e: e.tensor_tensor(out=a2, in0=x1, in1=sb_, op=ALU.mult), [qn, sinb], [t2])
                        yield
                        c.V(lambda e: e.tensor_tensor(out=dst4[:, :, 1, :], in0=a1, in1=a2, op=ALU.add), [t1, t2], [dbuf])
                        yield

                    def transposes(srcb, nblk, dstD):
                        pt = ptr[it % 2]; ts_ = trs[it % 2]
                        for m in range(nblk):
                            c.mm(lambda e: e.transpose(out=pt.a[:, m, :], in_=srcb.a[:, m * 128:(m + 1) * 128], identity=ident.a), [srcb, ident], [pt], last=(m == nblk - 1))
                            yield
                        c.A(lambda e: e.copy(out=ts_.a[:, :nblk, :], in_=pt.a[:, :nblk, :]), [pt], [ts_])
                        yield
                        if nblk == 1:
                            c.dma("sp", dstD.a[:, i * 128:(i + 1) * 128], ts_.a[:, 0, :], dstD, ts_)
                        else:
                            c.dma("sp", dstD.a[:, :, i * 128:(i + 1) * 128], ts_.a[:, :nblk, :], dstD, ts_)
                        yield

                    if kind == "Aq":
                        yield from qknorm(512, 0, qn.a[:, :512])
                        q_ = qr_[it % 2]
                        d5 = q_.a.rearrange("p (m g t f) -> p g m t f", g=2, t=2, f=32)
                        for g in range(2):
                            yield from rope(qn.a[:, g * 256:(g + 1) * 256], 4, d5[:, g], q_)
                        yield from transposes(q_, 4, QAT)
                    elif kind == "Akv":
                        yield from qknorm(128, 1, qn.a[:, :128])
                        q_ = qr_[it % 2]
                        yield from rope(qn.a[:, :128], 2, q_.a[:, :128].rearrange("p (h t f) -> p h t f", t=2, f=32), q_)
                        yield from transposes(q_, 1, KAT)
                        v_ = vst[it % 2]
                        c.A(lambda e: e.copy(out=v_.a[:, 0:2, 0:64], in_=p_.a[:, 128:256].rearrange("p (h d) -> p h d", d=64)), [p_], [v_])
                        yield
                        c.dma("sp", VA.a[tok], v_.a[:, 0:2, :], VA, v_)
                        yield
                    elif kind in ("Bq", "Bk"):
                        yield from qknorm(512, 2 if kind == "Bq" else 3, qn.a[:, :512])
                        q_ = qr_[it % 2]
                        c.A(lambda e: e.copy(out=q_.a, in_=qn.a[:, :512]), [qn], [q_])
                        yield
                        yield from transposes(q_, 4, QBT if kind == "Bq" else KBT)
                    elif kind == "Bv":
                        v_ = vst[it % 2]
                        c.A(lambda e: e.copy(out=v_.a[:, :, 0:64], in_=p_.a.rearrange("p (h d) -> p h d", d=64)), [p_], [v_])
                        yield
                        c.dma("sp", VB.a[tok], v_.a, VB, v_)
                        yield
                    elif kind in ("Cq", "Ck"):
                        q_ = qr_[it % 2]
                        c.A(lambda e: e.activation(out=q_.a, in_=p_.a, func=AF.Identity, scale=(1.0 if kind == "Cq" else 128 ** -0.5)), [p_], [q_])
                        yield
                        if kind == "Ck":
                            c.dma("sp", MK.a[tok], q_.a, MK, q_)
                            yield
                        yield from transposes(q_, 4, MQT if kind == "Cq" else MKT)
                    elif kind == "Cv":
                        v_ = mvst[it % 2]
                        c.A(lambda e: e.copy(out=v_.a[:, :, 0:128], in_=p_.a.rearrange("p (h d) -> p h d", d=128)), [p_], [v_])
                        yield
                        c.dma("sp", MV.a[tok], v_.a, MV, v_)
                        yield
                    elif kind == "Co":
                        g_ = gst[it % 2]
                        c.A(lambda e: e.activation(out=g_.a, in_=p_.a, func=AF.Sigmoid), [p_], [g_])
                        yield
                        c.dma("sp", MO.a[tok], g_.a, MO, g_)
                        yield
                    elif kind == "Cg":
                        c.V(lambda e: e.tensor_tensor(out=mg1.a, in0=p_.a[:, :16], in1=bml.a, op=ALU.add), [p_, bml], [mg1])
                        yield
                        fv = mg1.a.rearrange("p (d t h) -> p d t h", d=2, t=2)[:, :, 1, :]
                        m2 = mg2.a.rearrange("p (d h) -> p d h", d=2)
                        c.A(lambda e: e.activation(out=m2, in_=fv, func=AF.Exp, scale=-1.0), [mg1], [mg2])
                        yield
                        c.A(lambda e: e.activation(out=m2, in_=m2, func=AF.Ln, bias=1.0), [mg2], [mg2])
                        yield
                        c.V(lambda e: e.tensor_scalar(out=fv, in0=m2, scalar1=-1.0, scalar2=None, op0=ALU.mult), [mg2], [mg1])
                        yield
                        c.dma("sp", MG.a[tok], mg1.a, MG, mg1)
                        yield
                    else:
                        m = int(kind[2:])
                        g_ = gst[it % 2]
                        c.V(lambda e: e.tensor_tensor(out=gpre.a, in0=p_.a, in1=bmg.a[:, m * 512:(m + 1) * 512], op=ALU.add), [p_, bmg], [gpre])
                        yield
                        c.A(lambda e: e.activation(out=g_.a, in_=gpre.a, func=AF.Sigmoid), [gpre], [g_])
                        yield
                        c.dma("sp", GATES.a[tok, m * 512:(m + 1) * 512], g_.a, GATES, g_)
                        yield

                def wload(ci):
                    c0, cw, kind = chunks[ci]
                    w_ = wc[ci % 2]
                    c.dma("pool", w_.a[:, :, :cw], w_in.a[l, :, c0:c0 + cw].rearrange("(k p) n -> p k n", p=128), w_, w_in)

                items = [(ci, i) for ci in range(len(chunks)) for i in range(NT)]
                wload(0)
                loaded = 1

                def mm_of(n):
                    ci, i = items[n]
                    return mmgen(i, n, chunks[ci][1], wc[ci % 2])

                rr(mm_of(0), mm_of(1))
                for n in range(0, len(items), 2):
                    ci_next = items[min(n + 3, len(items) - 1)][0]
                    while loaded <= min(ci_next + 1, len(chunks) - 1):
                        wload(loaded)
                        loaded += 1
                    gens = [ptile(items[n][1], n, chunks[items[n][0]][2]), ptile(items[n + 1][1], n + 1, chunks[items[n + 1][0]][2])]
                    for m_ in (n + 2, n + 3):
                        if m_ < len(items):
                            gens.append(mm_of(m_))
                    rr(*gens)
                c.pop()
                if stop_after == "p1":
                    break

                c.push()
                kat = c.sb("kat", [128, T], BF16, grp="ka"); va = c.sb("va", [128, NT, 2, 65], BF16, grp="ka")
                c.dma("sp", kat.a, KAT.a, kat, KAT)
                c.dma("sp", va.a, VA.a.rearrange("(i p) g d -> p i g d", p=128), va, VA)
                qa = [[c.sb("qa%d_%d" % (i, g), [128, 4, 128], BF16, grp="qa%d" % i) for g in range(2)] for i in range(2)]
                for i in range(2):
                    for g in range(2):
                        c.V(lambda e: e.memset(qa[i][g].a, 0.0), [], [qa[i][g]])
                ND = 2
                pS = [c.ps("pS%d" % i, [128, 1024], F32) for i in range(ND)]
                pe_ = [c.sb("pe%d" % i, [128, 1024], BF16) for i in range(3)]
                pO = [c.ps("pO%d" % i, [128, 512], F32) for i in range(2)]
                pbc = c.ps("pbc", [128, 512], F32)
                ones_f = c.sb("ones_f", [128, 64], F32)
                c.V(lambda e: e.memset(ones_f.a, 1.0), [], [ones_f])
                dn = [c.sb("dn%d" % i, [128, 512], F32) for i in range(2)]
                bcs = [c.sb("bcs%d" % i, [64, 512], F32) for i in range(2)]
                yTa = [c.sb("yTa%d" % i, [64, 512], BF16) for i in range(2)]
                steps = []
                for i in range(first_tile, NT):
                    kts = list(range(0, 2)) if i < 2 else list(range(0, NT))
                    prs = [kts[j:j + 2] for j in range(0, len(kts), 2)]
                    for g in range(2):
                        for pi, pr in enumerate(prs):
                            steps.append((i, g, pr, pi == 0, pi == len(prs) - 1))

                def a_s1(si):
                    i, g, pr, first, last = steps[si]
                    q_ = qa[i % 2][g]
                    if g == 0 and first:
                        for g2 in range(2):
                            c.dma("sp", qa[i % 2][g2].a[g2 * 64:(g2 + 1) * 64], QAT.a[g2 * 64:(g2 + 1) * 64, :, i * 128:(i + 1) * 128], qa[i % 2][g2], QAT)
                            yield
                    ps_ = pS[si % ND]; e_ = pe_[si % 3]
                    for j, kt in enumerate(pr):
                        c.mm(lambda e: e.matmul(ps_.a[:, j * 512:(j + 1) * 512], lhsT=kat.a[:, kt * 128:(kt + 1) * 128], rhs=q_.a.rearrange("p m t -> p (m t)"), start=True, stop=True), [kat, q_], [ps_], last=(j == len(pr) - 1))
                        yield
                    c.A(lambda e: e.activation(out=e_.a, in_=ps_.a, func=AF.Exp, scale=0.125), [ps_], [e_])
                    yield

                def a_s2(si):
                    i, g, pr, first, last = steps[si]
                    e_ = pe_[si % 3]
                    po = pO[g]
                    for j, kt in enumerate(pr):
                        c.mm(lambda e: e.matmul(po.a[0:65, :], lhsT=va.a[:, kt, g, :], rhs=e_.a[:, j * 512:(j + 1) * 512], start=(first and j == 0), stop=(last and j == len(pr) - 1)), [e_, va], [po], last=(j == len(pr) - 1))
                        yield
                    if not last:
                        return
                    d_ = dn[g]; b_ = bcs[g]; y_ = yTa[g]
                    c.A(lambda e: e.copy(out=d_.a[64:65, :], in_=po.a[64:65, :]), [po], [d_])
                    yield
                    c.V(lambda e: e.reciprocal(out=d_.a[64:65, :], in_=d_.a[64:65, :]), [d_], [d_])
                    yield
                    c.mm(lambda e: e.matmul(pbc.a[0:64, :], lhsT=ones_f.a[64:65, :], rhs=d_.a[64:65, :], start=True, stop=True), [ones_f, d_], [pbc])
                    yield
                    c.A(lambda e: e.copy(out=b_.a, in_=pbc.a[0:64, :]), [pbc], [b_])
                    yield
                    c.V(lambda e: e.tensor_tensor(out=y_.a, in0=po.a[0:64, :], in1=b_.a, op=ALU.mult), [po, b_], [y_])
                    yield
                    for m in range(4):
                        c.dma("sp", YAT.a[g * 2 + m // 2, (m % 2) * 64:(m % 2) * 64 + 64, i * 128:(i + 1) * 128], y_.a[:, m * 128:(m + 1) * 128], YAT, y_)
                        yield

                ns_ = len(steps)
                rr(a_s1(0))
                if ns_ > 1:
                    rr(a_s1(1))
                for si in range(ns_):
                    if si + 2 < ns_:
                        rr(a_s1(si + 2))
                    rr(a_s2(si))
                c.pop()
                if stop_after == "p2a":
                    break

                c.push()
                kbt = c.sb("kbt", [128, 4, T], BF16, grp="kb"); vb = c.sb("vb", [128, NT, 8, 65], BF16, grp="kb")
                c.dma("sp", kbt.a, KBT.a, kbt, KBT)
                c.dma("sp", vb.a, VB.a.rearrange("(i p) h d -> p i h d", p=128), vb, VB)
                TP = c.sb("TP", [128, 8, 15, 64], F32, grp="kb")
                c.dma("sp", TP.a, TPD.a, TP, TPD)
                tabI = c.sb("tabI", [128, 5, 8, 128], F32); tabE = c.sb("tabE", [128, 5, 8, 128], F32)

                def build_tab(tab, plan):
                    for di, (kt, blocks) in enumerate(plan):
                        for (a, b2), dr in blocks.items():
                            o = tab.a[a * 64:(a + 1) * 64, di, :, b2 * 64:(b2 + 1) * 64]
                            if dr is None:
                                c.G(lambda e: e.memset(o, NEG), [], [tab])
                            else:
                                c.V(lambda e: e.tensor_copy(out=o, in_=TP.a[a * 64:(a + 1) * 64, :, dr, :]), [TP], [tab])

                build_tab(tabI, na_plan(5))
                qz = [[c.sb("qz%d_%d" % (i, p), [128, 4, 128], BF16, grp="qz%d" % i) for p in range(2)] for i in range(2)]
                for i in range(2):
                    for p in range(2):
                        c.V(lambda e: e.memset(qz[i][p].a, 0.0), [], [qz[i][p]])
                pS = [c.ps("pSb%d" % i, [128, 8, 128], F32) for i in range(2)]
                sS = c.sb("sS", [128, 8, 128], F32)
                pe_ = [c.sb("peb%d" % i, [128, 8, 128], BF16) for i in range(2)]
                pOb = c.ps("pOb", [128, 2, 512], F32)
                pO3 = [pOb.a[:, hh, 0:260].rearrange("p (m d) -> p m d", d=65) for hh in range(2)]
                rd = c.sb("rdb", [128, 4], F32)
                ya = [c.sb("yb%d" % i, [128, 512], BF16) for i in range(2)]
                pY = c.ps("pYb", [128, 4, 128], BF16); yT = c.sb("yTb", [128, 4, 128], BF16)
                steps = []
                for i in range(first_tile, NT):
                    keys = [(0, None, None), (1, None, None)]
                    plan = None
                    if i >= 2:
                        j = i - 2
                        plan = na_plan(j)
                        tab = tabI if 2 <= j <= 29 else tabE
                        keys = [(kt + 2, tab, di) for di, (kt, _) in enumerate(plan)] + keys
                    for ki, (kt, tab, di) in enumerate(keys):
                        steps.append((i, ki, kt, tab, di, len(keys), plan))

                def b_s1(si):
                    i, ki, kt, tab, di, nk, plan = steps[si]
                    qz_ = qz[i % 2]
                    if ki == 0:
                        for p in range(2):
                            c.dma("sp", qz_[p].a[p * 64:(p + 1) * 64], QBT.a[p * 64:(p + 1) * 64, :, i * 128:(i + 1) * 128], qz_[p], QBT)
                        if tab is tabE:
                            build_tab(tabE, plan)
                    ps_ = pS[si % 2]; e_ = pe_[si % 2]
                    for h in range(8):
                        par = h % 2; pr = h // 2
                        c.mm(lambda e: e.matmul(ps_.a[:, h, :], lhsT=kbt.a[:, pr, kt * 128:(kt + 1) * 128], rhs=qz_[par].a[:, pr, :], start=True, stop=True), [kbt, qz_[par]], [ps_], last=(h == 7))
                    if tab is not None:
                        c.V(lambda e: e.scalar_tensor_tensor(out=sS.a, in0=ps_.a, scalar=0.125, in1=tab.a[:, di], op0=ALU.mult, op1=ALU.add), [ps_, tab], [sS])
                        c.A(lambda e: e.activation(out=e_.a, in_=sS.a, func=AF.Exp), [sS], [e_])
                    else:
                        c.A(lambda e: e.activation(out=e_.a, in_=ps_.a, func=AF.Exp, scale=0.125), [ps_], [e_])

                def b_s2(si):
                    i, ki, kt, tab, di, nk, plan = steps[si]
                    e_ = pe_[si % 2]; y_ = ya[i % 2]
                    for h in range(8):
                        c.mm(lambda e: e.matmul(pO3[h // 4][:, h % 4, :], lhsT=e_.a[:, h, :], rhs=vb.a[:, kt, h, :], start=(ki == 0 and h % 4 == 0), stop=(ki == nk - 1)), [e_, vb], [pOb], last=(h == 7))
                    if ki != nk - 1:
                        return
                    for hh in range(2):
                        po3 = pO3[hh]
                        c.V(lambda e: e.reciprocal(out=rd.a, in_=po3[:, :, 64]), [pOb], [rd])
                        c.V(lambda e: e.tensor_tensor(out=y_.a[:, hh * 256:(hh + 1) * 256].rearrange("p (m d) -> p m d", d=64), in0=po3[:, :, 0:64], in1=rd.a.unsqueeze(2).to_broadcast([128, 4, 64]), op=ALU.mult), [pOb, rd], [y_])
                    for m in range(4):
                        c.mm(lambda e: e.transpose(out=pY.a[:, m, :], in_=y_.a[:, m * 128:(m + 1) * 128], identity=ident.a), [y_, ident], [pY], last=(m == 3))
                    c.A(lambda e: e.copy(out=yT.a, in_=pY.a), [pY], [yT])
                    c.dma("sp", YBT.a[:, :, i * 128:(i + 1) * 128].rearrange("m p t -> p m t"), yT.a, YBT, yT)

                pipeline(len(steps), b_s1, b_s2, 2)
                c.pop()
                if stop_after == "p2b":
                    break

                c.push()
                Cst = c.sb("Cst", [128, 4, 129], F32)
                Cb = [c.sb("Cb%d" % i, [128, 4, 129], BF16) for i in range(2)]
                mq = [c.sb("mq%d" % i, [128, 4, 128], BF16, grp="m%d" % i) for i in range(3)]
                mk = [c.sb("mk%d" % i, [128, 4, 128], BF16, grp="m%d" % i) for i in range(3)]
                mkt = [c.sb("mkt%d" % i, [128, 512], BF16, grp="m%d" % i) for i in range(3)]
                mv = [c.sb("mv%d" % i, [128, 4, 129], BF16, grp="m%d" % i) for i in range(3)]
                mg = [c.sb("mg%d" % i, [128, 16], F32, grp="m%d" % i) for i in range(3)]
                mo = [c.sb("mo%d" % i, [128, 512], BF16, grp="m%d" % i) for i in range(3)]
                hs = [c.sb("hs%d" % i, [128, 512], F32, grp="m%d" % i) for i in range(3)]
                pb = c.ps("pb", [128, 4], F32); pbB = c.ps("pbB", [128, 4, 128], F32); pST = c.ps("pST", [128, 4, 128], F32)
                pH = c.ps("pH", [128, 2, 512], F32); pC = c.ps("pC", [128, 2, 512], F32)
                pY = c.ps("pYc", [128, 4, 128], BF16)
                biasc = [c.sb("biasc%d" % i, [128, 4], F32) for i in range(3)]
                gcol = [c.sb("gcol%d" % i, [128, 4], F32) for i in range(3)]
                ebend = [c.sb("ebend%d" % i, [128, 4], F32) for i in range(3)]
                EB_L = [c.sb("EB%d" % i_, [128, 4, 128], F32) for i_ in range(2)]; EB = EB_L[0]
                qs = [c.sb("qs%d" % i, [128, 4, 128], BF16) for i in range(3)]
                DT_L = [c.sb("DT%d" % i_, [128, 4, 128], F32) for i_ in range(2)]; DT = DT_L[0]; DTm_L = [c.sb("DTm%d" % i_, [128, 4, 128], F32) for i_ in range(2)]; DTm = DTm_L[0]
                SD = [c.sb("SD%d" % i, [128, 4, 128], BF16) for i in range(3)]
                kg_L = [c.sb("kg%d" % i_, [128, 4, 128], BF16) for i_ in range(2)]; kg = kg_L[0]
                pCs = [c.sb("pCs%d" % i, [128, 2, 258], F32) for i in range(3)]
                den_L = [c.sb("den%d" % i_, [128, 4], F32) for i_ in range(2)]; den = den_L[0]; hout = [c.sb("hout%d" % i, [128, 512], F32) for i in range(2)]
                hsq_L = [c.sb("hsq%d" % i_, [128, 512], F32) for i_ in range(2)]; hsq = hsq_L[0]; hss_L = [c.sb("hss%d" % i_, [128, 4], F32) for i_ in range(2)]; hss = hss_L[0]; hn_L = [c.sb("hn%d" % i_, [128, 512], F32) for i_ in range(2)]; hn = hn_L[0]
                yc_L = [c.sb("yc%d" % i_, [128, 512], BF16) for i_ in range(2)]; yc = yc_L[0]; yT_L = [c.sb("yTc%d" % i_, [128, 4, 128], BF16) for i_ in range(2)]; yT = yT_L[0]

                def hreg(pt, h):
                    return pt.a[:, h // 2, (h % 2) * 129:(h % 2) * 129 + 129]

                def sreg(pt, h):
                    return pt.a[:, h // 2, (h % 2) * 129:(h % 2) * 129 + 129]

                for d in range(2):
                    order = list(range(NT)) if d == 0 else [1, 0] + list(range(NT - 1, 1, -1))
                    tend = 127 if d == 0 else 0
                    Ud = U.a[:, d, :]
                    c.V(lambda e: e.memset(Cst.a, 0.0), [], [Cst])
                    c.V(lambda e: e.memset(Cb[1].a, 0.0), [], [Cb[1]])

                    def l_s1(n):
                        kg_c = kg_L[n % 2]; EB_c = EB_L[n % 2]; DT_c = DT_L[n % 2]; DTm_c = DTm_L[n % 2]
                        i = order[n]
                        q_ = mq[n % 3]; k_ = mk[n % 3]; kt_ = mkt[n % 3]; v_ = mv[n % 3]; g_ = mg[n % 3]
                        bc_ = biasc[n % 3]; gc_ = gcol[n % 3]; eb_ = ebend[n % 3]; qs_ = qs[n % 3]; SD_ = SD[n % 3]; pcs = pCs[n % 3]
                        tok = slice(i * 128, (i + 1) * 128)
                        need_out = i >= first_tile
                        c.dma("sp", q_.a, MQT.a[:, :, tok], q_, MQT)
                        yield
                        c.dma("sp", k_.a, MKT.a[:, :, tok], k_, MKT)
                        yield
                        c.dma("sp", kt_.a, MK.a[tok], kt_, MK)
                        yield
                        c.dma("sp", v_.a, MV.a[tok], v_, MV)
                        yield
                        c.dma("sp", g_.a, MG.a[tok], g_, MG)
                        yield
                        if need_out and d == 1:
                            c.dma("sp", hs[n % 3].a, HS.a[tok], hs[n % 3], HS)
                            yield
                            c.dma("sp", mo[n % 3].a, MO.a[tok], mo[n % 3], MO)
                            yield
                        gv = g_.a.rearrange("p (d t h) -> p d t h", d=2, t=2)
                        ig = gv[:, d, 0, :]; lf = gv[:, d, 1, :]
                        c.mm(lambda e: e.matmul(pb.a, lhsT=Ud, rhs=lf, start=True, stop=True), [U, g_], [pb])
                        yield
                        for h in range(4):
                            c.mm(lambda e: e.matmul(pbB.a[:, h, :], lhsT=gv[:, d, 1, h:h + 1].to_broadcast([128, 128]), rhs=Ud, start=True, stop=True), [g_, U], [pbB], last=(h == 3))
                            yield
                        if need_out:
                            for h in range(4):
                                c.mm(lambda e: e.matmul(pST.a[:, h, :], lhsT=k_.a[:, h, :], rhs=q_.a[:, h, :], start=True, stop=True), [k_, q_], [pST], last=(h == 3))
                                yield
                        c.V(lambda e: e.tensor_tensor(out=bc_.a, in0=ig, in1=pb.a, op=ALU.subtract), [g_, pb], [bc_])
                        yield
                        c.V(lambda e: e.tensor_tensor(out=gc_.a, in0=pbB.a[:, :, tend], in1=bc_.a, op=ALU.add), [pbB, bc_], [gc_])
                        yield
                        c.A(lambda e: e.activation(out=gc_.a, in_=gc_.a, func=AF.Exp), [gc_], [gc_])
                        yield
                        c.A(lambda e: e.activation(out=eb_.a, in_=pbB.a[:, :, tend], func=AF.Exp), [pbB], [eb_])
                        yield
                        c.V(lambda e: e.tensor_tensor(out=kg_c.a, in0=kt_.a.rearrange("p (h d) -> p h d", d=128), in1=gc_.a.unsqueeze(2).to_broadcast([128, 4, 128]), op=ALU.mult), [kt_, gc_], [kg_c])
                        yield
                        for h in range(4):
                            c.mm(lambda e: e.matmul(hreg(pC, h), lhsT=kg_c.a[:, h, :], rhs=v_.a[:, h, :], start=True, stop=True), [kg_c, v_], [pC], last=(h == 3))
                            yield
                        for hh in range(2):
                            c.A(lambda e: e.copy(out=pcs.a[:, hh, :], in_=pC.a[:, hh, 0:258]), [pC], [pcs])
                            yield
                        if need_out:
                            c.A(lambda e: e.activation(out=EB_c.a, in_=pbB.a, func=AF.Exp), [pbB], [EB_c])
                            yield
                            c.V(lambda e: e.tensor_tensor(out=qs_.a, in0=q_.a, in1=EB_c.a, op=ALU.mult), [q_, EB_c], [qs_])
                            yield
                            for h in range(4):
                                c.A(lambda e: e.activation(out=DT_c.a[:, h, :], in_=pbB.a[:, h, :], func=AF.Exp, bias=bc_.a[:, h:h + 1]), [pbB, bc_], [DT_c])
                                yield
                            c.V(lambda e: e.tensor_tensor(out=DTm_c.a, in0=DT_c.a, in1=U.a[:, d:d + 1, :].to_broadcast([128, 4, 128]), op=ALU.mult), [DT_c, U], [DTm_c])
                            yield
                            c.V(lambda e: e.tensor_tensor(out=SD_.a, in0=DTm_c.a, in1=pST.a, op=ALU.mult), [DTm_c, pST], [SD_])
                            yield

                    def l_s2(n):
                        den_c = den_L[n % 2]; hsq_c = hsq_L[n % 2]; hss_c = hss_L[n % 2]; hn_c = hn_L[n % 2]; yc_c = yc_L[n % 2]; yT_c = yT_L[n % 2]
                        i = order[n]
                        v_ = mv[n % 3]; eb_ = ebend[n % 3]; qs_ = qs[n % 3]; SD_ = SD[n % 3]; pcs = pCs[n % 3]
                        cb_old = Cb[(n + 1) % 2]; cb_new = Cb[n % 2]
                        ho = hout[n % 2]
                        tok = slice(i * 128, (i + 1) * 128)
                        need_out = i >= first_tile
                        if need_out:
                            for h in range(4):
                                c.mm(lambda e: e.matmul(hreg(pH, h), lhsT=qs_.a[:, h, :], rhs=cb_old.a[:, h, :], start=True, stop=False), [qs_, cb_old], [pH], last=False)
                                yield
                                c.mm(lambda e: e.matmul(hreg(pH, h), lhsT=SD_.a[:, h, :], rhs=v_.a[:, h, :], start=False, stop=True), [SD_, v_], [pH], last=(h == 3))
                                yield
                        for h in range(4):
                            c.V(lambda e: e.scalar_tensor_tensor(out=Cst.a[:, h, :], in0=Cst.a[:, h, :], scalar=eb_.a[:, h:h + 1], in1=sreg(pcs, h), op0=ALU.mult, op1=ALU.add), [Cst, eb_, pcs], [Cst])
                            yield
                        c.A(lambda e: e.copy(out=cb_new.a, in_=Cst.a), [Cst], [cb_new])
                        yield
                        if not need_out:
                            return
                        for h in range(4):
                            c.A(lambda e: e.activation(out=den_c.a[:, h:h + 1], in_=hreg(pH, h)[:, 128:129], func=AF.Abs), [pH], [den_c])
                            yield
                        c.V(lambda e: e.tensor_scalar_max(out=den_c.a, in0=den_c.a, scalar1=1.0), [den_c], [den_c])
                        yield
                        c.V(lambda e: e.reciprocal(out=den_c.a, in_=den_c.a), [den_c], [den_c])
                        yield
                        for h in range(4):
                            c.V(lambda e: e.tensor_scalar(out=ho.a[:, h * 128:(h + 1) * 128], in0=hreg(pH, h)[:, 0:128], scalar1=den_c.a[:, h:h + 1], scalar2=None, op0=ALU.mult), [pH, den_c], [ho])
                            yield
                        if d == 0:
                            c.dma("sp", HS.a[tok], ho.a, HS, ho)
                            yield
                            return
                        h_ = hs[n % 3]; o_ = mo[n % 3]
                        c.V(lambda e: e.tensor_tensor(out=ho.a, in0=ho.a, in1=h_.a, op=ALU.add), [ho, h_], [ho])
                        yield
                        c.A(lambda e: e.activation(out=hsq_c.a, in_=ho.a, func=AF.Square), [ho], [hsq_c])
                        yield
                        c.V(lambda e: e.tensor_reduce(out=hss_c.a, in_=hsq_c.a.rearrange("p (h d) -> p h d", d=128), axis=AX.X, op=ALU.add), [hsq_c], [hss_c])
                        yield
                        c.V(lambda e: e.tensor_scalar(out=hss_c.a, in0=hss_c.a, scalar1=1.0 / 128, scalar2=1e-6, op0=ALU.mult, op1=ALU.add), [hss_c], [hss_c])
                        yield
                        c.A(lambda e: e.activation(out=hss_c.a, in_=hss_c.a, func=AF.Sqrt), [hss_c], [hss_c])
                        yield
                        c.V(lambda e: e.reciprocal(out=hss_c.a, in_=hss_c.a), [hss_c], [hss_c])
                        yield
                        c.V(lambda e: e.tensor_tensor(out=hn_c.a.rearrange("p (h d) -> p h d", d=128), in0=ho.a.rearrange("p (h d) -> p h d", d=128), in1=hss_c.a.unsqueeze(2).to_broadcast([128, 4, 128]), op=ALU.mult), [ho, hss_c], [hn_c])
                        yield
                        c.V(lambda e: e.tensor_tensor(out=hn_c.a, in0=hn_c.a, in1=gml.a, op=ALU.mult), [hn_c, gml], [hn_c])
                        yield
                        c.V(lambda e: e.tensor_tensor(out=yc_c.a, in0=hn_c.a, in1=o_.a, op=ALU.mult), [hn_c, o_], [yc_c])
                        yield
                        for m in range(4):
                            c.mm(lambda e: e.transpose(out=pY.a[:, m, :], in_=yc_c.a[:, m * 128:(m + 1) * 128], identity=ident.a), [yc_c, ident], [pY], last=(m == 3))
                            yield
                        c.A(lambda e: e.copy(out=yT_c.a, in_=pY.a), [pY], [yT_c])
                        yield
                        c.dma("sp", YCT.a[:, :, tok].rearrange("m p t -> p m t"), yT_c.a, YCT, yT_c)
                        yield

                    rr(l_s1(0))
                    if NT > 1:
                        rr(l_s1(1))
                    for n in range(NT):
                        rr(l_s2(n), l_s1(n + 2) if n + 2 < NT else None)
                c.pop()
                if stop_after == "p2c":
                    break

                c.push()
                wbr = c.sb("wbr", [128, 3, 4, D], BF16, grp="w3"); wo = c.sb("wo", [128, 8, D], BF16, grp="w3")
                for br in range(3):
                    c.dma("pool", wbr.a[:, br], w_branch.a[l, br].rearrange("(k p) n -> p k n", p=128), wbr, w_branch)
                c.dma("pool", wo.a, w_out.a[l].rearrange("(k p) n -> p k n", p=128), wo, w_out)
                ybr = [c.sb("ybr%d" % i, [128, 3, 4, 128], BF16, grp="l3%d" % i) for i in range(2)]
                gt_ = [c.sb("gt%d" % i, [128, 3072], BF16, grp="l3%d" % i) for i in range(2)]
                xt = [c.sb("x3%d" % i, [128, D], F32, grp="l3%d" % i) for i in range(2)]
                pB = [c.ps("pB%d" % i, [128, 512], F32) for i in range(2)]
                mrg_L = [c.sb("mrg%d" % i_, [128, D], F32) for i_ in range(2)]; mrg = mrg_L[0]; mtmp_L = [c.sb("mtmp%d" % i_, [128, 512], F32) for i_ in range(2)]; mtmp = mtmp_L[0]; mrb_L = [c.sb("mrb%d" % i_, [128, D], BF16) for i_ in range(2)]; mrb = mrb_L[0]
                pT = c.ps("pT3", [128, 8, 128], BF16); mT_L = [c.sb("mT%d" % i_, [128, 8, 128], BF16) for i_ in range(2)]; mT = mT_L[0]
                pYo = c.ps("pYo", [128, D], F32)
                x1 = [c.sb("x1%d" % i, [128, D], F32) for i in range(2)]
                junk_L = [c.sb("junk3%d" % i_, [128, D], F32) for i_ in range(2)]; junk = junk_L[0]; ss_L = [c.sb("ss3%d" % i_, [128, 1], F32) for i_ in range(2)]; ss = ss_L[0]; rstd_L = [c.sb("rstd3%d" % i_, [128, 1], F32) for i_ in range(2)]; rstd = rstd_L[0]
                xnf_L = [c.sb("xnf%d" % i_, [128, D], F32) for i_ in range(2)]; xnf = xnf_L[0]
                pTf = c.ps("pTf", [128, 8, 128], F32)
                h2f_L = [c.sb("h2f%d" % i_, [128, 8, 128], F32) for i_ in range(2)]; h2f = h2f_L[0]; h2b = [c.sb("h2b%d" % i, [128, 8, 128], BF16) for i in range(2)]
                pL = c.ps("pL", [128, 36], F32)
                lg_L = [c.sb("lg%d" % i_, [128, 36], F32) for i_ in range(2)]; lg = lg_L[0]; gmax_L = [c.sb("gmax%d" % i_, [128, 1], F32) for i_ in range(2)]; gmax = gmax_L[0]; ngmax_L = [c.sb("ngmax%d" % i_, [128, 1], F32) for i_ in range(2)]; ngmax = ngmax_L[0]
                eg_L = [c.sb("eg%d" % i_, [128, 4], F32) for i_ in range(2)]; eg = eg_L[0]; sg_L = [c.sb("sg%d" % i_, [128, 1], F32) for i_ in range(2)]; sg = sg_L[0]; ohg_L = [c.sb("ohg%d" % i_, [128, 4], F32) for i_ in range(2)]; ohg = ohg_L[0]
                lem_L = [c.sb("lem%d" % i_, [128, 4, 8], F32) for i_ in range(2)]; lem = lem_L[0]; les_L = [c.sb("les%d" % i_, [128, 8], F32) for i_ in range(2)]; les = les_L[0]; m8_L = [c.sb("m8%d" % i_, [128, 8], F32) for i_ in range(2)]; m8 = m8_L[0]
                nv0_L = [c.sb("nv0%d" % i_, [128, 1], F32) for i_ in range(2)]; nv0 = nv0_L[0]; e8_L = [c.sb("e8%d" % i_, [128, 8], F32) for i_ in range(2)]; e8 = e8_L[0]; mk2_L = [c.sb("mk2%d" % i_, [128, 8], F32) for i_ in range(2)]; mk2 = mk2_L[0]
                w8_L = [c.sb("w8%d" % i_, [128, 8], F32) for i_ in range(2)]; w8 = w8_L[0]; sden_L = [c.sb("sden%d" % i_, [128, 1], F32) for i_ in range(2)]; sden = sden_L[0]
                dw = [c.sb("dw%d" % i, [128, 4, 8], F32) for i in range(2)]
                def genA(i):
                    tok = slice(i * 128, (i + 1) * 128)
                    j3 = 2 if i < 2 else b
                    mrg = mrg_L[i % 2]; mtmp = mtmp_L[i % 2]; mrb = mrb_L[i % 2]; mT = mT_L[i % 2]; junk = junk_L[i % 2]; ss = ss_L[i % 2]; rstd = rstd_L[i % 2]; xnf = xnf_L[i % 2]; h2f = h2f_L[i % 2]; lg = lg_L[i % 2]; gmax = gmax_L[i % 2]; ngmax = ngmax_L[i % 2]; eg = eg_L[i % 2]; sg = sg_L[i % 2]; ohg = ohg_L[i % 2]; lem = lem_L[i % 2]; les = les_L[i % 2]; m8 = m8_L[i % 2]; nv0 = nv0_L[i % 2]; e8 = e8_L[i % 2]; mk2 = mk2_L[i % 2]; w8 = w8_L[i % 2]; sden = sden_L[i % 2]
                    y_ = ybr[i % 2]; g_ = gt_[i % 2]; x_ = xt[i % 2]; xo = x1[i % 2]; hb = h2b[i % 2]; dw_ = dw[i % 2]
                    for br, YT_ in enumerate((YAT, YBT, YCT)):
                        c.dma("sp", y_.a[:, br], YT_.a[:, :, tok].rearrange("m p t -> p m t"), y_, YT_)
                        yield
                    c.dma("sp", g_.a, GATES.a[tok], g_, GATES)
                    yield
                    src, ap = tile_src(l, b, i)
                    c.dma("sp", x_.a, ap, x_, src)
                    yield
                    for half in range(2):
                        cs = slice(half * 512, (half + 1) * 512)
                        for br in range(3):
                            p_ = pB[(half * 3 + br) % 2]
                            for k in range(4):
                                c.mm(lambda e: e.matmul(p_.a, lhsT=y_.a[:, br, k, :], rhs=wbr.a[:, br, k, cs], start=(k == 0), stop=(k == 3)), [y_, wbr], [p_], last=(k == 3))
                                yield
                            gsl = g_.a[:, br * 1024 + half * 512: br * 1024 + (half + 1) * 512]
                            if br == 0:
                                c.V(lambda e: e.tensor_tensor(out=mrg.a[:, cs], in0=p_.a, in1=gsl, op=ALU.mult), [p_, g_], [mrg])
                                yield
                            else:
                                c.V(lambda e: e.tensor_tensor(out=mtmp.a, in0=p_.a, in1=gsl, op=ALU.mult), [p_, g_], [mtmp])
                                yield
                                c.V(lambda e: e.tensor_tensor(out=mrg.a[:, cs], in0=mrg.a[:, cs], in1=mtmp.a, op=ALU.add), [mrg, mtmp], [mrg])
                                yield
                    c.A(lambda e: e.copy(out=mrb.a, in_=mrg.a), [mrg], [mrb])
                    yield
                    for k in range(8):
                        c.mm(lambda e: e.transpose(out=pT.a[:, k, :], in_=mrb.a[:, k * 128:(k + 1) * 128], identity=ident.a), [mrb, ident], [pT], last=(k == 7))
                        yield
                    c.A(lambda e: e.copy(out=mT.a, in_=pT.a), [pT], [mT])
                    yield
                    for half in range(2):
                        cs = slice(half * 512, (half + 1) * 512)
                        for k in range(8):
                            c.mm(lambda e: e.matmul(pYo.a[:, cs], lhsT=mT.a[:, k, :], rhs=wo.a[:, k, cs], start=(k == 0), stop=(k == 7)), [mT, wo], [pYo], last=(k == 7 and half == 1))
                            yield
                    c.V(lambda e: e.tensor_tensor(out=xo.a, in0=pYo.a, in1=GT.a[:, 0, j3, :], op=ALU.mult), [pYo, GT], [xo])
                    yield
                    c.V(lambda e: e.tensor_tensor(out=xo.a, in0=xo.a, in1=x_.a, op=ALU.add), [xo, x_], [xo])
                    yield
                    c.dma("sp", XS.a[b, tok, :], xo.a, XS, xo)
                    yield
                def genB(i):
                    tok = slice(i * 128, (i + 1) * 128)
                    j3 = 2 if i < 2 else b
                    mrg = mrg_L[i % 2]; mtmp = mtmp_L[i % 2]; mrb = mrb_L[i % 2]; mT = mT_L[i % 2]; junk = junk_L[i % 2]; ss = ss_L[i % 2]; rstd = rstd_L[i % 2]; xnf = xnf_L[i % 2]; h2f = h2f_L[i % 2]; lg = lg_L[i % 2]; gmax = gmax_L[i % 2]; ngmax = ngmax_L[i % 2]; eg = eg_L[i % 2]; sg = sg_L[i % 2]; ohg = ohg_L[i % 2]; lem = lem_L[i % 2]; les = les_L[i % 2]; m8 = m8_L[i % 2]; nv0 = nv0_L[i % 2]; e8 = e8_L[i % 2]; mk2 = mk2_L[i % 2]; w8 = w8_L[i % 2]; sden = sden_L[i % 2]
                    y_ = ybr[i % 2]; g_ = gt_[i % 2]; x_ = xt[i % 2]; xo = x1[i % 2]; hb = h2b[i % 2]; dw_ = dw[i % 2]
                    c.V(lambda e: e.memset(ss.a, 0.0), [], [ss])
                    yield
                    c.A(lambda e: e.activation(out=junk.a, in_=xo.a, func=AF.Square, accum_out=ss.a), [xo], [junk, ss])
                    yield
                    c.V(lambda e: e.tensor_scalar(out=rstd.a, in0=ss.a, scalar1=1.0 / D, scalar2=1e-6, op0=ALU.mult, op1=ALU.add), [ss], [rstd])
                    yield
                    c.A(lambda e: e.activation(out=rstd.a, in_=rstd.a, func=AF.Sqrt), [rstd], [rstd])
                    yield
                    c.V(lambda e: e.reciprocal(out=rstd.a, in_=rstd.a), [rstd], [rstd])
                    yield
                    c.V(lambda e: e.tensor_scalar(out=xnf.a, in0=xo.a, scalar1=rstd.a[:, 0:1], scalar2=None, op0=ALU.mult), [xo, rstd], [xnf])
                    yield
                    for k in range(8):
                        c.mm(lambda e: e.transpose(out=pTf.a[:, k, :], in_=xnf.a[:, k * 128:(k + 1) * 128], identity=ident_f.a), [xnf, ident_f], [pTf], last=(k == 7))
                        yield
                    for k in range(8):
                        c.V(lambda e: e.tensor_scalar(out=h2f.a[:, k, :], in0=pTf.a[:, k, :], scalar1=G2.a[:, k, j3:j3 + 1], scalar2=modT.a[:, 3, k, j3:j3 + 1], op0=ALU.mult, op1=ALU.add), [pTf, G2, modT], [h2f])
                        yield
                    c.A(lambda e: e.copy(out=hb.a, in_=h2f.a), [h2f], [hb])
                    yield
                    c.dma("sp", H2T.a[:, :, tok], hb.a, H2T, hb)
                    yield
                    for k in range(8):
                        c.mm(lambda e: e.matmul(pL.a, lhsT=h2f.a[:, k, :], rhs=wrt.a[:, k, :], start=(k == 0), stop=(k == 7)), [h2f, wrt], [pL], last=(k == 7))
                        yield
                    c.V(lambda e: e.tensor_tensor(out=lg.a, in0=pL.a, in1=brt.a, op=ALU.add), [pL, brt], [lg])
                    yield
                    c.V(lambda e: e.tensor_reduce(out=gmax.a, in_=lg.a[:, 0:4], axis=AX.X, op=ALU.max), [lg], [gmax])
                    yield
                    c.V(lambda e: e.tensor_scalar(out=ngmax.a, in0=gmax.a, scalar1=-1.0, scalar2=None, op0=ALU.mult), [gmax], [ngmax])
                    yield
                    c.V(lambda e: e.memset(sg.a, 0.0), [], [sg])
                    yield
                    c.A(lambda e: e.activation(out=eg.a, in_=lg.a[:, 0:4], func=AF.Exp, bias=ngmax.a[:, 0:1], accum_out=sg.a), [lg, ngmax], [eg, sg])
                    yield
                    c.V(lambda e: e.tensor_scalar(out=ohg.a, in0=lg.a[:, 0:4], scalar1=gmax.a[:, 0:1], scalar2=None, op0=ALU.is_ge), [lg, gmax], [ohg])
                    yield
                    c.V(lambda e: e.tensor_tensor(out=lem.a, in0=lg.a[:, 4:36].rearrange("p (g e) -> p g e", e=8), in1=ohg.a.unsqueeze(2).to_broadcast([128, 4, 8]), op=ALU.mult), [lg, ohg], [lem])
                    yield
                    c.V(lambda e: e.tensor_reduce(out=les.a, in_=lem.a.rearrange("p g e -> p e g"), axis=AX.X, op=ALU.add), [lem], [les])
                    yield
                    c.V(lambda e: e.max(out=m8.a, in_=les.a), [les], [m8])
                    yield
                    c.V(lambda e: e.tensor_scalar(out=nv0.a, in0=m8.a[:, 0:1], scalar1=-1.0, scalar2=None, op0=ALU.mult), [m8], [nv0])
                    yield
                    c.A(lambda e: e.activation(out=e8.a, in_=les.a, func=AF.Exp, bias=nv0.a[:, 0:1]), [les, nv0], [e8])
                    yield
                    c.V(lambda e: e.tensor_scalar(out=mk2.a, in0=les.a, scalar1=m8.a[:, 1:2], scalar2=None, op0=ALU.is_ge), [les, m8], [mk2])
                    yield
                    c.V(lambda e: e.tensor_tensor(out=w8.a, in0=e8.a, in1=mk2.a, op=ALU.mult), [e8, mk2], [w8])
                    yield
                    c.V(lambda e: e.tensor_reduce(out=sden.a, in_=w8.a, axis=AX.X, op=ALU.add), [w8], [sden])
                    yield
                    c.V(lambda e: e.tensor_tensor(out=sden.a, in0=sden.a, in1=sg.a, op=ALU.mult), [sden, sg], [sden])
                    yield
                    c.V(lambda e: e.reciprocal(out=sden.a, in_=sden.a), [sden], [sden])
                    yield
                    c.V(lambda e: e.tensor_scalar(out=w8.a, in0=w8.a, scalar1=sden.a[:, 0:1], scalar2=None, op0=ALU.mult), [w8, sden], [w8])
                    yield
                    c.V(lambda e: e.tensor_tensor(out=dw_.a, in0=ohg.a.unsqueeze(2).to_broadcast([128, 4, 8]), in1=w8.a.unsqueeze(1).to_broadcast([128, 4, 8]), op=ALU.mult), [ohg, w8], [dw_])
                    yield
                    c.dma("sp", DW.a[tok].rearrange("p (g e) -> p g e", e=8), dw_.a, DW, dw_)
                    yield
                tl = list(range(first_tile, NT))
                rr(genA(tl[0]))
                for k_ in range(len(tl)):
                    rr(genB(tl[k_]), genA(tl[k_ + 1]) if k_ + 1 < len(tl) else None)
                c.pop()
                if stop_after == "p3a":
                    break

                tiles = list(range(first_tile, NT))
                ng = 2
                per = (len(tiles) + ng - 1) // ng
                for gi in range(ng):
                    grp = tiles[gi * per:(gi + 1) * per]
                    t0 = grp[0]; G_ = len(grp)
                    c.push()
                    h2 = c.sb("h2", [128, 8, G_ * 128], BF16, grp="g4")
                    dwg = c.sb("dwg", [128, G_, 32], F32, grp="g4")
                    acc = c.sb("acc", [128, G_, D], F32)
                    c.dma("sp", h2.a, H2T.a[:, :, t0 * 128:(t0 + G_) * 128], h2, H2T)
                    c.dma("sp", dwg.a, DW.a[t0 * 128:(t0 + G_) * 128].rearrange("(i p) e -> p i e", p=128), dwg, DW)
                    wg = [c.sb("wg%d" % i, [128, 8, 512], BF16, grp="we%d" % i) for i in range(2)]
                    wd = [c.sb("wd%d" % i, [128, 2, D], BF16, grp="we%d" % i) for i in range(2)]
                    pGU = [c.ps("pGU%d" % i, [128, 2, 512], F32) for i in range(2)]
                    sl = [c.sb("sl%d" % i, [128, 512], F32) for i in range(2)]
                    aT = [c.sb("aT%d" % i, [128, 2, 512], BF16) for i in range(3)]
                    pD = [c.ps("pD%d" % i, [128, D], F32) for i in range(2)]
                    xt = [c.sb("x4%d" % i, [128, D], F32) for i in range(2)]
                    quads = [(tq, min(4, G_ - tq)) for tq in range(0, G_, 4)]
                    steps = [(e_, qi) for e_ in range(32) for qi in range(len(quads))]

                    def m_s1(si):
                        e_, qi = steps[si]
                        tq, nt = quads[qi]; N = nt * 128
                        g_ = wg[e_ % 2]; d_ = wd[e_ % 2]; at_ = aT[si % 3]
                        if qi == 0:
                            c.dma("sp", g_.a, WGU.a[e_], g_, WGU)
                            yield
                            c.dma("sp", d_.a, WDN.a[e_], d_, WDN)
                            yield
                        for cch in range(2):
                            pg = pGU[cch]; s_ = sl[cch]
                            for which in range(2):
                                col0 = which * 256 + cch * 128
                                for k in range(8):
                                    c.mm(lambda e: e.matmul(pg.a[:, which, :N], lhsT=g_.a[:, k, col0:col0 + 128], rhs=h2.a[:, k, tq * 128:tq * 128 + N], start=(k == 0), stop=(k == 7)), [g_, h2], [pg], last=(k == 7 and which == 1))
                                    yield
                            c.A(lambda e: e.activation(out=s_.a[:, :N], in_=pg.a[:, 0, :N], func=AF.Silu), [pg], [s_])
                            yield
                            c.V(lambda e: e.tensor_tensor(out=at_.a[:, cch, :N], in0=s_.a[:, :N], in1=pg.a[:, 1, :N], op=ALU.mult), [s_, pg], [at_])
                            yield

                    def m_s2(si):
                        e_, qi = steps[si]
                        tq, nt = quads[qi]
                        d_ = wd[e_ % 2]; at_ = aT[si % 3]
                        for tj in range(nt):
                            ti = tq + tj
                            pd = pD[tj % 2]
                            for half in range(2):
                                cs = slice(half * 512, (half + 1) * 512)
                                for k in range(2):
                                    c.mm(lambda e: e.matmul(pd.a[:, cs], lhsT=at_.a[:, k, tj * 128:(tj + 1) * 128], rhs=d_.a[:, k, cs], start=(k == 0), stop=(k == 1)), [at_, d_], [pd], last=(k == 1 and half == 1))
                                    yield
                            if e_ == 0:
                                c.V(lambda e: e.tensor_scalar(out=acc.a[:, ti, :], in0=pd.a, scalar1=dwg.a[:, ti, e_:e_ + 1], scalar2=None, op0=ALU.mult), [pd, dwg], [acc])
                                yield
                            else:
                                c.V(lambda e: e.scalar_tensor_tensor(out=acc.a[:, ti, :], in0=pd.a, scalar=dwg.a[:, ti, e_:e_ + 1], in1=acc.a[:, ti, :], op0=ALU.mult, op1=ALU.add), [pd, dwg, acc], [acc])
                                yield

                    ns_ = len(steps)
                    rr(m_s1(0))
                    if ns_ > 1:
                        rr(m_s1(1))
                    for si in range(ns_):
                        rr(m_s2(si), m_s1(si + 2) if si + 2 < ns_ else None)
                    for ti in range(G_):
                        i = t0 + ti
                        j3 = 2 if i < 2 else b
                        x_ = xt[ti % 2]
                        c.dma("sp", x_.a, XS.a[b, i * 128:(i + 1) * 128, :], x_, XS)
                        c.V(lambda e: e.tensor_tensor(out=acc.a[:, ti, :], in0=acc.a[:, ti, :], in1=GT.a[:, 1, j3, :], op=ALU.mult), [acc, GT], [acc])
                        c.V(lambda e: e.tensor_tensor(out=x_.a, in0=x_.a, in1=acc.a[:, ti, :], op=ALU.add), [x_, acc], [x_])
                        if last_layer:
                            c.dma("sp", y_out.a[b, (i - 2) * 128:(i - 1) * 128, :], x_.a, y_out, x_)
                        else:
                            c.dma("sp", XS.a[b, i * 128:(i + 1) * 128, :], x_.a, XS, x_)
                    c.pop()
            if stop_after is not None:
                break
            c.pop()
        c.barrier()
        while len(c.stack) > 1:
            c.stack.pop().__exit__(None, None, None)
        print("instructions:", c.ninst, "sems:", len(c.sem))
    nc._trace = c.trace
    return nc


def host_consts():
    t = np.arange(4096)
    row = (t // 64).astype(np.float32); col = (t % 64).astype(np.float32)
    inv = (10000.0 ** (-np.arange(16, dtype=np.float32) / 16)).astype(np.float32)
    ang = np.concatenate([row[:, None] * inv, col[:, None] * inv], axis=-1).astype(np.float32)
    cos = np.ones((T, 32), np.float32); sin = np.zeros((T, 32), np.float32)
    cos[256:] = np.cos(ang); sin[256:] = np.sin(ang)
    s = np.arange(128)
    u = np.stack([(s[:, None] <= s[None, :]), (s[:, None] >= s[None, :])]).astype(np.float32)
    kc = np.arange(64)[:, None]; qc = np.arange(64)[None, :]
    cs = np.clip(qc - 8, 0, 48)
    valid = (kc >= cs) & (kc < cs + 16)
    cm = np.where(valid, 0.0, NEG).astype(np.float32)
    jd = np.zeros((64, 128), np.float32)
    for kc in range(64):
        jd[63 - kc, kc] = 1.0; jd[63 - kc, 64 + kc] = 1.0
    return {"k_jd": jd, "k_cos": cos, "k_sin": sin, "k_u": u, "k_id": np.eye(128, dtype=np.float32), "k_colmask": np.concatenate([cm, cm], 0)}


def make_in_maps(inputs, nb, cores):
    consts = host_consts()
    maps = []
    for ci in cores:
        m = dict(consts)
        for k, v in inputs.items():
            v = np.ascontiguousarray(v, dtype=np.float32)
            if k in ("x", "c", "ctx"):
                m[k] = np.ascontiguousarray(v[ci * nb:(ci + 1) * nb])
            elif k == "b_mlstm":
                m[k] = v.reshape(2, 16)
            else:
                m[k] = v
        maps.append(m)
    return maps


def kernel(**inputs):
    nb = 2
    nc = build(nb=nb, nl=2)
    in_maps = make_in_maps(inputs, nb, list(range(8)))
    res = run_bass_kernel_spmd(nc, in_maps, core_ids=list(range(8)))
    return np.concatenate([r["y"] for r in res.results], axis=0).astype(np.float32)
```
